# Optimizing a Trainium2 kernel written in Bass

```python
import jax, jax.numpy as jnp
from jax import lax
import numpy as np

D_MODEL = 1024
BATCH = 8
SEQ = 4096
DEPTH = 1

CONV_WIDTH = D_MODEL // 2
CONV_KERNEL = 31
FOURIER_WIDTH = D_MODEL // 2
FOURIER_GROUPS = 4
FOURIER_GROUP_DIM = FOURIER_WIDTH // FOURIER_GROUPS
N_BRANCHES = 2
IN_COLS = 2 * CONV_WIDTH + FOURIER_WIDTH + N_BRANCHES * D_MODEL
N_EXPERTS = 16
CAPACITY_FACTOR = 2
D_FF_EXPERT = 2 * D_MODEL
N_MOD = 6
RMS_EPS = 1e-6
LN_EPS = 1e-5

kernel_name = "hybrid_conformer_fnet_ec_moe_block"


def rms_norm(x, g):
    xf = x.astype(jnp.float32)
    y = xf * lax.rsqrt(jnp.mean(xf * xf, axis=-1, keepdims=True) + RMS_EPS)
    return (y * g.astype(jnp.float32)).astype(x.dtype)


def layer_norm(x, g, b):
    xf = x.astype(jnp.float32)
    mu = jnp.mean(xf, axis=-1, keepdims=True)
    xc = xf - mu
    var = jnp.mean(xc * xc, axis=-1, keepdims=True)
    y = xc * lax.rsqrt(var + LN_EPS)
    return (y * g.astype(jnp.float32) + b.astype(jnp.float32)).astype(x.dtype)


def modulate(u, shift, scale):
    return u * (1.0 + scale[:, None, :]) + shift[:, None, :]


def conformer_branch(v2, dw_w, dw_b, ln_g, ln_b, w_o, b_o):
    a, g = jnp.split(v2, 2, axis=-1)
    v = a * jax.nn.sigmoid(g)
    pad = CONV_KERNEL // 2
    v = lax.conv_general_dilated(
        v, dw_w[:, None, :].astype(v.dtype), window_strides=(1,),
        padding=[(pad, pad)], dimension_numbers=("NWC", "WIO", "NWC"),
        feature_group_count=CONV_WIDTH) + dw_b
    v = jax.nn.silu(layer_norm(v, ln_g, ln_b))
    return v @ w_o + b_o


def fourier_branch(f, w_f, b_f):
    B, S, _ = f.shape
    fg = f.reshape(B, S, FOURIER_GROUPS, FOURIER_GROUP_DIM).astype(jnp.float32)
    fr = jnp.fft.fft2(fg, axes=(1, 3), norm="ortho").real
    fr = fr.reshape(B, S, FOURIER_WIDTH).astype(f.dtype)
    return fr @ w_f + b_f


def expert_choice_ffn(u, router_w, w_gate, w_up, w_down):
    B, S, D = u.shape
    cap = CAPACITY_FACTOR * S // N_EXPERTS
    logits = jnp.einsum("bsd,de->bse", u.astype(jnp.float32), router_w.astype(jnp.float32))
    aff = jax.nn.softmax(logits, axis=-1)
    aff_t = jnp.transpose(aff, (0, 2, 1))
    w, idx = lax.top_k(aff_t, cap)
    bidx = jnp.arange(B)[:, None, None]
    tok = u[bidx, idx]
    hg = jnp.einsum("becd,edf->becf", tok, w_gate)
    hu = jnp.einsum("becd,edf->becf", tok, w_up)
    out = jnp.einsum("becf,efd->becd", jax.nn.silu(hg) * hu, w_down)
    out = out * w[..., None].astype(out.dtype)
    return jnp.zeros_like(u).at[bidx, idx].add(out)


def setup_inputs(seed: int = 0) -> dict:
    key = jax.random.key(seed)
    ks = jax.random.split(key, 24)
    D, L, E, F = D_MODEL, DEPTH, N_EXPERTS, D_FF_EXPERT
    nrm = lambda k, shape, s: jax.random.normal(k, shape, jnp.float32) * s
    return {
        "x": nrm(ks[0], (BATCH, SEQ, D), 1.0),
        "c": nrm(ks[1], (BATCH, D), 1.0),
        "ada_w": nrm(ks[2], (L, D, N_MOD * D), 0.5 * D ** -0.5),
        "ada_b": nrm(ks[3], (L, N_MOD * D), 0.01),
        "norm1_g": 1.0 + nrm(ks[4], (L, D), 0.01),
        "w_in": nrm(ks[5], (L, D, IN_COLS), D ** -0.5),
        "b_in": nrm(ks[6], (L, IN_COLS), 0.01),
        "conv_dw_w": nrm(ks[7], (L, CONV_KERNEL, CONV_WIDTH), CONV_KERNEL ** -0.5),
        "conv_dw_b": nrm(ks[8], (L, CONV_WIDTH), 0.01),
        "conv_ln_g": 1.0 + nrm(ks[9], (L, CONV_WIDTH), 0.01),
        "conv_ln_b": nrm(ks[10], (L, CONV_WIDTH), 0.01),
        "conv_w_out": nrm(ks[11], (L, CONV_WIDTH, D), CONV_WIDTH ** -0.5),
        "conv_b_out": nrm(ks[12], (L, D), 0.01),
        "fourier_w": nrm(ks[13], (L, FOURIER_WIDTH, D), FOURIER_WIDTH ** -0.5),
        "fourier_b": nrm(ks[14], (L, D), 0.01),
        "w_out": nrm(ks[15], (L, D, D), D ** -0.5),
        "b_out": nrm(ks[16], (L, D), 0.01),
        "norm2_g": 1.0 + nrm(ks[17], (L, D), 0.01),
        "router_w": nrm(ks[18], (L, D, E), D ** -0.5),
        "expert_w_gate": nrm(ks[19], (L, E, D, F), D ** -0.5),
        "expert_w_up": nrm(ks[20], (L, E, D, F), D ** -0.5),
        "expert_w_down": nrm(ks[21], (L, E, F, D), F ** -0.5),
        "final_norm_g": 1.0 + nrm(ks[22], (D,), 0.01),
    }


def reference(x, c, ada_w, ada_b, norm1_g, w_in, b_in, conv_dw_w, conv_dw_b,
              conv_ln_g, conv_ln_b, conv_w_out, conv_b_out, fourier_w, fourier_b,
              w_out, b_out, norm2_g, router_w, expert_w_gate, expert_w_up,
              expert_w_down, final_norm_g):
    B, S, D = x.shape
    h = x
    c_act = jax.nn.silu(c)
    for l in range(DEPTH):
        mod = (c_act @ ada_w[l] + ada_b[l]).reshape(B, N_MOD, D)
        shift1, scale1, gate1 = mod[:, 0], mod[:, 1], mod[:, 2]
        shift2, scale2, gate2 = mod[:, 3], mod[:, 4], mod[:, 5]

        u = modulate(rms_norm(h, norm1_g[l]), shift1, scale1)
        proj = u @ w_in[l] + b_in[l]
        c0 = 2 * CONV_WIDTH
        c1 = c0 + FOURIER_WIDTH
        y_conv = conformer_branch(proj[..., :c0], conv_dw_w[l], conv_dw_b[l],
                                  conv_ln_g[l], conv_ln_b[l], conv_w_out[l], conv_b_out[l])
        y_four = fourier_branch(proj[..., c0:c1], fourier_w[l], fourier_b[l])
        gates = jax.nn.sigmoid(proj[..., c1:].reshape(B, S, N_BRANCHES, D))
        merged = gates[:, :, 0] * y_conv + gates[:, :, 1] * y_four
        h = h + gate1[:, None, :] * (merged @ w_out[l] + b_out[l])

        u2 = modulate(rms_norm(h, norm2_g[l]), shift2, scale2)
        y_ffn = expert_choice_ffn(u2, router_w[l], expert_w_gate[l],
                                  expert_w_up[l], expert_w_down[l])
        h = h + gate2[:, None, :] * y_ffn
    return rms_norm(h, final_norm_g)
```

```python
import os
from contextlib import ExitStack
import numpy as np
import ml_dtypes
import concourse.bass as bass
import concourse.mybir as mybir
from concourse.bass_utils import run_bass_kernel_spmd

F32 = mybir.dt.float32
BF16 = mybir.dt.bfloat16
I32 = mybir.dt.int32
I16 = mybir.dt.int16
AF = mybir.ActivationFunctionType
ALU = mybir.AluOpType

S = 4096
D = 1024
NT = 32
E = 16
CAP = 512
FF = 2048
KW = 31
ENGS = ("pe", "act", "dve", "pool", "sp")


class Prog:
    def __init__(self, nc, ctx):
        self.nc = nc
        self.ctx = ctx
        self.lists = {e: [] for e in ENGS}
        self.sem = {e: ctx.enter_context(nc.semaphore("prog_" + e)) for e in ENGS}
        self.cnt = {e: 0 for e in ENGS}
        self.waited = {}
        self.dma_sems = {}
        self.dma_cnt = {}
        self.dma_waited = {}
        self.last_w = {}
        self.readers = {}

    def _emit_waits(self, eng, deps):
        for d in deps:
            if d is None:
                continue
            if d[0] == "dma":
                _, name, val = d
                key = (eng, name)
                if self.dma_waited.get(key, 0) >= val:
                    continue
                self.dma_waited[key] = val
                sem = self.dma_sems[name]
                self.lists[eng].append(lambda e, sem=sem, val=val: e.wait_ge(sem, val))
            else:
                src, val = d
                key = (eng, src)
                if self.waited.get(key, 0) >= val:
                    continue
                self.waited[key] = val
                sem = self.sem[src]
                self.lists[eng].append(lambda e, sem=sem, val=val: e.wait_ge(sem, val))

    def _deps(self, r, w, extra):
        deps = list(extra)
        for k in r:
            if k in self.last_w:
                deps.append(self.last_w[k])
        for k in w:
            if k in self.last_w:
                deps.append(self.last_w[k])
            deps.extend(self.readers.get(k, []))
        return deps

    def _commit(self, tok, r, w):
        for k in r:
            self.readers.setdefault(k, []).append(tok)
        for k in w:
            self.last_w[k] = tok
            self.readers[k] = []

    def op(self, eng, fn, r=(), w=(), extra=()):
        self._emit_waits(eng, self._deps(r, w, extra))
        self.cnt[eng] += 1
        sem = self.sem[eng]
        self.lists[eng].append(lambda e, fn=fn, sem=sem: fn(e).then_inc(sem, 1))
        tok = (eng, self.cnt[eng])
        self._commit(tok, r, w)
        return tok

    def group(self, eng, fns, r=(), w=(), extra=()):
        self._emit_waits(eng, self._deps(r, w, extra))
        for fn in fns[:-1]:
            self.lists[eng].append(lambda e, fn=fn: fn(e))
        self.cnt[eng] += 1
        sem = self.sem[eng]
        fn = fns[-1]
        self.lists[eng].append(lambda e, fn=fn, sem=sem: fn(e).then_inc(sem, 1))
        tok = (eng, self.cnt[eng])
        self._commit(tok, r, w)
        return tok

    def dma(self, eng, fn, semname, r=(), w=(), extra=()):
        if semname not in self.dma_sems:
            self.dma_sems[semname] = self.ctx.enter_context(self.nc.semaphore("d_" + semname))
            self.dma_cnt[semname] = 0
        self._emit_waits(eng, self._deps(r, w, extra))
        self.dma_cnt[semname] += 16
        sem = self.dma_sems[semname]
        self.lists[eng].append(lambda e, fn=fn, sem=sem: fn(e).then_inc(sem, 16))
        tok = ("dma", semname, self.dma_cnt[semname])
        self._commit(tok, r, w)
        return tok

    def wait(self, eng, deps):
        self._emit_waits(eng, deps)

    def barrier(self):
        deps = [(src, self.cnt[src]) for src in ENGS if self.cnt[src] > 0]
        deps += [("dma", n, v) for n, v in self.dma_cnt.items() if v > 0]
        for eng in ENGS:
            self._emit_waits(eng, [d for d in deps if d[0] != eng])

    def emit(self):
        with self.nc.Block() as block:
            @block.tensor
            def _(e):
                for f in self.lists["pe"]:
                    f(e)

            @block.scalar
            def _(e):
                for f in self.lists["act"]:
                    f(e)

            @block.vector
            def _(e):
                for f in self.lists["dve"]:
                    f(e)

            @block.gpsimd
            def _(e):
                for f in self.lists["pool"]:
                    f(e)

            @block.sync
            def _(e):
                for f in self.lists["sp"]:
                    f(e)


class Arena:
    def __init__(self, big, total_bytes):
        self.big = big
        self.total = total_bytes
        self.off = 0

    def alloc(self, shape, dt):
        n = int(np.prod(shape))
        esz = 2 if dt in (BF16, I16) else 4
        nbytes = (n * esz + 63) // 64 * 64
        off = self.off
        self.off += nbytes
        self.hw = max(getattr(self, "hw", 0), self.off)
        assert self.off <= self.total, ("SBUF arena overflow", self.off, self.total)
        v = self.big[:, off // 4:(off + nbytes) // 4]
        if dt != F32:
            v = v.bitcast(dt)
        v = v[:, 0:n]
        if len(shape) == 2:
            v = v.rearrange("p (a b) -> p a b", a=shape[0])
        elif len(shape) == 3:
            v = v.rearrange("p (a b c) -> p a b c", a=shape[0], b=shape[1])
        return v

    def mark(self):
        return self.off

    def release(self, m):
        self.off = m


ARENA_BYTES = 200 * 1024


def build_program(stage=99):
    nc = bass.Bass("TRN2", target_bir_lowering=False)

    def din(name, shape, dt=F32):
        return nc.dram_tensor(name, list(shape), dt, kind="ExternalInput").ap()

    x = din("x", [S, D])
    c_col = din("c_col", [128, 8])
    ada_w = din("ada_w", [D, 6 * D])
    ada_b = din("ada_b", [1, 6 * D])
    norm1_g = din("norm1_g", [1, D])
    w_in = din("w_in", [D, 3584])
    smallrows = din("smallrows", [56, 128])
    dw_rows = din("dw_rows", [124, 128])
    conv_w_out = din("conv_w_out", [512, D])
    fourier_w = din("fourier_w", [512, D])
    w_out = din("w_out", [D, D])
    b_out = din("b_out", [1, D])
    norm2_g = din("norm2_g", [1, D])
    router_w = din("router_w", [D, E])
    ew_gate = din("expert_w_gate", [E, D, FF])
    ew_up = din("expert_w_up", [E, D, FF])
    ew_down = din("expert_w_down", [E, FF, D])
    final_g = din("final_norm_g", [1, D])
    cs128_d = din("cs128", [128, 256], BF16)
    CSd = din("dft_cos", [S, S], BF16)
    SSd = din("dft_sin", [S, S], BF16)
    maskM_d = din("maskM", [128, 128])
    maskM2_d = din("maskM2", [128, 128])
    cvals_d = din("cvals", [128, 4])

    out = nc.dram_tensor("out", [S, D], F32, kind="ExternalOutput").ap()
    dbg = None
    if stage < 99:
        dbg = nc.dram_tensor("dbg", [128, 16384], F32, kind="ExternalOutput").ap()

    modd = nc.dram_tensor("modd", [1, 9 * D], F32, kind="Internal").ap()
    frd = nc.dram_tensor("frd", [512, S], BF16, kind="Internal").ap()
    scr = nc.dram_tensor("scr", [S, 1056], BF16, kind="Internal").ap()
    hd = nc.dram_tensor("hd", [S, D], F32, kind="Internal").ap()
    cumd = nc.dram_tensor("cumd", [E, 8, 512], I16, kind="Internal").ap()

    with ExitStack() as ctx:
        P = Prog(nc, ctx)
        big = ctx.enter_context(nc.sbuf_tensor("big", [128, ARENA_BYTES // 4], F32))
        A = Arena(big, ARENA_BYTES)
        psb = [ctx.enter_context(nc.psum_tensor("ps%d" % i, [128, 512], F32)) for i in range(8)]
        PS = [p[:, :] for p in psb]
        PSB = [p[:, :].bitcast(BF16) for p in psb]
        pk = ["ps%d" % i for i in range(8)]

        ident = A.alloc([128], F32)
        identb = A.alloc([128], BF16)
        smallcol = A.alloc([56], F32)
        dwcol = A.alloc([124], F32)
        rstd1 = A.alloc([NT], F32)
        ss1 = A.alloc([NT], F32)
        ss2 = A.alloc([NT], F32)
        rstd2 = A.alloc([NT], F32)
        logits = A.alloc([NT, E], F32)
        aff = A.alloc([NT, E], F32)
        maskM = A.alloc([128], F32)
        maskM2 = A.alloc([128], F32)
        cvals = A.alloc([4], F32)
        ones_ln = A.alloc([128], F32)
        nhalf = A.alloc([256], F32)
        eps6 = A.alloc([1], F32)
        cact = A.alloc([8], F32)
        rw = A.alloc([8, E], F32)
        idxf = A.alloc([E * 4], F32)
        idxp = A.alloc([E * 8], F32)
        idxi = A.alloc([E * 4], I32)
        wsel = A.alloc([E * 4], F32)
        cs128 = A.alloc([256], BF16)
        junk = A.alloc([2048], BF16)
        BC_GS1, BC_SH1, BC_G1, BC_GB1, BC_GS2, BC_SH2, BC_G2, BC_FG = range(8)
        bcs = [None] * 8
        bcs[BC_G2] = A.alloc([D], F32)
        bcs[BC_FG] = A.alloc([D], F32)
        m_p1 = A.mark()
        for i in (BC_GS1, BC_SH1, BC_G1, BC_GB1, BC_GS2, BC_SH2):
            bcs[i] = A.alloc([D], F32)
        m_p2 = A.mark()

        P.op("pool", lambda e: e.memset(ident, 1.0), w=["ident"])
        P.op("pool", lambda e: e.affine_select(out=ident, in_=ident, pattern=[[-1, 128]], compare_op=ALU.is_equal,
                                               fill=0.0, base=0, channel_multiplier=1), w=["ident"])
        P.op("dve", lambda e: e.tensor_copy(out=identb, in_=ident), r=["ident"], w=["identb"])
        P.op("pool", lambda e: e.memset(ones_ln, 1.0 / 512.0), w=["ones_ln"])
        P.op("pool", lambda e: e.memset(nhalf, -0.5), w=["nhalf"])
        P.op("pool", lambda e: e.memset(eps6, 1e-6), w=["eps6"])
        P.dma("sp", lambda e: e.dma_start(out=maskM, in_=maskM_d[:, :]), "c_maskM", w=["maskM"])
        P.dma("sp", lambda e: e.dma_start(out=maskM2, in_=maskM2_d[:, :]), "c_maskM2", w=["maskM2"])
        P.dma("sp", lambda e: e.dma_start(out=cvals, in_=cvals_d[:, :]), "c_cvals", w=["cvals"])
        P.dma("sp", lambda e: e.dma_start(out=cs128, in_=cs128_d[:, :]), "c_cs128", w=["cs128"])
        P.dma("sp", lambda e: e.dma_start(out=cact, in_=c_col[:, :]), "c_cact", w=["cact"])
        P.dma("sp", lambda e: e.dma_start(out=rw, in_=router_w.rearrange("(k p) n -> p k n", p=128)), "c_rw", w=["rw"])

        m0 = A.mark()
        modrow = A.alloc([9 * D], F32)
        adab = A.alloc([6 * D], F32)
        g1row = A.alloc([D], F32)
        g2row = A.alloc([D], F32)
        borow = A.alloc([D], F32)
        awb = [A.alloc([8, 512], F32) for _ in range(2)]
        rows_T = A.alloc([128], F32)
        P.dma("act", lambda e: e.dma_start(out=adab[0:1, :], in_=ada_b[:, :]), "c_adab", w=["adab"])
        P.dma("act", lambda e: e.dma_start(out=g1row[0:1, :], in_=norm1_g[:, :]), "c_g1", w=["g1row"])
        P.dma("act", lambda e: e.dma_start(out=g2row[0:1, :], in_=norm2_g[:, :]), "c_g2", w=["g2row"])
        P.dma("act", lambda e: e.dma_start(out=borow[0:1, :], in_=b_out[:, :]), "c_bo", w=["borow"])
        P.op("act", lambda e: e.activation(out=cact, in_=cact, func=AF.Silu), r=["cact"], w=["cact"])
        adaw_v = ada_w.rearrange("(k p) n -> p k n", p=128)
        for pc in range(12):
            b = pc % 2
            P.dma("sp", lambda e, pc=pc, b=b: e.dma_start(out=awb[b], in_=adaw_v[:, :, pc * 512:(pc + 1) * 512]),
                  "awb%d" % b, w=["awb%d" % b])
            P.group("pe", [lambda e, k=k, b=b: e.matmul(PS[0][0:1, :], lhsT=cact[:, k:k + 1], rhs=awb[b][:, k, :],
                                                       start=(k == 0), stop=(k == 7)) for k in range(8)],
                    r=["cact", "awb%d" % b], w=[pk[0]])
            P.op("dve", lambda e, pc=pc: e.tensor_tensor(out=modrow[0:1, pc * 512:(pc + 1) * 512], in0=PS[0][0:1, :],
                                                        in1=adab[0:1, pc * 512:(pc + 1) * 512], op=ALU.add),
                 r=[pk[0], "adab"], w=["modrow"])
        P.op("dve", lambda e: e.scalar_tensor_tensor(out=modrow[0:1, 6 * D:7 * D], in0=modrow[0:1, D:2 * D], scalar=1.0,
                                                     in1=g1row[0:1, :], op0=ALU.add, op1=ALU.mult),
             r=["modrow", "g1row"], w=["modrow"])
        P.op("dve", lambda e: e.tensor_tensor(out=modrow[0:1, 7 * D:8 * D], in0=modrow[0:1, 2 * D:3 * D], in1=borow[0:1, :],
                                              op=ALU.mult), r=["modrow", "borow"], w=["modrow"])
        P.op("dve", lambda e: e.scalar_tensor_tensor(out=modrow[0:1, 8 * D:9 * D], in0=modrow[0:1, 4 * D:5 * D], scalar=1.0,
                                                     in1=g2row[0:1, :], op0=ALU.add, op1=ALU.mult),
             r=["modrow", "g2row"], w=["modrow"])
        P.dma("sp", lambda e: e.dma_start(out=modd[:, :], in_=modrow[0:1, :]), "st_modrow", r=["modrow"], w=["modd"])
        srcs = {BC_GS1: 6, BC_SH1: 0, BC_G1: 2, BC_GB1: 7, BC_GS2: 8, BC_SH2: 3, BC_G2: 5}
        for i, off in srcs.items():
            P.dma("sp" if i % 2 == 0 else "act",
                  lambda e, i=i, off=off: e.dma_start(out=bcs[i], in_=modd[0, off * D:(off + 1) * D].partition_broadcast(128)),
                  "bc%d" % i, r=["modd"], w=["bc%d" % i])
        P.dma("act", lambda e: e.dma_start(out=bcs[BC_FG], in_=final_g[0, :].partition_broadcast(128)), "bc7", w=["bc7"])
        P.dma("sp", lambda e: e.dma_start(out=rows_T[0:56, :], in_=smallrows[:, :]), "c_rowsT", w=["rows_T"])
        P.group("pe", [lambda e: e.transpose(out=PS[1][:, 0:56], in_=rows_T[0:56, :], identity=ident[0:56, 0:56])],
                r=["rows_T", "ident"], w=[pk[1]])
        P.op("dve", lambda e: e.tensor_copy(out=smallcol, in_=PS[1][:, 0:56]), r=[pk[1]], w=["smallcol"])
        P.dma("sp", lambda e: e.dma_start(out=rows_T[0:124, :], in_=dw_rows[:, :]), "c_rowsT", w=["rows_T"])
        P.group("pe", [lambda e: e.transpose(out=PS[1][:, 0:124], in_=rows_T[0:124, :], identity=ident[0:124, 0:124])],
                r=["rows_T", "ident"], w=[pk[1]])
        P.op("dve", lambda e: e.tensor_copy(out=dwcol, in_=PS[1][:, 0:124]), r=[pk[1]], w=["dwcol"])
        SC_BIN, SC_DWB, SC_LNG, SC_LNB, SC_CBO, SC_FB = 0, 28, 32, 36, 40, 48

        if stage == 0:
            P.dma("sp", lambda e: e.dma_start(out=dbg[0:1, 0:9 * D], in_=modrow[0:1, :]), "dbg0", r=["modrow"])
            P.dma("sp", lambda e: e.dma_start(out=dbg[:, 9 * D:9 * D + 56], in_=smallcol), "dbg1", r=["smallcol"])
            P.dma("sp", lambda e: e.dma_start(out=dbg[:, 10 * D:10 * D + 124], in_=dwcol), "dbg2", r=["dwcol"])
            P.dma("sp", lambda e: e.dma_start(out=dbg[:, 11 * D:12 * D], in_=bcs[BC_GS1]), "dbg3", r=["bc0"])
            P.barrier()
            P.emit()
            return nc
        P.barrier()
        A.release(m0)

        def xhat_tile(T, xin, xin_k, tmp, xm, xhatT_dst, xhatT_k, psbank, first_pass):
            P.dma("sp", lambda e: e.dma_start(out=xin, in_=x[T * 128:(T + 1) * 128, :]), "ld_" + xin_k, w=[xin_k])
            if first_pass:
                P.op("act", lambda e: e.activation(out=xm, in_=xin, func=AF.Square, accum_out=ss1[:, T:T + 1]),
                     r=[xin_k], w=["xm", "ss1_%d" % T])
                P.op("dve", lambda e: e.tensor_scalar(out=ss1[:, T:T + 1], in0=ss1[:, T:T + 1], scalar1=1.0 / D, scalar2=1e-6,
                                                      op0=ALU.mult, op1=ALU.add), r=["ss1_%d" % T], w=["ss1_%d" % T])
                P.op("pool", lambda e: e.tensor_tensor(out=rstd1[:, T:T + 1], in0=ss1[:, T:T + 1], in1=nhalf[:, 0:1], op=ALU.pow),
                     r=["ss1_%d" % T, "nhalf"], w=["rstd1_%d" % T])
            P.op("dve", lambda e: e.scalar_tensor_tensor(out=tmp, in0=xin, scalar=rstd1[:, T:T + 1], in1=bcs[BC_GS1],
                                                         op0=ALU.mult, op1=ALU.mult),
                 r=[xin_k, "rstd1_%d" % T, "bc0"], w=["tmp"])
            P.op("pool", lambda e: e.tensor_tensor(out=xm, in0=tmp, in1=bcs[BC_SH1], op=ALU.add),
                 r=["tmp", "bc1"], w=["xm"])
            P.group("pe", [lambda e, k=k: e.transpose(out=PSB[psbank][:, k * 128:(k + 1) * 128], in_=xm[:, k * 128:(k + 1) * 128],
                                                     identity=identb) for k in range(8)],
                    r=["xm", "identb"], w=[pk[psbank]])
            P.op("act", lambda e: e.activation(out=xhatT_dst, in_=PSB[psbank].rearrange("p (k t) -> p k t", k=8), func=AF.Copy),
                 r=[pk[psbank]], w=[xhatT_k])

        mA = A.mark()
        vT = A.alloc([4, S + 30], BF16)
        mV = A.mark()
        G = A.alloc([NT, 4, 256], BF16)
        mA2 = A.mark()
        wA = A.alloc([8, 1536], BF16)
        xin = [A.alloc([D], F32) for _ in range(2)]
        tmp = A.alloc([D], F32)
        xm = A.alloc([D], BF16)
        xhatT = [A.alloc([8, 512], BF16)] * 2
        sg = [A.alloc([512], F32) for _ in range(2)]
        fT = [A.alloc([4, 512], BF16)] * 2

        P.op("pool", lambda e: e.memset(vT[:, :, 0:15], 0.0), w=["vT"])
        P.op("pool", lambda e: e.memset(vT[:, :, S + 15:S + 30], 0.0), w=["vT"])
        win_v = w_in.rearrange("(k p) n -> p k n", p=128)
        for k in range(8):
            P.dma("pool", lambda e, k=k: e.dma_start(out=wA[:, k, :], in_=win_v[:, k, 0:1536]), "ld_wA", w=["wA"])

        for sc in range(8):
            xb_ = sc % 2
            for tt in range(4):
                T = sc * 4 + tt
                xhat_tile(T, xin[T % 2], "xin%d" % (T % 2), tmp, xm, xhatT[xb_][:, :, tt * 128:(tt + 1) * 128],
                          "xhatT", T % 2, True)
            xk = "xhatT"
            for c in range(4):
                ba, bg = 2 + 2 * (c % 2), 3 + 2 * (c % 2)
                P.group("pe", [lambda e, k=k, c=c, ba=ba: e.matmul(PS[ba], lhsT=wA[:, k, c * 128:(c + 1) * 128], rhs=xhatT[xb_][:, k, :],
                                                                  start=(k == 0), stop=(k == 7)) for k in range(8)],
                        r=["wA", xk], w=[pk[ba]])
                P.group("pe", [lambda e, k=k, c=c, bg=bg: e.matmul(PS[bg], lhsT=wA[:, k, 512 + c * 128:512 + (c + 1) * 128], rhs=xhatT[xb_][:, k, :],
                                                                  start=(k == 0), stop=(k == 7)) for k in range(8)],
                        r=["wA", xk], w=[pk[bg]])
                sgi = c % 2
                P.op("act", lambda e, c=c, bg=bg, sgi=sgi: e.activation(out=sg[sgi], in_=PS[bg], func=AF.Sigmoid,
                                                                      bias=smallcol[:, SC_BIN + 4 + c:SC_BIN + 5 + c], scale=1.0),
                     r=[pk[bg], "smallcol"], w=["sg%d" % sgi])
                P.op("dve", lambda e, c=c, ba=ba, sgi=sgi, sc=sc: e.scalar_tensor_tensor(
                    out=vT[:, c, 15 + sc * 512:15 + (sc + 1) * 512], in0=PS[ba], scalar=smallcol[:, SC_BIN + c:SC_BIN + c + 1],
                    in1=sg[sgi], op0=ALU.add, op1=ALU.mult),
                    r=[pk[ba], "sg%d" % sgi, "smallcol"], w=["vT_%d" % sc])
            fb_ = sc % 2
            for g in range(4):
                bf = 2 + (g % 4)
                P.group("pe", [lambda e, k=k, g=g, bf=bf: e.matmul(PS[bf], lhsT=wA[:, k, 1024 + g * 128:1024 + (g + 1) * 128], rhs=xhatT[xb_][:, k, :],
                                                                  start=(k == 0), stop=(k == 7)) for k in range(8)],
                        r=["wA", xk], w=[pk[bf]])
                P.op("act", lambda e, g=g, bf=bf: e.activation(out=fT[fb_][:, g, :], in_=PS[bf], func=AF.Identity,
                                                              bias=smallcol[:, SC_BIN + 8 + g:SC_BIN + 9 + g], scale=1.0),
                     r=[pk[bf], "smallcol"], w=["fT"])
            for tt in range(4):
                T = sc * 4 + tt
                for hb in range(2):
                    bank = 6 + hb
                    P.group("pe", [lambda e, g=g, tt=tt, bank=bank: e.matmul(
                        PS[bank][:, (g % 2) * 256:(g % 2 + 1) * 256], lhsT=fT[fb_][:, g, tt * 128:(tt + 1) * 128], rhs=cs128,
                        start=True, stop=True) for g in (2 * hb, 2 * hb + 1)],
                        r=["fT", "cs128"], w=[pk[bank]])
                    eng = "dve" if hb == 0 else "act"
                    if eng == "dve":
                        P.op("dve", lambda e, T=T, hb=hb, bank=bank: e.tensor_copy(
                            out=G[:, T, 2 * hb:2 * hb + 2, :], in_=PS[bank].rearrange("p (a b) -> p a b", a=2)),
                            r=[pk[bank]], w=["G_%d" % T])
                    else:
                        P.op("act", lambda e, T=T, hb=hb, bank=bank: e.activation(
                            out=G[:, T, 2 * hb:2 * hb + 2, :], in_=PS[bank].rearrange("p (a b) -> p a b", a=2), func=AF.Copy),
                            r=[pk[bank]], w=["G_%d" % T])

        if stage == 1:
            P.barrier()
            dtmp = xin[0]
            n = [0]

            def dump(src, col0, ncols):
                n[0] += 1
                P.op("dve", lambda e: e.tensor_copy(out=dtmp[:, 0:ncols], in_=src), r=["xin0"], w=["xin0"])
                P.dma("sp", lambda e: e.dma_start(out=dbg[:, col0:col0 + ncols], in_=dtmp[:, 0:ncols]), "dbg%d" % n[0], r=["xin0"])
            for q in range(4):
                dump(vT[:, 1, 15 + q * 1024:15 + (q + 1) * 1024], q * 1024, 1024)
            dump(G[:, 5, :, :].rearrange("p a b -> p (a b)"), 4096, 1024)
            dump(rstd1, 5120, NT)
            P.barrier()
            P.emit()
            return nc

        A.release(mA2)
        P.barrier()
        csb = [A.alloc([8, 512], BF16) for _ in range(2)]
        ssb = [A.alloc([8, 512], BF16) for _ in range(2)]
        frs = [A.alloc([4, 512], BF16) for _ in range(2)]
        CSv = CSd.rearrange("(t p) n -> p t n", p=128)
        SSv = SSd.rearrange("(t p) n -> p t n", p=128)
        Gkeys = ["G_%d" % t for t in range(NT)]
        it = 0
        for kc in range(8):
            bset = 4 * (kc % 2)
            for tg in range(4):
                b = it % 2
                it += 1
                P.dma("sp", lambda e, b=b, tg=tg, kc=kc: e.dma_start(out=csb[b], in_=CSv[:, tg * 8:(tg + 1) * 8, kc * 512:(kc + 1) * 512]),
                      "ld_csb%d" % b, w=["csb%d" % b])
                P.dma("act", lambda e, b=b, tg=tg, kc=kc: e.dma_start(out=ssb[b], in_=SSv[:, tg * 8:(tg + 1) * 8, kc * 512:(kc + 1) * 512]),
                      "ld_ssb%d" % b, w=["ssb%d" % b])
                fns = []
                for t8 in range(8):
                    t = tg * 8 + t8
                    for g in range(4):
                        fns.append(lambda e, t=t, t8=t8, g=g, b=b, bset=bset: e.matmul(
                            PS[bset + g], lhsT=G[:, t, g, 0:128], rhs=csb[b][:, t8, :], start=(t == 0), stop=False))
                        fns.append(lambda e, t=t, t8=t8, g=g, b=b, bset=bset: e.matmul(
                            PS[bset + g], lhsT=G[:, t, g, 128:256], rhs=ssb[b][:, t8, :], start=False, stop=(t == NT - 1)))
                P.group("pe", fns, r=Gkeys + ["csb%d" % b, "ssb%d" % b], w=[pk[bset + g] for g in range(4)])
            fb_ = kc % 2
            for g in range(4):
                if g % 2 == 0:
                    P.op("dve", lambda e, g=g, bset=bset, fb_=fb_: e.tensor_copy(out=frs[fb_][:, g, :], in_=PS[bset + g]),
                         r=[pk[bset + g]], w=["frs%d" % fb_])
                else:
                    P.op("act", lambda e, g=g, bset=bset, fb_=fb_: e.activation(out=frs[fb_][:, g, :], in_=PS[bset + g], func=AF.Copy),
                         r=[pk[bset + g]], w=["frs%d" % fb_])
            P.dma("sp", lambda e, kc=kc, fb_=fb_: e.dma_start(
                out=frd.rearrange("(g j) n -> j g n", j=128)[:, :, kc * 512:(kc + 1) * 512], in_=frs[fb_]),
                "st_frs%d" % fb_, r=["frs%d" % fb_], w=["frd_%d" % kc])

        if stage == 2:
            P.barrier()
            ld = A.alloc([S], BF16)
            dtmp = A.alloc([D], F32)
            P.dma("sp", lambda e: e.dma_start(out=ld, in_=frd[128:256, :]), "dbgL", w=["ld"])
            for q in range(4):
                P.op("dve", lambda e, q=q: e.tensor_copy(out=dtmp, in_=ld[:, q * 1024:(q + 1) * 1024]), r=["ld", "dtmp"], w=["dtmp"])
                P.dma("sp", lambda e, q=q: e.dma_start(out=dbg[:, q * 1024:(q + 1) * 1024], in_=dtmp), "dbgA%d" % q, r=["dtmp"])
            P.barrier()
            P.emit()
            return nc

        A.release(mV)
        P.barrier()
        vT2 = vT
        wg = A.alloc([8, 2048], BF16)
        cwo = A.alloc([4, D], BF16)
        fw = A.alloc([4, D], BF16)
        wo = A.alloc([8, D], BF16)
        SCB = 256
        xinB = A.alloc([D], F32)
        xres = A.alloc([D], F32)
        tmpB = A.alloc([D], F32)
        xmB = A.alloc([D], BF16)
        xhB = A.alloc([8, SCB], BF16)
        acc = A.alloc([4, SCB], F32)
        sq = A.alloc([4, SCB], F32)
        mean_sb = A.alloc([SCB], F32)
        var_sb = A.alloc([SCB], F32)
        rstd_ln = A.alloc([SCB], F32)
        ycen = A.alloc([SCB], F32)
        vact = A.alloc([4, SCB], BF16)
        sg0 = A.alloc([SCB], F32)
        sg1 = A.alloc([SCB], F32)
        mm1 = A.alloc([SCB], F32)
        mm2 = A.alloc([SCB], F32)
        merged = A.alloc([8, SCB], BF16)
        hbuf = A.alloc([D], F32)
        u2 = A.alloc([D], F32)
        u2b = A.alloc([D], BF16)
        u2T = A.alloc([8, 128], F32)
        frc = A.alloc([4, SCB], BF16)

        Gk_all = Gkeys
        for k in range(8):
            P.dma("pool", lambda e, k=k: e.dma_start(out=wg[:, k, :], in_=win_v[:, k, 1536:3584]), "ld_wg", w=["wg"])
            P.dma("pool", lambda e, k=k: e.dma_start(out=wo[:, k, :], in_=w_out.rearrange("(k p) n -> p k n", p=128)[:, k, :]), "ld_wo", w=["wo"])
        for c in range(4):
            P.dma("pool", lambda e, c=c: e.dma_start(out=cwo[:, c, :], in_=conv_w_out.rearrange("(k p) n -> p k n", p=128)[:, c, :]), "ld_cwo", w=["cwo"])
            P.dma("pool", lambda e, c=c: e.dma_start(out=fw[:, c, :], in_=fourier_w.rearrange("(k p) n -> p k n", p=128)[:, c, :]), "ld_fw", w=["fw"])
        vkeys = ["vT_%d" % i for i in range(8)] + ["vT"]
        stageB_first = True
        frd_v = frd.rearrange("(g j) n -> j g n", j=128)
        for sc in range(S // SCB):
            T0 = sc * SCB
            first_deps = []
            P.dma("act", lambda e, T0=T0: e.dma_start(out=frc, in_=frd_v[:, :, T0:T0 + SCB]), "ld_frc",
                  r=["frd_%d" % (T0 // 512)] + first_deps, w=["frc"])
            for tt in range(SCB // 128):
                T = sc * (SCB // 128) + tt
                xhat_tile(T, xinB, "xinB", tmpB, xmB, xhB[:, :, tt * 128:(tt + 1) * 128], "xhB", 0, False)
            for c in range(4):
                def tapfn(e, c=c, tap=0, T0=T0):
                    return e.tensor_scalar(out=acc[:, c, :], in0=vT2[:, c, T0:T0 + SCB], scalar1=dwcol[:, c:c + 1],
                                           scalar2=smallcol[:, SC_DWB + c:SC_DWB + c + 1], op0=ALU.mult, op1=ALU.add)
                fns = [tapfn]
                for tap in range(1, KW):
                    fns.append(lambda e, c=c, tap=tap, T0=T0: e.scalar_tensor_tensor(
                        out=acc[:, c, :], in0=vT2[:, c, T0 + tap:T0 + tap + SCB], scalar=dwcol[:, tap * 4 + c:tap * 4 + c + 1],
                        in1=acc[:, c, :], op0=ALU.mult, op1=ALU.add))
                for fn in fns:
                    P.op("dve", fn, r=vkeys + ["dwcol", "smallcol", "acc"], w=["acc"])
            P.op("act", lambda e: e.activation(out=sq, in_=acc, func=AF.Square), r=["acc"], w=["sq"])
            P.group("pe", [lambda e, c=c: e.matmul(PS[1][:, 0:SCB], lhsT=ones_ln, rhs=acc[:, c, :], start=(c == 0), stop=(c == 3)) for c in range(4)]
                    + [lambda e, c=c: e.matmul(PS[1][:, SCB:2 * SCB], lhsT=ones_ln, rhs=sq[:, c, :], start=(c == 0), stop=(c == 3)) for c in range(4)],
                    r=["acc", "sq", "ones_ln"], w=[pk[1]])
            P.op("act", lambda e: e.activation(out=mean_sb, in_=PS[1][:, 0:SCB], func=AF.Copy), r=[pk[1]], w=["mean_sb"])
            P.op("dve", lambda e: e.tensor_tensor(out=var_sb, in0=mean_sb, in1=mean_sb, op=ALU.mult), r=["mean_sb"], w=["var_sb"])
            P.op("dve", lambda e: e.tensor_tensor(out=var_sb, in0=PS[1][:, SCB:2 * SCB], in1=var_sb, op=ALU.subtract), r=[pk[1], "var_sb"], w=["var_sb"])
            P.op("dve", lambda e: e.tensor_scalar(out=var_sb, in0=var_sb, scalar1=1e-5, scalar2=None, op0=ALU.add), r=["var_sb"], w=["var_sb"])
            P.op("pool", lambda e: e.tensor_tensor(out=rstd_ln, in0=var_sb, in1=nhalf[:, 0:SCB], op=ALU.pow), r=["var_sb", "nhalf"], w=["rstd_ln"])
            for c in range(4):
                P.op("dve", lambda e, c=c: e.tensor_tensor(out=ycen, in0=acc[:, c, :], in1=mean_sb, op=ALU.subtract), r=["acc", "mean_sb"], w=["ycen"])
                P.op("dve", lambda e, c=c: e.tensor_tensor(out=ycen, in0=ycen, in1=rstd_ln, op=ALU.mult), r=["ycen", "rstd_ln"], w=["ycen"])
                P.op("act", lambda e, c=c: e.activation(out=vact[:, c, :], in_=ycen, func=AF.Silu,
                                                       bias=smallcol[:, SC_LNB + c:SC_LNB + c + 1], scale=smallcol[:, SC_LNG + c:SC_LNG + c + 1]),
                     r=["ycen", "smallcol"], w=["vact"])
            for dc in range(8):
                b0 = 2 + 2 * (dc % 2)
                b1 = b0 + 1
                P.group("pe", [lambda e, c=c, dc=dc, b0=b0: e.matmul(PS[b0][:, 0:SCB], lhsT=cwo[:, c, dc * 128:(dc + 1) * 128], rhs=vact[:, c, :],
                                                                    start=(c == 0), stop=(c == 3)) for c in range(4)]
                        + [lambda e, g=g, dc=dc, b0=b0: e.matmul(PS[b0][:, SCB:2 * SCB], lhsT=fw[:, g, dc * 128:(dc + 1) * 128], rhs=frc[:, g, :],
                                                                  start=(g == 0), stop=(g == 3)) for g in range(4)],
                        r=["cwo", "fw", "vact", "frc"], w=[pk[b0]])
                P.group("pe", [lambda e, k=k, dc=dc, b1=b1: e.matmul(PS[b1][:, 0:SCB], lhsT=wg[:, k, dc * 128:(dc + 1) * 128], rhs=xhB[:, k, :],
                                                                    start=(k == 0), stop=(k == 7)) for k in range(8)]
                        + [lambda e, k=k, dc=dc, b1=b1: e.matmul(PS[b1][:, SCB:2 * SCB], lhsT=wg[:, k, 1024 + dc * 128:1024 + (dc + 1) * 128], rhs=xhB[:, k, :],
                                                                  start=(k == 0), stop=(k == 7)) for k in range(8)],
                        r=["wg", "xhB"], w=[pk[b1]])
                P.op("act", lambda e, dc=dc, b1=b1: e.activation(out=sg0, in_=PS[b1][:, 0:SCB], func=AF.Sigmoid,
                                                                bias=smallcol[:, SC_BIN + 12 + dc:SC_BIN + 13 + dc], scale=1.0),
                     r=[pk[b1], "smallcol"], w=["sg0B"])
                P.op("act", lambda e, dc=dc, b1=b1: e.activation(out=sg1, in_=PS[b1][:, SCB:2 * SCB], func=AF.Sigmoid,
                                                                bias=smallcol[:, SC_BIN + 20 + dc:SC_BIN + 21 + dc], scale=1.0),
                     r=[pk[b1], "smallcol"], w=["sg1B"])
                P.op("dve", lambda e, dc=dc, b0=b0: e.scalar_tensor_tensor(out=mm1, in0=PS[b0][:, 0:SCB], scalar=smallcol[:, SC_CBO + dc:SC_CBO + dc + 1],
                                                                          in1=sg0, op0=ALU.add, op1=ALU.mult),
                     r=[pk[b0], "sg0B", "smallcol"], w=["mm1"])
                P.op("dve", lambda e, dc=dc, b0=b0: e.scalar_tensor_tensor(out=mm2, in0=PS[b0][:, SCB:2 * SCB], scalar=smallcol[:, SC_FB + dc:SC_FB + dc + 1],
                                                                          in1=sg1, op0=ALU.add, op1=ALU.mult),
                     r=[pk[b0], "sg1B", "smallcol"], w=["mm2"])
                P.op("pool", lambda e, dc=dc: e.tensor_tensor(out=merged[:, dc, :], in0=mm1, in1=mm2, op=ALU.add),
                     r=["mm1", "mm2"], w=["merged"])
            for tt in range(SCB // 128):
                T = sc * (SCB // 128) + tt
                P.dma("sp", lambda e, T=T: e.dma_start(out=xres, in_=x[T * 128:(T + 1) * 128, :]), "ld_xres", w=["xres"])
                P.op("pool", lambda e: e.tensor_tensor(out=xres, in0=xres, in1=bcs[BC_GB1], op=ALU.add), r=["xres", "bc3"], w=["xres"])
                for dh in range(2):
                    bh = 6 + dh
                    P.group("pe", [lambda e, k=k, tt=tt, dh=dh, bh=bh: e.matmul(PS[bh], lhsT=merged[:, k, tt * 128:(tt + 1) * 128],
                                                                               rhs=wo[:, k, dh * 512:(dh + 1) * 512], start=(k == 0), stop=(k == 7))
                                   for k in range(8)], r=["merged", "wo"], w=[pk[bh]])
                    P.op("dve", lambda e, dh=dh, bh=bh: e.tensor_tensor(out=hbuf[:, dh * 512:(dh + 1) * 512], in0=PS[bh],
                                                                       in1=bcs[BC_G1][:, dh * 512:(dh + 1) * 512], op=ALU.mult),
                         r=[pk[bh], "bc2"], w=["hbuf"])
                    P.op("dve", lambda e, dh=dh: e.tensor_tensor(out=hbuf[:, dh * 512:(dh + 1) * 512], in0=hbuf[:, dh * 512:(dh + 1) * 512],
                                                                in1=xres[:, dh * 512:(dh + 1) * 512], op=ALU.add),
                         r=["hbuf", "xres"], w=["hbuf"])
                P.dma("sp", lambda e, T=T: e.dma_start(out=hd[T * 128:(T + 1) * 128, :], in_=hbuf), "st_hbuf", r=["hbuf"], w=["hd_%d" % T])
                P.op("act", lambda e, T=T: e.activation(out=u2b, in_=hbuf, func=AF.Square, accum_out=ss2[:, T:T + 1]),
                     r=["hbuf"], w=["u2b", "ss2_%d" % T])
                P.op("dve", lambda e, T=T: e.tensor_scalar(out=ss2[:, T:T + 1], in0=ss2[:, T:T + 1], scalar1=1.0 / D, scalar2=1e-6,
                                                           op0=ALU.mult, op1=ALU.add), r=["ss2_%d" % T], w=["ss2_%d" % T])
                P.op("pool", lambda e, T=T: e.tensor_tensor(out=rstd2[:, T:T + 1], in0=ss2[:, T:T + 1], in1=nhalf[:, 0:1], op=ALU.pow),
                     r=["ss2_%d" % T, "nhalf"], w=["rstd2_%d" % T])
                P.op("dve", lambda e, T=T: e.scalar_tensor_tensor(out=u2, in0=hbuf, scalar=rstd2[:, T:T + 1], in1=bcs[BC_GS2],
                                                                  op0=ALU.mult, op1=ALU.mult), r=["hbuf", "rstd2_%d" % T, "bc4"], w=["u2"])
                P.op("pool", lambda e: e.tensor_tensor(out=u2, in0=u2, in1=bcs[BC_SH2], op=ALU.add), r=["u2", "bc5"], w=["u2"])
                P.op("act", lambda e: e.activation(out=u2b, in_=u2, func=AF.Copy), r=["u2"], w=["u2b"])
                P.dma("sp", lambda e, T=T: e.dma_start(out=scr[T * 128:(T + 1) * 128, 0:D], in_=u2b), "st_u2b", r=["u2b"], w=["scr_%d" % T])
                for hf in range(2):
                    P.group("pe", [lambda e, k=k, hf=hf: e.transpose(out=PS[4 + hf][:, (k % 4) * 128:(k % 4 + 1) * 128],
                                                                    in_=u2[:, k * 128:(k + 1) * 128], identity=ident)
                                   for k in range(4 * hf, 4 * hf + 4)], r=["u2", "ident"], w=[pk[4 + hf]])
                    if hf == 0:
                        P.op("dve", lambda e: e.tensor_copy(out=u2T[:, 0:4, :], in_=PS[4].rearrange("p (a b) -> p a b", a=4)), r=[pk[4]], w=["u2T"])
                    else:
                        P.op("act", lambda e: e.activation(out=u2T[:, 4:8, :], in_=PS[5].rearrange("p (a b) -> p a b", a=4), func=AF.Copy), r=[pk[5]], w=["u2T"])
                P.group("pe", [lambda e, k=k: e.matmul(PS[1][:, 0:E], lhsT=u2T[:, k, :], rhs=rw[:, k, :], start=(k == 0), stop=(k == 7)) for k in range(8)],
                        r=["u2T", "rw"], w=[pk[1]])
                P.op("dve", lambda e, T=T: e.tensor_copy(out=logits[:, T, :], in_=PS[1][:, 0:E]), r=[pk[1]], w=["logits"])

        if stage == 3:
            P.barrier()
            P.dma("sp", lambda e: e.dma_start(out=dbg[:, 0:NT * E], in_=logits.rearrange("p a b -> p (a b)")), "dbgA")
            P.dma("sp", lambda e: e.dma_start(out=hbuf, in_=hd[3 * 128:4 * 128, :]), "dbgL", w=["hbuf"])
            P.dma("sp", lambda e: e.dma_start(out=dbg[:, 1024:2048], in_=hbuf), "dbgC", r=["hbuf"])
            P.dma("sp", lambda e: e.dma_start(out=u2b, in_=scr[3 * 128:4 * 128, 0:D]), "dbgL2", w=["u2b"])
            P.op("dve", lambda e: e.tensor_copy(out=u2, in_=u2b), r=["u2b"], w=["u2"])
            P.dma("sp", lambda e: e.dma_start(out=dbg[:, 2048:3072], in_=u2), "dbgD", r=["u2"])
            P.barrier()
            P.emit()
            return nc

        A.release(m_p2)
        P.barrier()
        aff_es = A.alloc([512], F32)
        maskt = A.alloc([512], F32)
        onest = A.alloc([512], F32)
        cum = A.alloc([512], F32)
        affp = A.alloc([4, 128], F32)
        lo = A.alloc([1], F32)
        mid = A.alloc([1], F32)
        cntp = A.alloc([1], F32)
        pred = A.alloc([1], F32)
        offs = A.alloc([1], F32)
        mx = A.alloc([NT], F32)
        sm = A.alloc([NT], F32)
        cum16 = A.alloc([512], I16)
        P.op("dve", lambda e: e.tensor_reduce(out=mx, in_=logits, axis=mybir.AxisListType.X, op=ALU.max), r=["logits"], w=["mx"])
        P.op("dve", lambda e: e.tensor_scalar(out=mx, in0=mx, scalar1=-1.0, scalar2=None, op0=ALU.mult), r=["mx"], w=["mx"])
        for T in range(NT):
            P.op("act", lambda e, T=T: e.activation(out=aff[:, T, :], in_=logits[:, T, :], func=AF.Exp, bias=mx[:, T:T + 1], scale=1.0,
                                                   accum_out=sm[:, T:T + 1]), r=["logits", "mx"], w=["aff", "sm"])
        P.op("dve", lambda e: e.reciprocal(out=sm, in_=sm), r=["sm"], w=["sm"])
        for T in range(NT):
            P.op("dve", lambda e, T=T: e.tensor_scalar(out=aff[:, T, :], in0=aff[:, T, :], scalar1=sm[:, T:T + 1], scalar2=None, op0=ALU.mult),
                 r=["aff", "sm"], w=["aff"])
        scr_v = scr.rearrange("(t p) c -> p t c", p=128)
        aff_b = aff.rearrange("p a b -> p (a b)").bitcast(BF16).rearrange("p (a b) -> p a b", a=NT)
        for q in range(4):
            P.dma("sp", lambda e, q=q: e.dma_start(out=scr_v[:, q * 8:(q + 1) * 8, D:D + 32], in_=aff_b[:, q * 8:(q + 1) * 8, :]),
                  "st_aff", r=["aff"], w=["scra_%d" % q])
        aff4 = aff.rearrange("p (s c) e -> p s c e", c=4)
        for tc in range(4):
            P.op("dve", lambda e, tc=tc: e.tensor_copy(out=affp[:, tc, :].rearrange("p (s e) -> p s e", e=E), in_=aff4[:, :, tc, :]),
                 r=["aff"], w=["affp"])
        P.group("pe", [lambda e, tc=tc: e.transpose(out=PS[0][:, tc * 128:(tc + 1) * 128], in_=affp[:, tc, :], identity=ident) for tc in range(4)],
                r=["affp", "ident"], w=[pk[0]])
        P.op("dve", lambda e: e.tensor_copy(out=aff_es, in_=PS[0]), r=[pk[0]], w=["aff_es"])
        P.op("dve", lambda e: e.memset(lo, 0.0), w=["lo"])
        P.op("pool", lambda e: e.memset(onest, 1.0), w=["onest"])
        for i in range(26):
            step = 2.0 ** -(i + 1)
            P.op("dve", lambda e, step=step: e.tensor_scalar(out=mid, in0=lo, scalar1=step, scalar2=None, op0=ALU.add), r=["lo"], w=["mid"])
            P.op("dve", lambda e: e.tensor_scalar(out=junk[:, 0:512], in0=aff_es, scalar1=mid[:, 0:1], scalar2=0.0, op0=ALU.is_ge, op1=ALU.add,
                                                  accum_out=cntp), r=["aff_es", "mid"], w=["junk", "cntp"])
            P.group("pe", [lambda e: e.matmul(PS[1][:, 0:1], lhsT=maskM, rhs=cntp, start=True, stop=True)], r=["maskM", "cntp"], w=[pk[1]])
            P.op("dve", lambda e: e.tensor_scalar(out=pred, in0=PS[1][:, 0:1], scalar1=511.5, scalar2=None, op0=ALU.is_ge), r=[pk[1]], w=["pred"])
            P.op("dve", lambda e, step=step: e.scalar_tensor_tensor(out=lo, in0=pred, scalar=step, in1=lo, op0=ALU.mult, op1=ALU.add),
                 r=["pred", "lo"], w=["lo"])
        P.op("dve", lambda e: e.tensor_scalar(out=maskt, in0=aff_es, scalar1=lo[:, 0:1], scalar2=None, op0=ALU.is_ge), r=["aff_es", "lo"], w=["maskt"])
        P.op("dve", lambda e: e.tensor_tensor_scan(out=cum, data0=onest, data1=maskt, initial=0.0, op0=ALU.mult, op1=ALU.add),
             r=["onest", "maskt"], w=["cum"])
        P.group("pe", [lambda e: e.matmul(PS[1][:, 0:1], lhsT=maskM2, rhs=cum[:, 511:512], start=True, stop=True)], r=["maskM2", "cum"], w=[pk[1]])
        P.op("dve", lambda e: e.tensor_copy(out=offs, in_=PS[1][:, 0:1]), r=[pk[1]], w=["offs"])
        P.op("dve", lambda e: e.tensor_scalar(out=cum, in0=cum, scalar1=offs[:, 0:1], scalar2=None, op0=ALU.add), r=["cum", "offs"], w=["cum"])
        P.op("dve", lambda e: e.tensor_copy(out=cum16, in_=cum), r=["cum"], w=["cum16"])
        for s8 in range(8):
            P.dma("sp", lambda e, s8=s8: e.dma_start(out=cumd[:, s8, :], in_=cum16[s8 * 16:(s8 + 1) * 16, :]), "st_cum", r=["cum16"], w=["cumd"])

        if stage == 4:
            P.barrier()
            P.dma("sp", lambda e: e.dma_start(out=dbg[:, 0:512], in_=aff_es), "dbgA")
            P.dma("sp", lambda e: e.dma_start(out=dbg[:, 512:1024], in_=cum), "dbgB")
            P.dma("sp", lambda e: e.dma_start(out=dbg[:, 1024:1025], in_=lo, allow_slow_non_contiguous=True), "dbgC")
            P.barrier()
            P.emit()
            return nc

        A.release(m_p1)
        P.barrier()
        cb = A.alloc([S], I16)
        gath = A.alloc([4, 1056], BF16)
        tokT = [A.alloc([8, 512], BF16) for _ in range(2)]
        wgs = [A.alloc([8, 1024], BF16) for _ in range(2)]
        wus = [A.alloc([8, 1024], BF16) for _ in range(2)]
        wds = [A.alloc([8, 1024], BF16) for _ in range(2)]
        slu = [A.alloc([512], F32) for _ in range(2)]
        actT = A.alloc([8, 512], BF16)
        out1 = A.alloc([4, D], F32)
        outw = [A.alloc([D], F32) for _ in range(2)]
        otmp = A.alloc([512], F32)
        gath_f = gath.rearrange("p a b -> p (a b)").bitcast(F32).rearrange("p (a b) -> p a b", a=4)

        def load_weights(u):
            e_, fh = u // 2, u % 2
            s_ = u % 2
            gv = ew_gate[e_].rearrange("(k p) n -> p k n", p=128)
            uv = ew_up[e_].rearrange("(k p) n -> p k n", p=128)
            dv = ew_down[e_].rearrange("(k p) n -> p k n", p=128)
            for k in range(8):
                P.dma("pool", lambda e, k=k: e.dma_start(out=wgs[s_][:, k, :], in_=gv[:, k, fh * 1024:(fh + 1) * 1024]), "ld_wgs%d" % s_, w=["wgs%d" % s_])
            for k in range(8):
                P.dma("pool", lambda e, k=k: e.dma_start(out=wus[s_][:, k, :], in_=uv[:, k, fh * 1024:(fh + 1) * 1024]), "ld_wus%d" % s_, w=["wus%d" % s_])
            for k in range(8):
                P.dma("pool", lambda e, k=k: e.dma_start(out=wds[s_][:, k, :], in_=dv[:, fh * 8 + k, :]), "ld_wds%d" % s_, w=["wds%d" % s_])

        def route_idx(e_):
            P.dma("sp", lambda e: e.dma_start(out=cb, in_=cumd[e_].rearrange("s n -> (s n)").partition_broadcast(128)), "ld_cb",
                  r=["cumd"], w=["cb"])
            for cc in range(4):
                for hf in range(2):
                    P.op("dve", lambda e, cc=cc, hf=hf: e.tensor_scalar(
                        out=junk, in0=cb[:, hf * 2048:(hf + 1) * 2048], scalar1=cvals[:, cc:cc + 1], scalar2=0.0, op0=ALU.is_le, op1=ALU.add,
                        accum_out=idxp[:, e_ * 8 + cc * 2 + hf:e_ * 8 + cc * 2 + hf + 1]), r=["cb", "cvals"], w=["junk", "idxp%d" % e_])
            idxp_v = idxp[:, e_ * 8:(e_ + 1) * 8].rearrange("p (c h) -> p c h", h=2)
            P.op("dve", lambda e: e.tensor_tensor(out=idxf[:, e_ * 4:(e_ + 1) * 4], in0=idxp_v[:, :, 0], in1=idxp_v[:, :, 1], op=ALU.add),
                 r=["idxp%d" % e_], w=["idxf%d" % e_])
            P.op("dve", lambda e: e.tensor_copy(out=idxi[:, e_ * 4:(e_ + 1) * 4], in_=idxf[:, e_ * 4:(e_ + 1) * 4]), r=["idxf%d" % e_], w=["idxi%d" % e_])

        def gather_rows(e_):
            scr_keys = ["scr_%d" % t for t in range(NT)] + ["scra_%d" % q for q in range(4)]
            for cc in range(4):
                P.dma("pool", lambda e, cc=cc: e.indirect_dma_start(
                    out=gath[:, cc, :], out_offset=None, in_=scr[:, :],
                    in_offset=bass.IndirectOffsetOnAxis(ap=idxi[:, e_ * 4 + cc:e_ * 4 + cc + 1], axis=0)),
                    "ld_gath", r=["idxi%d" % e_] + scr_keys, w=["gath"])
            P.op("dve", lambda e: e.tensor_copy(out=wsel[:, e_ * 4:(e_ + 1) * 4], in_=gath_f[:, :, 512 + e_]), r=["gath"], w=["wsel%d" % e_])

        def transpose_tok(e_):
            tb = e_ % 2
            for cc in range(4):
                bank = 6 + (cc % 2)
                P.group("pe", [lambda e, k=k, cc=cc, bank=bank: e.transpose(out=PSB[bank][:, k * 128:(k + 1) * 128],
                                                                           in_=gath[:, cc, k * 128:(k + 1) * 128], identity=identb)
                               for k in range(8)], r=["gath", "identb"], w=[pk[bank]])
                if cc % 2 == 0:
                    P.op("act", lambda e, cc=cc, bank=bank: e.activation(out=tokT[tb][:, :, cc * 128:(cc + 1) * 128],
                                                                        in_=PSB[bank].rearrange("p (k t) -> p k t", k=8), func=AF.Copy),
                         r=[pk[bank]], w=["tokT%d" % tb])
                else:
                    P.op("dve", lambda e, cc=cc, bank=bank: e.tensor_copy(out=tokT[tb][:, :, cc * 128:(cc + 1) * 128],
                                                                         in_=PSB[bank].rearrange("p (k t) -> p k t", k=8)),
                         r=[pk[bank]], w=["tokT%d" % tb])

        hd_keys = ["hd_%d" % t for t in range(NT)]
        sc_keys = ["hd_sc0", "hd_sc1", "hd_sc2", "hd_sc3"]
        ocnt = [0]

        def unit(u):
            e_, fh = u // 2, u % 2
            s_ = u % 2
            tb = e_ % 2
            for fc in range(8):
                bg_, bu_ = 2 * (fc % 2), 2 * (fc % 2) + 1
                P.group("pe", [lambda e, k=k, fc=fc, bg_=bg_: e.matmul(PS[bg_], lhsT=wgs[s_][:, k, fc * 128:(fc + 1) * 128], rhs=tokT[tb][:, k, :],
                                                                      start=(k == 0), stop=(k == 7)) for k in range(8)],
                        r=["wgs%d" % s_, "tokT%d" % tb], w=[pk[bg_]])
                P.group("pe", [lambda e, k=k, fc=fc, bu_=bu_: e.matmul(PS[bu_], lhsT=wus[s_][:, k, fc * 128:(fc + 1) * 128], rhs=tokT[tb][:, k, :],
                                                                      start=(k == 0), stop=(k == 7)) for k in range(8)],
                        r=["wus%d" % s_, "tokT%d" % tb], w=[pk[bu_]])
                si = fc % 2
                P.op("act", lambda e, bg_=bg_, si=si: e.activation(out=slu[si], in_=PS[bg_], func=AF.Silu), r=[pk[bg_]], w=["slu%d" % si])
                P.op("dve", lambda e, fc=fc, bu_=bu_, si=si: e.tensor_tensor(out=actT[:, fc, :], in0=slu[si], in1=PS[bu_], op=ALU.mult),
                     r=["slu%d" % si, pk[bu_]], w=["actT"])
            for cc in range(4):
                if fh == 1:
                    ob = ocnt[0] % 2
                    ocnt[0] += 1
                for dh in range(2):
                    bo = 4 + dh
                    P.group("pe", [lambda e, fc=fc, cc=cc, dh=dh, bo=bo: e.matmul(PS[bo], lhsT=actT[:, fc, cc * 128:(cc + 1) * 128],
                                                                                 rhs=wds[s_][:, fc, dh * 512:(dh + 1) * 512],
                                                                                 start=(fc == 0), stop=(fc == 7)) for fc in range(8)],
                            r=["actT", "wds%d" % s_], w=[pk[bo]])
                    if fh == 0:
                        P.op("act", lambda e, cc=cc, dh=dh, bo=bo: e.activation(out=out1[:, cc, dh * 512:(dh + 1) * 512], in_=PS[bo], func=AF.Copy),
                             r=[pk[bo]], w=["out1"])
                    else:
                        P.op("dve", lambda e, cc=cc, dh=dh, bo=bo: e.tensor_tensor(out=otmp, in0=PS[bo], in1=out1[:, cc, dh * 512:(dh + 1) * 512], op=ALU.add),
                             r=[pk[bo], "out1"], w=["otmp"])
                        P.op("dve", lambda e, cc=cc, dh=dh, ob=ob: e.scalar_tensor_tensor(
                            out=outw[ob][:, dh * 512:(dh + 1) * 512], in0=otmp, scalar=wsel[:, e_ * 4 + cc:e_ * 4 + cc + 1],
                            in1=bcs[BC_G2][:, dh * 512:(dh + 1) * 512], op0=ALU.mult, op1=ALU.mult),
                            r=["otmp", "wsel%d" % e_, "bc6"], w=["outw%d" % ob])
                if fh == 1:
                    P.dma("pool", lambda e, cc=cc, ob=ob: e.indirect_dma_start(
                        out=hd[:, :], out_offset=bass.IndirectOffsetOnAxis(ap=idxi[:, e_ * 4 + cc:e_ * 4 + cc + 1], axis=0),
                        in_=outw[ob], in_offset=None, compute_op=ALU.add),
                        "sc_outw%d" % ob, r=["outw%d" % ob, "idxi%d" % e_] + hd_keys + (sc_keys if cc == 0 else []),
                        w=["hd_sc%d" % cc])

        P.op("dve", lambda e: e.memset(idxp, 0.0), w=["idxp%d" % i for i in range(E)])
        load_weights(0)
        load_weights(1)
        route_idx(0)
        gather_rows(0)
        transpose_tok(0)
        for e_ in range(E):
            if e_ + 1 < E:
                route_idx(e_ + 1)
            unit(2 * e_)
            if e_ + 1 < E:
                gather_rows(e_ + 1)
            if 2 * e_ + 2 < 2 * E:
                load_weights(2 * e_ + 2)
            if e_ + 1 < E:
                transpose_tok(e_ + 1)
            unit(2 * e_ + 1)
            if 2 * e_ + 3 < 2 * E:
                load_weights(2 * e_ + 3)

        fin = [A.alloc([D], F32) for _ in range(2)]
        fss = A.alloc([NT], F32)
        frs_ = A.alloc([NT], F32)
        last = []
        for T in range(NT):
            b = T % 2
            P.dma("sp", lambda e, T=T, b=b: e.dma_start(out=fin[b], in_=hd[T * 128:(T + 1) * 128, :]), "ld_fin%d" % b,
                  r=sc_keys + ["hd_%d" % T], w=["fin%d" % b])
            P.op("act", lambda e, T=T, b=b: e.activation(out=junk[:, 0:D], in_=fin[b], func=AF.Square, accum_out=fss[:, T:T + 1]),
                 r=["fin%d" % b], w=["junk", "fss%d" % T])
            P.op("dve", lambda e, T=T: e.tensor_scalar(out=fss[:, T:T + 1], in0=fss[:, T:T + 1], scalar1=1.0 / D, scalar2=1e-6,
                                                       op0=ALU.mult, op1=ALU.add), r=["fss%d" % T], w=["fss%d" % T])
            P.op("pool", lambda e, T=T: e.tensor_tensor(out=frs_[:, T:T + 1], in0=fss[:, T:T + 1], in1=nhalf[:, 0:1], op=ALU.pow),
                 r=["fss%d" % T, "nhalf"], w=["frs%d" % T])
            P.op("dve", lambda e, T=T, b=b: e.scalar_tensor_tensor(out=fin[b], in0=fin[b], scalar=frs_[:, T:T + 1], in1=bcs[BC_FG],
                                                                   op0=ALU.mult, op1=ALU.mult), r=["fin%d" % b, "frs%d" % T, "bc7"], w=["fin%d" % b])
            last.append(P.dma("act", lambda e, T=T, b=b: e.dma_start(out=out[T * 128:(T + 1) * 128, :], in_=fin[b]), "st_fin%d" % b,
                              r=["fin%d" % b], w=["out_%d" % T]))
        P.barrier()
        P.emit()
        print("arena high-water bytes", A.hw, "of", ARENA_BYTES)
    return nc


_CONST = {}


def _constants():
    if _CONST:
        return _CONST
    bf = ml_dtypes.bfloat16
    d = np.arange(128)
    ang = 2.0 * np.pi * ((d[:, None] * d[None, :]) % 128) / 128.0
    cs128 = np.concatenate([np.cos(ang), np.sin(ang)], axis=1) / np.sqrt(128.0)
    s = np.arange(S, dtype=np.int64)
    m = (s[:, None] * s[None, :]) % S
    tab_c = np.cos(2.0 * np.pi * np.arange(S) / S) / 64.0
    tab_s = -np.sin(2.0 * np.pi * np.arange(S) / S) / 64.0
    q = np.arange(128)
    e_of = q % 16
    s_of = q // 16
    maskM = (e_of[:, None] == e_of[None, :]).astype(np.float32)
    maskM2 = ((e_of[:, None] == e_of[None, :]) & (s_of[:, None] < s_of[None, :])).astype(np.float32)
    cvals = (np.arange(4)[None, :] * 128 + np.arange(128)[:, None]).astype(np.float32)
    _CONST.update(
        cs128=cs128.astype(bf),
        dft_cos=tab_c[m].astype(bf),
        dft_sin=tab_s[m].astype(bf),
        maskM=maskM, maskM2=maskM2, cvals=cvals,
    )
    return _CONST


def make_in_map(b, inp):
    f = lambda a: np.ascontiguousarray(np.asarray(a, dtype=np.float32))
    C = _constants()
    smallrows = np.concatenate([
        f(inp["b_in"][0]).reshape(28, 128), f(inp["conv_dw_b"][0]).reshape(4, 128), f(inp["conv_ln_g"][0]).reshape(4, 128),
        f(inp["conv_ln_b"][0]).reshape(4, 128), f(inp["conv_b_out"][0]).reshape(8, 128), f(inp["fourier_b"][0]).reshape(8, 128)], axis=0)
    return {
        "x": f(inp["x"][b]),
        "c_col": np.ascontiguousarray(f(inp["c"][b]).reshape(8, 128).T),
        "ada_w": f(inp["ada_w"][0]),
        "ada_b": f(inp["ada_b"][0]).reshape(1, -1),
        "norm1_g": f(inp["norm1_g"][0]).reshape(1, -1),
        "w_in": f(inp["w_in"][0]),
        "smallrows": np.ascontiguousarray(smallrows),
        "dw_rows": f(inp["conv_dw_w"][0]).reshape(124, 128),
        "conv_w_out": f(inp["conv_w_out"][0]),
        "fourier_w": f(inp["fourier_w"][0]),
        "w_out": f(inp["w_out"][0]),
        "b_out": f(inp["b_out"][0]).reshape(1, -1),
        "norm2_g": f(inp["norm2_g"][0]).reshape(1, -1),
        "router_w": f(inp["router_w"][0]),
        "expert_w_gate": f(inp["expert_w_gate"][0]),
        "expert_w_up": f(inp["expert_w_up"][0]),
        "expert_w_down": f(inp["expert_w_down"][0]),
        "final_norm_g": f(inp["final_norm_g"]).reshape(1, -1),
        "cs128": C["cs128"], "dft_cos": C["dft_cos"], "dft_sin": C["dft_sin"],
        "maskM": C["maskM"], "maskM2": C["maskM2"], "cvals": C["cvals"],
    }


def kernel(**inputs):
    nc = build_program()
    in_maps = [make_in_map(b, inputs) for b in range(8)]
    res = run_bass_kernel_spmd(nc, in_maps, core_ids=list(range(8)))
    return np.stack([np.asarray(r["out"], dtype=np.float32) for r in res.results], axis=0)
```

```python
import os
from contextlib import ExitStack
import numpy as np
import ml_dtypes
import concourse.bass as bass
import concourse.mybir as mybir
from concourse.bass_utils import run_bass_kernel_spmd

F32 = mybir.dt.float32
BF16 = mybir.dt.bfloat16
I32 = mybir.dt.int32
I16 = mybir.dt.int16
AF = mybir.ActivationFunctionType
ALU = mybir.AluOpType

S = 4096
D = 1024
NT = 32
E = 16
CAP = 512
FF = 2048
KW = 31
ENGS = ("pe", "act", "dve", "pool", "sp")


class Prog:
    def __init__(self, nc, ctx):
        self.nc = nc
        self.ctx = ctx
        self.lists = {e: [] for e in ENGS}
        self.sem = {e: ctx.enter_context(nc.semaphore("prog_" + e)) for e in ENGS}
        self.cnt = {e: 0 for e in ENGS}
        self.waited = {}
        self.dma_sems = {}
        self.dma_cnt = {}
        self.dma_waited = {}
        self.last_w = {}
        self.readers = {}

    def _emit_waits(self, eng, deps):
        for d in deps:
            if d is None:
                continue
            if d[0] == "dma":
                _, name, val = d
                key = (eng, name)
                if self.dma_waited.get(key, 0) >= val:
                    continue
                self.dma_waited[key] = val
                sem = self.dma_sems[name]
                self.lists[eng].append(lambda e, sem=sem, val=val: e.wait_ge(sem, val))
            else:
                src, val = d
                key = (eng, src)
                if self.waited.get(key, 0) >= val:
                    continue
                self.waited[key] = val
                sem = self.sem[src]
                self.lists[eng].append(lambda e, sem=sem, val=val: e.wait_ge(sem, val))

    def _deps(self, r, w, extra):
        deps = list(extra)
        for k in r:
            if k in self.last_w:
                deps.append(self.last_w[k])
        for k in w:
            if k in self.last_w:
                deps.append(self.last_w[k])
            deps.extend(self.readers.get(k, []))
        return deps

    def _commit(self, tok, r, w):
        for k in r:
            self.readers.setdefault(k, []).append(tok)
        for k in w:
            self.last_w[k] = tok
            self.readers[k] = []

    def op(self, eng, fn, r=(), w=(), extra=()):
        self._emit_waits(eng, self._deps(r, w, extra))
        self.cnt[eng] += 1
        sem = self.sem[eng]
        self.lists[eng].append(lambda e, fn=fn, sem=sem: fn(e).then_inc(sem, 1))
        tok = (eng, self.cnt[eng])
        self._commit(tok, r, w)
        return tok

    def group(self, eng, fns, r=(), w=(), extra=()):
        self._emit_waits(eng, self._deps(r, w, extra))
        for fn in fns[:-1]:
            self.lists[eng].append(lambda e, fn=fn: fn(e))
        self.cnt[eng] += 1
        sem = self.sem[eng]
        fn = fns[-1]
        self.lists[eng].append(lambda e, fn=fn, sem=sem: fn(e).then_inc(sem, 1))
        tok = (eng, self.cnt[eng])
        self._commit(tok, r, w)
        return tok

    def dma(self, eng, fn, semname, r=(), w=(), extra=()):
        if semname not in self.dma_sems:
            self.dma_sems[semname] = self.ctx.enter_context(self.nc.semaphore("d_" + semname))
            self.dma_cnt[semname] = 0
        self._emit_waits(eng, self._deps(r, w, extra))
        self.dma_cnt[semname] += 16
        sem = self.dma_sems[semname]
        self.lists[eng].append(lambda e, fn=fn, sem=sem: fn(e).then_inc(sem, 16))
        tok = ("dma", semname, self.dma_cnt[semname])
        self._commit(tok, r, w)
        return tok

    def wait(self, eng, deps):
        self._emit_waits(eng, deps)

    def barrier(self):
        deps = [(src, self.cnt[src]) for src in ENGS if self.cnt[src] > 0]
        deps += [("dma", n, v) for n, v in self.dma_cnt.items() if v > 0]
        for eng in ENGS:
            self._emit_waits(eng, [d for d in deps if d[0] != eng])

    def emit(self):
        with self.nc.Block() as block:
            @block.tensor
            def _(e):
                for f in self.lists["pe"]:
                    f(e)

            @block.scalar
            def _(e):
                for f in self.lists["act"]:
                    f(e)

            @block.vector
            def _(e):
                for f in self.lists["dve"]:
                    f(e)

            @block.gpsimd
            def _(e):
                for f in self.lists["pool"]:
                    f(e)

            @block.sync
            def _(e):
                for f in self.lists["sp"]:
                    f(e)


class Arena:
    def __init__(self, big, total_bytes):
        self.big = big
        self.total = total_bytes
        self.off = 0

    def alloc(self, shape, dt):
        n = int(np.prod(shape))
        esz = 2 if dt in (BF16, I16) else 4
        nbytes = (n * esz + 63) // 64 * 64
        off = self.off
        self.off += nbytes
        self.hw = max(getattr(self, "hw", 0), self.off)
        assert self.off <= self.total, ("SBUF arena overflow", self.off, self.total)
        v = self.big[:, off // 4:(off + nbytes) // 4]
        if dt != F32:
            v = v.bitcast(dt)
        v = v[:, 0:n]
        if len(shape) == 2:
            v = v.rearrange("p (a b) -> p a b", a=shape[0])
        elif len(shape) == 3:
            v = v.rearrange("p (a b c) -> p a b c", a=shape[0], b=shape[1])
        return v

    def mark(self):
        return self.off

    def release(self, m):
        self.off = m


ARENA_BYTES = 200 * 1024


def build_program(stage=99):
    nc = bass.Bass("TRN2", target_bir_lowering=False)

    def din(name, shape, dt=F32):
        return nc.dram_tensor(name, list(shape), dt, kind="ExternalInput").ap()

    x = din("x", [S, D])
    c_col = din("c_col", [128, 8])
    ada_w = din("ada_w_l", [12, 128, 8 * 512])
    ada_b = din("ada_b", [1, 6 * D])
    norm1_g = din("norm1_g", [1, D])
    w_in = din("w_in", [D, 3584])
    smallrows = din("smallrows", [56, 128])
    dw_rows = din("dw_rows", [124, 128])
    conv_w_out = din("conv_w_out", [512, D])
    fourier_w = din("fourier_w", [512, D])
    w_out = din("w_out", [D, D])
    b_out = din("b_out", [1, D])
    norm2_g = din("norm2_g", [1, D])
    router_w = din("router_w", [D, E])
    ew_gate = din("expert_w_gate_l", [E * 2, 128, 8 * 1024])
    ew_up = din("expert_w_up_l", [E * 2, 128, 8 * 1024])
    ew_down = din("expert_w_down_l", [E * 2, 128, 8 * 1024])
    final_g = din("final_norm_g", [1, D])
    cs128_d = din("cs128", [128, 256], BF16)
    CSd = din("dft_cos_l", [32, 128, 4096], BF16)
    SSd = din("dft_sin_l", [32, 128, 4096], BF16)
    maskM_d = din("maskM", [128, 128])
    maskM2_d = din("maskM2", [128, 128])
    cvals_d = din("cvals", [128, 4])

    out = nc.dram_tensor("out", [S, D], F32, kind="ExternalOutput").ap()
    dbg = None
    if stage < 99:
        dbg = nc.dram_tensor("dbg", [128, 16384], F32, kind="ExternalOutput").ap()

    modd = nc.dram_tensor("modd", [1, 9 * D], F32, kind="Internal").ap()
    frd = nc.dram_tensor("frd", [512, S], BF16, kind="Internal").ap()
    scr = nc.dram_tensor("scr", [S, 1056], BF16, kind="Internal").ap()
    hd = nc.dram_tensor("hd", [S, D], F32, kind="Internal").ap()
    cumd = nc.dram_tensor("cumd", [E, 8, 512], I16, kind="Internal").ap()

    with ExitStack() as ctx:
        P = Prog(nc, ctx)
        big = ctx.enter_context(nc.sbuf_tensor("big", [128, ARENA_BYTES // 4], F32))
        A = Arena(big, ARENA_BYTES)
        psb = [ctx.enter_context(nc.psum_tensor("ps%d" % i, [128, 512], F32)) for i in range(8)]
        PS = [p[:, :] for p in psb]
        PSB = [p[:, :].bitcast(BF16) for p in psb]
        pk = ["ps%d" % i for i in range(8)]

        ident = A.alloc([128], F32)
        identb = A.alloc([128], BF16)
        smallcol = A.alloc([56], F32)
        dwcol = A.alloc([124], F32)
        rstd1 = A.alloc([NT], F32)
        ss1 = A.alloc([NT], F32)
        ss2 = A.alloc([NT], F32)
        rstd2 = A.alloc([NT], F32)
        logits = A.alloc([NT, E], F32)
        aff = A.alloc([NT, E], F32)
        maskM = A.alloc([128], F32)
        maskM2 = A.alloc([128], F32)
        cvals = A.alloc([4], F32)
        ones_ln = A.alloc([128], F32)
        nhalf = A.alloc([256], F32)
        eps6 = A.alloc([1], F32)
        cact = A.alloc([8], F32)
        rw = A.alloc([8, E], F32)
        idxf = A.alloc([E * 4], F32)
        idxp = A.alloc([E * 8], F32)
        idxi = A.alloc([E * 4], I32)
        wsel = A.alloc([E * 4], F32)
        cs128 = A.alloc([256], BF16)
        junk = A.alloc([2048], BF16)
        BC_GS1, BC_SH1, BC_G1, BC_GB1, BC_GS2, BC_SH2, BC_G2, BC_FG = range(8)
        bcs = [None] * 8
        bcs[BC_G2] = A.alloc([D], F32)
        bcs[BC_FG] = A.alloc([D], F32)
        m_p1 = A.mark()
        for i in (BC_GS1, BC_SH1, BC_G1, BC_GB1, BC_GS2, BC_SH2):
            bcs[i] = A.alloc([D], F32)
        m_p2 = A.mark()

        P.op("pool", lambda e: e.memset(ident, 1.0), w=["ident"])
        P.op("pool", lambda e: e.affine_select(out=ident, in_=ident, pattern=[[-1, 128]], compare_op=ALU.is_equal,
                                               fill=0.0, base=0, channel_multiplier=1), w=["ident"])
        P.op("dve", lambda e: e.tensor_copy(out=identb, in_=ident), r=["ident"], w=["identb"])
        P.op("pool", lambda e: e.memset(ones_ln, 1.0 / 512.0), w=["ones_ln"])
        P.op("pool", lambda e: e.memset(nhalf, -0.5), w=["nhalf"])
        P.op("pool", lambda e: e.memset(eps6, 1e-6), w=["eps6"])
        P.dma("sp", lambda e: e.dma_start(out=maskM, in_=maskM_d[:, :]), "c_maskM", w=["maskM"])
        P.dma("sp", lambda e: e.dma_start(out=maskM2, in_=maskM2_d[:, :]), "c_maskM2", w=["maskM2"])
        P.dma("sp", lambda e: e.dma_start(out=cvals, in_=cvals_d[:, :]), "c_cvals", w=["cvals"])
        P.dma("sp", lambda e: e.dma_start(out=cs128, in_=cs128_d[:, :]), "c_cs128", w=["cs128"])
        P.dma("sp", lambda e: e.dma_start(out=cact, in_=c_col[:, :]), "c_cact", w=["cact"])
        P.dma("sp", lambda e: e.dma_start(out=rw, in_=router_w.rearrange("(k p) n -> p k n", p=128)), "c_rw", w=["rw"])

        m0 = A.mark()
        modrow = A.alloc([9 * D], F32)
        adab = A.alloc([6 * D], F32)
        g1row = A.alloc([D], F32)
        g2row = A.alloc([D], F32)
        borow = A.alloc([D], F32)
        awb = [A.alloc([8, 512], F32) for _ in range(2)]
        rows_T = A.alloc([128], F32)
        P.dma("act", lambda e: e.dma_start(out=adab[0:1, :], in_=ada_b[:, :]), "c_adab", w=["adab"])
        P.dma("act", lambda e: e.dma_start(out=g1row[0:1, :], in_=norm1_g[:, :]), "c_g1", w=["g1row"])
        P.dma("act", lambda e: e.dma_start(out=g2row[0:1, :], in_=norm2_g[:, :]), "c_g2", w=["g2row"])
        P.dma("act", lambda e: e.dma_start(out=borow[0:1, :], in_=b_out[:, :]), "c_bo", w=["borow"])
        P.op("act", lambda e: e.activation(out=cact, in_=cact, func=AF.Silu), r=["cact"], w=["cact"])
        for pc in range(12):
            b = pc % 2
            P.dma("sp", lambda e, pc=pc, b=b: e.dma_start(out=awb[b].rearrange("p a b -> p (a b)"), in_=ada_w[pc]),
                  "awb%d" % b, w=["awb%d" % b])
            P.group("pe", [lambda e, k=k, b=b: e.matmul(PS[0][0:1, :], lhsT=cact[:, k:k + 1], rhs=awb[b][:, k, :],
                                                       start=(k == 0), stop=(k == 7)) for k in range(8)],
                    r=["cact", "awb%d" % b], w=[pk[0]])
            P.op("dve", lambda e, pc=pc: e.tensor_tensor(out=modrow[0:1, pc * 512:(pc + 1) * 512], in0=PS[0][0:1, :],
                                                        in1=adab[0:1, pc * 512:(pc + 1) * 512], op=ALU.add),
                 r=[pk[0], "adab"], w=["modrow"])
        P.op("dve", lambda e: e.scalar_tensor_tensor(out=modrow[0:1, 6 * D:7 * D], in0=modrow[0:1, D:2 * D], scalar=1.0,
                                                     in1=g1row[0:1, :], op0=ALU.add, op1=ALU.mult),
             r=["modrow", "g1row"], w=["modrow"])
        P.op("dve", lambda e: e.tensor_tensor(out=modrow[0:1, 7 * D:8 * D], in0=modrow[0:1, 2 * D:3 * D], in1=borow[0:1, :],
                                              op=ALU.mult), r=["modrow", "borow"], w=["modrow"])
        P.op("dve", lambda e: e.scalar_tensor_tensor(out=modrow[0:1, 8 * D:9 * D], in0=modrow[0:1, 4 * D:5 * D], scalar=1.0,
                                                     in1=g2row[0:1, :], op0=ALU.add, op1=ALU.mult),
             r=["modrow", "g2row"], w=["modrow"])
        P.dma("sp", lambda e: e.dma_start(out=modd[:, :], in_=modrow[0:1, :]), "st_modrow", r=["modrow"], w=["modd"])
        srcs = {BC_GS1: 6, BC_SH1: 0, BC_G1: 2, BC_GB1: 7, BC_GS2: 8, BC_SH2: 3, BC_G2: 5}
        for i, off in srcs.items():
            P.dma("sp" if i % 2 == 0 else "act",
                  lambda e, i=i, off=off: e.dma_start(out=bcs[i], in_=modd[0, off * D:(off + 1) * D].partition_broadcast(128)),
                  "bc%d" % i, r=["modd"], w=["bc%d" % i])
        P.dma("act", lambda e: e.dma_start(out=bcs[BC_FG], in_=final_g[0, :].partition_broadcast(128)), "bc7", w=["bc7"])
        P.dma("sp", lambda e: e.dma_start(out=rows_T[0:56, :], in_=smallrows[:, :]), "c_rowsT", w=["rows_T"])
        P.group("pe", [lambda e: e.transpose(out=PS[1][:, 0:56], in_=rows_T[0:56, :], identity=ident[0:56, 0:56])],
                r=["rows_T", "ident"], w=[pk[1]])
        P.op("dve", lambda e: e.tensor_copy(out=smallcol, in_=PS[1][:, 0:56]), r=[pk[1]], w=["smallcol"])
        P.dma("sp", lambda e: e.dma_start(out=rows_T[0:124, :], in_=dw_rows[:, :]), "c_rowsT", w=["rows_T"])
        P.group("pe", [lambda e: e.transpose(out=PS[1][:, 0:124], in_=rows_T[0:124, :], identity=ident[0:124, 0:124])],
                r=["rows_T", "ident"], w=[pk[1]])
        P.op("dve", lambda e: e.tensor_copy(out=dwcol, in_=PS[1][:, 0:124]), r=[pk[1]], w=["dwcol"])
        SC_BIN, SC_DWB, SC_LNG, SC_LNB, SC_CBO, SC_FB = 0, 28, 32, 36, 40, 48

        if stage == 0:
            P.dma("sp", lambda e: e.dma_start(out=dbg[0:1, 0:9 * D], in_=modrow[0:1, :]), "dbg0", r=["modrow"])
            P.dma("sp", lambda e: e.dma_start(out=dbg[:, 9 * D:9 * D + 56], in_=smallcol), "dbg1", r=["smallcol"])
            P.dma("sp", lambda e: e.dma_start(out=dbg[:, 10 * D:10 * D + 124], in_=dwcol), "dbg2", r=["dwcol"])
            P.dma("sp", lambda e: e.dma_start(out=dbg[:, 11 * D:12 * D], in_=bcs[BC_GS1]), "dbg3", r=["bc0"])
            P.barrier()
            P.emit()
            return nc
        P.barrier()
        A.release(m0)

        def xhat_tile(T, xin, xin_k, tmp, xm, xhatT_dst, xhatT_k, psbank, first_pass):
            P.dma("sp", lambda e: e.dma_start(out=xin, in_=x[T * 128:(T + 1) * 128, :]), "ld_" + xin_k, w=[xin_k])
            if first_pass:
                P.op("act", lambda e: e.activation(out=xm, in_=xin, func=AF.Square, accum_out=ss1[:, T:T + 1]),
                     r=[xin_k], w=["xm", "ss1_%d" % T])
                P.op("dve", lambda e: e.tensor_scalar(out=ss1[:, T:T + 1], in0=ss1[:, T:T + 1], scalar1=1.0 / D, scalar2=1e-6,
                                                      op0=ALU.mult, op1=ALU.add), r=["ss1_%d" % T], w=["ss1_%d" % T])
                P.op("pool", lambda e: e.tensor_tensor(out=rstd1[:, T:T + 1], in0=ss1[:, T:T + 1], in1=nhalf[:, 0:1], op=ALU.pow),
                     r=["ss1_%d" % T, "nhalf"], w=["rstd1_%d" % T])
            P.op("dve", lambda e: e.scalar_tensor_tensor(out=tmp, in0=xin, scalar=rstd1[:, T:T + 1], in1=bcs[BC_GS1],
                                                         op0=ALU.mult, op1=ALU.mult),
                 r=[xin_k, "rstd1_%d" % T, "bc0"], w=["tmp"])
            P.op("pool", lambda e: e.tensor_tensor(out=xm, in0=tmp, in1=bcs[BC_SH1], op=ALU.add),
                 r=["tmp", "bc1"], w=["xm"])
            P.group("pe", [lambda e, k=k: e.transpose(out=PSB[psbank][:, k * 128:(k + 1) * 128], in_=xm[:, k * 128:(k + 1) * 128],
                                                     identity=identb) for k in range(8)],
                    r=["xm", "identb"], w=[pk[psbank]])
            P.op("act", lambda e: e.activation(out=xhatT_dst, in_=PSB[psbank].rearrange("p (k t) -> p k t", k=8), func=AF.Copy),
                 r=[pk[psbank]], w=[xhatT_k])

        mA = A.mark()
        vT = A.alloc([4, S + 30], BF16)
        mV = A.mark()
        G = A.alloc([NT, 4, 256], BF16)
        mA2 = A.mark()
        wA = A.alloc([8, 1536], BF16)
        xin = [A.alloc([D], F32) for _ in range(2)]
        tmp = A.alloc([D], F32)
        xm = A.alloc([D], BF16)
        xhatT = [A.alloc([8, 512], BF16)] * 2
        sg = [A.alloc([512], F32) for _ in range(2)]
        fT = [A.alloc([4, 512], BF16)] * 2

        P.op("pool", lambda e: e.memset(vT[:, :, 0:15], 0.0), w=["vT"])
        P.op("pool", lambda e: e.memset(vT[:, :, S + 15:S + 30], 0.0), w=["vT"])
        win_v = w_in.rearrange("(k p) n -> p k n", p=128)
        for k in range(8):
            P.dma("pool", lambda e, k=k: e.dma_start(out=wA[:, k, :], in_=win_v[:, k, 0:1536]), "ld_wA", w=["wA"])

        for sc in range(8):
            xb_ = sc % 2
            for tt in range(4):
                T = sc * 4 + tt
                xhat_tile(T, xin[T % 2], "xin%d" % (T % 2), tmp, xm, xhatT[xb_][:, :, tt * 128:(tt + 1) * 128],
                          "xhatT", T % 2, True)
            xk = "xhatT"
            for c in range(4):
                ba, bg = 2 + 2 * (c % 2), 3 + 2 * (c % 2)
                P.group("pe", [lambda e, k=k, c=c, ba=ba: e.matmul(PS[ba], lhsT=wA[:, k, c * 128:(c + 1) * 128], rhs=xhatT[xb_][:, k, :],
                                                                  start=(k == 0), stop=(k == 7)) for k in range(8)],
                        r=["wA", xk], w=[pk[ba]])
                P.group("pe", [lambda e, k=k, c=c, bg=bg: e.matmul(PS[bg], lhsT=wA[:, k, 512 + c * 128:512 + (c + 1) * 128], rhs=xhatT[xb_][:, k, :],
                                                                  start=(k == 0), stop=(k == 7)) for k in range(8)],
                        r=["wA", xk], w=[pk[bg]])
                sgi = c % 2
                P.op("act", lambda e, c=c, bg=bg, sgi=sgi: e.activation(out=sg[sgi], in_=PS[bg], func=AF.Sigmoid,
                                                                      bias=smallcol[:, SC_BIN + 4 + c:SC_BIN + 5 + c], scale=1.0),
                     r=[pk[bg], "smallcol"], w=["sg%d" % sgi])
                P.op("dve", lambda e, c=c, ba=ba, sgi=sgi, sc=sc: e.scalar_tensor_tensor(
                    out=vT[:, c, 15 + sc * 512:15 + (sc + 1) * 512], in0=PS[ba], scalar=smallcol[:, SC_BIN + c:SC_BIN + c + 1],
                    in1=sg[sgi], op0=ALU.add, op1=ALU.mult),
                    r=[pk[ba], "sg%d" % sgi, "smallcol"], w=["vT_%d" % sc])
            fb_ = sc % 2
            for g in range(4):
                bf = 2 + (g % 4)
                P.group("pe", [lambda e, k=k, g=g, bf=bf: e.matmul(PS[bf], lhsT=wA[:, k, 1024 + g * 128:1024 + (g + 1) * 128], rhs=xhatT[xb_][:, k, :],
                                                                  start=(k == 0), stop=(k == 7)) for k in range(8)],
                        r=["wA", xk], w=[pk[bf]])
                P.op("act", lambda e, g=g, bf=bf: e.activation(out=fT[fb_][:, g, :], in_=PS[bf], func=AF.Identity,
                                                              bias=smallcol[:, SC_BIN + 8 + g:SC_BIN + 9 + g], scale=1.0),
                     r=[pk[bf], "smallcol"], w=["fT"])
            for tt in range(4):
                T = sc * 4 + tt
                for hb in range(2):
                    bank = 6 + hb
                    P.group("pe", [lambda e, g=g, tt=tt, bank=bank: e.matmul(
                        PS[bank][:, (g % 2) * 256:(g % 2 + 1) * 256], lhsT=fT[fb_][:, g, tt * 128:(tt + 1) * 128], rhs=cs128,
                        start=True, stop=True) for g in (2 * hb, 2 * hb + 1)],
                        r=["fT", "cs128"], w=[pk[bank]])
                    eng = "dve" if hb == 0 else "act"
                    if eng == "dve":
                        P.op("dve", lambda e, T=T, hb=hb, bank=bank: e.tensor_copy(
                            out=G[:, T, 2 * hb:2 * hb + 2, :], in_=PS[bank].rearrange("p (a b) -> p a b", a=2)),
                            r=[pk[bank]], w=["G_%d" % T])
                    else:
                        P.op("act", lambda e, T=T, hb=hb, bank=bank: e.activation(
                            out=G[:, T, 2 * hb:2 * hb + 2, :], in_=PS[bank].rearrange("p (a b) -> p a b", a=2), func=AF.Copy),
                            r=[pk[bank]], w=["G_%d" % T])

        if stage == 1:
            P.barrier()
            dtmp = xin[0]
            n = [0]

            def dump(src, col0, ncols):
                n[0] += 1
                P.op("dve", lambda e: e.tensor_copy(out=dtmp[:, 0:ncols], in_=src), r=["xin0"], w=["xin0"])
                P.dma("sp", lambda e: e.dma_start(out=dbg[:, col0:col0 + ncols], in_=dtmp[:, 0:ncols]), "dbg%d" % n[0], r=["xin0"])
            for q in range(4):
                dump(vT[:, 1, 15 + q * 1024:15 + (q + 1) * 1024], q * 1024, 1024)
            dump(G[:, 5, :, :].rearrange("p a b -> p (a b)"), 4096, 1024)
            dump(rstd1, 5120, NT)
            P.barrier()
            P.emit()
            return nc

        A.release(mA2)
        P.barrier()
        csb = [A.alloc([8, 512], BF16) for _ in range(2)]
        ssb = [A.alloc([8, 512], BF16) for _ in range(2)]
        frs = [A.alloc([4, 512], BF16) for _ in range(2)]
        Gkeys = ["G_%d" % t for t in range(NT)]
        it = 0
        for kc in range(8):
            bset = 4 * (kc % 2)
            for tg in range(4):
                b = it % 2
                it += 1
                P.dma("sp", lambda e, b=b, tg=tg, kc=kc: e.dma_start(out=csb[b].rearrange("p a b -> p (a b)"), in_=CSd[kc * 4 + tg]),
                      "ld_csb%d" % b, w=["csb%d" % b])
                P.dma("act", lambda e, b=b, tg=tg, kc=kc: e.dma_start(out=ssb[b].rearrange("p a b -> p (a b)"), in_=SSd[kc * 4 + tg]),
                      "ld_ssb%d" % b, w=["ssb%d" % b])
                fns = []
                for t8 in range(8):
                    t = tg * 8 + t8
                    for g in range(4):
                        fns.append(lambda e, t=t, t8=t8, g=g, b=b, bset=bset: e.matmul(
                            PS[bset + g], lhsT=G[:, t, g, 0:128], rhs=csb[b][:, t8, :], start=(t == 0), stop=False))
                        fns.append(lambda e, t=t, t8=t8, g=g, b=b, bset=bset: e.matmul(
                            PS[bset + g], lhsT=G[:, t, g, 128:256], rhs=ssb[b][:, t8, :], start=False, stop=(t == NT - 1)))
                P.group("pe", fns, r=Gkeys + ["csb%d" % b, "ssb%d" % b], w=[pk[bset + g] for g in range(4)])
            fb_ = kc % 2
            for g in range(4):
                if g % 2 == 0:
                    P.op("dve", lambda e, g=g, bset=bset, fb_=fb_: e.tensor_copy(out=frs[fb_][:, g, :], in_=PS[bset + g]),
                         r=[pk[bset + g]], w=["frs%d" % fb_])
                else:
                    P.op("act", lambda e, g=g, bset=bset, fb_=fb_: e.activation(out=frs[fb_][:, g, :], in_=PS[bset + g], func=AF.Copy),
                         r=[pk[bset + g]], w=["frs%d" % fb_])
            P.dma("sp", lambda e, kc=kc, fb_=fb_: e.dma_start(
                out=frd.rearrange("(g j) n -> j g n", j=128)[:, :, kc * 512:(kc + 1) * 512], in_=frs[fb_]),
                "st_frs%d" % fb_, r=["frs%d" % fb_], w=["frd_%d" % kc])

        if stage == 2:
            P.barrier()
            ld = A.alloc([S], BF16)
            dtmp = A.alloc([D], F32)
            P.dma("sp", lambda e: e.dma_start(out=ld, in_=frd[128:256, :]), "dbgL", w=["ld"])
            for q in range(4):
                P.op("dve", lambda e, q=q: e.tensor_copy(out=dtmp, in_=ld[:, q * 1024:(q + 1) * 1024]), r=["ld", "dtmp"], w=["dtmp"])
                P.dma("sp", lambda e, q=q: e.dma_start(out=dbg[:, q * 1024:(q + 1) * 1024], in_=dtmp), "dbgA%d" % q, r=["dtmp"])
            P.barrier()
            P.emit()
            return nc

        A.release(mV)
        P.barrier()
        vT2 = vT
        wg = A.alloc([8, 2048], BF16)
        cwo = A.alloc([4, D], BF16)
        fw = A.alloc([4, D], BF16)
        wo = A.alloc([8, D], BF16)
        SCB = 256
        xinB = A.alloc([D], F32)
        xres = A.alloc([D], F32)
        tmpB = A.alloc([D], F32)
        xmB = A.alloc([D], BF16)
        xhB = A.alloc([8, SCB], BF16)
        acc = A.alloc([4, SCB], F32)
        sq = A.alloc([4, SCB], F32)
        mean_sb = A.alloc([SCB], F32)
        var_sb = A.alloc([SCB], F32)
        rstd_ln = A.alloc([SCB], F32)
        ycen = A.alloc([SCB], F32)
        vact = A.alloc([4, SCB], BF16)
        sg0 = A.alloc([SCB], F32)
        sg1 = A.alloc([SCB], F32)
        mm1 = A.alloc([SCB], F32)
        mm2 = A.alloc([SCB], F32)
        merged = A.alloc([8, SCB], BF16)
        hbuf = A.alloc([D], F32)
        u2 = A.alloc([D], F32)
        u2b = A.alloc([D], BF16)
        u2T = A.alloc([8, 128], F32)
        frc = A.alloc([4, SCB], BF16)

        Gk_all = Gkeys
        for k in range(8):
            P.dma("pool", lambda e, k=k: e.dma_start(out=wg[:, k, :], in_=win_v[:, k, 1536:3584]), "ld_wg", w=["wg"])
            P.dma("pool", lambda e, k=k: e.dma_start(out=wo[:, k, :], in_=w_out.rearrange("(k p) n -> p k n", p=128)[:, k, :]), "ld_wo", w=["wo"])
        for c in range(4):
            P.dma("pool", lambda e, c=c: e.dma_start(out=cwo[:, c, :], in_=conv_w_out.rearrange("(k p) n -> p k n", p=128)[:, c, :]), "ld_cwo", w=["cwo"])
            P.dma("pool", lambda e, c=c: e.dma_start(out=fw[:, c, :], in_=fourier_w.rearrange("(k p) n -> p k n", p=128)[:, c, :]), "ld_fw", w=["fw"])
        vkeys = ["vT_%d" % i for i in range(8)] + ["vT"]
        stageB_first = True
        frd_v = frd.rearrange("(g j) n -> j g n", j=128)
        for sc in range(S // SCB):
            T0 = sc * SCB
            first_deps = []
            P.dma("act", lambda e, T0=T0: e.dma_start(out=frc, in_=frd_v[:, :, T0:T0 + SCB]), "ld_frc",
                  r=["frd_%d" % (T0 // 512)] + first_deps, w=["frc"])
            for tt in range(SCB // 128):
                T = sc * (SCB // 128) + tt
                xhat_tile(T, xinB, "xinB", tmpB, xmB, xhB[:, :, tt * 128:(tt + 1) * 128], "xhB", 0, False)
            for c in range(4):
                def tapfn(e, c=c, tap=0, T0=T0):
                    return e.tensor_scalar(out=acc[:, c, :], in0=vT2[:, c, T0:T0 + SCB], scalar1=dwcol[:, c:c + 1],
                                           scalar2=smallcol[:, SC_DWB + c:SC_DWB + c + 1], op0=ALU.mult, op1=ALU.add)
                fns = [tapfn]
                for tap in range(1, KW):
                    fns.append(lambda e, c=c, tap=tap, T0=T0: e.scalar_tensor_tensor(
                        out=acc[:, c, :], in0=vT2[:, c, T0 + tap:T0 + tap + SCB], scalar=dwcol[:, tap * 4 + c:tap * 4 + c + 1],
                        in1=acc[:, c, :], op0=ALU.mult, op1=ALU.add))
                for fn in fns:
                    P.op("dve", fn, r=vkeys + ["dwcol", "smallcol", "acc"], w=["acc"])
            P.op("act", lambda e: e.activation(out=sq, in_=acc, func=AF.Square), r=["acc"], w=["sq"])
            P.group("pe", [lambda e, c=c: e.matmul(PS[1][:, 0:SCB], lhsT=ones_ln, rhs=acc[:, c, :], start=(c == 0), stop=(c == 3)) for c in range(4)]
                    + [lambda e, c=c: e.matmul(PS[1][:, SCB:2 * SCB], lhsT=ones_ln, rhs=sq[:, c, :], start=(c == 0), stop=(c == 3)) for c in range(4)],
                    r=["acc", "sq", "ones_ln"], w=[pk[1]])
            P.op("act", lambda e: e.activation(out=mean_sb, in_=PS[1][:, 0:SCB], func=AF.Copy), r=[pk[1]], w=["mean_sb"])
            P.op("dve", lambda e: e.tensor_tensor(out=var_sb, in0=mean_sb, in1=mean_sb, op=ALU.mult), r=["mean_sb"], w=["var_sb"])
            P.op("dve", lambda e: e.tensor_tensor(out=var_sb, in0=PS[1][:, SCB:2 * SCB], in1=var_sb, op=ALU.subtract), r=[pk[1], "var_sb"], w=["var_sb"])
            P.op("dve", lambda e: e.tensor_scalar(out=var_sb, in0=var_sb, scalar1=1e-5, scalar2=None, op0=ALU.add), r=["var_sb"], w=["var_sb"])
            P.op("pool", lambda e: e.tensor_tensor(out=rstd_ln, in0=var_sb, in1=nhalf[:, 0:SCB], op=ALU.pow), r=["var_sb", "nhalf"], w=["rstd_ln"])
            for c in range(4):
                P.op("dve", lambda e, c=c: e.tensor_tensor(out=ycen, in0=acc[:, c, :], in1=mean_sb, op=ALU.subtract), r=["acc", "mean_sb"], w=["ycen"])
                P.op("dve", lambda e, c=c: e.tensor_tensor(out=ycen, in0=ycen, in1=rstd_ln, op=ALU.mult), r=["ycen", "rstd_ln"], w=["ycen"])
                P.op("act", lambda e, c=c: e.activation(out=vact[:, c, :], in_=ycen, func=AF.Silu,
                                                       bias=smallcol[:, SC_LNB + c:SC_LNB + c + 1], scale=smallcol[:, SC_LNG + c:SC_LNG + c + 1]),
                     r=["ycen", "smallcol"], w=["vact"])
            for dc in range(8):
                b0 = 2 + 2 * (dc % 2)
                b1 = b0 + 1
                P.group("pe", [lambda e, c=c, dc=dc, b0=b0: e.matmul(PS[b0][:, 0:SCB], lhsT=cwo[:, c, dc * 128:(dc + 1) * 128], rhs=vact[:, c, :],
                                                                    start=(c == 0), stop=(c == 3)) for c in range(4)]
                        + [lambda e, g=g, dc=dc, b0=b0: e.matmul(PS[b0][:, SCB:2 * SCB], lhsT=fw[:, g, dc * 128:(dc + 1) * 128], rhs=frc[:, g, :],
                                                                  start=(g == 0), stop=(g == 3)) for g in range(4)],
                        r=["cwo", "fw", "vact", "frc"], w=[pk[b0]])
                P.group("pe", [lambda e, k=k, dc=dc, b1=b1: e.matmul(PS[b1][:, 0:SCB], lhsT=wg[:, k, dc * 128:(dc + 1) * 128], rhs=xhB[:, k, :],
                                                                    start=(k == 0), stop=(k == 7)) for k in range(8)]
                        + [lambda e, k=k, dc=dc, b1=b1: e.matmul(PS[b1][:, SCB:2 * SCB], lhsT=wg[:, k, 1024 + dc * 128:1024 + (dc + 1) * 128], rhs=xhB[:, k, :],
                                                                  start=(k == 0), stop=(k == 7)) for k in range(8)],
                        r=["wg", "xhB"], w=[pk[b1]])
                P.op("act", lambda e, dc=dc, b1=b1: e.activation(out=sg0, in_=PS[b1][:, 0:SCB], func=AF.Sigmoid,
                                                                bias=smallcol[:, SC_BIN + 12 + dc:SC_BIN + 13 + dc], scale=1.0),
                     r=[pk[b1], "smallcol"], w=["sg0B"])
                P.op("act", lambda e, dc=dc, b1=b1: e.activation(out=sg1, in_=PS[b1][:, SCB:2 * SCB], func=AF.Sigmoid,
                                                                bias=smallcol[:, SC_BIN + 20 + dc:SC_BIN + 21 + dc], scale=1.0),
                     r=[pk[b1], "smallcol"], w=["sg1B"])
                P.op("dve", lambda e, dc=dc, b0=b0: e.scalar_tensor_tensor(out=mm1, in0=PS[b0][:, 0:SCB], scalar=smallcol[:, SC_CBO + dc:SC_CBO + dc + 1],
                                                                          in1=sg0, op0=ALU.add, op1=ALU.mult),
                     r=[pk[b0], "sg0B", "smallcol"], w=["mm1"])
                P.op("dve", lambda e, dc=dc, b0=b0: e.scalar_tensor_tensor(out=mm2, in0=PS[b0][:, SCB:2 * SCB], scalar=smallcol[:, SC_FB + dc:SC_FB + dc + 1],
                                                                          in1=sg1, op0=ALU.add, op1=ALU.mult),
                     r=[pk[b0], "sg1B", "smallcol"], w=["mm2"])
                P.op("pool", lambda e, dc=dc: e.tensor_tensor(out=merged[:, dc, :], in0=mm1, in1=mm2, op=ALU.add),
                     r=["mm1", "mm2"], w=["merged"])
            for tt in range(SCB // 128):
                T = sc * (SCB // 128) + tt
                P.dma("sp", lambda e, T=T: e.dma_start(out=xres, in_=x[T * 128:(T + 1) * 128, :]), "ld_xres", w=["xres"])
                P.op("pool", lambda e: e.tensor_tensor(out=xres, in0=xres, in1=bcs[BC_GB1], op=ALU.add), r=["xres", "bc3"], w=["xres"])
                for dh in range(2):
                    bh = 6 + dh
                    P.group("pe", [lambda e, k=k, tt=tt, dh=dh, bh=bh: e.matmul(PS[bh], lhsT=merged[:, k, tt * 128:(tt + 1) * 128],
                                                                               rhs=wo[:, k, dh * 512:(dh + 1) * 512], start=(k == 0), stop=(k == 7))
                                   for k in range(8)], r=["merged", "wo"], w=[pk[bh]])
                    P.op("dve", lambda e, dh=dh, bh=bh: e.tensor_tensor(out=hbuf[:, dh * 512:(dh + 1) * 512], in0=PS[bh],
                                                                       in1=bcs[BC_G1][:, dh * 512:(dh + 1) * 512], op=ALU.mult),
                         r=[pk[bh], "bc2"], w=["hbuf"])
                    P.op("dve", lambda e, dh=dh: e.tensor_tensor(out=hbuf[:, dh * 512:(dh + 1) * 512], in0=hbuf[:, dh * 512:(dh + 1) * 512],
                                                                in1=xres[:, dh * 512:(dh + 1) * 512], op=ALU.add),
                         r=["hbuf", "xres"], w=["hbuf"])
                P.dma("sp", lambda e, T=T: e.dma_start(out=hd[T * 128:(T + 1) * 128, :], in_=hbuf), "st_hbuf", r=["hbuf"], w=["hd_%d" % T])
                P.op("act", lambda e, T=T: e.activation(out=u2b, in_=hbuf, func=AF.Square, accum_out=ss2[:, T:T + 1]),
                     r=["hbuf"], w=["u2b", "ss2_%d" % T])
                P.op("dve", lambda e, T=T: e.tensor_scalar(out=ss2[:, T:T + 1], in0=ss2[:, T:T + 1], scalar1=1.0 / D, scalar2=1e-6,
                                                           op0=ALU.mult, op1=ALU.add), r=["ss2_%d" % T], w=["ss2_%d" % T])
                P.op("pool", lambda e, T=T: e.tensor_tensor(out=rstd2[:, T:T + 1], in0=ss2[:, T:T + 1], in1=nhalf[:, 0:1], op=ALU.pow),
                     r=["ss2_%d" % T, "nhalf"], w=["rstd2_%d" % T])
                P.op("dve", lambda e, T=T: e.scalar_tensor_tensor(out=u2, in0=hbuf, scalar=rstd2[:, T:T + 1], in1=bcs[BC_GS2],
                                                                  op0=ALU.mult, op1=ALU.mult), r=["hbuf", "rstd2_%d" % T, "bc4"], w=["u2"])
                P.op("pool", lambda e: e.tensor_tensor(out=u2, in0=u2, in1=bcs[BC_SH2], op=ALU.add), r=["u2", "bc5"], w=["u2"])
                P.op("act", lambda e: e.activation(out=u2b, in_=u2, func=AF.Copy), r=["u2"], w=["u2b"])
                P.dma("sp", lambda e, T=T: e.dma_start(out=scr[T * 128:(T + 1) * 128, 0:D], in_=u2b), "st_u2b", r=["u2b"], w=["scr_%d" % T])
                for hf in range(2):
                    P.group("pe", [lambda e, k=k, hf=hf: e.transpose(out=PS[4 + hf][:, (k % 4) * 128:(k % 4 + 1) * 128],
                                                                    in_=u2[:, k * 128:(k + 1) * 128], identity=ident)
                                   for k in range(4 * hf, 4 * hf + 4)], r=["u2", "ident"], w=[pk[4 + hf]])
                    if hf == 0:
                        P.op("dve", lambda e: e.tensor_copy(out=u2T[:, 0:4, :], in_=PS[4].rearrange("p (a b) -> p a b", a=4)), r=[pk[4]], w=["u2T"])
                    else:
                        P.op("act", lambda e: e.activation(out=u2T[:, 4:8, :], in_=PS[5].rearrange("p (a b) -> p a b", a=4), func=AF.Copy), r=[pk[5]], w=["u2T"])
                P.group("pe", [lambda e, k=k: e.matmul(PS[1][:, 0:E], lhsT=u2T[:, k, :], rhs=rw[:, k, :], start=(k == 0), stop=(k == 7)) for k in range(8)],
                        r=["u2T", "rw"], w=[pk[1]])
                P.op("dve", lambda e, T=T: e.tensor_copy(out=logits[:, T, :], in_=PS[1][:, 0:E]), r=[pk[1]], w=["logits"])

        if stage == 3:
            P.barrier()
            P.dma("sp", lambda e: e.dma_start(out=dbg[:, 0:NT * E], in_=logits.rearrange("p a b -> p (a b)")), "dbgA")
            P.dma("sp", lambda e: e.dma_start(out=hbuf, in_=hd[3 * 128:4 * 128, :]), "dbgL", w=["hbuf"])
            P.dma("sp", lambda e: e.dma_start(out=dbg[:, 1024:2048], in_=hbuf), "dbgC", r=["hbuf"])
            P.dma("sp", lambda e: e.dma_start(out=u2b, in_=scr[3 * 128:4 * 128, 0:D]), "dbgL2", w=["u2b"])
            P.op("dve", lambda e: e.tensor_copy(out=u2, in_=u2b), r=["u2b"], w=["u2"])
            P.dma("sp", lambda e: e.dma_start(out=dbg[:, 2048:3072], in_=u2), "dbgD", r=["u2"])
            P.barrier()
            P.emit()
            return nc

        A.release(m_p2)
        P.barrier()
        aff_es = A.alloc([512], F32)
        maskt = A.alloc([512], F32)
        onest = A.alloc([512], F32)
        cum = A.alloc([512], F32)
        affp = A.alloc([4, 128], F32)
        lo = A.alloc([1], F32)
        mid = A.alloc([1], F32)
        cntp = A.alloc([1], F32)
        pred = A.alloc([1], F32)
        offs = A.alloc([1], F32)
        mx = A.alloc([NT], F32)
        sm = A.alloc([NT], F32)
        cum16 = A.alloc([512], I16)
        P.op("dve", lambda e: e.tensor_reduce(out=mx, in_=logits, axis=mybir.AxisListType.X, op=ALU.max), r=["logits"], w=["mx"])
        P.op("dve", lambda e: e.tensor_scalar(out=mx, in0=mx, scalar1=-1.0, scalar2=None, op0=ALU.mult), r=["mx"], w=["mx"])
        for T in range(NT):
            P.op("act", lambda e, T=T: e.activation(out=aff[:, T, :], in_=logits[:, T, :], func=AF.Exp, bias=mx[:, T:T + 1], scale=1.0,
                                                   accum_out=sm[:, T:T + 1]), r=["logits", "mx"], w=["aff", "sm"])
        P.op("dve", lambda e: e.reciprocal(out=sm, in_=sm), r=["sm"], w=["sm"])
        for T in range(NT):
            P.op("dve", lambda e, T=T: e.tensor_scalar(out=aff[:, T, :], in0=aff[:, T, :], scalar1=sm[:, T:T + 1], scalar2=None, op0=ALU.mult),
                 r=["aff", "sm"], w=["aff"])
        scr_v = scr.rearrange("(t p) c -> p t c", p=128)
        aff_b = aff.rearrange("p a b -> p (a b)").bitcast(BF16).rearrange("p (a b) -> p a b", a=NT)
        for q in range(4):
            P.dma("sp", lambda e, q=q: e.dma_start(out=scr_v[:, q * 8:(q + 1) * 8, D:D + 32], in_=aff_b[:, q * 8:(q + 1) * 8, :]),
                  "st_aff", r=["aff"], w=["scra_%d" % q])
        aff4 = aff.rearrange("p (s c) e -> p s c e", c=4)
        for tc in range(4):
            P.op("dve", lambda e, tc=tc: e.tensor_copy(out=affp[:, tc, :].rearrange("p (s e) -> p s e", e=E), in_=aff4[:, :, tc, :]),
                 r=["aff"], w=["affp"])
        P.group("pe", [lambda e, tc=tc: e.transpose(out=PS[0][:, tc * 128:(tc + 1) * 128], in_=affp[:, tc, :], identity=ident) for tc in range(4)],
                r=["affp", "ident"], w=[pk[0]])
        P.op("dve", lambda e: e.tensor_copy(out=aff_es, in_=PS[0]), r=[pk[0]], w=["aff_es"])
        P.op("dve", lambda e: e.memset(lo, 0.0), w=["lo"])
        P.op("pool", lambda e: e.memset(onest, 1.0), w=["onest"])
        for i in range(26):
            step = 2.0 ** -(i + 1)
            P.op("dve", lambda e, step=step: e.tensor_scalar(out=mid, in0=lo, scalar1=step, scalar2=None, op0=ALU.add), r=["lo"], w=["mid"])
            P.op("dve", lambda e: e.tensor_scalar(out=junk[:, 0:512], in0=aff_es, scalar1=mid[:, 0:1], scalar2=0.0, op0=ALU.is_ge, op1=ALU.add,
                                                  accum_out=cntp), r=["aff_es", "mid"], w=["junk", "cntp"])
            P.group("pe", [lambda e: e.matmul(PS[1][:, 0:1], lhsT=maskM, rhs=cntp, start=True, stop=True)], r=["maskM", "cntp"], w=[pk[1]])
            P.op("dve", lambda e: e.tensor_scalar(out=pred, in0=PS[1][:, 0:1], scalar1=511.5, scalar2=None, op0=ALU.is_ge), r=[pk[1]], w=["pred"])
            P.op("dve", lambda e, step=step: e.scalar_tensor_tensor(out=lo, in0=pred, scalar=step, in1=lo, op0=ALU.mult, op1=ALU.add),
                 r=["pred", "lo"], w=["lo"])
        P.op("dve", lambda e: e.tensor_scalar(out=maskt, in0=aff_es, scalar1=lo[:, 0:1], scalar2=None, op0=ALU.is_ge), r=["aff_es", "lo"], w=["maskt"])
        P.op("dve", lambda e: e.tensor_tensor_scan(out=cum, data0=onest, data1=maskt, initial=0.0, op0=ALU.mult, op1=ALU.add),
             r=["onest", "maskt"], w=["cum"])
        P.group("pe", [lambda e: e.matmul(PS[1][:, 0:1], lhsT=maskM2, rhs=cum[:, 511:512], start=True, stop=True)], r=["maskM2", "cum"], w=[pk[1]])
        P.op("dve", lambda e: e.tensor_copy(out=offs, in_=PS[1][:, 0:1]), r=[pk[1]], w=["offs"])
        P.op("dve", lambda e: e.tensor_scalar(out=cum, in0=cum, scalar1=offs[:, 0:1], scalar2=None, op0=ALU.add), r=["cum", "offs"], w=["cum"])
        P.op("dve", lambda e: e.tensor_copy(out=cum16, in_=cum), r=["cum"], w=["cum16"])
        for s8 in range(8):
            P.dma("sp", lambda e, s8=s8: e.dma_start(out=cumd[:, s8, :], in_=cum16[s8 * 16:(s8 + 1) * 16, :]), "st_cum", r=["cum16"], w=["cumd"])

        if stage == 4:
            P.barrier()
            P.dma("sp", lambda e: e.dma_start(out=dbg[:, 0:512], in_=aff_es), "dbgA")
            P.dma("sp", lambda e: e.dma_start(out=dbg[:, 512:1024], in_=cum), "dbgB")
            P.dma("sp", lambda e: e.dma_start(out=dbg[:, 1024:1025], in_=lo, allow_slow_non_contiguous=True), "dbgC")
            P.barrier()
            P.emit()
            return nc

        A.release(m_p1)
        P.barrier()
        cb = A.alloc([S], I16)
        gath = A.alloc([4, 1056], BF16)
        tokT = [A.alloc([8, 512], BF16) for _ in range(2)]
        wgs = [A.alloc([8, 1024], BF16) for _ in range(2)]
        wus = [A.alloc([8, 1024], BF16) for _ in range(2)]
        wds = [A.alloc([8, 1024], BF16) for _ in range(2)]
        slu = [A.alloc([512], F32) for _ in range(2)]
        actT = A.alloc([8, 512], BF16)
        out1 = A.alloc([4, D], F32)
        outw = [A.alloc([D], F32) for _ in range(2)]
        otmp = A.alloc([512], F32)
        gath_f = gath.rearrange("p a b -> p (a b)").bitcast(F32).rearrange("p (a b) -> p a b", a=4)

        def load_weights(u):
            s_ = u % 2
            for nm, src, dst in (("wgs", ew_gate, wgs), ("wus", ew_up, wus), ("wds", ew_down, wds)):
                P.dma("pool", lambda e, src=src, dst=dst: e.dma_start(
                    out=dst[s_].rearrange("p a b -> p (a b)").rearrange("p (a b) -> p a b", b=2048),
                    in_=src[u].rearrange("p (a b) -> p a b", b=2048)), "ld_%s%d" % (nm, s_), w=["%s%d" % (nm, s_)])

        def route_idx(e_):
            P.dma("sp", lambda e: e.dma_start(out=cb, in_=cumd[e_].rearrange("s n -> (s n)").partition_broadcast(128)), "ld_cb",
                  r=["cumd"], w=["cb"])
            for cc in range(4):
                for hf in range(2):
                    P.op("dve", lambda e, cc=cc, hf=hf: e.tensor_scalar(
                        out=junk, in0=cb[:, hf * 2048:(hf + 1) * 2048], scalar1=cvals[:, cc:cc + 1], scalar2=0.0, op0=ALU.is_le, op1=ALU.add,
                        accum_out=idxp[:, e_ * 8 + cc * 2 + hf:e_ * 8 + cc * 2 + hf + 1]), r=["cb", "cvals"], w=["junk", "idxp%d" % e_])
            idxp_v = idxp[:, e_ * 8:(e_ + 1) * 8].rearrange("p (c h) -> p c h", h=2)
            P.op("dve", lambda e: e.tensor_tensor(out=idxf[:, e_ * 4:(e_ + 1) * 4], in0=idxp_v[:, :, 0], in1=idxp_v[:, :, 1], op=ALU.add),
                 r=["idxp%d" % e_], w=["idxf%d" % e_])
            P.op("dve", lambda e: e.tensor_copy(out=idxi[:, e_ * 4:(e_ + 1) * 4], in_=idxf[:, e_ * 4:(e_ + 1) * 4]), r=["idxf%d" % e_], w=["idxi%d" % e_])

        def gather_rows(e_):
            scr_keys = ["scr_%d" % t for t in range(NT)] + ["scra_%d" % q for q in range(4)]
            for cc in range(4):
                P.dma("pool", lambda e, cc=cc: e.indirect_dma_start(
                    out=gath[:, cc, :], out_offset=None, in_=scr[:, :],
                    in_offset=bass.IndirectOffsetOnAxis(ap=idxi[:, e_ * 4 + cc:e_ * 4 + cc + 1], axis=0)),
                    "ld_gath", r=["idxi%d" % e_] + scr_keys, w=["gath"])
            P.op("dve", lambda e: e.tensor_copy(out=wsel[:, e_ * 4:(e_ + 1) * 4], in_=gath_f[:, :, 512 + e_]), r=["gath"], w=["wsel%d" % e_])

        def transpose_tok(e_):
            tb = e_ % 2
            for cc in range(4):
                bank = 6 + (cc % 2)
                P.group("pe", [lambda e, k=k, cc=cc, bank=bank: e.transpose(out=PSB[bank][:, k * 128:(k + 1) * 128],
                                                                           in_=gath[:, cc, k * 128:(k + 1) * 128], identity=identb)
                               for k in range(8)], r=["gath", "identb"], w=[pk[bank]])
                if cc % 2 == 0:
                    P.op("act", lambda e, cc=cc, bank=bank: e.activation(out=tokT[tb][:, :, cc * 128:(cc + 1) * 128],
                                                                        in_=PSB[bank].rearrange("p (k t) -> p k t", k=8), func=AF.Copy),
                         r=[pk[bank]], w=["tokT%d" % tb])
                else:
                    P.op("dve", lambda e, cc=cc, bank=bank: e.tensor_copy(out=tokT[tb][:, :, cc * 128:(cc + 1) * 128],
                                                                         in_=PSB[bank].rearrange("p (k t) -> p k t", k=8)),
                         r=[pk[bank]], w=["tokT%d" % tb])

        hd_keys = ["hd_%d" % t for t in range(NT)]
        sc_keys = ["hd_sc0", "hd_sc1", "hd_sc2", "hd_sc3"]
        ocnt = [0]

        def unit(u):
            e_, fh = u // 2, u % 2
            s_ = u % 2
            tb = e_ % 2
            for fc in range(8):
                bg_, bu_ = 2 * (fc % 2), 2 * (fc % 2) + 1
                P.group("pe", [lambda e, k=k, fc=fc, bg_=bg_: e.matmul(PS[bg_], lhsT=wgs[s_][:, k, fc * 128:(fc + 1) * 128], rhs=tokT[tb][:, k, :],
                                                                      start=(k == 0), stop=(k == 7)) for k in range(8)],
                        r=["wgs%d" % s_, "tokT%d" % tb], w=[pk[bg_]])
                P.group("pe", [lambda e, k=k, fc=fc, bu_=bu_: e.matmul(PS[bu_], lhsT=wus[s_][:, k, fc * 128:(fc + 1) * 128], rhs=tokT[tb][:, k, :],
                                                                      start=(k == 0), stop=(k == 7)) for k in range(8)],
                        r=["wus%d" % s_, "tokT%d" % tb], w=[pk[bu_]])
                si = fc % 2
                P.op("act", lambda e, bg_=bg_, si=si: e.activation(out=slu[si], in_=PS[bg_], func=AF.Silu), r=[pk[bg_]], w=["slu%d" % si])
                P.op("dve", lambda e, fc=fc, bu_=bu_, si=si: e.tensor_tensor(out=actT[:, fc, :], in0=slu[si], in1=PS[bu_], op=ALU.mult),
                     r=["slu%d" % si, pk[bu_]], w=["actT"])
            for cc in range(4):
                if fh == 1:
                    ob = ocnt[0] % 2
                    ocnt[0] += 1
                for dh in range(2):
                    bo = 4 + dh
                    P.group("pe", [lambda e, fc=fc, cc=cc, dh=dh, bo=bo: e.matmul(PS[bo], lhsT=actT[:, fc, cc * 128:(cc + 1) * 128],
                                                                                 rhs=wds[s_][:, fc, dh * 512:(dh + 1) * 512],
                                                                                 start=(fc == 0), stop=(fc == 7)) for fc in range(8)],
                            r=["actT", "wds%d" % s_], w=[pk[bo]])
                    if fh == 0:
                        P.op("act", lambda e, cc=cc, dh=dh, bo=bo: e.activation(out=out1[:, cc, dh * 512:(dh + 1) * 512], in_=PS[bo], func=AF.Copy),
                             r=[pk[bo]], w=["out1"])
                    else:
                        P.op("dve", lambda e, cc=cc, dh=dh, bo=bo: e.tensor_tensor(out=otmp, in0=PS[bo], in1=out1[:, cc, dh * 512:(dh + 1) * 512], op=ALU.add),
                             r=[pk[bo], "out1"], w=["otmp"])
                        P.op("dve", lambda e, cc=cc, dh=dh, ob=ob: e.scalar_tensor_tensor(
                            out=outw[ob][:, dh * 512:(dh + 1) * 512], in0=otmp, scalar=wsel[:, e_ * 4 + cc:e_ * 4 + cc + 1],
                            in1=bcs[BC_G2][:, dh * 512:(dh + 1) * 512], op0=ALU.mult, op1=ALU.mult),
                            r=["otmp", "wsel%d" % e_, "bc6"], w=["outw%d" % ob])
                if fh == 1:
                    P.dma("pool", lambda e, cc=cc, ob=ob: e.indirect_dma_start(
                        out=hd[:, :], out_offset=bass.IndirectOffsetOnAxis(ap=idxi[:, e_ * 4 + cc:e_ * 4 + cc + 1], axis=0),
                        in_=outw[ob], in_offset=None, compute_op=ALU.add),
                        "sc_outw%d" % ob, r=["outw%d" % ob, "idxi%d" % e_] + hd_keys + (sc_keys if cc == 0 else []),
                        w=["hd_sc%d" % cc])

        P.op("dve", lambda e: e.memset(idxp, 0.0), w=["idxp%d" % i for i in range(E)])
        load_weights(0)
        load_weights(1)
        route_idx(0)
        gather_rows(0)
        transpose_tok(0)
        NEXP = int(os.environ.get('K_NEXP', E))
        for e_ in range(NEXP):
            if e_ + 1 < E:
                route_idx(e_ + 1)
            unit(2 * e_)
            if e_ + 1 < E:
                gather_rows(e_ + 1)
            if 2 * e_ + 2 < 2 * E:
                load_weights(2 * e_ + 2)
            if e_ + 1 < E:
                transpose_tok(e_ + 1)
            unit(2 * e_ + 1)
            if 2 * e_ + 3 < 2 * E:
                load_weights(2 * e_ + 3)

        fin = [A.alloc([D], F32) for _ in range(2)]
        fss = A.alloc([NT], F32)
        frs_ = A.alloc([NT], F32)
        last = []
        for T in range(NT):
            b = T % 2
            P.dma("sp", lambda e, T=T, b=b: e.dma_start(out=fin[b], in_=hd[T * 128:(T + 1) * 128, :]), "ld_fin%d" % b,
                  r=sc_keys + ["hd_%d" % T], w=["fin%d" % b])
            P.op("act", lambda e, T=T, b=b: e.activation(out=junk[:, 0:D], in_=fin[b], func=AF.Square, accum_out=fss[:, T:T + 1]),
                 r=["fin%d" % b], w=["junk", "fss%d" % T])
            P.op("dve", lambda e, T=T: e.tensor_scalar(out=fss[:, T:T + 1], in0=fss[:, T:T + 1], scalar1=1.0 / D, scalar2=1e-6,
                                                       op0=ALU.mult, op1=ALU.add), r=["fss%d" % T], w=["fss%d" % T])
            P.op("pool", lambda e, T=T: e.tensor_tensor(out=frs_[:, T:T + 1], in0=fss[:, T:T + 1], in1=nhalf[:, 0:1], op=ALU.pow),
                 r=["fss%d" % T, "nhalf"], w=["frs%d" % T])
            P.op("dve", lambda e, T=T, b=b: e.scalar_tensor_tensor(out=fin[b], in0=fin[b], scalar=frs_[:, T:T + 1], in1=bcs[BC_FG],
                                                                   op0=ALU.mult, op1=ALU.mult), r=["fin%d" % b, "frs%d" % T, "bc7"], w=["fin%d" % b])
            last.append(P.dma("act", lambda e, T=T, b=b: e.dma_start(out=out[T * 128:(T + 1) * 128, :], in_=fin[b]), "st_fin%d" % b,
                              r=["fin%d" % b], w=["out_%d" % T]))
        P.barrier()
        P.emit()
        print("arena high-water bytes", A.hw, "of", ARENA_BYTES)
    return nc


_CONST = {}


def _dft_layout(M):
    A_ = M.reshape(4, 8, 128, 8, 512)
    return np.ascontiguousarray(A_.transpose(3, 0, 2, 1, 4)).reshape(32, 128, 4096)


_WCACHE = {}


def _weight_layouts(inp):
    key = id(inp["expert_w_gate"])
    if _WCACHE.get("key") == key:
        return _WCACHE["val"]
    f = lambda a: np.asarray(a, dtype=np.float32)
    g = f(inp["expert_w_gate"][0]).reshape(E, 8, 128, 2, 1024).transpose(0, 3, 2, 1, 4)
    u = f(inp["expert_w_up"][0]).reshape(E, 8, 128, 2, 1024).transpose(0, 3, 2, 1, 4)
    d = f(inp["expert_w_down"][0]).reshape(E, 2, 8, 128, 1024).transpose(0, 1, 3, 2, 4)
    aw = f(inp["ada_w"][0]).reshape(8, 128, 12, 512).transpose(2, 1, 0, 3)
    val = dict(
        expert_w_gate_l=np.ascontiguousarray(g).reshape(E * 2, 128, 8192),
        expert_w_up_l=np.ascontiguousarray(u).reshape(E * 2, 128, 8192),
        expert_w_down_l=np.ascontiguousarray(d).reshape(E * 2, 128, 8192),
        ada_w_l=np.ascontiguousarray(aw).reshape(12, 128, 4096),
    )
    _WCACHE["key"] = key
    _WCACHE["val"] = val
    return val


def _constants():
    if _CONST:
        return _CONST
    bf = ml_dtypes.bfloat16
    d = np.arange(128)
    ang = 2.0 * np.pi * ((d[:, None] * d[None, :]) % 128) / 128.0
    cs128 = np.concatenate([np.cos(ang), np.sin(ang)], axis=1) / np.sqrt(128.0)
    s = np.arange(S, dtype=np.int64)
    m = (s[:, None] * s[None, :]) % S
    tab_c = np.cos(2.0 * np.pi * np.arange(S) / S) / 64.0
    tab_s = -np.sin(2.0 * np.pi * np.arange(S) / S) / 64.0
    q = np.arange(128)
    e_of = q % 16
    s_of = q // 16
    maskM = (e_of[:, None] == e_of[None, :]).astype(np.float32)
    maskM2 = ((e_of[:, None] == e_of[None, :]) & (s_of[:, None] < s_of[None, :])).astype(np.float32)
    cvals = (np.arange(4)[None, :] * 128 + np.arange(128)[:, None]).astype(np.float32)
    _CONST.update(
        cs128=cs128.astype(bf),
        dft_cos_l=_dft_layout(tab_c[m].astype(bf)),
        dft_sin_l=_dft_layout(tab_s[m].astype(bf)),
        maskM=maskM, maskM2=maskM2, cvals=cvals,
    )
    return _CONST


def make_in_map(b, inp):
    f = lambda a: np.ascontiguousarray(np.asarray(a, dtype=np.float32))
    C = _constants()
    WL = _weight_layouts(inp)
    smallrows = np.concatenate([
        f(inp["b_in"][0]).reshape(28, 128), f(inp["conv_dw_b"][0]).reshape(4, 128), f(inp["conv_ln_g"][0]).reshape(4, 128),
        f(inp["conv_ln_b"][0]).reshape(4, 128), f(inp["conv_b_out"][0]).reshape(8, 128), f(inp["fourier_b"][0]).reshape(8, 128)], axis=0)
    return {
        "x": f(inp["x"][b]),
        "c_col": np.ascontiguousarray(f(inp["c"][b]).reshape(8, 128).T),
        "ada_w_l": WL["ada_w_l"],
        "ada_b": f(inp["ada_b"][0]).reshape(1, -1),
        "norm1_g": f(inp["norm1_g"][0]).reshape(1, -1),
        "w_in": f(inp["w_in"][0]),
        "smallrows": np.ascontiguousarray(smallrows),
        "dw_rows": f(inp["conv_dw_w"][0]).reshape(124, 128),
        "conv_w_out": f(inp["conv_w_out"][0]),
        "fourier_w": f(inp["fourier_w"][0]),
        "w_out": f(inp["w_out"][0]),
        "b_out": f(inp["b_out"][0]).reshape(1, -1),
        "norm2_g": f(inp["norm2_g"][0]).reshape(1, -1),
        "router_w": f(inp["router_w"][0]),
        "expert_w_gate_l": WL["expert_w_gate_l"],
        "expert_w_up_l": WL["expert_w_up_l"],
        "expert_w_down_l": WL["expert_w_down_l"],
        "final_norm_g": f(inp["final_norm_g"]).reshape(1, -1),
        "cs128": C["cs128"], "dft_cos_l": C["dft_cos_l"], "dft_sin_l": C["dft_sin_l"],
        "maskM": C["maskM"], "maskM2": C["maskM2"], "cvals": C["cvals"],
    }


def kernel(**inputs):
    nc = build_program()
    in_maps = [make_in_map(b, inputs) for b in range(8)]
    res = run_bass_kernel_spmd(nc, in_maps, core_ids=list(range(8)))
    return np.stack([np.asarray(r["out"], dtype=np.float32) for r in res.results], axis=0)
```

```python
import os
from contextlib import ExitStack
import numpy as np
import ml_dtypes
import concourse.bass as bass
import concourse.mybir as mybir
from concourse.bass_utils import run_bass_kernel_spmd

F32 = mybir.dt.float32
BF16 = mybir.dt.bfloat16
I32 = mybir.dt.int32
I16 = mybir.dt.int16
AF = mybir.ActivationFunctionType
ALU = mybir.AluOpType

S = 4096
D = 1024
NT = 32
E = 16
CAP = 512
FF = 2048
KW = 31
ENGS = ("pe", "act", "dve", "pool", "sp")


class Prog:
    def __init__(self, nc, ctx):
        self.nc = nc
        self.ctx = ctx
        self.lists = {e: [] for e in ENGS}
        self.sem = {e: ctx.enter_context(nc.semaphore("prog_" + e)) for e in ENGS}
        self.cnt = {e: 0 for e in ENGS}
        self.waited = {}
        self.dma_sems = {}
        self.dma_cnt = {}
        self.dma_waited = {}
        self.last_w = {}
        self.readers = {}

    def _emit_waits(self, eng, deps):
        for d in deps:
            if d is None:
                continue
            if d[0] == "dma":
                _, name, val = d
                key = (eng, name)
                if self.dma_waited.get(key, 0) >= val:
                    continue
                self.dma_waited[key] = val
                sem = self.dma_sems[name]
                self.lists[eng].append(lambda e, sem=sem, val=val: e.wait_ge(sem, val))
            else:
                src, val = d
                key = (eng, src)
                if self.waited.get(key, 0) >= val:
                    continue
                self.waited[key] = val
                sem = self.sem[src]
                self.lists[eng].append(lambda e, sem=sem, val=val: e.wait_ge(sem, val))

    def _deps(self, r, w, extra):
        deps = list(extra)
        for k in r:
            if k in self.last_w:
                deps.append(self.last_w[k])
        for k in w:
            if k in self.last_w:
                deps.append(self.last_w[k])
            deps.extend(self.readers.get(k, []))
        return deps

    def _commit(self, tok, r, w):
        for k in r:
            self.readers.setdefault(k, []).append(tok)
        for k in w:
            self.last_w[k] = tok
            self.readers[k] = []

    def op(self, eng, fn, r=(), w=(), extra=()):
        self._emit_waits(eng, self._deps(r, w, extra))
        self.cnt[eng] += 1
        sem = self.sem[eng]
        self.lists[eng].append(lambda e, fn=fn, sem=sem: fn(e).then_inc(sem, 1))
        tok = (eng, self.cnt[eng])
        self._commit(tok, r, w)
        return tok

    def group(self, eng, fns, r=(), w=(), extra=()):
        self._emit_waits(eng, self._deps(r, w, extra))
        for fn in fns[:-1]:
            self.lists[eng].append(lambda e, fn=fn: fn(e))
        self.cnt[eng] += 1
        sem = self.sem[eng]
        fn = fns[-1]
        self.lists[eng].append(lambda e, fn=fn, sem=sem: fn(e).then_inc(sem, 1))
        tok = (eng, self.cnt[eng])
        self._commit(tok, r, w)
        return tok

    def dma(self, eng, fn, semname, r=(), w=(), extra=()):
        if semname not in self.dma_sems:
            self.dma_sems[semname] = self.ctx.enter_context(self.nc.semaphore("d_" + semname))
            self.dma_cnt[semname] = 0
        self._emit_waits(eng, self._deps(r, w, extra))
        self.dma_cnt[semname] += 16
        sem = self.dma_sems[semname]
        self.lists[eng].append(lambda e, fn=fn, sem=sem: fn(e).then_inc(sem, 16))
        tok = ("dma", semname, self.dma_cnt[semname])
        self._commit(tok, r, w)
        return tok

    def wait(self, eng, deps):
        self._emit_waits(eng, deps)

    def barrier(self):
        deps = [(src, self.cnt[src]) for src in ENGS if self.cnt[src] > 0]
        deps += [("dma", n, v) for n, v in self.dma_cnt.items() if v > 0]
        for eng in ENGS:
            self._emit_waits(eng, [d for d in deps if d[0] != eng])

    def emit(self):
        with self.nc.Block() as block:
            @block.tensor
            def _(e):
                for f in self.lists["pe"]:
                    f(e)

            @block.scalar
            def _(e):
                for f in self.lists["act"]:
                    f(e)

            @block.vector
            def _(e):
                for f in self.lists["dve"]:
                    f(e)

            @block.gpsimd
            def _(e):
                for f in self.lists["pool"]:
                    f(e)

            @block.sync
            def _(e):
                for f in self.lists["sp"]:
                    f(e)


class Arena:
    def __init__(self, big, total_bytes):
        self.big = big
        self.total = total_bytes
        self.off = 0

    def alloc(self, shape, dt):
        n = int(np.prod(shape))
        esz = 2 if dt in (BF16, I16) else 4
        nbytes = (n * esz + 63) // 64 * 64
        off = self.off
        self.off += nbytes
        self.hw = max(getattr(self, "hw", 0), self.off)
        assert self.off <= self.total, ("SBUF arena overflow", self.off, self.total)
        v = self.big[:, off // 4:(off + nbytes) // 4]
        if dt != F32:
            v = v.bitcast(dt)
        v = v[:, 0:n]
        if len(shape) == 2:
            v = v.rearrange("p (a b) -> p a b", a=shape[0])
        elif len(shape) == 3:
            v = v.rearrange("p (a b c) -> p a b c", a=shape[0], b=shape[1])
        return v

    def mark(self):
        return self.off

    def release(self, m):
        self.off = m


ARENA_BYTES = 200 * 1024


def build_program(stage=99):
    nc = bass.Bass("TRN2", target_bir_lowering=False)

    def din(name, shape, dt=F32):
        return nc.dram_tensor(name, list(shape), dt, kind="ExternalInput").ap()

    x = din("x", [S, D])
    c_col = din("c_col", [128, 8])
    ada_w = din("ada_w_l", [12, 128, 8 * 512])
    ada_b = din("ada_b", [1, 6 * D])
    norm1_g = din("norm1_g", [1, D])
    w_in = din("w_in", [D, 3584])
    smallrows = din("smallrows", [56, 128])
    dw_rows = din("dw_rows", [124, 128])
    conv_w_out = din("conv_w_out", [512, D])
    fourier_w = din("fourier_w", [512, D])
    w_out = din("w_out", [D, D])
    b_out = din("b_out", [1, D])
    norm2_g = din("norm2_g", [1, D])
    router_w = din("router_w", [D, E])
    ew_gate = din("expert_w_gate_l", [E * 2, 128, 8 * 1024])
    ew_up = din("expert_w_up_l", [E * 2, 128, 8 * 1024])
    ew_down = din("expert_w_down_l", [E * 2, 128, 8 * 1024])
    final_g = din("final_norm_g", [1, D])
    cs128_d = din("cs128", [128, 256], BF16)
    CSd = din("dft_cos_l", [32, 128, 4096], BF16)
    SSd = din("dft_sin_l", [32, 128, 4096], BF16)
    maskM_d = din("maskM", [128, 128])
    maskM2_d = din("maskM2", [128, 128])
    cvals_d = din("cvals", [128, 4])

    out = nc.dram_tensor("out", [S, D], F32, kind="ExternalOutput").ap()
    dbg = None
    if stage < 99:
        dbg = nc.dram_tensor("dbg", [128, 16384], F32, kind="ExternalOutput").ap()

    modd = nc.dram_tensor("modd", [1, 9 * D], F32, kind="Internal").ap()
    frd = nc.dram_tensor("frd", [512, S], BF16, kind="Internal").ap()
    scr = nc.dram_tensor("scr", [S, 1056], BF16, kind="Internal").ap()
    hd = nc.dram_tensor("hd", [S, D], F32, kind="Internal").ap()
    cumd = nc.dram_tensor("cumd", [E, 8, 512], I16, kind="Internal").ap()

    with ExitStack() as ctx:
        P = Prog(nc, ctx)
        big = ctx.enter_context(nc.sbuf_tensor("big", [128, ARENA_BYTES // 4], F32))
        A = Arena(big, ARENA_BYTES)
        psb = [ctx.enter_context(nc.psum_tensor("ps%d" % i, [128, 512], F32)) for i in range(8)]
        PS = [p[:, :] for p in psb]
        PSB = [p[:, :].bitcast(BF16) for p in psb]
        pk = ["ps%d" % i for i in range(8)]

        ident = A.alloc([128], F32)
        identb = A.alloc([128], BF16)
        smallcol = A.alloc([56], F32)
        dwcol = A.alloc([124], F32)
        rstd1 = A.alloc([NT], F32)
        ss1 = A.alloc([NT], F32)
        ss2 = A.alloc([NT], F32)
        rstd2 = A.alloc([NT], F32)
        logits = A.alloc([NT, E], F32)
        aff = A.alloc([NT, E], F32)
        maskM = A.alloc([128], F32)
        maskM2 = A.alloc([128], F32)
        cvals = A.alloc([4], F32)
        ones_ln = A.alloc([128], F32)
        nhalf = A.alloc([256], F32)
        eps6 = A.alloc([1], F32)
        cact = A.alloc([8], F32)
        rw = A.alloc([8, E], F32)
        idxf = A.alloc([E * 4], F32)
        idxp = A.alloc([E * 8], F32)
        idxi = A.alloc([E * 4], I32)
        wsel = A.alloc([E * 4], F32)
        cs128 = A.alloc([256], BF16)
        ones_lnb = A.alloc([128], BF16)
        BC_GS1, BC_SH1, BC_G1, BC_GB1, BC_GS2, BC_SH2, BC_G2, BC_FG = range(8)
        bcs = [None] * 8
        m_p1 = A.mark()
        for i in (BC_GS1, BC_SH1, BC_G1, BC_GB1, BC_GS2, BC_SH2):
            bcs[i] = A.alloc([D], F32)
        m_p2 = A.mark()

        P.op("pool", lambda e: e.memset(ident, 1.0), w=["ident"])
        P.op("pool", lambda e: e.affine_select(out=ident, in_=ident, pattern=[[-1, 128]], compare_op=ALU.is_equal,
                                               fill=0.0, base=0, channel_multiplier=1), w=["ident"])
        P.op("dve", lambda e: e.tensor_copy(out=identb, in_=ident), r=["ident"], w=["identb"])
        P.op("pool", lambda e: e.memset(ones_ln, 1.0 / 512.0), w=["ones_ln"])
        P.op("pool", lambda e: e.memset(ones_lnb, 1.0 / 512.0), w=["ones_lnb"])
        P.op("pool", lambda e: e.memset(nhalf, -0.5), w=["nhalf"])
        P.op("pool", lambda e: e.memset(eps6, 1e-6), w=["eps6"])
        P.dma("sp", lambda e: e.dma_start(out=maskM, in_=maskM_d[:, :]), "c_maskM", w=["maskM"])
        P.dma("sp", lambda e: e.dma_start(out=maskM2, in_=maskM2_d[:, :]), "c_maskM2", w=["maskM2"])
        P.dma("sp", lambda e: e.dma_start(out=cvals, in_=cvals_d[:, :]), "c_cvals", w=["cvals"])
        P.dma("sp", lambda e: e.dma_start(out=cs128, in_=cs128_d[:, :]), "c_cs128", w=["cs128"])
        P.dma("sp", lambda e: e.dma_start(out=cact, in_=c_col[:, :]), "c_cact", w=["cact"])
        P.dma("sp", lambda e: e.dma_start(out=rw, in_=router_w.rearrange("(k p) n -> p k n", p=128)), "c_rw", w=["rw"])

        m0 = A.mark()
        modrow = A.alloc([9 * D], F32)
        adab = A.alloc([6 * D], F32)
        g1row = A.alloc([D], F32)
        g2row = A.alloc([D], F32)
        borow = A.alloc([D], F32)
        awb = [A.alloc([8, 512], F32) for _ in range(2)]
        rows_T = A.alloc([128], F32)
        P.dma("act", lambda e: e.dma_start(out=adab[0:1, :], in_=ada_b[:, :]), "c_adab", w=["adab"])
        P.dma("act", lambda e: e.dma_start(out=g1row[0:1, :], in_=norm1_g[:, :]), "c_g1", w=["g1row"])
        P.dma("act", lambda e: e.dma_start(out=g2row[0:1, :], in_=norm2_g[:, :]), "c_g2", w=["g2row"])
        P.dma("act", lambda e: e.dma_start(out=borow[0:1, :], in_=b_out[:, :]), "c_bo", w=["borow"])
        P.op("act", lambda e: e.activation(out=cact, in_=cact, func=AF.Silu), r=["cact"], w=["cact"])
        for pc in range(12):
            b = pc % 2
            P.dma("sp", lambda e, pc=pc, b=b: e.dma_start(out=awb[b].rearrange("p a b -> p (a b)"), in_=ada_w[pc]),
                  "awb%d" % b, w=["awb%d" % b])
            P.group("pe", [lambda e, k=k, b=b: e.matmul(PS[0][0:1, :], lhsT=cact[:, k:k + 1], rhs=awb[b][:, k, :],
                                                       start=(k == 0), stop=(k == 7)) for k in range(8)],
                    r=["cact", "awb%d" % b], w=[pk[0]])
            P.op("dve", lambda e, pc=pc: e.tensor_tensor(out=modrow[0:1, pc * 512:(pc + 1) * 512], in0=PS[0][0:1, :],
                                                        in1=adab[0:1, pc * 512:(pc + 1) * 512], op=ALU.add),
                 r=[pk[0], "adab"], w=["modrow"])
        P.op("dve", lambda e: e.scalar_tensor_tensor(out=modrow[0:1, 6 * D:7 * D], in0=modrow[0:1, D:2 * D], scalar=1.0,
                                                     in1=g1row[0:1, :], op0=ALU.add, op1=ALU.mult),
             r=["modrow", "g1row"], w=["modrow"])
        P.op("dve", lambda e: e.tensor_tensor(out=modrow[0:1, 7 * D:8 * D], in0=modrow[0:1, 2 * D:3 * D], in1=borow[0:1, :],
                                              op=ALU.mult), r=["modrow", "borow"], w=["modrow"])
        P.op("dve", lambda e: e.scalar_tensor_tensor(out=modrow[0:1, 8 * D:9 * D], in0=modrow[0:1, 4 * D:5 * D], scalar=1.0,
                                                     in1=g2row[0:1, :], op0=ALU.add, op1=ALU.mult),
             r=["modrow", "g2row"], w=["modrow"])
        P.dma("sp", lambda e: e.dma_start(out=modd[:, :], in_=modrow[0:1, :]), "st_modrow", r=["modrow"], w=["modd"])
        srcs = {BC_GS1: 6, BC_SH1: 0, BC_G1: 2, BC_GB1: 7, BC_GS2: 8, BC_SH2: 3}
        for i, off in srcs.items():
            P.dma("sp" if i % 2 == 0 else "act",
                  lambda e, i=i, off=off: e.dma_start(out=bcs[i], in_=modd[0, off * D:(off + 1) * D].partition_broadcast(128)),
                  "bc%d" % i, r=["modd"], w=["bc%d" % i])
        P.dma("sp", lambda e: e.dma_start(out=rows_T[0:56, :], in_=smallrows[:, :]), "c_rowsT", w=["rows_T"])
        P.group("pe", [lambda e: e.transpose(out=PS[1][:, 0:56], in_=rows_T[0:56, :], identity=ident[0:56, 0:56])],
                r=["rows_T", "ident"], w=[pk[1]])
        P.op("dve", lambda e: e.tensor_copy(out=smallcol, in_=PS[1][:, 0:56]), r=[pk[1]], w=["smallcol"])
        P.dma("sp", lambda e: e.dma_start(out=rows_T[0:124, :], in_=dw_rows[:, :]), "c_rowsT", w=["rows_T"])
        P.group("pe", [lambda e: e.transpose(out=PS[1][:, 0:124], in_=rows_T[0:124, :], identity=ident[0:124, 0:124])],
                r=["rows_T", "ident"], w=[pk[1]])
        P.op("dve", lambda e: e.tensor_copy(out=dwcol, in_=PS[1][:, 0:124]), r=[pk[1]], w=["dwcol"])
        SC_BIN, SC_DWB, SC_LNG, SC_LNB, SC_CBO, SC_FB = 0, 28, 32, 36, 40, 48

        if stage == 0:
            P.dma("sp", lambda e: e.dma_start(out=dbg[0:1, 0:9 * D], in_=modrow[0:1, :]), "dbg0", r=["modrow"])
            P.dma("sp", lambda e: e.dma_start(out=dbg[:, 9 * D:9 * D + 56], in_=smallcol), "dbg1", r=["smallcol"])
            P.dma("sp", lambda e: e.dma_start(out=dbg[:, 10 * D:10 * D + 124], in_=dwcol), "dbg2", r=["dwcol"])
            P.dma("sp", lambda e: e.dma_start(out=dbg[:, 11 * D:12 * D], in_=bcs[BC_GS1]), "dbg3", r=["bc0"])
            P.barrier()
            P.emit()
            return nc
        P.barrier()
        A.release(m0)

        def xhat_tile(T, xin, xin_k, tmp, xm, xhatT_dst, xhatT_k, psbank, first_pass, tmp_k="tmp"):
            P.dma("sp", lambda e: e.dma_start(out=xin, in_=x[T * 128:(T + 1) * 128, :]), "ld_" + xin_k, w=[xin_k])
            if first_pass:
                P.op("act", lambda e: e.activation(out=xm, in_=xin, func=AF.Square, accum_out=ss1[:, T:T + 1]),
                     r=[xin_k], w=["xm", "ss1_%d" % T])
                P.op("dve", lambda e: e.tensor_scalar(out=ss1[:, T:T + 1], in0=ss1[:, T:T + 1], scalar1=1.0 / D, scalar2=1e-6,
                                                      op0=ALU.mult, op1=ALU.add), r=["ss1_%d" % T], w=["ss1_%d" % T])
                P.op("pool", lambda e: e.tensor_tensor(out=rstd1[:, T:T + 1], in0=ss1[:, T:T + 1], in1=nhalf[:, 0:1], op=ALU.pow),
                     r=["ss1_%d" % T, "nhalf"], w=["rstd1_%d" % T])
            P.op("dve", lambda e: e.scalar_tensor_tensor(out=tmp, in0=xin, scalar=rstd1[:, T:T + 1], in1=bcs[BC_GS1],
                                                         op0=ALU.mult, op1=ALU.mult),
                 r=[xin_k, "rstd1_%d" % T, "bc0"], w=[tmp_k])
            P.op("pool", lambda e: e.tensor_tensor(out=xm, in0=tmp, in1=bcs[BC_SH1], op=ALU.add),
                 r=[tmp_k, "bc1"], w=["xm"])
            P.group("pe", [lambda e, k=k: e.transpose(out=PSB[psbank][:, k * 128:(k + 1) * 128], in_=xm[:, k * 128:(k + 1) * 128],
                                                     identity=identb) for k in range(8)],
                    r=["xm", "identb"], w=[pk[psbank]])
            P.op("act", lambda e: e.activation(out=xhatT_dst, in_=PSB[psbank].rearrange("p (k t) -> p k t", k=8), func=AF.Copy),
                 r=[pk[psbank]], w=[xhatT_k])

        mA = A.mark()
        vT = A.alloc([4, S + 30], BF16)
        mV = A.mark()
        G = A.alloc([NT, 4, 256], BF16)
        mA2 = A.mark()
        wA = A.alloc([8, 1536], BF16)
        xin = [A.alloc([D], F32) for _ in range(2)]
        tmp = A.alloc([D], F32)
        xm = A.alloc([D], BF16)
        xhatT = [A.alloc([8, 512], BF16)] * 2
        sg = [A.alloc([512], F32) for _ in range(2)]
        fT = [A.alloc([4, 512], BF16)] * 2

        P.op("pool", lambda e: e.memset(vT[:, :, 0:15], 0.0), w=["vT"])
        P.op("pool", lambda e: e.memset(vT[:, :, S + 15:S + 30], 0.0), w=["vT"])
        win_v = w_in.rearrange("(k p) n -> p k n", p=128)
        for k in range(8):
            P.dma("pool", lambda e, k=k: e.dma_start(out=wA[:, k, :], in_=win_v[:, k, 0:1536]), "ld_wA", w=["wA"])

        for sc in range(8):
            xb_ = sc % 2
            for tt in range(4):
                T = sc * 4 + tt
                xhat_tile(T, xin[T % 2], "xin%d" % (T % 2), tmp, xm, xhatT[xb_][:, :, tt * 128:(tt + 1) * 128],
                          "xhatT", T % 2, True)
            xk = "xhatT"
            for c in range(4):
                ba, bg = 2 + 2 * (c % 2), 3 + 2 * (c % 2)
                P.group("pe", [lambda e, k=k, c=c, ba=ba: e.matmul(PS[ba], lhsT=wA[:, k, c * 128:(c + 1) * 128], rhs=xhatT[xb_][:, k, :],
                                                                  start=(k == 0), stop=(k == 7)) for k in range(8)],
                        r=["wA", xk], w=[pk[ba]])
                P.group("pe", [lambda e, k=k, c=c, bg=bg: e.matmul(PS[bg], lhsT=wA[:, k, 512 + c * 128:512 + (c + 1) * 128], rhs=xhatT[xb_][:, k, :],
                                                                  start=(k == 0), stop=(k == 7)) for k in range(8)],
                        r=["wA", xk], w=[pk[bg]])
                sgi = c % 2
                P.op("act", lambda e, c=c, bg=bg, sgi=sgi: e.activation(out=sg[sgi], in_=PS[bg], func=AF.Sigmoid,
                                                                      bias=smallcol[:, SC_BIN + 4 + c:SC_BIN + 5 + c], scale=1.0),
                     r=[pk[bg], "smallcol"], w=["sg%d" % sgi])
                P.op("dve", lambda e, c=c, ba=ba, sgi=sgi, sc=sc: e.scalar_tensor_tensor(
                    out=vT[:, c, 15 + sc * 512:15 + (sc + 1) * 512], in0=PS[ba], scalar=smallcol[:, SC_BIN + c:SC_BIN + c + 1],
                    in1=sg[sgi], op0=ALU.add, op1=ALU.mult),
                    r=[pk[ba], "sg%d" % sgi, "smallcol"], w=["vT_%d" % sc])
            fb_ = sc % 2
            for g in range(4):
                bf = 2 + (g % 4)
                P.group("pe", [lambda e, k=k, g=g, bf=bf: e.matmul(PS[bf], lhsT=wA[:, k, 1024 + g * 128:1024 + (g + 1) * 128], rhs=xhatT[xb_][:, k, :],
                                                                  start=(k == 0), stop=(k == 7)) for k in range(8)],
                        r=["wA", xk], w=[pk[bf]])
                P.op("act", lambda e, g=g, bf=bf: e.activation(out=fT[fb_][:, g, :], in_=PS[bf], func=AF.Identity,
                                                              bias=smallcol[:, SC_BIN + 8 + g:SC_BIN + 9 + g], scale=1.0),
                     r=[pk[bf], "smallcol"], w=["fT"])
            for tt in range(4):
                T = sc * 4 + tt
                for hb in range(2):
                    bank = 6 + hb
                    P.group("pe", [lambda e, g=g, tt=tt, bank=bank: e.matmul(
                        PS[bank][:, (g % 2) * 256:(g % 2 + 1) * 256], lhsT=fT[fb_][:, g, tt * 128:(tt + 1) * 128], rhs=cs128,
                        start=True, stop=True) for g in (2 * hb, 2 * hb + 1)],
                        r=["fT", "cs128"], w=[pk[bank]])
                    eng = "dve" if hb == 0 else "act"
                    if eng == "dve":
                        P.op("dve", lambda e, T=T, hb=hb, bank=bank: e.tensor_copy(
                            out=G[:, T, 2 * hb:2 * hb + 2, :], in_=PS[bank].rearrange("p (a b) -> p a b", a=2)),
                            r=[pk[bank]], w=["G_%d" % T])
                    else:
                        P.op("act", lambda e, T=T, hb=hb, bank=bank: e.activation(
                            out=G[:, T, 2 * hb:2 * hb + 2, :], in_=PS[bank].rearrange("p (a b) -> p a b", a=2), func=AF.Copy),
                            r=[pk[bank]], w=["G_%d" % T])

        if stage == 1:
            P.barrier()
            dtmp = xin[0]
            n = [0]

            def dump(src, col0, ncols):
                n[0] += 1
                P.op("dve", lambda e: e.tensor_copy(out=dtmp[:, 0:ncols], in_=src), r=["xin0"], w=["xin0"])
                P.dma("sp", lambda e: e.dma_start(out=dbg[:, col0:col0 + ncols], in_=dtmp[:, 0:ncols]), "dbg%d" % n[0], r=["xin0"])
            for q in range(4):
                dump(vT[:, 1, 15 + q * 1024:15 + (q + 1) * 1024], q * 1024, 1024)
            dump(G[:, 5, :, :].rearrange("p a b -> p (a b)"), 4096, 1024)
            dump(rstd1, 5120, NT)
            P.barrier()
            P.emit()
            return nc

        A.release(mA2)
        P.barrier()
        csb = [A.alloc([8, 512], BF16) for _ in range(2)]
        ssb = [A.alloc([8, 512], BF16) for _ in range(2)]
        frs = [A.alloc([4, 512], BF16) for _ in range(2)]
        Gkeys = ["G_%d" % t for t in range(NT)]
        it = 0
        for kc in range(8):
            bset = 4 * (kc % 2)
            for tg in range(4):
                b = it % 2
                it += 1
                P.dma("sp", lambda e, b=b, tg=tg, kc=kc: e.dma_start(out=csb[b].rearrange("p a b -> p (a b)"), in_=CSd[kc * 4 + tg]),
                      "ld_csb%d" % b, w=["csb%d" % b])
                P.dma("act", lambda e, b=b, tg=tg, kc=kc: e.dma_start(out=ssb[b].rearrange("p a b -> p (a b)"), in_=SSd[kc * 4 + tg]),
                      "ld_ssb%d" % b, w=["ssb%d" % b])
                fns = []
                for t8 in range(8):
                    t = tg * 8 + t8
                    for g in range(4):
                        fns.append(lambda e, t=t, t8=t8, g=g, b=b, bset=bset: e.matmul(
                            PS[bset + g], lhsT=G[:, t, g, 0:128], rhs=csb[b][:, t8, :], start=(t == 0), stop=False))
                        fns.append(lambda e, t=t, t8=t8, g=g, b=b, bset=bset: e.matmul(
                            PS[bset + g], lhsT=G[:, t, g, 128:256], rhs=ssb[b][:, t8, :], start=False, stop=(t == NT - 1)))
                P.group("pe", fns, r=Gkeys + ["csb%d" % b, "ssb%d" % b], w=[pk[bset + g] for g in range(4)])
            fb_ = kc % 2
            for g in range(4):
                if g % 2 == 0:
                    P.op("dve", lambda e, g=g, bset=bset, fb_=fb_: e.tensor_copy(out=frs[fb_][:, g, :], in_=PS[bset + g]),
                         r=[pk[bset + g]], w=["frs%d" % fb_])
                else:
                    P.op("act", lambda e, g=g, bset=bset, fb_=fb_: e.activation(out=frs[fb_][:, g, :], in_=PS[bset + g], func=AF.Copy),
                         r=[pk[bset + g]], w=["frs%d" % fb_])
            P.dma("sp", lambda e, kc=kc, fb_=fb_: e.dma_start(
                out=frd.rearrange("(g j) n -> j g n", j=128)[:, :, kc * 512:(kc + 1) * 512], in_=frs[fb_]),
                "st_frs%d" % fb_, r=["frs%d" % fb_], w=["frd_%d" % kc])

        if stage == 2:
            P.barrier()
            ld = A.alloc([S], BF16)
            dtmp = A.alloc([D], F32)
            P.dma("sp", lambda e: e.dma_start(out=ld, in_=frd[128:256, :]), "dbgL", w=["ld"])
            for q in range(4):
                P.op("dve", lambda e, q=q: e.tensor_copy(out=dtmp, in_=ld[:, q * 1024:(q + 1) * 1024]), r=["ld", "dtmp"], w=["dtmp"])
                P.dma("sp", lambda e, q=q: e.dma_start(out=dbg[:, q * 1024:(q + 1) * 1024], in_=dtmp), "dbgA%d" % q, r=["dtmp"])
            P.barrier()
            P.emit()
            return nc

        A.release(mV)
        P.barrier()
        vkeys = ["vT_%d" % i for i in range(8)] + ["vT"]
        convT = A.alloc([4, S], BF16)
        conv_end = A.mark()
        diag = A.alloc([4, KW, 128], BF16)
        n_ = 0
        for c in range(4):
            for tap in range(KW):
                eng = "dve"
                n_ += 1
                P.op(eng, lambda e, c=c, tap=tap: e.tensor_scalar(out=diag[:, c, tap, :], in0=identb, scalar1=dwcol[:, tap * 4 + c:tap * 4 + c + 1],
                                                                scalar2=None, op0=ALU.mult), r=["identb", "dwcol"], w=["diag%d" % c])
        ev = 0
        for c in range(4):
            for blk in range(8):
                bank = ev % 8
                P.group("pe", [lambda e, c=c, blk=blk, tap=tap, bank=bank: e.matmul(
                    PS[bank], lhsT=diag[:, c, tap, :], rhs=vT[:, c, blk * 512 + tap:blk * 512 + tap + 512], start=(tap == 0), stop=(tap == KW - 1))
                    for tap in range(KW)], r=["diag%d" % c] + vkeys, w=[pk[bank]])
                if ev % 2 == 0:
                    P.op("act", lambda e, c=c, blk=blk, bank=bank: e.activation(out=convT[:, c, blk * 512:(blk + 1) * 512], in_=PS[bank], func=AF.Identity,
                                                                              bias=smallcol[:, SC_DWB + c:SC_DWB + c + 1], scale=1.0),
                         r=[pk[bank], "smallcol"], w=["convT"])
                else:
                    P.op("dve", lambda e, c=c, blk=blk, bank=bank: e.tensor_scalar(out=convT[:, c, blk * 512:(blk + 1) * 512], in0=PS[bank],
                                                                                 scalar1=smallcol[:, SC_DWB + c:SC_DWB + c + 1], scalar2=None, op0=ALU.add),
                         r=[pk[bank], "smallcol"], w=["convT"])
                ev += 1

        if stage == 25:
            P.barrier()
            A.release(mA2)
            dtmp = A.alloc([D], F32)
            for q in range(4):
                P.op("dve", lambda e, q=q: e.tensor_copy(out=dtmp, in_=convT[:, 1, q * 1024:(q + 1) * 1024]), r=["dtmp"], w=["dtmp"])
                P.dma("sp", lambda e, q=q: e.dma_start(out=dbg[:, q * 1024:(q + 1) * 1024], in_=dtmp), "dbgA%d" % q, r=["dtmp"])
            P.barrier()
            P.emit()
            return nc

        P.barrier()
        A.release(mA)
        cwo = A.alloc([4, D], BF16)
        fw = A.alloc([4, D], BF16)
        wo = A.alloc([8, D], BF16)
        assert A.mark() <= mV
        A.release(conv_end)
        wg = A.alloc([8, 2048], BF16)
        SCB = 256
        NSC = S // SCB
        TPS = SCB // 128
        xinB = [A.alloc([D], F32) for _ in range(2)]
        xmB = A.alloc([D], BF16)
        xhB = [A.alloc([8, SCB], BF16) for _ in range(2)]
        sq = A.alloc([4, SCB], F32)
        mean_sb = A.alloc([SCB], F32)
        var_sb = A.alloc([SCB], F32)
        rstd_ln = A.alloc([SCB], F32)
        ycen = [A.alloc([SCB], F32) for _ in range(2)]
        vact = [A.alloc([4, SCB], BF16) for _ in range(2)]
        sg0 = [A.alloc([SCB], F32) for _ in range(2)]
        sg1 = [A.alloc([SCB], F32) for _ in range(2)]
        mm1 = A.alloc([SCB], F32)
        mm2 = A.alloc([SCB], F32)
        merged = A.alloc([8, SCB], BF16)
        hbuf = [A.alloc([D], F32) for _ in range(2)]
        htmp = A.alloc([512], F32)
        u2b = [A.alloc([D], BF16) for _ in range(2)]
        u2T = A.alloc([8, 128], F32)
        frc = [A.alloc([4, SCB], BF16) for _ in range(2)]

        for k in range(8):
            P.dma("pool", lambda e, k=k: e.dma_start(out=wg[:, k, :], in_=win_v[:, k, 1536:3584]), "ld_wg", w=["wg"])
            P.dma("pool", lambda e, k=k: e.dma_start(out=wo[:, k, :], in_=w_out.rearrange("(k p) n -> p k n", p=128)[:, k, :]), "ld_wo", w=["wo"])
        for c in range(4):
            P.dma("pool", lambda e, c=c: e.dma_start(out=cwo[:, c, :], in_=conv_w_out.rearrange("(k p) n -> p k n", p=128)[:, c, :]), "ld_cwo", w=["cwo"])
            P.dma("pool", lambda e, c=c: e.dma_start(out=fw[:, c, :], in_=fourier_w.rearrange("(k p) n -> p k n", p=128)[:, c, :]), "ld_fw", w=["fw"])
        frd_v = frd.rearrange("(g j) n -> j g n", j=128)

        def stage_X(sc):
            p = sc % 2
            T0 = sc * SCB
            P.dma("act", lambda e: e.dma_start(out=frc[p], in_=frd_v[:, :, T0:T0 + SCB]), "ld_frc%d" % p, w=["frc%d" % p])
            for tt in range(TPS):
                T = sc * TPS + tt
                xk = "xinB%d" % (T % 2)
                xhat_tile(T, xinB[T % 2], xk, xinB[T % 2], xmB, xhB[p][:, :, tt * 128:(tt + 1) * 128], "xhB%d" % p, 0, False, tmp_k=xk)

        def stage_L(sc):
            p = sc % 2
            T0 = sc * SCB
            P.op("act", lambda e: e.activation(out=sq, in_=convT[:, :, T0:T0 + SCB], func=AF.Square), r=["convT"], w=["sq"])
            P.group("pe", [lambda e, c=c: e.matmul(PS[1][:, 0:SCB], lhsT=ones_lnb, rhs=convT[:, c, T0:T0 + SCB], start=(c == 0), stop=(c == 3)) for c in range(4)]
                    + [lambda e, c=c: e.matmul(PS[1][:, SCB:2 * SCB], lhsT=ones_ln, rhs=sq[:, c, :], start=(c == 0), stop=(c == 3)) for c in range(4)],
                    r=["convT", "sq", "ones_ln", "ones_lnb"], w=[pk[1]])
            P.op("act", lambda e: e.activation(out=mean_sb, in_=PS[1][:, 0:SCB], func=AF.Copy), r=[pk[1]], w=["mean_sb"])
            P.op("dve", lambda e: e.tensor_tensor(out=var_sb, in0=mean_sb, in1=mean_sb, op=ALU.mult), r=["mean_sb"], w=["var_sb"])
            P.op("dve", lambda e: e.tensor_tensor(out=var_sb, in0=PS[1][:, SCB:2 * SCB], in1=var_sb, op=ALU.subtract), r=[pk[1], "var_sb"], w=["var_sb"])
            P.op("dve", lambda e: e.tensor_scalar(out=var_sb, in0=var_sb, scalar1=1e-5, scalar2=None, op0=ALU.add), r=["var_sb"], w=["var_sb"])
            P.op("pool", lambda e: e.tensor_tensor(out=rstd_ln, in0=var_sb, in1=nhalf[:, 0:SCB], op=ALU.pow), r=["var_sb", "nhalf"], w=["rstd_ln"])
            for c in range(4):
                y = ycen[c % 2]
                yk = "ycen%d" % (c % 2)
                P.op("dve", lambda e, c=c, y=y: e.tensor_tensor(out=y, in0=convT[:, c, T0:T0 + SCB], in1=mean_sb, op=ALU.subtract), r=["convT", "mean_sb"], w=[yk])
                P.op("dve", lambda e, y=y: e.tensor_tensor(out=y, in0=y, in1=rstd_ln, op=ALU.mult), r=[yk, "rstd_ln"], w=[yk])
                P.op("act", lambda e, c=c, y=y: e.activation(out=vact[p][:, c, :], in_=y, func=AF.Silu,
                                                            bias=smallcol[:, SC_LNB + c:SC_LNB + c + 1], scale=smallcol[:, SC_LNG + c:SC_LNG + c + 1]),
                     r=[yk, "smallcol"], w=["vact%d" % p])

        def stage_M(sc):
            p = sc % 2
            for dc in range(8):
                q = dc % 2
                b0 = 2 + 2 * q
                b1 = b0 + 1
                P.group("pe", [lambda e, c=c, dc=dc, b0=b0: e.matmul(PS[b0][:, 0:SCB], lhsT=cwo[:, c, dc * 128:(dc + 1) * 128], rhs=vact[p][:, c, :],
                                                                    start=(c == 0), stop=(c == 3)) for c in range(4)]
                        + [lambda e, g=g, dc=dc, b0=b0: e.matmul(PS[b0][:, SCB:2 * SCB], lhsT=fw[:, g, dc * 128:(dc + 1) * 128], rhs=frc[p][:, g, :],
                                                                  start=(g == 0), stop=(g == 3)) for g in range(4)],
                        r=["cwo", "fw", "vact%d" % p, "frc%d" % p], w=[pk[b0]])
                P.group("pe", [lambda e, k=k, dc=dc, b1=b1: e.matmul(PS[b1][:, 0:SCB], lhsT=wg[:, k, dc * 128:(dc + 1) * 128], rhs=xhB[p][:, k, :],
                                                                    start=(k == 0), stop=(k == 7)) for k in range(8)]
                        + [lambda e, k=k, dc=dc, b1=b1: e.matmul(PS[b1][:, SCB:2 * SCB], lhsT=wg[:, k, 1024 + dc * 128:1024 + (dc + 1) * 128], rhs=xhB[p][:, k, :],
                                                                  start=(k == 0), stop=(k == 7)) for k in range(8)],
                        r=["wg", "xhB%d" % p], w=[pk[b1]])
                P.op("act", lambda e, dc=dc, b1=b1, q=q: e.activation(out=sg0[q], in_=PS[b1][:, 0:SCB], func=AF.Sigmoid,
                                                                     bias=smallcol[:, SC_BIN + 12 + dc:SC_BIN + 13 + dc], scale=1.0),
                     r=[pk[b1], "smallcol"], w=["sg0B%d" % q])
                P.op("act", lambda e, dc=dc, b1=b1, q=q: e.activation(out=sg1[q], in_=PS[b1][:, SCB:2 * SCB], func=AF.Sigmoid,
                                                                     bias=smallcol[:, SC_BIN + 20 + dc:SC_BIN + 21 + dc], scale=1.0),
                     r=[pk[b1], "smallcol"], w=["sg1B%d" % q])
                P.op("dve", lambda e, dc=dc, b0=b0, q=q: e.scalar_tensor_tensor(out=mm1, in0=PS[b0][:, 0:SCB], scalar=smallcol[:, SC_CBO + dc:SC_CBO + dc + 1],
                                                                               in1=sg0[q], op0=ALU.add, op1=ALU.mult),
                     r=[pk[b0], "sg0B%d" % q, "smallcol"], w=["mm1"])
                P.op("dve", lambda e, dc=dc, b0=b0, q=q: e.scalar_tensor_tensor(out=mm2, in0=PS[b0][:, SCB:2 * SCB], scalar=smallcol[:, SC_FB + dc:SC_FB + dc + 1],
                                                                               in1=sg1[q], op0=ALU.add, op1=ALU.mult),
                     r=[pk[b0], "sg1B%d" % q, "smallcol"], w=["mm2"])
                P.op("pool", lambda e, dc=dc: e.tensor_tensor(out=merged[:, dc, :], in0=mm1, in1=mm2, op=ALU.add),
                     r=["mm1", "mm2"], w=["merged"])

        def router(T):
            hb = hbuf[T % 2]
            hk = "hbuf%d" % (T % 2)
            for hf in range(2):
                P.group("pe", [lambda e, k=k, hf=hf: e.transpose(out=PS[hf][:, (k % 4) * 128:(k % 4 + 1) * 128],
                                                                in_=hb[:, k * 128:(k + 1) * 128], identity=ident)
                               for k in range(4 * hf, 4 * hf + 4)], r=[hk, "ident"], w=[pk[hf]])
                if hf == 0:
                    P.op("dve", lambda e: e.tensor_copy(out=u2T[:, 0:4, :], in_=PS[0].rearrange("p (a b) -> p a b", a=4)), r=[pk[0]], w=["u2T"])
                else:
                    P.op("act", lambda e: e.activation(out=u2T[:, 4:8, :], in_=PS[1].rearrange("p (a b) -> p a b", a=4), func=AF.Copy), r=[pk[1]], w=["u2T"])
            P.group("pe", [lambda e, k=k: e.matmul(PS[1][:, 0:E], lhsT=u2T[:, k, :], rhs=rw[:, k, :], start=(k == 0), stop=(k == 7)) for k in range(8)],
                    r=["u2T", "rw"], w=[pk[1]])
            P.op("dve", lambda e: e.tensor_copy(out=logits[:, T, :], in_=PS[1][:, 0:E]), r=[pk[1]], w=["logits"])

        def stage_H(sc):
            for tt in range(TPS):
                T = sc * TPS + tt
                hb = hbuf[T % 2]
                hk = "hbuf%d" % (T % 2)
                ub = u2b[T % 2]
                uk = "u2b%d" % (T % 2)
                P.dma("sp", lambda e, hb=hb, T=T: e.dma_start(out=hb, in_=x[T * 128:(T + 1) * 128, :]), "ld_" + hk, w=[hk])
                P.op("pool", lambda e, hb=hb: e.tensor_tensor(out=hb, in0=hb, in1=bcs[BC_GB1], op=ALU.add), r=[hk, "bc3"], w=[hk])
                for dh in range(2):
                    bh = 6 + dh
                    P.group("pe", [lambda e, k=k, tt=tt, dh=dh, bh=bh: e.matmul(PS[bh], lhsT=merged[:, k, tt * 128:(tt + 1) * 128],
                                                                               rhs=wo[:, k, dh * 512:(dh + 1) * 512], start=(k == 0), stop=(k == 7))
                                   for k in range(8)], r=["merged", "wo"], w=[pk[bh]])
                    P.op("dve", lambda e, dh=dh, bh=bh: e.tensor_tensor(out=htmp, in0=PS[bh], in1=bcs[BC_G1][:, dh * 512:(dh + 1) * 512], op=ALU.mult),
                         r=[pk[bh], "bc2"], w=["htmp"])
                    P.op("dve", lambda e, dh=dh, hb=hb: e.tensor_tensor(out=hb[:, dh * 512:(dh + 1) * 512], in0=hb[:, dh * 512:(dh + 1) * 512],
                                                                       in1=htmp, op=ALU.add), r=[hk, "htmp"], w=[hk])
                P.dma("sp", lambda e, T=T, hb=hb: e.dma_start(out=hd[T * 128:(T + 1) * 128, :], in_=hb), "st_" + hk, r=[hk], w=["hd_%d" % T])
                P.op("act", lambda e, T=T, hb=hb, ub=ub: e.activation(out=ub, in_=hb, func=AF.Square, accum_out=ss2[:, T:T + 1]),
                     r=[hk], w=[uk, "ss2_%d" % T])
                P.op("dve", lambda e, T=T: e.tensor_scalar(out=ss2[:, T:T + 1], in0=ss2[:, T:T + 1], scalar1=1.0 / D, scalar2=1e-6,
                                                           op0=ALU.mult, op1=ALU.add), r=["ss2_%d" % T], w=["ss2_%d" % T])
                P.op("pool", lambda e, T=T: e.tensor_tensor(out=rstd2[:, T:T + 1], in0=ss2[:, T:T + 1], in1=nhalf[:, 0:1], op=ALU.pow),
                     r=["ss2_%d" % T, "nhalf"], w=["rstd2_%d" % T])
                P.op("dve", lambda e, T=T, hb=hb: e.scalar_tensor_tensor(out=hb, in0=hb, scalar=rstd2[:, T:T + 1], in1=bcs[BC_GS2],
                                                                        op0=ALU.mult, op1=ALU.mult), r=[hk, "rstd2_%d" % T, "bc4"], w=[hk])
                P.op("pool", lambda e, hb=hb: e.tensor_tensor(out=hb, in0=hb, in1=bcs[BC_SH2], op=ALU.add), r=[hk, "bc5"], w=[hk])
                P.op("act", lambda e, hb=hb, ub=ub: e.activation(out=ub, in_=hb, func=AF.Copy), r=[hk], w=[uk])
                P.dma("sp", lambda e, T=T, ub=ub: e.dma_start(out=scr[T * 128:(T + 1) * 128, 0:D], in_=ub), "st_" + uk, r=[uk], w=["scr_%d" % T])
                if T > 0:
                    router(T - 1)

        stage_X(0)
        stage_L(0)
        for sc in range(NSC):
            if sc + 1 < NSC:
                stage_X(sc + 1)
                stage_L(sc + 1)
            stage_M(sc)
            stage_H(sc)
        router(NT - 1)

        if stage == 3:
            P.barrier()
            P.dma("sp", lambda e: e.dma_start(out=dbg[:, 0:NT * E], in_=logits.rearrange("p a b -> p (a b)")), "dbgA")
            P.dma("sp", lambda e: e.dma_start(out=hbuf[0], in_=hd[3 * 128:4 * 128, :]), "dbgL", w=["hb0x"])
            P.dma("sp", lambda e: e.dma_start(out=dbg[:, 1024:2048], in_=hbuf[0]), "dbgC", r=["hb0x"])
            P.dma("sp", lambda e: e.dma_start(out=u2b[0], in_=scr[3 * 128:4 * 128, 0:D]), "dbgL2", w=["ub0x"])
            P.op("dve", lambda e: e.tensor_copy(out=hbuf[1], in_=u2b[0]), r=["ub0x"], w=["hb1x"])
            P.dma("sp", lambda e: e.dma_start(out=dbg[:, 2048:3072], in_=hbuf[1]), "dbgD", r=["hb1x"])
            P.barrier()
            P.emit()
            return nc

        A.release(m_p2)
        P.barrier()
        junk = A.alloc([2048], BF16)
        aff_es = A.alloc([512], F32)
        maskt = A.alloc([512], F32)
        onest = A.alloc([512], F32)
        cum = A.alloc([512], F32)
        affp = A.alloc([4, 128], F32)
        lo = A.alloc([1], F32)
        mid = A.alloc([1], F32)
        cntp = A.alloc([1], F32)
        pred = A.alloc([1], F32)
        offs = A.alloc([1], F32)
        mx = A.alloc([NT], F32)
        sm = A.alloc([NT], F32)
        cum16 = A.alloc([512], I16)
        P.op("dve", lambda e: e.tensor_reduce(out=mx, in_=logits, axis=mybir.AxisListType.X, op=ALU.max), r=["logits"], w=["mx"])
        P.op("dve", lambda e: e.tensor_scalar(out=mx, in0=mx, scalar1=-1.0, scalar2=None, op0=ALU.mult), r=["mx"], w=["mx"])
        for T in range(NT):
            P.op("act", lambda e, T=T: e.activation(out=aff[:, T, :], in_=logits[:, T, :], func=AF.Exp, bias=mx[:, T:T + 1], scale=1.0,
                                                   accum_out=sm[:, T:T + 1]), r=["logits", "mx"], w=["aff", "sm"])
        P.op("dve", lambda e: e.reciprocal(out=sm, in_=sm), r=["sm"], w=["sm"])
        for T in range(NT):
            P.op("dve", lambda e, T=T: e.tensor_scalar(out=aff[:, T, :], in0=aff[:, T, :], scalar1=sm[:, T:T + 1], scalar2=None, op0=ALU.mult),
                 r=["aff", "sm"], w=["aff"])
        scr_v = scr.rearrange("(t p) c -> p t c", p=128)
        aff_b = aff.rearrange("p a b -> p (a b)").bitcast(BF16).rearrange("p (a b) -> p a b", a=NT)
        for q in range(4):
            P.dma("sp", lambda e, q=q: e.dma_start(out=scr_v[:, q * 8:(q + 1) * 8, D:D + 32], in_=aff_b[:, q * 8:(q + 1) * 8, :]),
                  "st_aff", r=["aff"], w=["scra_%d" % q])
        aff4 = aff.rearrange("p (s c) e -> p s c e", c=4)
        for tc in range(4):
            P.op("dve", lambda e, tc=tc: e.tensor_copy(out=affp[:, tc, :].rearrange("p (s e) -> p s e", e=E), in_=aff4[:, :, tc, :]),
                 r=["aff"], w=["affp"])
        P.group("pe", [lambda e, tc=tc: e.transpose(out=PS[0][:, tc * 128:(tc + 1) * 128], in_=affp[:, tc, :], identity=ident) for tc in range(4)],
                r=["affp", "ident"], w=[pk[0]])
        P.op("dve", lambda e: e.tensor_copy(out=aff_es, in_=PS[0]), r=[pk[0]], w=["aff_es"])
        P.op("dve", lambda e: e.memset(lo, 0.0), w=["lo"])
        P.op("pool", lambda e: e.memset(onest, 1.0), w=["onest"])
        for i in range(26):
            step = 2.0 ** -(i + 1)
            P.op("dve", lambda e, step=step: e.tensor_scalar(out=mid, in0=lo, scalar1=step, scalar2=None, op0=ALU.add), r=["lo"], w=["mid"])
            P.op("dve", lambda e: e.tensor_scalar(out=junk[:, 0:512], in0=aff_es, scalar1=mid[:, 0:1], scalar2=0.0, op0=ALU.is_ge, op1=ALU.add,
                                                  accum_out=cntp), r=["aff_es", "mid"], w=["junk", "cntp"])
            P.group("pe", [lambda e: e.matmul(PS[1][:, 0:1], lhsT=maskM, rhs=cntp, start=True, stop=True)], r=["maskM", "cntp"], w=[pk[1]])
            P.op("dve", lambda e: e.tensor_scalar(out=pred, in0=PS[1][:, 0:1], scalar1=511.5, scalar2=None, op0=ALU.is_ge), r=[pk[1]], w=["pred"])
            P.op("dve", lambda e, step=step: e.scalar_tensor_tensor(out=lo, in0=pred, scalar=step, in1=lo, op0=ALU.mult, op1=ALU.add),
                 r=["pred", "lo"], w=["lo"])
        P.op("dve", lambda e: e.tensor_scalar(out=maskt, in0=aff_es, scalar1=lo[:, 0:1], scalar2=None, op0=ALU.is_ge), r=["aff_es", "lo"], w=["maskt"])
        P.op("dve", lambda e: e.tensor_tensor_scan(out=cum, data0=onest, data1=maskt, initial=0.0, op0=ALU.mult, op1=ALU.add),
             r=["onest", "maskt"], w=["cum"])
        P.group("pe", [lambda e: e.matmul(PS[1][:, 0:1], lhsT=maskM2, rhs=cum[:, 511:512], start=True, stop=True)], r=["maskM2", "cum"], w=[pk[1]])
        P.op("dve", lambda e: e.tensor_copy(out=offs, in_=PS[1][:, 0:1]), r=[pk[1]], w=["offs"])
        P.op("dve", lambda e: e.tensor_scalar(out=cum, in0=cum, scalar1=offs[:, 0:1], scalar2=None, op0=ALU.add), r=["cum", "offs"], w=["cum"])
        P.op("dve", lambda e: e.tensor_copy(out=cum16, in_=cum), r=["cum"], w=["cum16"])
        for s8 in range(8):
            P.dma("sp", lambda e, s8=s8: e.dma_start(out=cumd[:, s8, :], in_=cum16[s8 * 16:(s8 + 1) * 16, :]), "st_cum", r=["cum16"], w=["cumd"])

        if stage == 4:
            P.barrier()
            P.dma("sp", lambda e: e.dma_start(out=dbg[:, 0:512], in_=aff_es), "dbgA")
            P.dma("sp", lambda e: e.dma_start(out=dbg[:, 512:1024], in_=cum), "dbgB")
            P.dma("sp", lambda e: e.dma_start(out=dbg[:, 1024:1025], in_=lo, allow_slow_non_contiguous=True), "dbgC")
            P.barrier()
            P.emit()
            return nc

        A.release(m_p1)
        P.barrier()
        junk = A.alloc([2048], BF16)
        bcs[BC_G2] = A.alloc([D], F32)
        bcs[BC_FG] = A.alloc([D], F32)
        P.dma("sp", lambda e: e.dma_start(out=bcs[BC_G2], in_=modd[0, 5 * D:6 * D].partition_broadcast(128)), "bc6", w=["bc6"])
        P.dma("act", lambda e: e.dma_start(out=bcs[BC_FG], in_=final_g[0, :].partition_broadcast(128)), "bc7", w=["bc7"])
        cb = A.alloc([S], I16)
        gath = A.alloc([4, 1056], BF16)
        tokT = [A.alloc([8, 512], BF16) for _ in range(2)]
        wgs = [A.alloc([8, 1024], BF16) for _ in range(2)]
        wus = [A.alloc([8, 1024], BF16) for _ in range(2)]
        wds = [A.alloc([8, 1024], BF16) for _ in range(2)]
        slu = [A.alloc([512], F32) for _ in range(2)]
        actT = A.alloc([8, 512], BF16)
        out1 = A.alloc([4, D], F32)
        outw = [A.alloc([D], F32) for _ in range(2)]
        otmp = A.alloc([512], F32)
        gath_f = gath.rearrange("p a b -> p (a b)").bitcast(F32).rearrange("p (a b) -> p a b", a=4)

        def load_weights(u):
            s_ = u % 2
            for nm, src, dst in (("wgs", ew_gate, wgs), ("wus", ew_up, wus), ("wds", ew_down, wds)):
                P.dma("pool", lambda e, src=src, dst=dst: e.dma_start(
                    out=dst[s_].rearrange("p a b -> p (a b)").rearrange("p (a b) -> p a b", b=2048),
                    in_=src[u].rearrange("p (a b) -> p a b", b=2048)), "ld_%s%d" % (nm, s_), w=["%s%d" % (nm, s_)])

        def route_idx(e_):
            P.dma("sp", lambda e: e.dma_start(out=cb, in_=cumd[e_].rearrange("s n -> (s n)").partition_broadcast(128)), "ld_cb",
                  r=["cumd"], w=["cb"])
            for cc in range(4):
                for hf in range(2):
                    P.op("dve", lambda e, cc=cc, hf=hf: e.tensor_scalar(
                        out=junk, in0=cb[:, hf * 2048:(hf + 1) * 2048], scalar1=cvals[:, cc:cc + 1], scalar2=0.0, op0=ALU.is_le, op1=ALU.add,
                        accum_out=idxp[:, e_ * 8 + cc * 2 + hf:e_ * 8 + cc * 2 + hf + 1]), r=["cb", "cvals"], w=["junk", "idxp%d" % e_])
            idxp_v = idxp[:, e_ * 8:(e_ + 1) * 8].rearrange("p (c h) -> p c h", h=2)
            P.op("dve", lambda e: e.tensor_tensor(out=idxf[:, e_ * 4:(e_ + 1) * 4], in0=idxp_v[:, :, 0], in1=idxp_v[:, :, 1], op=ALU.add),
                 r=["idxp%d" % e_], w=["idxf%d" % e_])
            P.op("dve", lambda e: e.tensor_copy(out=idxi[:, e_ * 4:(e_ + 1) * 4], in_=idxf[:, e_ * 4:(e_ + 1) * 4]), r=["idxf%d" % e_], w=["idxi%d" % e_])

        def gather_rows(e_):
            scr_keys = ["scr_%d" % t for t in range(NT)] + ["scra_%d" % q for q in range(4)]
            for cc in range(4):
                P.dma("pool", lambda e, cc=cc: e.indirect_dma_start(
                    out=gath[:, cc, :], out_offset=None, in_=scr[:, :],
                    in_offset=bass.IndirectOffsetOnAxis(ap=idxi[:, e_ * 4 + cc:e_ * 4 + cc + 1], axis=0)),
                    "ld_gath", r=["idxi%d" % e_] + scr_keys, w=["gath"])
            P.op("dve", lambda e: e.tensor_copy(out=wsel[:, e_ * 4:(e_ + 1) * 4], in_=gath_f[:, :, 512 + e_]), r=["gath"], w=["wsel%d" % e_])

        def transpose_tok(e_):
            tb = e_ % 2
            for cc in range(4):
                bank = 6 + (cc % 2)
                P.group("pe", [lambda e, k=k, cc=cc, bank=bank: e.transpose(out=PSB[bank][:, k * 128:(k + 1) * 128],
                                                                           in_=gath[:, cc, k * 128:(k + 1) * 128], identity=identb)
                               for k in range(8)], r=["gath", "identb"], w=[pk[bank]])
                if cc % 2 == 0:
                    P.op("act", lambda e, cc=cc, bank=bank: e.activation(out=tokT[tb][:, :, cc * 128:(cc + 1) * 128],
                                                                        in_=PSB[bank].rearrange("p (k t) -> p k t", k=8), func=AF.Copy),
                         r=[pk[bank]], w=["tokT%d" % tb])
                else:
                    P.op("dve", lambda e, cc=cc, bank=bank: e.tensor_copy(out=tokT[tb][:, :, cc * 128:(cc + 1) * 128],
                                                                         in_=PSB[bank].rearrange("p (k t) -> p k t", k=8)),
                         r=[pk[bank]], w=["tokT%d" % tb])

        hd_keys = ["hd_%d" % t for t in range(NT)]
        sc_keys = ["hd_sc0", "hd_sc1", "hd_sc2", "hd_sc3"]
        ocnt = [0]

        def unit(u):
            e_, fh = u // 2, u % 2
            s_ = u % 2
            tb = e_ % 2
            for fc in range(8):
                bg_, bu_ = 2 * (fc % 2), 2 * (fc % 2) + 1
                P.group("pe", [lambda e, k=k, fc=fc, bg_=bg_: e.matmul(PS[bg_], lhsT=wgs[s_][:, k, fc * 128:(fc + 1) * 128], rhs=tokT[tb][:, k, :],
                                                                      start=(k == 0), stop=(k == 7)) for k in range(8)],
                        r=["wgs%d" % s_, "tokT%d" % tb], w=[pk[bg_]])
                P.group("pe", [lambda e, k=k, fc=fc, bu_=bu_: e.matmul(PS[bu_], lhsT=wus[s_][:, k, fc * 128:(fc + 1) * 128], rhs=tokT[tb][:, k, :],
                                                                      start=(k == 0), stop=(k == 7)) for k in range(8)],
                        r=["wus%d" % s_, "tokT%d" % tb], w=[pk[bu_]])
                si = fc % 2
                P.op("act", lambda e, bg_=bg_, si=si: e.activation(out=slu[si], in_=PS[bg_], func=AF.Silu), r=[pk[bg_]], w=["slu%d" % si])
                P.op("dve", lambda e, fc=fc, bu_=bu_, si=si: e.tensor_tensor(out=actT[:, fc, :], in0=slu[si], in1=PS[bu_], op=ALU.mult),
                     r=["slu%d" % si, pk[bu_]], w=["actT"])
            for cc in range(4):
                if fh == 1:
                    ob = ocnt[0] % 2
                    ocnt[0] += 1
                for dh in range(2):
                    bo = 4 + dh
                    P.group("pe", [lambda e, fc=fc, cc=cc, dh=dh, bo=bo: e.matmul(PS[bo], lhsT=actT[:, fc, cc * 128:(cc + 1) * 128],
                                                                                 rhs=wds[s_][:, fc, dh * 512:(dh + 1) * 512],
                                                                                 start=(fc == 0), stop=(fc == 7)) for fc in range(8)],
                            r=["actT", "wds%d" % s_], w=[pk[bo]])
                    if fh == 0:
                        P.op("act", lambda e, cc=cc, dh=dh, bo=bo: e.activation(out=out1[:, cc, dh * 512:(dh + 1) * 512], in_=PS[bo], func=AF.Copy),
                             r=[pk[bo]], w=["out1"])
                    else:
                        P.op("dve", lambda e, cc=cc, dh=dh, bo=bo: e.tensor_tensor(out=otmp, in0=PS[bo], in1=out1[:, cc, dh * 512:(dh + 1) * 512], op=ALU.add),
                             r=[pk[bo], "out1"], w=["otmp"])
                        P.op("dve", lambda e, cc=cc, dh=dh, ob=ob: e.scalar_tensor_tensor(
                            out=outw[ob][:, dh * 512:(dh + 1) * 512], in0=otmp, scalar=wsel[:, e_ * 4 + cc:e_ * 4 + cc + 1],
                            in1=bcs[BC_G2][:, dh * 512:(dh + 1) * 512], op0=ALU.mult, op1=ALU.mult),
                            r=["otmp", "wsel%d" % e_, "bc6"], w=["outw%d" % ob])
                if fh == 1:
                    P.dma("pool", lambda e, cc=cc, ob=ob: e.indirect_dma_start(
                        out=hd[:, :], out_offset=bass.IndirectOffsetOnAxis(ap=idxi[:, e_ * 4 + cc:e_ * 4 + cc + 1], axis=0),
                        in_=outw[ob], in_offset=None, compute_op=ALU.add),
                        "sc_outw%d" % ob, r=["outw%d" % ob, "idxi%d" % e_] + hd_keys + (sc_keys if cc == 0 else []),
                        w=["hd_sc%d" % cc])

        P.op("dve", lambda e: e.memset(idxp, 0.0), w=["idxp%d" % i for i in range(E)])
        load_weights(0)
        load_weights(1)
        route_idx(0)
        gather_rows(0)
        transpose_tok(0)
        NEXP = int(os.environ.get('K_NEXP', E))
        for e_ in range(NEXP):
            if e_ + 1 < E:
                route_idx(e_ + 1)
            unit(2 * e_)
            if e_ + 1 < E:
                gather_rows(e_ + 1)
            if 2 * e_ + 2 < 2 * E:
                load_weights(2 * e_ + 2)
            if e_ + 1 < E:
                transpose_tok(e_ + 1)
            unit(2 * e_ + 1)
            if 2 * e_ + 3 < 2 * E:
                load_weights(2 * e_ + 3)

        fin = [A.alloc([D], F32) for _ in range(2)]
        fss = A.alloc([NT], F32)
        frs_ = A.alloc([NT], F32)
        last = []
        for T in range(NT):
            b = T % 2
            P.dma("sp", lambda e, T=T, b=b: e.dma_start(out=fin[b], in_=hd[T * 128:(T + 1) * 128, :]), "ld_fin%d" % b,
                  r=sc_keys + ["hd_%d" % T], w=["fin%d" % b])
            P.op("act", lambda e, T=T, b=b: e.activation(out=junk[:, 0:D], in_=fin[b], func=AF.Square, accum_out=fss[:, T:T + 1]),
                 r=["fin%d" % b], w=["junk", "fss%d" % T])
            P.op("dve", lambda e, T=T: e.tensor_scalar(out=fss[:, T:T + 1], in0=fss[:, T:T + 1], scalar1=1.0 / D, scalar2=1e-6,
                                                       op0=ALU.mult, op1=ALU.add), r=["fss%d" % T], w=["fss%d" % T])
            P.op("pool", lambda e, T=T: e.tensor_tensor(out=frs_[:, T:T + 1], in0=fss[:, T:T + 1], in1=nhalf[:, 0:1], op=ALU.pow),
                 r=["fss%d" % T, "nhalf"], w=["frs%d" % T])
            P.op("dve", lambda e, T=T, b=b: e.scalar_tensor_tensor(out=fin[b], in0=fin[b], scalar=frs_[:, T:T + 1], in1=bcs[BC_FG],
                                                                   op0=ALU.mult, op1=ALU.mult), r=["fin%d" % b, "frs%d" % T, "bc7"], w=["fin%d" % b])
            last.append(P.dma("act", lambda e, T=T, b=b: e.dma_start(out=out[T * 128:(T + 1) * 128, :], in_=fin[b]), "st_fin%d" % b,
                              r=["fin%d" % b], w=["out_%d" % T]))
        P.barrier()
        P.emit()
        print("arena high-water bytes", A.hw, "of", ARENA_BYTES)
    return nc


_CONST = {}


def _dft_layout(M):
    A_ = M.reshape(4, 8, 128, 8, 512)
    return np.ascontiguousarray(A_.transpose(3, 0, 2, 1, 4)).reshape(32, 128, 4096)


_WCACHE = {}


def _weight_layouts(inp):
    key = id(inp["expert_w_gate"])
    if _WCACHE.get("key") == key:
        return _WCACHE["val"]
    f = lambda a: np.asarray(a, dtype=np.float32)
    g = f(inp["expert_w_gate"][0]).reshape(E, 8, 128, 2, 1024).transpose(0, 3, 2, 1, 4)
    u = f(inp["expert_w_up"][0]).reshape(E, 8, 128, 2, 1024).transpose(0, 3, 2, 1, 4)
    d = f(inp["expert_w_down"][0]).reshape(E, 2, 8, 128, 1024).transpose(0, 1, 3, 2, 4)
    aw = f(inp["ada_w"][0]).reshape(8, 128, 12, 512).transpose(2, 1, 0, 3)
    val = dict(
        expert_w_gate_l=np.ascontiguousarray(g).reshape(E * 2, 128, 8192),
        expert_w_up_l=np.ascontiguousarray(u).reshape(E * 2, 128, 8192),
        expert_w_down_l=np.ascontiguousarray(d).reshape(E * 2, 128, 8192),
        ada_w_l=np.ascontiguousarray(aw).reshape(12, 128, 4096),
    )
    _WCACHE["key"] = key
    _WCACHE["val"] = val
    return val


def _constants():
    if _CONST:
        return _CONST
    bf = ml_dtypes.bfloat16
    d = np.arange(128)
    ang = 2.0 * np.pi * ((d[:, None] * d[None, :]) % 128) / 128.0
    cs128 = np.concatenate([np.cos(ang), np.sin(ang)], axis=1) / np.sqrt(128.0)
    s = np.arange(S, dtype=np.int64)
    m = (s[:, None] * s[None, :]) % S
    tab_c = np.cos(2.0 * np.pi * np.arange(S) / S) / 64.0
    tab_s = -np.sin(2.0 * np.pi * np.arange(S) / S) / 64.0
    q = np.arange(128)
    e_of = q % 16
    s_of = q // 16
    maskM = (e_of[:, None] == e_of[None, :]).astype(np.float32)
    maskM2 = ((e_of[:, None] == e_of[None, :]) & (s_of[:, None] < s_of[None, :])).astype(np.float32)
    cvals = (np.arange(4)[None, :] * 128 + np.arange(128)[:, None]).astype(np.float32)
    _CONST.update(
        cs128=cs128.astype(bf),
        dft_cos_l=_dft_layout(tab_c[m].astype(bf)),
        dft_sin_l=_dft_layout(tab_s[m].astype(bf)),
        maskM=maskM, maskM2=maskM2, cvals=cvals,
    )
    return _CONST


def make_in_map(b, inp):
    f = lambda a: np.ascontiguousarray(np.asarray(a, dtype=np.float32))
    C = _constants()
    WL = _weight_layouts(inp)
    smallrows = np.concatenate([
        f(inp["b_in"][0]).reshape(28, 128), f(inp["conv_dw_b"][0]).reshape(4, 128), f(inp["conv_ln_g"][0]).reshape(4, 128),
        f(inp["conv_ln_b"][0]).reshape(4, 128), f(inp["conv_b_out"][0]).reshape(8, 128), f(inp["fourier_b"][0]).reshape(8, 128)], axis=0)
    return {
        "x": f(inp["x"][b]),
        "c_col": np.ascontiguousarray(f(inp["c"][b]).reshape(8, 128).T),
        "ada_w_l": WL["ada_w_l"],
        "ada_b": f(inp["ada_b"][0]).reshape(1, -1),
        "norm1_g": f(inp["norm1_g"][0]).reshape(1, -1),
        "w_in": f(inp["w_in"][0]),
        "smallrows": np.ascontiguousarray(smallrows),
        "dw_rows": f(inp["conv_dw_w"][0]).reshape(124, 128),
        "conv_w_out": f(inp["conv_w_out"][0]),
        "fourier_w": f(inp["fourier_w"][0]),
        "w_out": f(inp["w_out"][0]),
        "b_out": f(inp["b_out"][0]).reshape(1, -1),
        "norm2_g": f(inp["norm2_g"][0]).reshape(1, -1),
        "router_w": f(inp["router_w"][0]),
        "expert_w_gate_l": WL["expert_w_gate_l"],
        "expert_w_up_l": WL["expert_w_up_l"],
        "expert_w_down_l": WL["expert_w_down_l"],
        "final_norm_g": f(inp["final_norm_g"]).reshape(1, -1),
        "cs128": C["cs128"], "dft_cos_l": C["dft_cos_l"], "dft_sin_l": C["dft_sin_l"],
        "maskM": C["maskM"], "maskM2": C["maskM2"], "cvals": C["cvals"],
    }


def kernel(**inputs):
    nc = build_program()
    in_maps = [make_in_map(b, inputs) for b in range(8)]
    res = run_bass_kernel_spmd(nc, in_maps, core_ids=list(range(8)))
    return np.stack([np.asarray(r["out"], dtype=np.float32) for r in res.results], axis=0)
```

```python
import os
from contextlib import ExitStack
import numpy as np
import ml_dtypes
import concourse.bass as bass
import concourse.mybir as mybir
from concourse.bass_utils import run_bass_kernel_spmd

F32 = mybir.dt.float32
BF16 = mybir.dt.bfloat16
I32 = mybir.dt.int32
I16 = mybir.dt.int16
AF = mybir.ActivationFunctionType
ALU = mybir.AluOpType

S = 4096
D = 1024
NT = 32
E = 16
CAP = 512
FF = 2048
KW = 31
ENGS = ("pe", "act", "dve", "pool", "sp")


class Prog:
    def __init__(self, nc, ctx):
        self.nc = nc
        self.ctx = ctx
        self.lists = {e: [] for e in ENGS}
        self.sem = {e: ctx.enter_context(nc.semaphore("prog_" + e)) for e in ENGS}
        self.cnt = {e: 0 for e in ENGS}
        self.waited = {}
        self.dma_sems = {}
        self.dma_cnt = {}
        self.dma_waited = {}
        self.last_w = {}
        self.readers = {}

    def _emit_waits(self, eng, deps):
        for d in deps:
            if d is None:
                continue
            if d[0] == "dma":
                _, name, val = d
                key = (eng, name)
                if self.dma_waited.get(key, 0) >= val:
                    continue
                self.dma_waited[key] = val
                sem = self.dma_sems[name]
                self.lists[eng].append(lambda e, sem=sem, val=val: e.wait_ge(sem, val))
            else:
                src, val = d
                key = (eng, src)
                if self.waited.get(key, 0) >= val:
                    continue
                self.waited[key] = val
                sem = self.sem[src]
                self.lists[eng].append(lambda e, sem=sem, val=val: e.wait_ge(sem, val))

    def _deps(self, r, w, extra):
        deps = list(extra)
        for k in r:
            if k in self.last_w:
                deps.append(self.last_w[k])
        for k in w:
            if k in self.last_w:
                deps.append(self.last_w[k])
            deps.extend(self.readers.get(k, []))
        return deps

    def _commit(self, tok, r, w):
        for k in r:
            self.readers.setdefault(k, []).append(tok)
        for k in w:
            self.last_w[k] = tok
            self.readers[k] = []

    def op(self, eng, fn, r=(), w=(), extra=()):
        self._emit_waits(eng, self._deps(r, w, extra))
        self.cnt[eng] += 1
        sem = self.sem[eng]
        self.lists[eng].append(lambda e, fn=fn, sem=sem: fn(e).then_inc(sem, 1))
        tok = (eng, self.cnt[eng])
        self._commit(tok, r, w)
        return tok

    def group(self, eng, fns, r=(), w=(), extra=()):
        self._emit_waits(eng, self._deps(r, w, extra))
        for fn in fns[:-1]:
            self.lists[eng].append(lambda e, fn=fn: fn(e))
        self.cnt[eng] += 1
        sem = self.sem[eng]
        fn = fns[-1]
        self.lists[eng].append(lambda e, fn=fn, sem=sem: fn(e).then_inc(sem, 1))
        tok = (eng, self.cnt[eng])
        self._commit(tok, r, w)
        return tok

    def dma(self, eng, fn, semname, r=(), w=(), extra=()):
        if semname not in self.dma_sems:
            self.dma_sems[semname] = self.ctx.enter_context(self.nc.semaphore("d_" + semname))
            self.dma_cnt[semname] = 0
        self._emit_waits(eng, self._deps(r, w, extra))
        self.dma_cnt[semname] += 16
        sem = self.dma_sems[semname]
        self.lists[eng].append(lambda e, fn=fn, sem=sem: fn(e).then_inc(sem, 16))
        tok = ("dma", semname, self.dma_cnt[semname])
        self._commit(tok, r, w)
        return tok

    def wait(self, eng, deps):
        self._emit_waits(eng, deps)

    def barrier(self):
        deps = [(src, self.cnt[src]) for src in ENGS if self.cnt[src] > 0]
        deps += [("dma", n, v) for n, v in self.dma_cnt.items() if v > 0]
        for eng in ENGS:
            self._emit_waits(eng, [d for d in deps if d[0] != eng])

    def emit(self):
        with self.nc.Block() as block:
            @block.tensor
            def _(e):
                for f in self.lists["pe"]:
                    f(e)

            @block.scalar
            def _(e):
                for f in self.lists["act"]:
                    f(e)

            @block.vector
            def _(e):
                for f in self.lists["dve"]:
                    f(e)

            @block.gpsimd
            def _(e):
                for f in self.lists["pool"]:
                    f(e)

            @block.sync
            def _(e):
                for f in self.lists["sp"]:
                    f(e)


class Arena:
    def __init__(self, big, total_bytes):
        self.big = big
        self.total = total_bytes
        self.off = 0

    def alloc(self, shape, dt):
        n = int(np.prod(shape))
        esz = 2 if dt in (BF16, I16) else 4
        nbytes = (n * esz + 63) // 64 * 64
        off = self.off
        self.off += nbytes
        self.hw = max(getattr(self, "hw", 0), self.off)
        assert self.off <= self.total, ("SBUF arena overflow", self.off, self.total)
        v = self.big[:, off // 4:(off + nbytes) // 4]
        if dt != F32:
            v = v.bitcast(dt)
        v = v[:, 0:n]
        if len(shape) == 2:
            v = v.rearrange("p (a b) -> p a b", a=shape[0])
        elif len(shape) == 3:
            v = v.rearrange("p (a b c) -> p a b c", a=shape[0], b=shape[1])
        return v

    def mark(self):
        return self.off

    def release(self, m):
        self.off = m


ARENA_BYTES = 200 * 1024


def build_program(stage=99):
    nc = bass.Bass("TRN2", target_bir_lowering=False)

    def din(name, shape, dt=F32):
        return nc.dram_tensor(name, list(shape), dt, kind="ExternalInput").ap()

    x = din("x", [S, D])
    c_col = din("c_col", [128, 8])
    ada_w = din("ada_w_l", [12, 128, 8 * 512])
    ada_b = din("ada_b", [1, 6 * D])
    norm1_g = din("norm1_g", [1, D])
    w_in = din("w_in", [D, 3584])
    smallrows = din("smallrows", [56, 128])
    dw_rows = din("dw_rows", [124, 128])
    conv_w_out = din("conv_w_out", [512, D])
    fourier_w = din("fourier_w", [512, D])
    w_out = din("w_out", [D, D])
    b_out = din("b_out", [1, D])
    norm2_g = din("norm2_g", [1, D])
    router_w = din("router_w", [D, E])
    ew_gate = din("expert_w_gate_l", [E * 2, 128, 8 * 1024])
    ew_up = din("expert_w_up_l", [E * 2, 128, 8 * 1024])
    ew_down = din("expert_w_down_l", [E * 2, 128, 8 * 1024])
    final_g = din("final_norm_g", [1, D])
    cs128_d = din("cs128", [128, 256], BF16)
    CSd = din("dft_cos_l", [32, 128, 4096], BF16)
    SSd = din("dft_sin_l", [32, 128, 4096], BF16)
    maskM_d = din("maskM", [128, 128])
    maskM2_d = din("maskM2", [128, 128])
    cvals_d = din("cvals", [128, 4])

    out = nc.dram_tensor("out", [S, D], F32, kind="ExternalOutput").ap()
    dbg = None
    if stage < 99:
        dbg = nc.dram_tensor("dbg", [128, 16384], F32, kind="ExternalOutput").ap()

    modd = nc.dram_tensor("modd", [1, 9 * D], F32, kind="Internal").ap()
    frd = nc.dram_tensor("frd", [512, S], BF16, kind="Internal").ap()
    scr = nc.dram_tensor("scr", [S, 1056], BF16, kind="Internal").ap()
    hd = nc.dram_tensor("hd", [S, D], F32, kind="Internal").ap()
    cumd = nc.dram_tensor("cumd", [E, 8, 512], I16, kind="Internal").ap()

    with ExitStack() as ctx:
        P = Prog(nc, ctx)
        big = ctx.enter_context(nc.sbuf_tensor("big", [128, ARENA_BYTES // 4], F32))
        A = Arena(big, ARENA_BYTES)
        psb = [ctx.enter_context(nc.psum_tensor("ps%d" % i, [128, 512], F32)) for i in range(8)]
        PS = [p[:, :] for p in psb]
        PSB = [p[:, :].bitcast(BF16) for p in psb]
        pk = ["ps%d" % i for i in range(8)]

        ident = A.alloc([128], F32)
        identb = A.alloc([128], BF16)
        smallcol = A.alloc([56], F32)
        dwcol = A.alloc([124], F32)
        rstd1 = A.alloc([NT], F32)
        ss1 = A.alloc([NT], F32)
        ss2 = A.alloc([NT], F32)
        rstd2 = A.alloc([NT], F32)
        logits = A.alloc([NT, E], F32)
        aff = A.alloc([NT, E], F32)
        maskM = A.alloc([128], F32)
        maskM2 = A.alloc([128], F32)
        cvals = A.alloc([4], F32)
        ones_ln = A.alloc([128], F32)
        nhalf = A.alloc([256], F32)
        eps6 = A.alloc([1], F32)
        eps5 = A.alloc([1], F32)
        cact = A.alloc([8], F32)
        rw = A.alloc([8, E], F32)
        idxf = A.alloc([E * 4], F32)
        idxp = A.alloc([E * 8], F32)
        idxi = A.alloc([E * 4], I32)
        wsel = A.alloc([E * 4], F32)
        cs128 = A.alloc([256], BF16)
        ones_lnb = A.alloc([128], BF16)
        BC_GS1, BC_SH1, BC_G1, BC_GB1, BC_GS2, BC_SH2, BC_G2, BC_FG = range(8)
        bcs = [None] * 8
        m_p1 = A.mark()
        for i in (BC_GS1, BC_SH1, BC_G1, BC_GB1, BC_GS2, BC_SH2):
            bcs[i] = A.alloc([D], F32)
        m_p2 = A.mark()

        P.op("pool", lambda e: e.memset(ident, 1.0), w=["ident"])
        P.op("pool", lambda e: e.affine_select(out=ident, in_=ident, pattern=[[-1, 128]], compare_op=ALU.is_equal,
                                               fill=0.0, base=0, channel_multiplier=1), w=["ident"])
        P.op("dve", lambda e: e.tensor_copy(out=identb, in_=ident), r=["ident"], w=["identb"])
        P.op("pool", lambda e: e.memset(ones_ln, 1.0 / 512.0), w=["ones_ln"])
        P.op("pool", lambda e: e.memset(ones_lnb, 1.0 / 512.0), w=["ones_lnb"])
        P.op("pool", lambda e: e.memset(nhalf, -0.5), w=["nhalf"])
        P.op("pool", lambda e: e.memset(eps6, 1e-6), w=["eps6"])
        P.op("pool", lambda e: e.memset(eps5, 1e-5), w=["eps5"])
        P.dma("sp", lambda e: e.dma_start(out=maskM, in_=maskM_d[:, :]), "c_maskM", w=["maskM"])
        P.dma("sp", lambda e: e.dma_start(out=maskM2, in_=maskM2_d[:, :]), "c_maskM2", w=["maskM2"])
        P.dma("sp", lambda e: e.dma_start(out=cvals, in_=cvals_d[:, :]), "c_cvals", w=["cvals"])
        P.dma("sp", lambda e: e.dma_start(out=cs128, in_=cs128_d[:, :]), "c_cs128", w=["cs128"])
        P.dma("sp", lambda e: e.dma_start(out=cact, in_=c_col[:, :]), "c_cact", w=["cact"])
        P.dma("sp", lambda e: e.dma_start(out=rw, in_=router_w.rearrange("(k p) n -> p k n", p=128)), "c_rw", w=["rw"])

        m0 = A.mark()
        modrow = A.alloc([9 * D], F32)
        adab = A.alloc([6 * D], F32)
        g1row = A.alloc([D], F32)
        g2row = A.alloc([D], F32)
        borow = A.alloc([D], F32)
        awb = [A.alloc([8, 512], F32) for _ in range(2)]
        rows_T = A.alloc([128], F32)
        P.dma("act", lambda e: e.dma_start(out=adab[0:1, :], in_=ada_b[:, :]), "c_adab", w=["adab"])
        P.dma("act", lambda e: e.dma_start(out=g1row[0:1, :], in_=norm1_g[:, :]), "c_g1", w=["g1row"])
        P.dma("act", lambda e: e.dma_start(out=g2row[0:1, :], in_=norm2_g[:, :]), "c_g2", w=["g2row"])
        P.dma("act", lambda e: e.dma_start(out=borow[0:1, :], in_=b_out[:, :]), "c_bo", w=["borow"])
        P.op("act", lambda e: e.activation(out=cact, in_=cact, func=AF.Silu), r=["cact"], w=["cact"])
        for pc in range(12):
            b = pc % 2
            P.dma("sp", lambda e, pc=pc, b=b: e.dma_start(out=awb[b].rearrange("p a b -> p (a b)"), in_=ada_w[pc]),
                  "awb%d" % b, w=["awb%d" % b])
            P.group("pe", [lambda e, k=k, b=b: e.matmul(PS[0][0:1, :], lhsT=cact[:, k:k + 1], rhs=awb[b][:, k, :],
                                                       start=(k == 0), stop=(k == 7)) for k in range(8)],
                    r=["cact", "awb%d" % b], w=[pk[0]])
            P.op("dve", lambda e, pc=pc: e.tensor_tensor(out=modrow[0:1, pc * 512:(pc + 1) * 512], in0=PS[0][0:1, :],
                                                        in1=adab[0:1, pc * 512:(pc + 1) * 512], op=ALU.add),
                 r=[pk[0], "adab"], w=["modrow"])
        P.op("dve", lambda e: e.scalar_tensor_tensor(out=modrow[0:1, 6 * D:7 * D], in0=modrow[0:1, D:2 * D], scalar=1.0,
                                                     in1=g1row[0:1, :], op0=ALU.add, op1=ALU.mult),
             r=["modrow", "g1row"], w=["modrow"])
        P.op("dve", lambda e: e.tensor_tensor(out=modrow[0:1, 7 * D:8 * D], in0=modrow[0:1, 2 * D:3 * D], in1=borow[0:1, :],
                                              op=ALU.mult), r=["modrow", "borow"], w=["modrow"])
        P.op("dve", lambda e: e.scalar_tensor_tensor(out=modrow[0:1, 8 * D:9 * D], in0=modrow[0:1, 4 * D:5 * D], scalar=1.0,
                                                     in1=g2row[0:1, :], op0=ALU.add, op1=ALU.mult),
             r=["modrow", "g2row"], w=["modrow"])
        P.dma("sp", lambda e: e.dma_start(out=modd[:, :], in_=modrow[0:1, :]), "st_modrow", r=["modrow"], w=["modd"])
        srcs = {BC_GS1: 6, BC_SH1: 0, BC_G1: 2, BC_GB1: 7, BC_GS2: 8, BC_SH2: 3}
        for i, off in srcs.items():
            P.dma("sp" if i % 2 == 0 else "act",
                  lambda e, i=i, off=off: e.dma_start(out=bcs[i], in_=modd[0, off * D:(off + 1) * D].partition_broadcast(128)),
                  "bc%d" % i, r=["modd"], w=["bc%d" % i])
        P.dma("sp", lambda e: e.dma_start(out=rows_T[0:56, :], in_=smallrows[:, :]), "c_rowsT", w=["rows_T"])
        P.group("pe", [lambda e: e.transpose(out=PS[1][:, 0:56], in_=rows_T[0:56, :], identity=ident[0:56, 0:56])],
                r=["rows_T", "ident"], w=[pk[1]])
        P.op("dve", lambda e: e.tensor_copy(out=smallcol, in_=PS[1][:, 0:56]), r=[pk[1]], w=["smallcol"])
        P.dma("sp", lambda e: e.dma_start(out=rows_T[0:124, :], in_=dw_rows[:, :]), "c_rowsT", w=["rows_T"])
        P.group("pe", [lambda e: e.transpose(out=PS[1][:, 0:124], in_=rows_T[0:124, :], identity=ident[0:124, 0:124])],
                r=["rows_T", "ident"], w=[pk[1]])
        P.op("dve", lambda e: e.tensor_copy(out=dwcol, in_=PS[1][:, 0:124]), r=[pk[1]], w=["dwcol"])
        SC_BIN, SC_DWB, SC_LNG, SC_LNB, SC_CBO, SC_FB = 0, 28, 32, 36, 40, 48

        if stage == 0:
            P.dma("sp", lambda e: e.dma_start(out=dbg[0:1, 0:9 * D], in_=modrow[0:1, :]), "dbg0", r=["modrow"])
            P.dma("sp", lambda e: e.dma_start(out=dbg[:, 9 * D:9 * D + 56], in_=smallcol), "dbg1", r=["smallcol"])
            P.dma("sp", lambda e: e.dma_start(out=dbg[:, 10 * D:10 * D + 124], in_=dwcol), "dbg2", r=["dwcol"])
            P.dma("sp", lambda e: e.dma_start(out=dbg[:, 11 * D:12 * D], in_=bcs[BC_GS1]), "dbg3", r=["bc0"])
            P.barrier()
            P.emit()
            return nc
        P.barrier()
        A.release(m0)

        def xhat_tile(T, xin, xin_k, tmp, xm, xhatT_dst, xhatT_k, psbank, first_pass, tmp_k="tmp"):
            P.dma("sp", lambda e: e.dma_start(out=xin, in_=x[T * 128:(T + 1) * 128, :]), "ld_" + xin_k, w=[xin_k])
            if first_pass:
                P.op("act", lambda e: e.activation(out=xm, in_=xin, func=AF.Square, accum_out=ss1[:, T:T + 1]),
                     r=[xin_k], w=["xm", "ss1_%d" % T])
                P.op("dve", lambda e: e.tensor_scalar(out=ss1[:, T:T + 1], in0=ss1[:, T:T + 1], scalar1=1.0 / D, scalar2=1e-6,
                                                      op0=ALU.mult, op1=ALU.add), r=["ss1_%d" % T], w=["ss1_%d" % T])
                P.op("pool", lambda e: e.tensor_tensor(out=rstd1[:, T:T + 1], in0=ss1[:, T:T + 1], in1=nhalf[:, 0:1], op=ALU.pow),
                     r=["ss1_%d" % T, "nhalf"], w=["rstd1_%d" % T])
            P.op("dve", lambda e: e.scalar_tensor_tensor(out=tmp, in0=xin, scalar=rstd1[:, T:T + 1], in1=bcs[BC_GS1],
                                                         op0=ALU.mult, op1=ALU.mult),
                 r=[xin_k, "rstd1_%d" % T, "bc0"], w=[tmp_k])
            P.op("pool", lambda e: e.tensor_tensor(out=xm, in0=tmp, in1=bcs[BC_SH1], op=ALU.add),
                 r=[tmp_k, "bc1"], w=["xm"])
            P.group("pe", [lambda e, k=k: e.transpose(out=PSB[psbank][:, k * 128:(k + 1) * 128], in_=xm[:, k * 128:(k + 1) * 128],
                                                     identity=identb) for k in range(8)],
                    r=["xm", "identb"], w=[pk[psbank]])
            P.op("act", lambda e: e.activation(out=xhatT_dst, in_=PSB[psbank].rearrange("p (k t) -> p k t", k=8), func=AF.Copy),
                 r=[pk[psbank]], w=[xhatT_k])

        mA = A.mark()
        vT = A.alloc([4, S + 30], BF16)
        mV = A.mark()
        G = A.alloc([NT, 4, 256], BF16)
        mA2 = A.mark()
        wA = A.alloc([8, 1536], BF16)
        xin = [A.alloc([D], F32) for _ in range(2)]
        tmp = A.alloc([D], F32)
        xm = A.alloc([D], BF16)
        xhatT = [A.alloc([8, 512], BF16)] * 2
        sg = [A.alloc([512], F32) for _ in range(2)]
        fT = [A.alloc([4, 512], BF16)] * 2

        P.op("pool", lambda e: e.memset(vT[:, :, 0:15], 0.0), w=["vT"])
        P.op("pool", lambda e: e.memset(vT[:, :, S + 15:S + 30], 0.0), w=["vT"])
        win_v = w_in.rearrange("(k p) n -> p k n", p=128)
        for k in range(8):
            P.dma("pool", lambda e, k=k: e.dma_start(out=wA[:, k, :], in_=win_v[:, k, 0:1536]), "ld_wA", w=["wA"])

        for sc in range(8):
            xb_ = sc % 2
            for tt in range(4):
                T = sc * 4 + tt
                xhat_tile(T, xin[T % 2], "xin%d" % (T % 2), tmp, xm, xhatT[xb_][:, :, tt * 128:(tt + 1) * 128],
                          "xhatT", T % 2, True)
            xk = "xhatT"
            for c in range(4):
                ba, bg = 2 + 2 * (c % 2), 3 + 2 * (c % 2)
                P.group("pe", [lambda e, k=k, c=c, ba=ba: e.matmul(PS[ba], lhsT=wA[:, k, c * 128:(c + 1) * 128], rhs=xhatT[xb_][:, k, :],
                                                                  start=(k == 0), stop=(k == 7)) for k in range(8)],
                        r=["wA", xk], w=[pk[ba]])
                P.group("pe", [lambda e, k=k, c=c, bg=bg: e.matmul(PS[bg], lhsT=wA[:, k, 512 + c * 128:512 + (c + 1) * 128], rhs=xhatT[xb_][:, k, :],
                                                                  start=(k == 0), stop=(k == 7)) for k in range(8)],
                        r=["wA", xk], w=[pk[bg]])
                sgi = c % 2
                P.op("act", lambda e, c=c, bg=bg, sgi=sgi: e.activation(out=sg[sgi], in_=PS[bg], func=AF.Sigmoid,
                                                                      bias=smallcol[:, SC_BIN + 4 + c:SC_BIN + 5 + c], scale=1.0),
                     r=[pk[bg], "smallcol"], w=["sg%d" % sgi])
                P.op("dve", lambda e, c=c, ba=ba, sgi=sgi, sc=sc: e.scalar_tensor_tensor(
                    out=vT[:, c, 15 + sc * 512:15 + (sc + 1) * 512], in0=PS[ba], scalar=smallcol[:, SC_BIN + c:SC_BIN + c + 1],
                    in1=sg[sgi], op0=ALU.add, op1=ALU.mult),
                    r=[pk[ba], "sg%d" % sgi, "smallcol"], w=["vT_%d" % sc])
            fb_ = sc % 2
            for g in range(4):
                bf = 2 + (g % 4)
                P.group("pe", [lambda e, k=k, g=g, bf=bf: e.matmul(PS[bf], lhsT=wA[:, k, 1024 + g * 128:1024 + (g + 1) * 128], rhs=xhatT[xb_][:, k, :],
                                                                  start=(k == 0), stop=(k == 7)) for k in range(8)],
                        r=["wA", xk], w=[pk[bf]])
                P.op("act", lambda e, g=g, bf=bf: e.activation(out=fT[fb_][:, g, :], in_=PS[bf], func=AF.Identity,
                                                              bias=smallcol[:, SC_BIN + 8 + g:SC_BIN + 9 + g], scale=1.0),
                     r=[pk[bf], "smallcol"], w=["fT"])
            for tt in range(4):
                T = sc * 4 + tt
                for hb in range(2):
                    bank = 6 + hb
                    P.group("pe", [lambda e, g=g, tt=tt, bank=bank: e.matmul(
                        PS[bank][:, (g % 2) * 256:(g % 2 + 1) * 256], lhsT=fT[fb_][:, g, tt * 128:(tt + 1) * 128], rhs=cs128,
                        start=True, stop=True) for g in (2 * hb, 2 * hb + 1)],
                        r=["fT", "cs128"], w=[pk[bank]])
                    eng = "dve" if hb == 0 else "act"
                    if eng == "dve":
                        P.op("dve", lambda e, T=T, hb=hb, bank=bank: e.tensor_copy(
                            out=G[:, T, 2 * hb:2 * hb + 2, :], in_=PS[bank].rearrange("p (a b) -> p a b", a=2)),
                            r=[pk[bank]], w=["G_%d" % T])
                    else:
                        P.op("act", lambda e, T=T, hb=hb, bank=bank: e.activation(
                            out=G[:, T, 2 * hb:2 * hb + 2, :], in_=PS[bank].rearrange("p (a b) -> p a b", a=2), func=AF.Copy),
                            r=[pk[bank]], w=["G_%d" % T])

        if stage == 1:
            P.barrier()
            dtmp = xin[0]
            n = [0]

            def dump(src, col0, ncols):
                n[0] += 1
                P.op("dve", lambda e: e.tensor_copy(out=dtmp[:, 0:ncols], in_=src), r=["xin0"], w=["xin0"])
                P.dma("sp", lambda e: e.dma_start(out=dbg[:, col0:col0 + ncols], in_=dtmp[:, 0:ncols]), "dbg%d" % n[0], r=["xin0"])
            for q in range(4):
                dump(vT[:, 1, 15 + q * 1024:15 + (q + 1) * 1024], q * 1024, 1024)
            dump(G[:, 5, :, :].rearrange("p a b -> p (a b)"), 4096, 1024)
            dump(rstd1, 5120, NT)
            P.barrier()
            P.emit()
            return nc

        A.release(mA2)
        P.barrier()
        csb = [A.alloc([8, 512], BF16) for _ in range(2)]
        ssb = [A.alloc([8, 512], BF16) for _ in range(2)]
        frs = [A.alloc([4, 512], BF16) for _ in range(2)]
        Gkeys = ["G_%d" % t for t in range(NT)]
        it = 0
        for kc in range(8):
            bset = 4 * (kc % 2)
            for tg in range(4):
                b = it % 2
                it += 1
                P.dma("sp", lambda e, b=b, tg=tg, kc=kc: e.dma_start(out=csb[b].rearrange("p a b -> p (a b)"), in_=CSd[kc * 4 + tg]),
                      "ld_csb%d" % b, w=["csb%d" % b])
                P.dma("act", lambda e, b=b, tg=tg, kc=kc: e.dma_start(out=ssb[b].rearrange("p a b -> p (a b)"), in_=SSd[kc * 4 + tg]),
                      "ld_ssb%d" % b, w=["ssb%d" % b])
                fns = []
                for t8 in range(8):
                    t = tg * 8 + t8
                    for g in range(4):
                        fns.append(lambda e, t=t, t8=t8, g=g, b=b, bset=bset: e.matmul(
                            PS[bset + g], lhsT=G[:, t, g, 0:128], rhs=csb[b][:, t8, :], start=(t == 0), stop=False))
                        fns.append(lambda e, t=t, t8=t8, g=g, b=b, bset=bset: e.matmul(
                            PS[bset + g], lhsT=G[:, t, g, 128:256], rhs=ssb[b][:, t8, :], start=False, stop=(t == NT - 1)))
                P.group("pe", fns, r=Gkeys + ["csb%d" % b, "ssb%d" % b], w=[pk[bset + g] for g in range(4)])
            fb_ = kc % 2
            for g in range(4):
                if g % 2 == 0:
                    P.op("dve", lambda e, g=g, bset=bset, fb_=fb_: e.tensor_copy(out=frs[fb_][:, g, :], in_=PS[bset + g]),
                         r=[pk[bset + g]], w=["frs%d" % fb_])
                else:
                    P.op("act", lambda e, g=g, bset=bset, fb_=fb_: e.activation(out=frs[fb_][:, g, :], in_=PS[bset + g], func=AF.Copy),
                         r=[pk[bset + g]], w=["frs%d" % fb_])
            P.dma("sp", lambda e, kc=kc, fb_=fb_: e.dma_start(
                out=frd.rearrange("(g j) n -> j g n", j=128)[:, :, kc * 512:(kc + 1) * 512], in_=frs[fb_]),
                "st_frs%d" % fb_, r=["frs%d" % fb_], w=["frd_%d" % kc])

        if stage == 2:
            P.barrier()
            ld = A.alloc([S], BF16)
            dtmp = A.alloc([D], F32)
            P.dma("sp", lambda e: e.dma_start(out=ld, in_=frd[128:256, :]), "dbgL", w=["ld"])
            for q in range(4):
                P.op("dve", lambda e, q=q: e.tensor_copy(out=dtmp, in_=ld[:, q * 1024:(q + 1) * 1024]), r=["ld", "dtmp"], w=["dtmp"])
                P.dma("sp", lambda e, q=q: e.dma_start(out=dbg[:, q * 1024:(q + 1) * 1024], in_=dtmp), "dbgA%d" % q, r=["dtmp"])
            P.barrier()
            P.emit()
            return nc

        A.release(mV)
        P.barrier()
        vkeys = ["vT_%d" % i for i in range(8)] + ["vT"]
        convT = A.alloc([4, S], BF16)
        conv_end = A.mark()
        diag = A.alloc([4, KW, 128], BF16)
        n_ = 0
        for c in range(4):
            for tap in range(KW):
                eng = "dve"
                n_ += 1
                P.op(eng, lambda e, c=c, tap=tap: e.tensor_scalar(out=diag[:, c, tap, :], in0=identb, scalar1=dwcol[:, tap * 4 + c:tap * 4 + c + 1],
                                                                scalar2=None, op0=ALU.mult), r=["identb", "dwcol"], w=["diag%d" % c])
        ev = 0
        for c in range(4):
            for blk in range(8):
                bank = ev % 8
                P.group("pe", [lambda e, c=c, blk=blk, tap=tap, bank=bank: e.matmul(
                    PS[bank], lhsT=diag[:, c, tap, :], rhs=vT[:, c, blk * 512 + tap:blk * 512 + tap + 512], start=(tap == 0), stop=(tap == KW - 1))
                    for tap in range(KW)], r=["diag%d" % c] + vkeys, w=[pk[bank]])
                if ev % 2 == 0:
                    P.op("act", lambda e, c=c, blk=blk, bank=bank: e.activation(out=convT[:, c, blk * 512:(blk + 1) * 512], in_=PS[bank], func=AF.Identity,
                                                                              bias=smallcol[:, SC_DWB + c:SC_DWB + c + 1], scale=1.0),
                         r=[pk[bank], "smallcol"], w=["convT"])
                else:
                    P.op("dve", lambda e, c=c, blk=blk, bank=bank: e.tensor_scalar(out=convT[:, c, blk * 512:(blk + 1) * 512], in0=PS[bank],
                                                                                 scalar1=smallcol[:, SC_DWB + c:SC_DWB + c + 1], scalar2=None, op0=ALU.add),
                         r=[pk[bank], "smallcol"], w=["convT"])
                ev += 1

        if stage == 25:
            P.barrier()
            A.release(mA2)
            dtmp = A.alloc([D], F32)
            for q in range(4):
                P.op("dve", lambda e, q=q: e.tensor_copy(out=dtmp, in_=convT[:, 1, q * 1024:(q + 1) * 1024]), r=["dtmp"], w=["dtmp"])
                P.dma("sp", lambda e, q=q: e.dma_start(out=dbg[:, q * 1024:(q + 1) * 1024], in_=dtmp), "dbgA%d" % q, r=["dtmp"])
            P.barrier()
            P.emit()
            return nc

        P.barrier()
        A.release(mA)
        cwo = A.alloc([4, D], BF16)
        fw = A.alloc([4, D], BF16)
        wo = A.alloc([8, D], BF16)
        assert A.mark() <= mV
        A.release(conv_end)
        wg = A.alloc([8, 2048], BF16)
        SCB = 256
        NSC = S // SCB
        TPS = SCB // 128
        xinB = [A.alloc([D], F32) for _ in range(2)]
        xmB = A.alloc([D], BF16)
        xhB = [A.alloc([8, SCB], BF16) for _ in range(2)]
        sq = A.alloc([4, SCB], F32)
        mean_sb = A.alloc([SCB], F32)
        var_sb = A.alloc([SCB], F32)
        rstd_ln = A.alloc([SCB], F32)
        ycen = [A.alloc([SCB], F32) for _ in range(2)]
        vact = [A.alloc([4, SCB], BF16) for _ in range(2)]
        sg0 = [A.alloc([SCB], F32) for _ in range(2)]
        sg1 = [A.alloc([SCB], F32) for _ in range(2)]
        mm1 = A.alloc([SCB], F32)
        mm2 = A.alloc([SCB], F32)
        merged = A.alloc([8, SCB], BF16)
        hbuf = [A.alloc([D], F32) for _ in range(2)]
        htmp = A.alloc([512], F32)
        u2b = [A.alloc([D], BF16) for _ in range(2)]
        u2T = A.alloc([8, 128], F32)
        frc = [A.alloc([4, SCB], BF16) for _ in range(2)]

        for k in range(8):
            P.dma("pool", lambda e, k=k: e.dma_start(out=wg[:, k, :], in_=win_v[:, k, 1536:3584]), "ld_wg", w=["wg"])
            P.dma("pool", lambda e, k=k: e.dma_start(out=wo[:, k, :], in_=w_out.rearrange("(k p) n -> p k n", p=128)[:, k, :]), "ld_wo", w=["wo"])
        for c in range(4):
            P.dma("pool", lambda e, c=c: e.dma_start(out=cwo[:, c, :], in_=conv_w_out.rearrange("(k p) n -> p k n", p=128)[:, c, :]), "ld_cwo", w=["cwo"])
            P.dma("pool", lambda e, c=c: e.dma_start(out=fw[:, c, :], in_=fourier_w.rearrange("(k p) n -> p k n", p=128)[:, c, :]), "ld_fw", w=["fw"])
        frd_v = frd.rearrange("(g j) n -> j g n", j=128)

        def stage_X(sc):
            p = sc % 2
            T0 = sc * SCB
            P.dma("act", lambda e: e.dma_start(out=frc[p], in_=frd_v[:, :, T0:T0 + SCB]), "ld_frc%d" % p, w=["frc%d" % p])
            for tt in range(TPS):
                T = sc * TPS + tt
                xk = "xinB%d" % (T % 2)
                xhat_tile(T, xinB[T % 2], xk, xinB[T % 2], xmB, xhB[p][:, :, tt * 128:(tt + 1) * 128], "xhB%d" % p, 0, False, tmp_k=xk)

        def stage_L(sc):
            p = sc % 2
            T0 = sc * SCB
            P.op("act", lambda e: e.activation(out=sq, in_=convT[:, :, T0:T0 + SCB], func=AF.Square), r=["convT"], w=["sq"])
            P.group("pe", [lambda e, c=c: e.matmul(PS[1][:, 0:SCB], lhsT=ones_lnb, rhs=convT[:, c, T0:T0 + SCB], start=(c == 0), stop=(c == 3)) for c in range(4)]
                    + [lambda e, c=c: e.matmul(PS[1][:, SCB:2 * SCB], lhsT=ones_ln, rhs=sq[:, c, :], start=(c == 0), stop=(c == 3)) for c in range(4)],
                    r=["convT", "sq", "ones_ln", "ones_lnb"], w=[pk[1]])
            P.op("act", lambda e: e.activation(out=mean_sb, in_=PS[1][:, 0:SCB], func=AF.Copy), r=[pk[1]], w=["mean_sb"])
            P.op("dve", lambda e: e.tensor_tensor(out=var_sb, in0=mean_sb, in1=mean_sb, op=ALU.mult), r=["mean_sb"], w=["var_sb"])
            P.op("dve", lambda e: e.tensor_tensor(out=var_sb, in0=PS[1][:, SCB:2 * SCB], in1=var_sb, op=ALU.subtract), r=[pk[1], "var_sb"], w=["var_sb"])
            P.op("act", lambda e: e.activation(out=rstd_ln, in_=var_sb, func=AF.Sqrt, bias=eps5[:, 0:1], scale=1.0), r=["var_sb", "eps5"], w=["rstd_ln"])
            P.op("dve", lambda e: e.reciprocal(out=rstd_ln, in_=rstd_ln), r=["rstd_ln"], w=["rstd_ln"])
            for c in range(4):
                y = ycen[c % 2]
                yk = "ycen%d" % (c % 2)
                P.op("dve", lambda e, c=c, y=y: e.tensor_tensor(out=y, in0=convT[:, c, T0:T0 + SCB], in1=mean_sb, op=ALU.subtract), r=["convT", "mean_sb"], w=[yk])
                P.op("dve", lambda e, y=y: e.tensor_tensor(out=y, in0=y, in1=rstd_ln, op=ALU.mult), r=[yk, "rstd_ln"], w=[yk])
                P.op("act", lambda e, c=c, y=y: e.activation(out=vact[p][:, c, :], in_=y, func=AF.Silu,
                                                            bias=smallcol[:, SC_LNB + c:SC_LNB + c + 1], scale=smallcol[:, SC_LNG + c:SC_LNG + c + 1]),
                     r=[yk, "smallcol"], w=["vact%d" % p])

        def stage_M(sc):
            p = sc % 2
            for dc in range(8):
                q = dc % 2
                b0 = 2 + 2 * q
                b1 = b0 + 1
                P.group("pe", [lambda e, c=c, dc=dc, b0=b0: e.matmul(PS[b0][:, 0:SCB], lhsT=cwo[:, c, dc * 128:(dc + 1) * 128], rhs=vact[p][:, c, :],
                                                                    start=(c == 0), stop=(c == 3)) for c in range(4)]
                        + [lambda e, g=g, dc=dc, b0=b0: e.matmul(PS[b0][:, SCB:2 * SCB], lhsT=fw[:, g, dc * 128:(dc + 1) * 128], rhs=frc[p][:, g, :],
                                                                  start=(g == 0), stop=(g == 3)) for g in range(4)],
                        r=["cwo", "fw", "vact%d" % p, "frc%d" % p], w=[pk[b0]])
                P.group("pe", [lambda e, k=k, dc=dc, b1=b1: e.matmul(PS[b1][:, 0:SCB], lhsT=wg[:, k, dc * 128:(dc + 1) * 128], rhs=xhB[p][:, k, :],
                                                                    start=(k == 0), stop=(k == 7)) for k in range(8)]
                        + [lambda e, k=k, dc=dc, b1=b1: e.matmul(PS[b1][:, SCB:2 * SCB], lhsT=wg[:, k, 1024 + dc * 128:1024 + (dc + 1) * 128], rhs=xhB[p][:, k, :],
                                                                  start=(k == 0), stop=(k == 7)) for k in range(8)],
                        r=["wg", "xhB%d" % p], w=[pk[b1]])
                P.op("act", lambda e, dc=dc, b1=b1, q=q: e.activation(out=sg0[q], in_=PS[b1][:, 0:SCB], func=AF.Sigmoid,
                                                                     bias=smallcol[:, SC_BIN + 12 + dc:SC_BIN + 13 + dc], scale=1.0),
                     r=[pk[b1], "smallcol"], w=["sg0B%d" % q])
                P.op("act", lambda e, dc=dc, b1=b1, q=q: e.activation(out=sg1[q], in_=PS[b1][:, SCB:2 * SCB], func=AF.Sigmoid,
                                                                     bias=smallcol[:, SC_BIN + 20 + dc:SC_BIN + 21 + dc], scale=1.0),
                     r=[pk[b1], "smallcol"], w=["sg1B%d" % q])
                P.op("dve", lambda e, dc=dc, b0=b0, q=q: e.scalar_tensor_tensor(out=mm1, in0=PS[b0][:, 0:SCB], scalar=smallcol[:, SC_CBO + dc:SC_CBO + dc + 1],
                                                                               in1=sg0[q], op0=ALU.add, op1=ALU.mult),
                     r=[pk[b0], "sg0B%d" % q, "smallcol"], w=["mm1"])
                P.op("dve", lambda e, dc=dc, b0=b0, q=q: e.scalar_tensor_tensor(out=mm2, in0=PS[b0][:, SCB:2 * SCB], scalar=smallcol[:, SC_FB + dc:SC_FB + dc + 1],
                                                                               in1=sg1[q], op0=ALU.add, op1=ALU.mult),
                     r=[pk[b0], "sg1B%d" % q, "smallcol"], w=["mm2"])
                P.op("pool", lambda e, dc=dc: e.tensor_tensor(out=merged[:, dc, :], in0=mm1, in1=mm2, op=ALU.add),
                     r=["mm1", "mm2"], w=["merged"])

        def router(T):
            hb = hbuf[T % 2]
            hk = "hbuf%d" % (T % 2)
            for hf in range(2):
                P.group("pe", [lambda e, k=k, hf=hf: e.transpose(out=PS[hf][:, (k % 4) * 128:(k % 4 + 1) * 128],
                                                                in_=hb[:, k * 128:(k + 1) * 128], identity=ident)
                               for k in range(4 * hf, 4 * hf + 4)], r=[hk, "ident"], w=[pk[hf]])
                if hf == 0:
                    P.op("dve", lambda e: e.tensor_copy(out=u2T[:, 0:4, :], in_=PS[0].rearrange("p (a b) -> p a b", a=4)), r=[pk[0]], w=["u2T"])
                else:
                    P.op("act", lambda e: e.activation(out=u2T[:, 4:8, :], in_=PS[1].rearrange("p (a b) -> p a b", a=4), func=AF.Copy), r=[pk[1]], w=["u2T"])
            P.group("pe", [lambda e, k=k: e.matmul(PS[1][:, 0:E], lhsT=u2T[:, k, :], rhs=rw[:, k, :], start=(k == 0), stop=(k == 7)) for k in range(8)],
                    r=["u2T", "rw"], w=[pk[1]])
            P.op("dve", lambda e: e.tensor_copy(out=logits[:, T, :], in_=PS[1][:, 0:E]), r=[pk[1]], w=["logits"])

        def stage_H(sc):
            for tt in range(TPS):
                T = sc * TPS + tt
                hb = hbuf[T % 2]
                hk = "hbuf%d" % (T % 2)
                ub = u2b[T % 2]
                uk = "u2b%d" % (T % 2)
                P.dma("sp", lambda e, hb=hb, T=T: e.dma_start(out=hb, in_=x[T * 128:(T + 1) * 128, :]), "ld_" + hk, w=[hk])
                P.op("pool", lambda e, hb=hb: e.tensor_tensor(out=hb, in0=hb, in1=bcs[BC_GB1], op=ALU.add), r=[hk, "bc3"], w=[hk])
                for dh in range(2):
                    bh = 6 + dh
                    P.group("pe", [lambda e, k=k, tt=tt, dh=dh, bh=bh: e.matmul(PS[bh], lhsT=merged[:, k, tt * 128:(tt + 1) * 128],
                                                                               rhs=wo[:, k, dh * 512:(dh + 1) * 512], start=(k == 0), stop=(k == 7))
                                   for k in range(8)], r=["merged", "wo"], w=[pk[bh]])
                    P.op("dve", lambda e, dh=dh, bh=bh: e.tensor_tensor(out=htmp, in0=PS[bh], in1=bcs[BC_G1][:, dh * 512:(dh + 1) * 512], op=ALU.mult),
                         r=[pk[bh], "bc2"], w=["htmp"])
                    P.op("dve", lambda e, dh=dh, hb=hb: e.tensor_tensor(out=hb[:, dh * 512:(dh + 1) * 512], in0=hb[:, dh * 512:(dh + 1) * 512],
                                                                       in1=htmp, op=ALU.add), r=[hk, "htmp"], w=[hk])
                P.dma("sp", lambda e, T=T, hb=hb: e.dma_start(out=hd[T * 128:(T + 1) * 128, :], in_=hb), "st_" + hk, r=[hk], w=["hd_%d" % T])
                P.op("act", lambda e, T=T, hb=hb, ub=ub: e.activation(out=ub, in_=hb, func=AF.Square, accum_out=ss2[:, T:T + 1]),
                     r=[hk], w=[uk, "ss2_%d" % T])
                P.op("dve", lambda e, T=T: e.tensor_scalar(out=ss2[:, T:T + 1], in0=ss2[:, T:T + 1], scalar1=1.0 / D, scalar2=1e-6,
                                                           op0=ALU.mult, op1=ALU.add), r=["ss2_%d" % T], w=["ss2_%d" % T])
                P.op("pool", lambda e, T=T: e.tensor_tensor(out=rstd2[:, T:T + 1], in0=ss2[:, T:T + 1], in1=nhalf[:, 0:1], op=ALU.pow),
                     r=["ss2_%d" % T, "nhalf"], w=["rstd2_%d" % T])
                P.op("dve", lambda e, T=T, hb=hb: e.scalar_tensor_tensor(out=hb, in0=hb, scalar=rstd2[:, T:T + 1], in1=bcs[BC_GS2],
                                                                        op0=ALU.mult, op1=ALU.mult), r=[hk, "rstd2_%d" % T, "bc4"], w=[hk])
                P.op("pool", lambda e, hb=hb: e.tensor_tensor(out=hb, in0=hb, in1=bcs[BC_SH2], op=ALU.add), r=[hk, "bc5"], w=[hk])
                P.op("act", lambda e, hb=hb, ub=ub: e.activation(out=ub, in_=hb, func=AF.Copy), r=[hk], w=[uk])
                P.dma("sp", lambda e, T=T, ub=ub: e.dma_start(out=scr[T * 128:(T + 1) * 128, 0:D], in_=ub), "st_" + uk, r=[uk], w=["scr_%d" % T])
                if T > 0:
                    router(T - 1)

        stage_X(0)
        stage_L(0)
        for sc in range(NSC):
            if sc + 1 < NSC:
                stage_X(sc + 1)
                stage_L(sc + 1)
            stage_M(sc)
            stage_H(sc)
        router(NT - 1)

        if stage == 3:
            P.barrier()
            P.dma("sp", lambda e: e.dma_start(out=dbg[:, 0:NT * E], in_=logits.rearrange("p a b -> p (a b)")), "dbgA")
            P.dma("sp", lambda e: e.dma_start(out=hbuf[0], in_=hd[3 * 128:4 * 128, :]), "dbgL", w=["hb0x"])
            P.dma("sp", lambda e: e.dma_start(out=dbg[:, 1024:2048], in_=hbuf[0]), "dbgC", r=["hb0x"])
            P.dma("sp", lambda e: e.dma_start(out=u2b[0], in_=scr[3 * 128:4 * 128, 0:D]), "dbgL2", w=["ub0x"])
            P.op("dve", lambda e: e.tensor_copy(out=hbuf[1], in_=u2b[0]), r=["ub0x"], w=["hb1x"])
            P.dma("sp", lambda e: e.dma_start(out=dbg[:, 2048:3072], in_=hbuf[1]), "dbgD", r=["hb1x"])
            P.barrier()
            P.emit()
            return nc

        A.release(m_p2)
        P.barrier()
        junk = A.alloc([2048], BF16)
        aff_es = A.alloc([512], F32)
        maskt = A.alloc([512], F32)
        onest = A.alloc([512], F32)
        cum = A.alloc([512], F32)
        affp = A.alloc([4, 128], F32)
        lo = A.alloc([1], F32)
        mid = A.alloc([1], F32)
        cntp = A.alloc([1], F32)
        pred = A.alloc([1], F32)
        offs = A.alloc([1], F32)
        mx = A.alloc([NT], F32)
        sm = A.alloc([NT], F32)
        cum16 = A.alloc([512], I16)
        P.op("dve", lambda e: e.tensor_reduce(out=mx, in_=logits, axis=mybir.AxisListType.X, op=ALU.max), r=["logits"], w=["mx"])
        P.op("dve", lambda e: e.tensor_scalar(out=mx, in0=mx, scalar1=-1.0, scalar2=None, op0=ALU.mult), r=["mx"], w=["mx"])
        for T in range(NT):
            P.op("act", lambda e, T=T: e.activation(out=aff[:, T, :], in_=logits[:, T, :], func=AF.Exp, bias=mx[:, T:T + 1], scale=1.0,
                                                   accum_out=sm[:, T:T + 1]), r=["logits", "mx"], w=["aff", "sm"])
        P.op("dve", lambda e: e.reciprocal(out=sm, in_=sm), r=["sm"], w=["sm"])
        for T in range(NT):
            P.op("dve", lambda e, T=T: e.tensor_scalar(out=aff[:, T, :], in0=aff[:, T, :], scalar1=sm[:, T:T + 1], scalar2=None, op0=ALU.mult),
                 r=["aff", "sm"], w=["aff"])
        scr_v = scr.rearrange("(t p) c -> p t c", p=128)
        aff_b = aff.rearrange("p a b -> p (a b)").bitcast(BF16).rearrange("p (a b) -> p a b", a=NT)
        for q in range(4):
            P.dma("sp", lambda e, q=q: e.dma_start(out=scr_v[:, q * 8:(q + 1) * 8, D:D + 32], in_=aff_b[:, q * 8:(q + 1) * 8, :]),
                  "st_aff", r=["aff"], w=["scra_%d" % q])
        aff4 = aff.rearrange("p (s c) e -> p s c e", c=4)
        for tc in range(4):
            P.op("dve", lambda e, tc=tc: e.tensor_copy(out=affp[:, tc, :].rearrange("p (s e) -> p s e", e=E), in_=aff4[:, :, tc, :]),
                 r=["aff"], w=["affp"])
        P.group("pe", [lambda e, tc=tc: e.transpose(out=PS[0][:, tc * 128:(tc + 1) * 128], in_=affp[:, tc, :], identity=ident) for tc in range(4)],
                r=["affp", "ident"], w=[pk[0]])
        P.op("dve", lambda e: e.tensor_copy(out=aff_es, in_=PS[0]), r=[pk[0]], w=["aff_es"])
        P.op("dve", lambda e: e.memset(lo, 0.0), w=["lo"])
        P.op("pool", lambda e: e.memset(onest, 1.0), w=["onest"])
        for i in range(26):
            step = 2.0 ** -(i + 1)
            P.op("dve", lambda e, step=step: e.tensor_scalar(out=mid, in0=lo, scalar1=step, scalar2=None, op0=ALU.add), r=["lo"], w=["mid"])
            P.op("dve", lambda e: e.tensor_scalar(out=junk[:, 0:512], in0=aff_es, scalar1=mid[:, 0:1], scalar2=0.0, op0=ALU.is_ge, op1=ALU.add,
                                                  accum_out=cntp), r=["aff_es", "mid"], w=["junk", "cntp"])
            P.group("pe", [lambda e: e.matmul(PS[1][:, 0:1], lhsT=maskM, rhs=cntp, start=True, stop=True)], r=["maskM", "cntp"], w=[pk[1]])
            P.op("dve", lambda e: e.tensor_scalar(out=pred, in0=PS[1][:, 0:1], scalar1=511.5, scalar2=None, op0=ALU.is_ge), r=[pk[1]], w=["pred"])
            P.op("dve", lambda e, step=step: e.scalar_tensor_tensor(out=lo, in0=pred, scalar=step, in1=lo, op0=ALU.mult, op1=ALU.add),
                 r=["pred", "lo"], w=["lo"])
        P.op("dve", lambda e: e.tensor_scalar(out=maskt, in0=aff_es, scalar1=lo[:, 0:1], scalar2=None, op0=ALU.is_ge), r=["aff_es", "lo"], w=["maskt"])
        P.op("dve", lambda e: e.tensor_tensor_scan(out=cum, data0=onest, data1=maskt, initial=0.0, op0=ALU.mult, op1=ALU.add),
             r=["onest", "maskt"], w=["cum"])
        P.group("pe", [lambda e: e.matmul(PS[1][:, 0:1], lhsT=maskM2, rhs=cum[:, 511:512], start=True, stop=True)], r=["maskM2", "cum"], w=[pk[1]])
        P.op("dve", lambda e: e.tensor_copy(out=offs, in_=PS[1][:, 0:1]), r=[pk[1]], w=["offs"])
        P.op("dve", lambda e: e.tensor_scalar(out=cum, in0=cum, scalar1=offs[:, 0:1], scalar2=None, op0=ALU.add), r=["cum", "offs"], w=["cum"])
        P.op("dve", lambda e: e.tensor_copy(out=cum16, in_=cum), r=["cum"], w=["cum16"])
        for s8 in range(8):
            P.dma("sp", lambda e, s8=s8: e.dma_start(out=cumd[:, s8, :], in_=cum16[s8 * 16:(s8 + 1) * 16, :]), "st_cum", r=["cum16"], w=["cumd"])

        if stage == 4:
            P.barrier()
            P.dma("sp", lambda e: e.dma_start(out=dbg[:, 0:512], in_=aff_es), "dbgA")
            P.dma("sp", lambda e: e.dma_start(out=dbg[:, 512:1024], in_=cum), "dbgB")
            P.dma("sp", lambda e: e.dma_start(out=dbg[:, 1024:1025], in_=lo, allow_slow_non_contiguous=True), "dbgC")
            P.barrier()
            P.emit()
            return nc

        A.release(m_p1)
        P.barrier()
        junk = A.alloc([2048], BF16)
        bcs[BC_G2] = A.alloc([D], F32)
        bcs[BC_FG] = A.alloc([D], F32)
        P.dma("sp", lambda e: e.dma_start(out=bcs[BC_G2], in_=modd[0, 5 * D:6 * D].partition_broadcast(128)), "bc6", w=["bc6"])
        P.dma("act", lambda e: e.dma_start(out=bcs[BC_FG], in_=final_g[0, :].partition_broadcast(128)), "bc7", w=["bc7"])
        cb = A.alloc([S], I16)
        gath = A.alloc([4, 1056], BF16)
        tokT = [A.alloc([8, 512], BF16) for _ in range(2)]
        wgs = [A.alloc([8, 1024], BF16) for _ in range(2)]
        wus = [A.alloc([8, 1024], BF16) for _ in range(2)]
        wds = [A.alloc([8, 1024], BF16) for _ in range(2)]
        slu = [A.alloc([512], F32) for _ in range(2)]
        actT = A.alloc([8, 512], BF16)
        out1 = A.alloc([4, D], F32)
        outw = [A.alloc([D], F32) for _ in range(2)]
        otmp = A.alloc([512], F32)
        gath_f = gath.rearrange("p a b -> p (a b)").bitcast(F32).rearrange("p (a b) -> p a b", a=4)

        def load_weights(u):
            s_ = u % 2
            for nm, src, dst in (("wgs", ew_gate, wgs), ("wus", ew_up, wus), ("wds", ew_down, wds)):
                P.dma("pool", lambda e, src=src, dst=dst: e.dma_start(
                    out=dst[s_].rearrange("p a b -> p (a b)").rearrange("p (a b) -> p a b", b=2048),
                    in_=src[u].rearrange("p (a b) -> p a b", b=2048)), "ld_%s%d" % (nm, s_), w=["%s%d" % (nm, s_)])

        def route_idx(e_):
            P.dma("sp", lambda e: e.dma_start(out=cb, in_=cumd[e_].rearrange("s n -> (s n)").partition_broadcast(128)), "ld_cb",
                  r=["cumd"], w=["cb"])
            for cc in range(4):
                for hf in range(2):
                    P.op("dve", lambda e, cc=cc, hf=hf: e.tensor_scalar(
                        out=junk, in0=cb[:, hf * 2048:(hf + 1) * 2048], scalar1=cvals[:, cc:cc + 1], scalar2=0.0, op0=ALU.is_le, op1=ALU.add,
                        accum_out=idxp[:, e_ * 8 + cc * 2 + hf:e_ * 8 + cc * 2 + hf + 1]), r=["cb", "cvals"], w=["junk", "idxp%d" % e_])
            idxp_v = idxp[:, e_ * 8:(e_ + 1) * 8].rearrange("p (c h) -> p c h", h=2)
            P.op("dve", lambda e: e.tensor_tensor(out=idxf[:, e_ * 4:(e_ + 1) * 4], in0=idxp_v[:, :, 0], in1=idxp_v[:, :, 1], op=ALU.add),
                 r=["idxp%d" % e_], w=["idxf%d" % e_])
            P.op("dve", lambda e: e.tensor_copy(out=idxi[:, e_ * 4:(e_ + 1) * 4], in_=idxf[:, e_ * 4:(e_ + 1) * 4]), r=["idxf%d" % e_], w=["idxi%d" % e_])

        def gather_rows(e_):
            scr_keys = ["scr_%d" % t for t in range(NT)] + ["scra_%d" % q for q in range(4)]
            for cc in range(4):
                P.dma("pool", lambda e, cc=cc: e.indirect_dma_start(
                    out=gath[:, cc, :], out_offset=None, in_=scr[:, :],
                    in_offset=bass.IndirectOffsetOnAxis(ap=idxi[:, e_ * 4 + cc:e_ * 4 + cc + 1], axis=0)),
                    "ld_gath", r=["idxi%d" % e_] + scr_keys, w=["gath"])
            P.op("dve", lambda e: e.tensor_copy(out=wsel[:, e_ * 4:(e_ + 1) * 4], in_=gath_f[:, :, 512 + e_]), r=["gath"], w=["wsel%d" % e_])

        def transpose_tok(e_):
            tb = e_ % 2
            for cc in range(4):
                bank = 6 + (cc % 2)
                P.group("pe", [lambda e, k=k, cc=cc, bank=bank: e.transpose(out=PSB[bank][:, k * 128:(k + 1) * 128],
                                                                           in_=gath[:, cc, k * 128:(k + 1) * 128], identity=identb)
                               for k in range(8)], r=["gath", "identb"], w=[pk[bank]])
                if cc % 2 == 0:
                    P.op("act", lambda e, cc=cc, bank=bank: e.activation(out=tokT[tb][:, :, cc * 128:(cc + 1) * 128],
                                                                        in_=PSB[bank].rearrange("p (k t) -> p k t", k=8), func=AF.Copy),
                         r=[pk[bank]], w=["tokT%d" % tb])
                else:
                    P.op("dve", lambda e, cc=cc, bank=bank: e.tensor_copy(out=tokT[tb][:, :, cc * 128:(cc + 1) * 128],
                                                                         in_=PSB[bank].rearrange("p (k t) -> p k t", k=8)),
                         r=[pk[bank]], w=["tokT%d" % tb])

        hd_keys = ["hd_%d" % t for t in range(NT)]
        sc_keys = ["hd_sc0", "hd_sc1", "hd_sc2", "hd_sc3"]
        ocnt = [0]

        def unit(u):
            e_, fh = u // 2, u % 2
            s_ = u % 2
            tb = e_ % 2
            for fc in range(8):
                bg_, bu_ = 2 * (fc % 2), 2 * (fc % 2) + 1
                P.group("pe", [lambda e, k=k, fc=fc, bg_=bg_: e.matmul(PS[bg_], lhsT=wgs[s_][:, k, fc * 128:(fc + 1) * 128], rhs=tokT[tb][:, k, :],
                                                                      start=(k == 0), stop=(k == 7)) for k in range(8)],
                        r=["wgs%d" % s_, "tokT%d" % tb], w=[pk[bg_]])
                P.group("pe", [lambda e, k=k, fc=fc, bu_=bu_: e.matmul(PS[bu_], lhsT=wus[s_][:, k, fc * 128:(fc + 1) * 128], rhs=tokT[tb][:, k, :],
                                                                      start=(k == 0), stop=(k == 7)) for k in range(8)],
                        r=["wus%d" % s_, "tokT%d" % tb], w=[pk[bu_]])
                si = fc % 2
                P.op("act", lambda e, bg_=bg_, si=si: e.activation(out=slu[si], in_=PS[bg_], func=AF.Silu), r=[pk[bg_]], w=["slu%d" % si])
                P.op("dve", lambda e, fc=fc, bu_=bu_, si=si: e.tensor_tensor(out=actT[:, fc, :], in0=slu[si], in1=PS[bu_], op=ALU.mult),
                     r=["slu%d" % si, pk[bu_]], w=["actT"])
            for cc in range(4):
                if fh == 1:
                    ob = ocnt[0] % 2
                    ocnt[0] += 1
                for dh in range(2):
                    bo = 4 + dh
                    P.group("pe", [lambda e, fc=fc, cc=cc, dh=dh, bo=bo: e.matmul(PS[bo], lhsT=actT[:, fc, cc * 128:(cc + 1) * 128],
                                                                                 rhs=wds[s_][:, fc, dh * 512:(dh + 1) * 512],
                                                                                 start=(fc == 0), stop=(fc == 7)) for fc in range(8)],
                            r=["actT", "wds%d" % s_], w=[pk[bo]])
                    if fh == 0:
                        P.op("act", lambda e, cc=cc, dh=dh, bo=bo: e.activation(out=out1[:, cc, dh * 512:(dh + 1) * 512], in_=PS[bo], func=AF.Copy),
                             r=[pk[bo]], w=["out1"])
                    else:
                        P.op("dve", lambda e, cc=cc, dh=dh, bo=bo: e.tensor_tensor(out=otmp, in0=PS[bo], in1=out1[:, cc, dh * 512:(dh + 1) * 512], op=ALU.add),
                             r=[pk[bo], "out1"], w=["otmp"])
                        P.op("dve", lambda e, cc=cc, dh=dh, ob=ob: e.scalar_tensor_tensor(
                            out=outw[ob][:, dh * 512:(dh + 1) * 512], in0=otmp, scalar=wsel[:, e_ * 4 + cc:e_ * 4 + cc + 1],
                            in1=bcs[BC_G2][:, dh * 512:(dh + 1) * 512], op0=ALU.mult, op1=ALU.mult),
                            r=["otmp", "wsel%d" % e_, "bc6"], w=["outw%d" % ob])
                if fh == 1:
                    P.dma("pool", lambda e, cc=cc, ob=ob: e.indirect_dma_start(
                        out=hd[:, :], out_offset=bass.IndirectOffsetOnAxis(ap=idxi[:, e_ * 4 + cc:e_ * 4 + cc + 1], axis=0),
                        in_=outw[ob], in_offset=None, compute_op=ALU.add),
                        "sc_outw%d" % ob, r=["outw%d" % ob, "idxi%d" % e_] + hd_keys + (sc_keys if cc == 0 else []),
                        w=["hd_sc%d" % cc])

        P.op("dve", lambda e: e.memset(idxp, 0.0), w=["idxp%d" % i for i in range(E)])
        load_weights(0)
        load_weights(1)
        route_idx(0)
        gather_rows(0)
        transpose_tok(0)
        NEXP = int(os.environ.get('K_NEXP', E))
        for e_ in range(NEXP):
            if e_ + 1 < E:
                route_idx(e_ + 1)
            unit(2 * e_)
            if e_ + 1 < E:
                gather_rows(e_ + 1)
            if 2 * e_ + 2 < 2 * E:
                load_weights(2 * e_ + 2)
            if e_ + 1 < E:
                transpose_tok(e_ + 1)
            unit(2 * e_ + 1)
            if 2 * e_ + 3 < 2 * E:
                load_weights(2 * e_ + 3)

        fin = [A.alloc([D], F32) for _ in range(2)]
        fss = A.alloc([NT], F32)
        frs_ = A.alloc([NT], F32)
        last = []
        for T in range(NT):
            b = T % 2
            P.dma("sp", lambda e, T=T, b=b: e.dma_start(out=fin[b], in_=hd[T * 128:(T + 1) * 128, :]), "ld_fin%d" % b,
                  r=sc_keys + ["hd_%d" % T], w=["fin%d" % b])
            P.op("act", lambda e, T=T, b=b: e.activation(out=junk[:, 0:D], in_=fin[b], func=AF.Square, accum_out=fss[:, T:T + 1]),
                 r=["fin%d" % b], w=["junk", "fss%d" % T])
            P.op("dve", lambda e, T=T: e.tensor_scalar(out=fss[:, T:T + 1], in0=fss[:, T:T + 1], scalar1=1.0 / D, scalar2=1e-6,
                                                       op0=ALU.mult, op1=ALU.add), r=["fss%d" % T], w=["fss%d" % T])
            P.op("pool", lambda e, T=T: e.tensor_tensor(out=frs_[:, T:T + 1], in0=fss[:, T:T + 1], in1=nhalf[:, 0:1], op=ALU.pow),
                 r=["fss%d" % T, "nhalf"], w=["frs%d" % T])
            P.op("dve", lambda e, T=T, b=b: e.scalar_tensor_tensor(out=fin[b], in0=fin[b], scalar=frs_[:, T:T + 1], in1=bcs[BC_FG],
                                                                   op0=ALU.mult, op1=ALU.mult), r=["fin%d" % b, "frs%d" % T, "bc7"], w=["fin%d" % b])
            last.append(P.dma("act", lambda e, T=T, b=b: e.dma_start(out=out[T * 128:(T + 1) * 128, :], in_=fin[b]), "st_fin%d" % b,
                              r=["fin%d" % b], w=["out_%d" % T]))
        P.barrier()
        P.emit()
        print("arena high-water bytes", A.hw, "of", ARENA_BYTES)
    return nc


_CONST = {}


def _dft_layout(M):
    A_ = M.reshape(4, 8, 128, 8, 512)
    return np.ascontiguousarray(A_.transpose(3, 0, 2, 1, 4)).reshape(32, 128, 4096)


_WCACHE = {}


def _weight_layouts(inp):
    key = id(inp["expert_w_gate"])
    if _WCACHE.get("key") == key:
        return _WCACHE["val"]
    f = lambda a: np.asarray(a, dtype=np.float32)
    g = f(inp["expert_w_gate"][0]).reshape(E, 8, 128, 2, 1024).transpose(0, 3, 2, 1, 4)
    u = f(inp["expert_w_up"][0]).reshape(E, 8, 128, 2, 1024).transpose(0, 3, 2, 1, 4)
    d = f(inp["expert_w_down"][0]).reshape(E, 2, 8, 128, 1024).transpose(0, 1, 3, 2, 4)
    aw = f(inp["ada_w"][0]).reshape(8, 128, 12, 512).transpose(2, 1, 0, 3)
    val = dict(
        expert_w_gate_l=np.ascontiguousarray(g).reshape(E * 2, 128, 8192),
        expert_w_up_l=np.ascontiguousarray(u).reshape(E * 2, 128, 8192),
        expert_w_down_l=np.ascontiguousarray(d).reshape(E * 2, 128, 8192),
        ada_w_l=np.ascontiguousarray(aw).reshape(12, 128, 4096),
    )
    _WCACHE["key"] = key
    _WCACHE["val"] = val
    return val


def _constants():
    if _CONST:
        return _CONST
    bf = ml_dtypes.bfloat16
    d = np.arange(128)
    ang = 2.0 * np.pi * ((d[:, None] * d[None, :]) % 128) / 128.0
    cs128 = np.concatenate([np.cos(ang), np.sin(ang)], axis=1) / np.sqrt(128.0)
    s = np.arange(S, dtype=np.int64)
    m = (s[:, None] * s[None, :]) % S
    tab_c = np.cos(2.0 * np.pi * np.arange(S) / S) / 64.0
    tab_s = -np.sin(2.0 * np.pi * np.arange(S) / S) / 64.0
    q = np.arange(128)
    e_of = q % 16
    s_of = q // 16
    maskM = (e_of[:, None] == e_of[None, :]).astype(np.float32)
    maskM2 = ((e_of[:, None] == e_of[None, :]) & (s_of[:, None] < s_of[None, :])).astype(np.float32)
    cvals = (np.arange(4)[None, :] * 128 + np.arange(128)[:, None]).astype(np.float32)
    _CONST.update(
        cs128=cs128.astype(bf),
        dft_cos_l=_dft_layout(tab_c[m].astype(bf)),
        dft_sin_l=_dft_layout(tab_s[m].astype(bf)),
        maskM=maskM, maskM2=maskM2, cvals=cvals,
    )
    return _CONST


def make_in_map(b, inp):
    f = lambda a: np.ascontiguousarray(np.asarray(a, dtype=np.float32))
    C = _constants()
    WL = _weight_layouts(inp)
    smallrows = np.concatenate([
        f(inp["b_in"][0]).reshape(28, 128), f(inp["conv_dw_b"][0]).reshape(4, 128), f(inp["conv_ln_g"][0]).reshape(4, 128),
        f(inp["conv_ln_b"][0]).reshape(4, 128), f(inp["conv_b_out"][0]).reshape(8, 128), f(inp["fourier_b"][0]).reshape(8, 128)], axis=0)
    return {
        "x": f(inp["x"][b]),
        "c_col": np.ascontiguousarray(f(inp["c"][b]).reshape(8, 128).T),
        "ada_w_l": WL["ada_w_l"],
        "ada_b": f(inp["ada_b"][0]).reshape(1, -1),
        "norm1_g": f(inp["norm1_g"][0]).reshape(1, -1),
        "w_in": f(inp["w_in"][0]),
        "smallrows": np.ascontiguousarray(smallrows),
        "dw_rows": f(inp["conv_dw_w"][0]).reshape(124, 128),
        "conv_w_out": f(inp["conv_w_out"][0]),
        "fourier_w": f(inp["fourier_w"][0]),
        "w_out": f(inp["w_out"][0]),
        "b_out": f(inp["b_out"][0]).reshape(1, -1),
        "norm2_g": f(inp["norm2_g"][0]).reshape(1, -1),
        "router_w": f(inp["router_w"][0]),
        "expert_w_gate_l": WL["expert_w_gate_l"],
        "expert_w_up_l": WL["expert_w_up_l"],
        "expert_w_down_l": WL["expert_w_down_l"],
        "final_norm_g": f(inp["final_norm_g"]).reshape(1, -1),
        "cs128": C["cs128"], "dft_cos_l": C["dft_cos_l"], "dft_sin_l": C["dft_sin_l"],
        "maskM": C["maskM"], "maskM2": C["maskM2"], "cvals": C["cvals"],
    }


def kernel(**inputs):
    nc = build_program()
    in_maps = [make_in_map(b, inputs) for b in range(8)]
    res = run_bass_kernel_spmd(nc, in_maps, core_ids=list(range(8)))
    return np.stack([np.asarray(r["out"], dtype=np.float32) for r in res.results], axis=0)
```

```python
import os
from contextlib import ExitStack
import numpy as np
import ml_dtypes
import concourse.bass as bass
import concourse.mybir as mybir
from concourse.bass_utils import run_bass_kernel_spmd

F32 = mybir.dt.float32
BF16 = mybir.dt.bfloat16
I32 = mybir.dt.int32
I16 = mybir.dt.int16
AF = mybir.ActivationFunctionType
ALU = mybir.AluOpType

S = 4096
D = 1024
NT = 32
E = 16
CAP = 512
FF = 2048
KW = 31
ENGS = ("pe", "act", "dve", "pool", "sp")


class Prog:
    def __init__(self, nc, ctx):
        self.nc = nc
        self.ctx = ctx
        self.lists = {e: [] for e in ENGS}
        self.sem = {e: ctx.enter_context(nc.semaphore("prog_" + e)) for e in ENGS}
        self.cnt = {e: 0 for e in ENGS}
        self.waited = {}
        self.dma_sems = {}
        self.dma_cnt = {}
        self.dma_waited = {}
        self.last_w = {}
        self.readers = {}

    def _emit_waits(self, eng, deps):
        for d in deps:
            if d is None:
                continue
            if d[0] == "dma":
                _, name, val = d
                key = (eng, name)
                if self.dma_waited.get(key, 0) >= val:
                    continue
                self.dma_waited[key] = val
                sem = self.dma_sems[name]
                self.lists[eng].append(lambda e, sem=sem, val=val: e.wait_ge(sem, val))
            else:
                src, val = d
                key = (eng, src)
                if self.waited.get(key, 0) >= val:
                    continue
                self.waited[key] = val
                sem = self.sem[src]
                self.lists[eng].append(lambda e, sem=sem, val=val: e.wait_ge(sem, val))

    def _deps(self, r, w, extra):
        deps = list(extra)
        for k in r:
            if k in self.last_w:
                deps.append(self.last_w[k])
        for k in w:
            if k in self.last_w:
                deps.append(self.last_w[k])
            deps.extend(self.readers.get(k, []))
        return deps

    def _commit(self, tok, r, w):
        for k in r:
            self.readers.setdefault(k, []).append(tok)
        for k in w:
            self.last_w[k] = tok
            self.readers[k] = []

    def op(self, eng, fn, r=(), w=(), extra=()):
        self._emit_waits(eng, self._deps(r, w, extra))
        self.cnt[eng] += 1
        sem = self.sem[eng]
        self.lists[eng].append(lambda e, fn=fn, sem=sem: fn(e).then_inc(sem, 1))
        tok = (eng, self.cnt[eng])
        self._commit(tok, r, w)
        return tok

    def group(self, eng, fns, r=(), w=(), extra=()):
        self._emit_waits(eng, self._deps(r, w, extra))
        for fn in fns[:-1]:
            self.lists[eng].append(lambda e, fn=fn: fn(e))
        self.cnt[eng] += 1
        sem = self.sem[eng]
        fn = fns[-1]
        self.lists[eng].append(lambda e, fn=fn, sem=sem: fn(e).then_inc(sem, 1))
        tok = (eng, self.cnt[eng])
        self._commit(tok, r, w)
        return tok

    def dma(self, eng, fn, semname, r=(), w=(), extra=()):
        if semname not in self.dma_sems:
            self.dma_sems[semname] = self.ctx.enter_context(self.nc.semaphore("d_" + semname))
            self.dma_cnt[semname] = 0
        self._emit_waits(eng, self._deps(r, w, extra))
        self.dma_cnt[semname] += 16
        sem = self.dma_sems[semname]
        self.lists[eng].append(lambda e, fn=fn, sem=sem: fn(e).then_inc(sem, 16))
        tok = ("dma", semname, self.dma_cnt[semname])
        self._commit(tok, r, w)
        return tok

    def wait(self, eng, deps):
        self._emit_waits(eng, deps)

    def barrier(self):
        deps = [(src, self.cnt[src]) for src in ENGS if self.cnt[src] > 0]
        deps += [("dma", n, v) for n, v in self.dma_cnt.items() if v > 0]
        for eng in ENGS:
            self._emit_waits(eng, [d for d in deps if d[0] != eng])

    def emit(self):
        with self.nc.Block() as block:
            @block.tensor
            def _(e):
                for f in self.lists["pe"]:
                    f(e)

            @block.scalar
            def _(e):
                for f in self.lists["act"]:
                    f(e)

            @block.vector
            def _(e):
                for f in self.lists["dve"]:
                    f(e)

            @block.gpsimd
            def _(e):
                for f in self.lists["pool"]:
                    f(e)

            @block.sync
            def _(e):
                for f in self.lists["sp"]:
                    f(e)


class Arena:
    def __init__(self, big, total_bytes):
        self.big = big
        self.total = total_bytes
        self.off = 0

    def alloc(self, shape, dt):
        n = int(np.prod(shape))
        esz = 2 if dt in (BF16, I16) else 4
        nbytes = (n * esz + 63) // 64 * 64
        off = self.off
        self.off += nbytes
        self.hw = max(getattr(self, "hw", 0), self.off)
        assert self.off <= self.total, ("SBUF arena overflow", self.off, self.total)
        v = self.big[:, off // 4:(off + nbytes) // 4]
        if dt != F32:
            v = v.bitcast(dt)
        v = v[:, 0:n]
        if len(shape) == 2:
            v = v.rearrange("p (a b) -> p a b", a=shape[0])
        elif len(shape) == 3:
            v = v.rearrange("p (a b c) -> p a b c", a=shape[0], b=shape[1])
        return v

    def mark(self):
        return self.off

    def release(self, m):
        self.off = m


ARENA_BYTES = 200 * 1024


def build_program(stage=99):
    nc = bass.Bass("TRN2", target_bir_lowering=False)

    def din(name, shape, dt=F32):
        return nc.dram_tensor(name, list(shape), dt, kind="ExternalInput").ap()

    x = din("x", [S, D])
    c_col = din("c_col", [128, 8])
    ada_w = din("ada_w_l", [12, 128, 8 * 512])
    ada_b = din("ada_b", [1, 6 * D])
    norm1_g = din("norm1_g", [1, D])
    w_in = din("w_in", [D, 3584])
    smallrows = din("smallrows", [56, 128])
    dw_rows = din("dw_rows", [124, 128])
    conv_w_out = din("conv_w_out", [512, D])
    fourier_w = din("fourier_w", [512, D])
    w_out = din("w_out", [D, D])
    b_out = din("b_out", [1, D])
    norm2_g = din("norm2_g", [1, D])
    router_w = din("router_w", [D, E])
    ew_gate = din("expert_w_gate_l", [E * 2, 128, 8 * 1024])
    ew_up = din("expert_w_up_l", [E * 2, 128, 8 * 1024])
    ew_down = din("expert_w_down_l", [E * 2, 128, 8 * 1024])
    final_g = din("final_norm_g", [1, D])
    cs128_d = din("cs128", [128, 256], BF16)
    CSd = din("dft_cos_l", [32, 128, 4096], BF16)
    SSd = din("dft_sin_l", [32, 128, 4096], BF16)
    maskM_d = din("maskM", [128, 128])
    maskM2_d = din("maskM2", [128, 128])
    cvals_d = din("cvals", [128, 4])

    out = nc.dram_tensor("out", [S, D], F32, kind="ExternalOutput").ap()
    dbg = None
    if stage < 99:
        dbg = nc.dram_tensor("dbg", [128, 16384], F32, kind="ExternalOutput").ap()

    modd = nc.dram_tensor("modd", [1, 9 * D], F32, kind="Internal").ap()
    frd = nc.dram_tensor("frd", [512, S], BF16, kind="Internal").ap()
    scr = nc.dram_tensor("scr", [S, 1056], BF16, kind="Internal").ap()
    hd = nc.dram_tensor("hd", [S, D], F32, kind="Internal").ap()
    cumd = nc.dram_tensor("cumd", [E, 8, 512], I16, kind="Internal").ap()

    with ExitStack() as ctx:
        P = Prog(nc, ctx)
        big = ctx.enter_context(nc.sbuf_tensor("big", [128, ARENA_BYTES // 4], F32))
        A = Arena(big, ARENA_BYTES)
        psb = [ctx.enter_context(nc.psum_tensor("ps%d" % i, [128, 512], F32)) for i in range(8)]
        PS = [p[:, :] for p in psb]
        PSB = [p[:, :].bitcast(BF16) for p in psb]
        pk = ["ps%d" % i for i in range(8)]

        ident = A.alloc([128], F32)
        identb = A.alloc([128], BF16)
        smallcol = A.alloc([56], F32)
        dwcol = A.alloc([124], F32)
        rstd1 = A.alloc([NT], F32)
        ss1 = A.alloc([NT], F32)
        ss2 = A.alloc([NT], F32)
        rstd2 = A.alloc([NT], F32)
        logits = A.alloc([NT, E], F32)
        aff = A.alloc([NT, E], F32)
        maskM = A.alloc([128], F32)
        maskM2 = A.alloc([128], F32)
        cvals = A.alloc([4], F32)
        ones_ln = A.alloc([128], F32)
        nhalf = A.alloc([256], F32)
        eps6 = A.alloc([1], F32)
        eps5 = A.alloc([1], F32)
        cact = A.alloc([8], F32)
        rw = A.alloc([8, E], F32)
        idxf = A.alloc([E * 4], F32)
        idxp = A.alloc([E * 8], F32)
        idxi = A.alloc([E * 4], I32)
        wsel = A.alloc([E * 4], F32)
        cs128 = A.alloc([256], BF16)
        ones_lnb = A.alloc([128], BF16)
        BC_GS1, BC_SH1, BC_G1, BC_GB1, BC_GS2, BC_SH2, BC_G2, BC_FG = range(8)
        bcs = [None] * 8
        m_p1 = A.mark()
        for i in (BC_GS1, BC_SH1, BC_G1, BC_GB1, BC_GS2, BC_SH2):
            bcs[i] = A.alloc([D], F32)
        m_p2 = A.mark()

        P.op("pool", lambda e: e.memset(ident, 1.0), w=["ident"])
        P.op("pool", lambda e: e.affine_select(out=ident, in_=ident, pattern=[[-1, 128]], compare_op=ALU.is_equal,
                                               fill=0.0, base=0, channel_multiplier=1), w=["ident"])
        P.op("dve", lambda e: e.tensor_copy(out=identb, in_=ident), r=["ident"], w=["identb"])
        P.op("pool", lambda e: e.memset(ones_ln, 1.0 / 512.0), w=["ones_ln"])
        P.op("pool", lambda e: e.memset(ones_lnb, 1.0 / 512.0), w=["ones_lnb"])
        P.op("pool", lambda e: e.memset(nhalf, -0.5), w=["nhalf"])
        P.op("pool", lambda e: e.memset(eps6, 1e-6), w=["eps6"])
        P.op("pool", lambda e: e.memset(eps5, 1e-5), w=["eps5"])
        P.dma("sp", lambda e: e.dma_start(out=maskM, in_=maskM_d[:, :]), "c_maskM", w=["maskM"])
        P.dma("sp", lambda e: e.dma_start(out=maskM2, in_=maskM2_d[:, :]), "c_maskM2", w=["maskM2"])
        P.dma("sp", lambda e: e.dma_start(out=cvals, in_=cvals_d[:, :]), "c_cvals", w=["cvals"])
        P.dma("sp", lambda e: e.dma_start(out=cs128, in_=cs128_d[:, :]), "c_cs128", w=["cs128"])
        P.dma("sp", lambda e: e.dma_start(out=cact, in_=c_col[:, :]), "c_cact", w=["cact"])
        P.dma("sp", lambda e: e.dma_start(out=rw, in_=router_w.rearrange("(k p) n -> p k n", p=128)), "c_rw", w=["rw"])

        m0 = A.mark()
        modrow = A.alloc([9 * D], F32)
        adab = A.alloc([6 * D], F32)
        g1row = A.alloc([D], F32)
        g2row = A.alloc([D], F32)
        borow = A.alloc([D], F32)
        awb = [A.alloc([8, 512], F32) for _ in range(2)]
        rows_T = A.alloc([128], F32)
        P.dma("act", lambda e: e.dma_start(out=adab[0:1, :], in_=ada_b[:, :]), "c_adab", w=["adab"])
        P.dma("act", lambda e: e.dma_start(out=g1row[0:1, :], in_=norm1_g[:, :]), "c_g1", w=["g1row"])
        P.dma("act", lambda e: e.dma_start(out=g2row[0:1, :], in_=norm2_g[:, :]), "c_g2", w=["g2row"])
        P.dma("act", lambda e: e.dma_start(out=borow[0:1, :], in_=b_out[:, :]), "c_bo", w=["borow"])
        P.op("act", lambda e: e.activation(out=cact, in_=cact, func=AF.Silu), r=["cact"], w=["cact"])
        for pc in range(12):
            b = pc % 2
            P.dma("sp", lambda e, pc=pc, b=b: e.dma_start(out=awb[b].rearrange("p a b -> p (a b)"), in_=ada_w[pc]),
                  "awb%d" % b, w=["awb%d" % b])
            P.group("pe", [lambda e, k=k, b=b: e.matmul(PS[0][0:1, :], lhsT=cact[:, k:k + 1], rhs=awb[b][:, k, :],
                                                       start=(k == 0), stop=(k == 7)) for k in range(8)],
                    r=["cact", "awb%d" % b], w=[pk[0]])
            P.op("dve", lambda e, pc=pc: e.tensor_tensor(out=modrow[0:1, pc * 512:(pc + 1) * 512], in0=PS[0][0:1, :],
                                                        in1=adab[0:1, pc * 512:(pc + 1) * 512], op=ALU.add),
                 r=[pk[0], "adab"], w=["modrow"])
        P.op("dve", lambda e: e.scalar_tensor_tensor(out=modrow[0:1, 6 * D:7 * D], in0=modrow[0:1, D:2 * D], scalar=1.0,
                                                     in1=g1row[0:1, :], op0=ALU.add, op1=ALU.mult),
             r=["modrow", "g1row"], w=["modrow"])
        P.op("dve", lambda e: e.tensor_tensor(out=modrow[0:1, 7 * D:8 * D], in0=modrow[0:1, 2 * D:3 * D], in1=borow[0:1, :],
                                              op=ALU.mult), r=["modrow", "borow"], w=["modrow"])
        P.op("dve", lambda e: e.scalar_tensor_tensor(out=modrow[0:1, 8 * D:9 * D], in0=modrow[0:1, 4 * D:5 * D], scalar=1.0,
                                                     in1=g2row[0:1, :], op0=ALU.add, op1=ALU.mult),
             r=["modrow", "g2row"], w=["modrow"])
        P.dma("sp", lambda e: e.dma_start(out=modd[:, :], in_=modrow[0:1, :]), "st_modrow", r=["modrow"], w=["modd"])
        srcs = {BC_GS1: 6, BC_SH1: 0, BC_G1: 2, BC_GB1: 7, BC_GS2: 8, BC_SH2: 3}
        for i, off in srcs.items():
            P.dma("sp" if i % 2 == 0 else "act",
                  lambda e, i=i, off=off: e.dma_start(out=bcs[i], in_=modd[0, off * D:(off + 1) * D].partition_broadcast(128)),
                  "bc%d" % i, r=["modd"], w=["bc%d" % i])
        P.dma("sp", lambda e: e.dma_start(out=rows_T[0:56, :], in_=smallrows[:, :]), "c_rowsT", w=["rows_T"])
        P.group("pe", [lambda e: e.transpose(out=PS[1][:, 0:56], in_=rows_T[0:56, :], identity=ident[0:56, 0:56])],
                r=["rows_T", "ident"], w=[pk[1]])
        P.op("dve", lambda e: e.tensor_copy(out=smallcol, in_=PS[1][:, 0:56]), r=[pk[1]], w=["smallcol"])
        P.dma("sp", lambda e: e.dma_start(out=rows_T[0:124, :], in_=dw_rows[:, :]), "c_rowsT", w=["rows_T"])
        P.group("pe", [lambda e: e.transpose(out=PS[1][:, 0:124], in_=rows_T[0:124, :], identity=ident[0:124, 0:124])],
                r=["rows_T", "ident"], w=[pk[1]])
        P.op("dve", lambda e: e.tensor_copy(out=dwcol, in_=PS[1][:, 0:124]), r=[pk[1]], w=["dwcol"])
        SC_BIN, SC_DWB, SC_LNG, SC_LNB, SC_CBO, SC_FB = 0, 28, 32, 36, 40, 48

        if stage == 0:
            P.dma("sp", lambda e: e.dma_start(out=dbg[0:1, 0:9 * D], in_=modrow[0:1, :]), "dbg0", r=["modrow"])
            P.dma("sp", lambda e: e.dma_start(out=dbg[:, 9 * D:9 * D + 56], in_=smallcol), "dbg1", r=["smallcol"])
            P.dma("sp", lambda e: e.dma_start(out=dbg[:, 10 * D:10 * D + 124], in_=dwcol), "dbg2", r=["dwcol"])
            P.dma("sp", lambda e: e.dma_start(out=dbg[:, 11 * D:12 * D], in_=bcs[BC_GS1]), "dbg3", r=["bc0"])
            P.barrier()
            P.emit()
            return nc
        P.barrier()
        A.release(m0)

        def xhat_tile(T, xin, xin_k, tmp, xm, xhatT_dst, xhatT_k, psbank, first_pass, tmp_k="tmp", part=None, add_eng="pool"):
            if part == "B":
                P.group("pe", [lambda e, k=k: e.transpose(out=PSB[psbank][:, k * 128:(k + 1) * 128], in_=xm[:, k * 128:(k + 1) * 128],
                                                         identity=identb) for k in range(8)],
                        r=["xm", "identb"], w=[pk[psbank]])
                P.op("act", lambda e: e.activation(out=xhatT_dst, in_=PSB[psbank].rearrange("p (k t) -> p k t", k=8), func=AF.Copy),
                     r=[pk[psbank]], w=[xhatT_k])
                return
            P.dma("sp", lambda e: e.dma_start(out=xin, in_=x[T * 128:(T + 1) * 128, :]), "ld_" + xin_k, w=[xin_k])
            if first_pass:
                P.op("act", lambda e: e.activation(out=xm, in_=xin, func=AF.Square, accum_out=ss1[:, T:T + 1]),
                     r=[xin_k], w=["xm", "ss1_%d" % T])
                P.op("dve", lambda e: e.tensor_scalar(out=ss1[:, T:T + 1], in0=ss1[:, T:T + 1], scalar1=1.0 / D, scalar2=1e-6,
                                                      op0=ALU.mult, op1=ALU.add), r=["ss1_%d" % T], w=["ss1_%d" % T])
                P.op("pool", lambda e: e.tensor_tensor(out=rstd1[:, T:T + 1], in0=ss1[:, T:T + 1], in1=nhalf[:, 0:1], op=ALU.pow),
                     r=["ss1_%d" % T, "nhalf"], w=["rstd1_%d" % T])
            P.op("dve", lambda e: e.scalar_tensor_tensor(out=tmp, in0=xin, scalar=rstd1[:, T:T + 1], in1=bcs[BC_GS1],
                                                         op0=ALU.mult, op1=ALU.mult),
                 r=[xin_k, "rstd1_%d" % T, "bc0"], w=[tmp_k])
            P.op(add_eng, lambda e: e.tensor_tensor(out=xm, in0=tmp, in1=bcs[BC_SH1], op=ALU.add),
                 r=[tmp_k, "bc1"], w=["xm"])
            if part == "A":
                return
            P.group("pe", [lambda e, k=k: e.transpose(out=PSB[psbank][:, k * 128:(k + 1) * 128], in_=xm[:, k * 128:(k + 1) * 128],
                                                     identity=identb) for k in range(8)],
                    r=["xm", "identb"], w=[pk[psbank]])
            P.op("act", lambda e: e.activation(out=xhatT_dst, in_=PSB[psbank].rearrange("p (k t) -> p k t", k=8), func=AF.Copy),
                 r=[pk[psbank]], w=[xhatT_k])

        mA = A.mark()
        vT = A.alloc([4, S + 30], BF16)
        mV = A.mark()
        G = A.alloc([NT, 4, 256], BF16)
        mA2 = A.mark()
        wA = A.alloc([8, 1536], BF16)
        xin = [A.alloc([D], F32) for _ in range(2)]
        tmp = A.alloc([D], F32)
        xm = A.alloc([D], BF16)
        xhatT = [A.alloc([8, 512], BF16)] * 2
        sg = [A.alloc([512], F32) for _ in range(2)]
        fT = [A.alloc([4, 512], BF16)] * 2

        P.op("pool", lambda e: e.memset(vT[:, :, 0:15], 0.0), w=["vT"])
        P.op("pool", lambda e: e.memset(vT[:, :, S + 15:S + 30], 0.0), w=["vT"])
        win_v = w_in.rearrange("(k p) n -> p k n", p=128)
        for k in range(8):
            P.dma("pool", lambda e, k=k: e.dma_start(out=wA[:, k, :], in_=win_v[:, k, 0:1536]), "ld_wA", w=["wA"])

        for sc in range(8):
            xb_ = sc % 2
            for tt in range(4):
                T = sc * 4 + tt
                xhat_tile(T, xin[T % 2], "xin%d" % (T % 2), tmp, xm, xhatT[xb_][:, :, tt * 128:(tt + 1) * 128],
                          "xhatT", T % 2, True)
            xk = "xhatT"
            for c in range(4):
                ba, bg = 2 + 2 * (c % 2), 3 + 2 * (c % 2)
                P.group("pe", [lambda e, k=k, c=c, ba=ba: e.matmul(PS[ba], lhsT=wA[:, k, c * 128:(c + 1) * 128], rhs=xhatT[xb_][:, k, :],
                                                                  start=(k == 0), stop=(k == 7)) for k in range(8)],
                        r=["wA", xk], w=[pk[ba]])
                P.group("pe", [lambda e, k=k, c=c, bg=bg: e.matmul(PS[bg], lhsT=wA[:, k, 512 + c * 128:512 + (c + 1) * 128], rhs=xhatT[xb_][:, k, :],
                                                                  start=(k == 0), stop=(k == 7)) for k in range(8)],
                        r=["wA", xk], w=[pk[bg]])
                sgi = c % 2
                P.op("act", lambda e, c=c, bg=bg, sgi=sgi: e.activation(out=sg[sgi], in_=PS[bg], func=AF.Sigmoid,
                                                                      bias=smallcol[:, SC_BIN + 4 + c:SC_BIN + 5 + c], scale=1.0),
                     r=[pk[bg], "smallcol"], w=["sg%d" % sgi])
                P.op("dve", lambda e, c=c, ba=ba, sgi=sgi, sc=sc: e.scalar_tensor_tensor(
                    out=vT[:, c, 15 + sc * 512:15 + (sc + 1) * 512], in0=PS[ba], scalar=smallcol[:, SC_BIN + c:SC_BIN + c + 1],
                    in1=sg[sgi], op0=ALU.add, op1=ALU.mult),
                    r=[pk[ba], "sg%d" % sgi, "smallcol"], w=["vT_%d" % sc])
            fb_ = sc % 2
            for g in range(4):
                bf = 2 + (g % 4)
                P.group("pe", [lambda e, k=k, g=g, bf=bf: e.matmul(PS[bf], lhsT=wA[:, k, 1024 + g * 128:1024 + (g + 1) * 128], rhs=xhatT[xb_][:, k, :],
                                                                  start=(k == 0), stop=(k == 7)) for k in range(8)],
                        r=["wA", xk], w=[pk[bf]])
                P.op("act", lambda e, g=g, bf=bf: e.activation(out=fT[fb_][:, g, :], in_=PS[bf], func=AF.Identity,
                                                              bias=smallcol[:, SC_BIN + 8 + g:SC_BIN + 9 + g], scale=1.0),
                     r=[pk[bf], "smallcol"], w=["fT"])
            for tt in range(4):
                T = sc * 4 + tt
                for hb in range(2):
                    bank = 6 + hb
                    P.group("pe", [lambda e, g=g, tt=tt, bank=bank: e.matmul(
                        PS[bank][:, (g % 2) * 256:(g % 2 + 1) * 256], lhsT=fT[fb_][:, g, tt * 128:(tt + 1) * 128], rhs=cs128,
                        start=True, stop=True) for g in (2 * hb, 2 * hb + 1)],
                        r=["fT", "cs128"], w=[pk[bank]])
                    eng = "dve" if hb == 0 else "act"
                    if eng == "dve":
                        P.op("dve", lambda e, T=T, hb=hb, bank=bank: e.tensor_copy(
                            out=G[:, T, 2 * hb:2 * hb + 2, :], in_=PS[bank].rearrange("p (a b) -> p a b", a=2)),
                            r=[pk[bank]], w=["G_%d" % T])
                    else:
                        P.op("act", lambda e, T=T, hb=hb, bank=bank: e.activation(
                            out=G[:, T, 2 * hb:2 * hb + 2, :], in_=PS[bank].rearrange("p (a b) -> p a b", a=2), func=AF.Copy),
                            r=[pk[bank]], w=["G_%d" % T])

        if stage == 1:
            P.barrier()
            dtmp = xin[0]
            n = [0]

            def dump(src, col0, ncols):
                n[0] += 1
                P.op("dve", lambda e: e.tensor_copy(out=dtmp[:, 0:ncols], in_=src), r=["xin0"], w=["xin0"])
                P.dma("sp", lambda e: e.dma_start(out=dbg[:, col0:col0 + ncols], in_=dtmp[:, 0:ncols]), "dbg%d" % n[0], r=["xin0"])
            for q in range(4):
                dump(vT[:, 1, 15 + q * 1024:15 + (q + 1) * 1024], q * 1024, 1024)
            dump(G[:, 5, :, :].rearrange("p a b -> p (a b)"), 4096, 1024)
            dump(rstd1, 5120, NT)
            P.barrier()
            P.emit()
            return nc

        A.release(mA2)
        P.barrier()
        csb = [A.alloc([8, 512], BF16) for _ in range(2)]
        ssb = [A.alloc([8, 512], BF16) for _ in range(2)]
        frs = [A.alloc([4, 512], BF16) for _ in range(2)]
        Gkeys = ["G_%d" % t for t in range(NT)]
        it = 0
        for kc in range(8):
            bset = 4 * (kc % 2)
            for tg in range(4):
                b = it % 2
                it += 1
                P.dma("sp", lambda e, b=b, tg=tg, kc=kc: e.dma_start(out=csb[b].rearrange("p a b -> p (a b)"), in_=CSd[kc * 4 + tg]),
                      "ld_csb%d" % b, w=["csb%d" % b])
                P.dma("act", lambda e, b=b, tg=tg, kc=kc: e.dma_start(out=ssb[b].rearrange("p a b -> p (a b)"), in_=SSd[kc * 4 + tg]),
                      "ld_ssb%d" % b, w=["ssb%d" % b])
                fns = []
                for t8 in range(8):
                    t = tg * 8 + t8
                    for g in range(4):
                        fns.append(lambda e, t=t, t8=t8, g=g, b=b, bset=bset: e.matmul(
                            PS[bset + g], lhsT=G[:, t, g, 0:128], rhs=csb[b][:, t8, :], start=(t == 0), stop=False))
                        fns.append(lambda e, t=t, t8=t8, g=g, b=b, bset=bset: e.matmul(
                            PS[bset + g], lhsT=G[:, t, g, 128:256], rhs=ssb[b][:, t8, :], start=False, stop=(t == NT - 1)))
                P.group("pe", fns, r=Gkeys + ["csb%d" % b, "ssb%d" % b], w=[pk[bset + g] for g in range(4)])
            fb_ = kc % 2
            for g in range(4):
                if g % 2 == 0:
                    P.op("dve", lambda e, g=g, bset=bset, fb_=fb_: e.tensor_copy(out=frs[fb_][:, g, :], in_=PS[bset + g]),
                         r=[pk[bset + g]], w=["frs%d" % fb_])
                else:
                    P.op("act", lambda e, g=g, bset=bset, fb_=fb_: e.activation(out=frs[fb_][:, g, :], in_=PS[bset + g], func=AF.Copy),
                         r=[pk[bset + g]], w=["frs%d" % fb_])
            P.dma("sp", lambda e, kc=kc, fb_=fb_: e.dma_start(
                out=frd.rearrange("(g j) n -> j g n", j=128)[:, :, kc * 512:(kc + 1) * 512], in_=frs[fb_]),
                "st_frs%d" % fb_, r=["frs%d" % fb_], w=["frd_%d" % kc])

        if stage == 2:
            P.barrier()
            ld = A.alloc([S], BF16)
            dtmp = A.alloc([D], F32)
            P.dma("sp", lambda e: e.dma_start(out=ld, in_=frd[128:256, :]), "dbgL", w=["ld"])
            for q in range(4):
                P.op("dve", lambda e, q=q: e.tensor_copy(out=dtmp, in_=ld[:, q * 1024:(q + 1) * 1024]), r=["ld", "dtmp"], w=["dtmp"])
                P.dma("sp", lambda e, q=q: e.dma_start(out=dbg[:, q * 1024:(q + 1) * 1024], in_=dtmp), "dbgA%d" % q, r=["dtmp"])
            P.barrier()
            P.emit()
            return nc

        A.release(mV)
        P.barrier()
        vkeys = ["vT_%d" % i for i in range(8)] + ["vT"]
        convT = A.alloc([4, S], BF16)
        conv_end = A.mark()
        diag = A.alloc([4, KW, 128], BF16)
        n_ = 0
        for c in range(4):
            for tap in range(KW):
                eng = "dve"
                n_ += 1
                P.op(eng, lambda e, c=c, tap=tap: e.tensor_scalar(out=diag[:, c, tap, :], in0=identb, scalar1=dwcol[:, tap * 4 + c:tap * 4 + c + 1],
                                                                scalar2=None, op0=ALU.mult), r=["identb", "dwcol"], w=["diag%d" % c])
        ev = 0
        for c in range(4):
            for blk in range(8):
                bank = ev % 8
                P.group("pe", [lambda e, c=c, blk=blk, tap=tap, bank=bank: e.matmul(
                    PS[bank], lhsT=diag[:, c, tap, :], rhs=vT[:, c, blk * 512 + tap:blk * 512 + tap + 512], start=(tap == 0), stop=(tap == KW - 1))
                    for tap in range(KW)], r=["diag%d" % c] + vkeys, w=[pk[bank]])
                if ev % 2 == 0:
                    P.op("act", lambda e, c=c, blk=blk, bank=bank: e.activation(out=convT[:, c, blk * 512:(blk + 1) * 512], in_=PS[bank], func=AF.Identity,
                                                                              bias=smallcol[:, SC_DWB + c:SC_DWB + c + 1], scale=1.0),
                         r=[pk[bank], "smallcol"], w=["convT"])
                else:
                    P.op("dve", lambda e, c=c, blk=blk, bank=bank: e.tensor_scalar(out=convT[:, c, blk * 512:(blk + 1) * 512], in0=PS[bank],
                                                                                 scalar1=smallcol[:, SC_DWB + c:SC_DWB + c + 1], scalar2=None, op0=ALU.add),
                         r=[pk[bank], "smallcol"], w=["convT"])
                ev += 1

        if stage == 25:
            P.barrier()
            A.release(mA2)
            dtmp = A.alloc([D], F32)
            for q in range(4):
                P.op("dve", lambda e, q=q: e.tensor_copy(out=dtmp, in_=convT[:, 1, q * 1024:(q + 1) * 1024]), r=["dtmp"], w=["dtmp"])
                P.dma("sp", lambda e, q=q: e.dma_start(out=dbg[:, q * 1024:(q + 1) * 1024], in_=dtmp), "dbgA%d" % q, r=["dtmp"])
            P.barrier()
            P.emit()
            return nc

        P.barrier()
        A.release(mA)
        cwo = A.alloc([4, D], BF16)
        fw = A.alloc([4, D], BF16)
        wo = A.alloc([8, D], BF16)
        assert A.mark() <= mV
        A.release(conv_end)
        wg = A.alloc([8, 2048], BF16)
        SCB = 256
        NSC = S // SCB
        TPS = SCB // 128
        xinB = [A.alloc([D], F32) for _ in range(2)]
        xmB = A.alloc([D], BF16)
        xhB = [A.alloc([8, SCB], BF16) for _ in range(2)]
        sq = A.alloc([4, SCB], F32)
        mean_sb = A.alloc([SCB], F32)
        var_sb = A.alloc([SCB], F32)
        rstd_ln = A.alloc([SCB], F32)
        ycen = [A.alloc([SCB], F32) for _ in range(2)]
        vact = [A.alloc([4, SCB], BF16) for _ in range(2)]
        sg0 = [A.alloc([SCB], F32) for _ in range(2)]
        sg1 = [A.alloc([SCB], F32) for _ in range(2)]
        mm1 = A.alloc([SCB], F32)
        mm2 = A.alloc([SCB], F32)
        merged = A.alloc([8, SCB], BF16)
        hbuf = [A.alloc([D], F32) for _ in range(2)]
        htmp = [A.alloc([512], F32) for _ in range(2)]
        u2b = [A.alloc([D], BF16) for _ in range(2)]
        u2T = A.alloc([8, 128], F32)
        frc = [A.alloc([4, SCB], BF16) for _ in range(2)]

        for k in range(8):
            P.dma("pool", lambda e, k=k: e.dma_start(out=wg[:, k, :], in_=win_v[:, k, 1536:3584]), "ld_wg", w=["wg"])
            P.dma("pool", lambda e, k=k: e.dma_start(out=wo[:, k, :], in_=w_out.rearrange("(k p) n -> p k n", p=128)[:, k, :]), "ld_wo", w=["wo"])
        for c in range(4):
            P.dma("pool", lambda e, c=c: e.dma_start(out=cwo[:, c, :], in_=conv_w_out.rearrange("(k p) n -> p k n", p=128)[:, c, :]), "ld_cwo", w=["cwo"])
            P.dma("pool", lambda e, c=c: e.dma_start(out=fw[:, c, :], in_=fourier_w.rearrange("(k p) n -> p k n", p=128)[:, c, :]), "ld_fw", w=["fw"])
        frd_v = frd.rearrange("(g j) n -> j g n", j=128)

        def chunks_X(sc):
            p = sc % 2
            T0 = sc * SCB
            ch = []

            def c0():
                P.dma("act", lambda e: e.dma_start(out=frc[p], in_=frd_v[:, :, T0:T0 + SCB]), "ld_frc%d" % p, w=["frc%d" % p])
            ch.append(c0)
            for tt in range(TPS):
                T = sc * TPS + tt
                xk = "xinB%d" % (T % 2)
                for part in ("A", "B"):
                    ch.append(lambda T=T, xk=xk, tt=tt, part=part: xhat_tile(
                        T, xinB[T % 2], xk, xinB[T % 2], xmB, xhB[p][:, :, tt * 128:(tt + 1) * 128], "xhB%d" % p, 0, False,
                        tmp_k=xk, part=part, add_eng="dve"))
            return ch

        def chunks_L(sc):
            p = sc % 2
            T0 = sc * SCB
            ch = []

            def l0():
                P.op("act", lambda e: e.activation(out=sq, in_=convT[:, :, T0:T0 + SCB], func=AF.Square), r=["convT"], w=["sq"])
                P.group("pe", [lambda e, c=c: e.matmul(PS[1][:, 0:SCB], lhsT=ones_lnb, rhs=convT[:, c, T0:T0 + SCB], start=(c == 0), stop=(c == 3)) for c in range(4)]
                        + [lambda e, c=c: e.matmul(PS[1][:, SCB:2 * SCB], lhsT=ones_ln, rhs=sq[:, c, :], start=(c == 0), stop=(c == 3)) for c in range(4)],
                        r=["convT", "sq", "ones_ln", "ones_lnb"], w=[pk[1]])
            ch.append(l0)

            def l1():
                P.op("act", lambda e: e.activation(out=mean_sb, in_=PS[1][:, 0:SCB], func=AF.Copy), r=[pk[1]], w=["mean_sb"])
                P.op("dve", lambda e: e.tensor_tensor(out=var_sb, in0=mean_sb, in1=mean_sb, op=ALU.mult), r=["mean_sb"], w=["var_sb"])
                P.op("dve", lambda e: e.tensor_tensor(out=var_sb, in0=PS[1][:, SCB:2 * SCB], in1=var_sb, op=ALU.subtract), r=[pk[1], "var_sb"], w=["var_sb"])
            ch.append(l1)

            def l2():
                P.op("act", lambda e: e.activation(out=rstd_ln, in_=var_sb, func=AF.Sqrt, bias=eps5[:, 0:1], scale=1.0), r=["var_sb", "eps5"], w=["rstd_ln"])
                P.op("dve", lambda e: e.reciprocal(out=rstd_ln, in_=rstd_ln), r=["rstd_ln"], w=["rstd_ln"])
            ch.append(l2)
            for c in range(4):
                def lc(c=c):
                    y = ycen[c % 2]
                    yk = "ycen%d" % (c % 2)
                    P.op("dve", lambda e: e.tensor_tensor(out=y, in0=convT[:, c, T0:T0 + SCB], in1=mean_sb, op=ALU.subtract), r=["convT", "mean_sb"], w=[yk])
                    P.op("dve", lambda e: e.tensor_tensor(out=y, in0=y, in1=rstd_ln, op=ALU.mult), r=[yk, "rstd_ln"], w=[yk])
                    P.op("act", lambda e: e.activation(out=vact[p][:, c, :], in_=y, func=AF.Silu,
                                                       bias=smallcol[:, SC_LNB + c:SC_LNB + c + 1], scale=smallcol[:, SC_LNG + c:SC_LNG + c + 1]),
                         r=[yk, "smallcol"], w=["vact%d" % p])
                ch.append(lc)
            return ch

        def stage_M(sc, inter=()):
            p = sc % 2
            inter = list(inter)
            per = (len(inter) + 7) // 8 if inter else 0
            for dc in range(8):
                for _ in range(per):
                    if inter:
                        inter.pop(0)()
                q = dc % 2
                b0 = 2 + 2 * q
                b1 = b0 + 1
                P.group("pe", [lambda e, c=c, dc=dc, b0=b0: e.matmul(PS[b0][:, 0:SCB], lhsT=cwo[:, c, dc * 128:(dc + 1) * 128], rhs=vact[p][:, c, :],
                                                                    start=(c == 0), stop=(c == 3)) for c in range(4)]
                        + [lambda e, g=g, dc=dc, b0=b0: e.matmul(PS[b0][:, SCB:2 * SCB], lhsT=fw[:, g, dc * 128:(dc + 1) * 128], rhs=frc[p][:, g, :],
                                                                  start=(g == 0), stop=(g == 3)) for g in range(4)],
                        r=["cwo", "fw", "vact%d" % p, "frc%d" % p], w=[pk[b0]])
                P.group("pe", [lambda e, k=k, dc=dc, b1=b1: e.matmul(PS[b1][:, 0:SCB], lhsT=wg[:, k, dc * 128:(dc + 1) * 128], rhs=xhB[p][:, k, :],
                                                                    start=(k == 0), stop=(k == 7)) for k in range(8)]
                        + [lambda e, k=k, dc=dc, b1=b1: e.matmul(PS[b1][:, SCB:2 * SCB], lhsT=wg[:, k, 1024 + dc * 128:1024 + (dc + 1) * 128], rhs=xhB[p][:, k, :],
                                                                  start=(k == 0), stop=(k == 7)) for k in range(8)],
                        r=["wg", "xhB%d" % p], w=[pk[b1]])
                P.op("act", lambda e, dc=dc, b1=b1, q=q: e.activation(out=sg0[q], in_=PS[b1][:, 0:SCB], func=AF.Sigmoid,
                                                                     bias=smallcol[:, SC_BIN + 12 + dc:SC_BIN + 13 + dc], scale=1.0),
                     r=[pk[b1], "smallcol"], w=["sg0B%d" % q])
                P.op("act", lambda e, dc=dc, b1=b1, q=q: e.activation(out=sg1[q], in_=PS[b1][:, SCB:2 * SCB], func=AF.Sigmoid,
                                                                     bias=smallcol[:, SC_BIN + 20 + dc:SC_BIN + 21 + dc], scale=1.0),
                     r=[pk[b1], "smallcol"], w=["sg1B%d" % q])
                P.op("dve", lambda e, dc=dc, b0=b0, q=q: e.scalar_tensor_tensor(out=mm1, in0=PS[b0][:, 0:SCB], scalar=smallcol[:, SC_CBO + dc:SC_CBO + dc + 1],
                                                                               in1=sg0[q], op0=ALU.add, op1=ALU.mult),
                     r=[pk[b0], "sg0B%d" % q, "smallcol"], w=["mm1"])
                P.op("dve", lambda e, dc=dc, b0=b0, q=q: e.scalar_tensor_tensor(out=mm2, in0=PS[b0][:, SCB:2 * SCB], scalar=smallcol[:, SC_FB + dc:SC_FB + dc + 1],
                                                                               in1=sg1[q], op0=ALU.add, op1=ALU.mult),
                     r=[pk[b0], "sg1B%d" % q, "smallcol"], w=["mm2"])
                P.op("pool", lambda e, dc=dc: e.tensor_tensor(out=merged[:, dc, :], in0=mm1, in1=mm2, op=ALU.add),
                     r=["mm1", "mm2"], w=["merged"])

        def router_tr(T):
            hb = hbuf[T % 2]
            hk = "hbuf%d" % (T % 2)
            for hf in range(2):
                P.group("pe", [lambda e, k=k, hf=hf: e.transpose(out=PS[hf][:, (k % 4) * 128:(k % 4 + 1) * 128],
                                                                in_=hb[:, k * 128:(k + 1) * 128], identity=ident)
                               for k in range(4 * hf, 4 * hf + 4)], r=[hk, "ident"], w=[pk[hf]])
                if hf == 0:
                    P.op("dve", lambda e: e.tensor_copy(out=u2T[:, 0:4, :], in_=PS[0].rearrange("p (a b) -> p a b", a=4)), r=[pk[0]], w=["u2T"])
                else:
                    P.op("act", lambda e: e.activation(out=u2T[:, 4:8, :], in_=PS[1].rearrange("p (a b) -> p a b", a=4), func=AF.Copy), r=[pk[1]], w=["u2T"])

        def router_mm(T):
            P.group("pe", [lambda e, k=k: e.matmul(PS[1][:, 0:E], lhsT=u2T[:, k, :], rhs=rw[:, k, :], start=(k == 0), stop=(k == 7)) for k in range(8)],
                    r=["u2T", "rw"], w=[pk[1]])
            P.op("dve", lambda e: e.tensor_copy(out=logits[:, T, :], in_=PS[1][:, 0:E]), r=[pk[1]], w=["logits"])

        def stage_H(sc):
            for tt in range(TPS):
                T = sc * TPS + tt
                hb = hbuf[T % 2]
                hk = "hbuf%d" % (T % 2)
                ub = u2b[T % 2]
                uk = "u2b%d" % (T % 2)
                P.dma("sp", lambda e, hb=hb, T=T: e.dma_start(out=hb, in_=x[T * 128:(T + 1) * 128, :]), "ld_" + hk, w=[hk])
                P.op("pool", lambda e, hb=hb: e.tensor_tensor(out=hb, in0=hb, in1=bcs[BC_GB1], op=ALU.add), r=[hk, "bc3"], w=[hk])
                if T > 0:
                    router_tr(T - 1)
                toks = []
                for dh in range(2):
                    bh = 6 + dh
                    P.group("pe", [lambda e, k=k, tt=tt, dh=dh, bh=bh: e.matmul(PS[bh], lhsT=merged[:, k, tt * 128:(tt + 1) * 128],
                                                                               rhs=wo[:, k, dh * 512:(dh + 1) * 512], start=(k == 0), stop=(k == 7))
                                   for k in range(8)], r=["merged", "wo"], w=[pk[bh]])
                if T > 0:
                    router_mm(T - 1)
                for dh in range(2):
                    bh = 6 + dh
                    P.op("dve", lambda e, dh=dh, bh=bh: e.tensor_tensor(out=htmp[dh], in0=PS[bh], in1=bcs[BC_G1][:, dh * 512:(dh + 1) * 512], op=ALU.mult),
                         r=[pk[bh], "bc2"], w=["htmp%d" % dh])
                    P.op("dve", lambda e, dh=dh, hb=hb: e.tensor_tensor(out=hb[:, dh * 512:(dh + 1) * 512], in0=hb[:, dh * 512:(dh + 1) * 512],
                                                                       in1=htmp[dh], op=ALU.add), r=[hk, "htmp%d" % dh], w=[hk])
                P.dma("sp", lambda e, T=T, hb=hb: e.dma_start(out=hd[T * 128:(T + 1) * 128, :], in_=hb), "st_" + hk, r=[hk], w=["hd_%d" % T])
                P.op("act", lambda e, T=T, hb=hb, ub=ub: e.activation(out=ub, in_=hb, func=AF.Square, accum_out=ss2[:, T:T + 1]),
                     r=[hk], w=[uk, "ss2_%d" % T])
                P.op("dve", lambda e, T=T: e.tensor_scalar(out=ss2[:, T:T + 1], in0=ss2[:, T:T + 1], scalar1=1.0 / D, scalar2=1e-6,
                                                           op0=ALU.mult, op1=ALU.add), r=["ss2_%d" % T], w=["ss2_%d" % T])
                P.op("pool", lambda e, T=T: e.tensor_tensor(out=rstd2[:, T:T + 1], in0=ss2[:, T:T + 1], in1=nhalf[:, 0:1], op=ALU.pow),
                     r=["ss2_%d" % T, "nhalf"], w=["rstd2_%d" % T])
                P.op("dve", lambda e, T=T, hb=hb: e.scalar_tensor_tensor(out=hb, in0=hb, scalar=rstd2[:, T:T + 1], in1=bcs[BC_GS2],
                                                                        op0=ALU.mult, op1=ALU.mult), r=[hk, "rstd2_%d" % T, "bc4"], w=[hk])
                P.op("dve", lambda e, hb=hb: e.tensor_tensor(out=hb, in0=hb, in1=bcs[BC_SH2], op=ALU.add), r=[hk, "bc5"], w=[hk])
                P.op("act", lambda e, hb=hb, ub=ub: e.activation(out=ub, in_=hb, func=AF.Copy), r=[hk], w=[uk])
                P.dma("sp", lambda e, T=T, ub=ub: e.dma_start(out=scr[T * 128:(T + 1) * 128, 0:D], in_=ub), "st_" + uk, r=[uk], w=["scr_%d" % T])

        for f in chunks_X(0) + chunks_L(0):
            f()
        for sc in range(NSC):
            inter = (chunks_X(sc + 1) + chunks_L(sc + 1)) if sc + 1 < NSC else []
            stage_M(sc, inter)
            stage_H(sc)
        router_tr(NT - 1)
        router_mm(NT - 1)

        if stage == 3:
            P.barrier()
            P.dma("sp", lambda e: e.dma_start(out=dbg[:, 0:NT * E], in_=logits.rearrange("p a b -> p (a b)")), "dbgA")
            P.dma("sp", lambda e: e.dma_start(out=hbuf[0], in_=hd[3 * 128:4 * 128, :]), "dbgL", w=["hb0x"])
            P.dma("sp", lambda e: e.dma_start(out=dbg[:, 1024:2048], in_=hbuf[0]), "dbgC", r=["hb0x"])
            P.dma("sp", lambda e: e.dma_start(out=u2b[0], in_=scr[3 * 128:4 * 128, 0:D]), "dbgL2", w=["ub0x"])
            P.op("dve", lambda e: e.tensor_copy(out=hbuf[1], in_=u2b[0]), r=["ub0x"], w=["hb1x"])
            P.dma("sp", lambda e: e.dma_start(out=dbg[:, 2048:3072], in_=hbuf[1]), "dbgD", r=["hb1x"])
            P.barrier()
            P.emit()
            return nc

        A.release(m_p2)
        P.barrier()
        junk = A.alloc([2048], BF16)
        aff_es = A.alloc([512], F32)
        maskt = A.alloc([512], F32)
        onest = A.alloc([512], F32)
        cum = A.alloc([512], F32)
        affp = A.alloc([4, 128], F32)
        lo = A.alloc([1], F32)
        mid = A.alloc([1], F32)
        cntp = A.alloc([1], F32)
        pred = A.alloc([1], F32)
        offs = A.alloc([1], F32)
        mx = A.alloc([NT], F32)
        sm = A.alloc([NT], F32)
        cum16 = A.alloc([512], I16)
        P.op("dve", lambda e: e.tensor_reduce(out=mx, in_=logits, axis=mybir.AxisListType.X, op=ALU.max), r=["logits"], w=["mx"])
        P.op("dve", lambda e: e.tensor_scalar(out=mx, in0=mx, scalar1=-1.0, scalar2=None, op0=ALU.mult), r=["mx"], w=["mx"])
        for T in range(NT):
            P.op("act", lambda e, T=T: e.activation(out=aff[:, T, :], in_=logits[:, T, :], func=AF.Exp, bias=mx[:, T:T + 1], scale=1.0,
                                                   accum_out=sm[:, T:T + 1]), r=["logits", "mx"], w=["aff", "sm"])
        P.op("dve", lambda e: e.reciprocal(out=sm, in_=sm), r=["sm"], w=["sm"])
        for T in range(NT):
            P.op("dve", lambda e, T=T: e.tensor_scalar(out=aff[:, T, :], in0=aff[:, T, :], scalar1=sm[:, T:T + 1], scalar2=None, op0=ALU.mult),
                 r=["aff", "sm"], w=["aff"])
        scr_v = scr.rearrange("(t p) c -> p t c", p=128)
        aff_b = aff.rearrange("p a b -> p (a b)").bitcast(BF16).rearrange("p (a b) -> p a b", a=NT)
        for q in range(4):
            P.dma("sp", lambda e, q=q: e.dma_start(out=scr_v[:, q * 8:(q + 1) * 8, D:D + 32], in_=aff_b[:, q * 8:(q + 1) * 8, :]),
                  "st_aff", r=["aff"], w=["scra_%d" % q])
        aff4 = aff.rearrange("p (s c) e -> p s c e", c=4)
        for tc in range(4):
            P.op("dve", lambda e, tc=tc: e.tensor_copy(out=affp[:, tc, :].rearrange("p (s e) -> p s e", e=E), in_=aff4[:, :, tc, :]),
                 r=["aff"], w=["affp"])
        P.group("pe", [lambda e, tc=tc: e.transpose(out=PS[0][:, tc * 128:(tc + 1) * 128], in_=affp[:, tc, :], identity=ident) for tc in range(4)],
                r=["affp", "ident"], w=[pk[0]])
        P.op("dve", lambda e: e.tensor_copy(out=aff_es, in_=PS[0]), r=[pk[0]], w=["aff_es"])
        P.op("dve", lambda e: e.memset(lo, 0.0), w=["lo"])
        P.op("pool", lambda e: e.memset(onest, 1.0), w=["onest"])
        for i in range(26):
            step = 2.0 ** -(i + 1)
            P.op("dve", lambda e, step=step: e.tensor_scalar(out=mid, in0=lo, scalar1=step, scalar2=None, op0=ALU.add), r=["lo"], w=["mid"])
            P.op("dve", lambda e: e.tensor_scalar(out=junk[:, 0:512], in0=aff_es, scalar1=mid[:, 0:1], scalar2=0.0, op0=ALU.is_ge, op1=ALU.add,
                                                  accum_out=cntp), r=["aff_es", "mid"], w=["junk", "cntp"])
            P.group("pe", [lambda e: e.matmul(PS[1][:, 0:1], lhsT=maskM, rhs=cntp, start=True, stop=True)], r=["maskM", "cntp"], w=[pk[1]])
            P.op("dve", lambda e: e.tensor_scalar(out=pred, in0=PS[1][:, 0:1], scalar1=511.5, scalar2=None, op0=ALU.is_ge), r=[pk[1]], w=["pred"])
            P.op("dve", lambda e, step=step: e.scalar_tensor_tensor(out=lo, in0=pred, scalar=step, in1=lo, op0=ALU.mult, op1=ALU.add),
                 r=["pred", "lo"], w=["lo"])
        P.op("dve", lambda e: e.tensor_scalar(out=maskt, in0=aff_es, scalar1=lo[:, 0:1], scalar2=None, op0=ALU.is_ge), r=["aff_es", "lo"], w=["maskt"])
        P.op("dve", lambda e: e.tensor_tensor_scan(out=cum, data0=onest, data1=maskt, initial=0.0, op0=ALU.mult, op1=ALU.add),
             r=["onest", "maskt"], w=["cum"])
        P.group("pe", [lambda e: e.matmul(PS[1][:, 0:1], lhsT=maskM2, rhs=cum[:, 511:512], start=True, stop=True)], r=["maskM2", "cum"], w=[pk[1]])
        P.op("dve", lambda e: e.tensor_copy(out=offs, in_=PS[1][:, 0:1]), r=[pk[1]], w=["offs"])
        P.op("dve", lambda e: e.tensor_scalar(out=cum, in0=cum, scalar1=offs[:, 0:1], scalar2=None, op0=ALU.add), r=["cum", "offs"], w=["cum"])
        P.op("dve", lambda e: e.tensor_copy(out=cum16, in_=cum), r=["cum"], w=["cum16"])
        for s8 in range(8):
            P.dma("sp", lambda e, s8=s8: e.dma_start(out=cumd[:, s8, :], in_=cum16[s8 * 16:(s8 + 1) * 16, :]), "st_cum", r=["cum16"], w=["cumd"])

        if stage == 4:
            P.barrier()
            P.dma("sp", lambda e: e.dma_start(out=dbg[:, 0:512], in_=aff_es), "dbgA")
            P.dma("sp", lambda e: e.dma_start(out=dbg[:, 512:1024], in_=cum), "dbgB")
            P.dma("sp", lambda e: e.dma_start(out=dbg[:, 1024:1025], in_=lo, allow_slow_non_contiguous=True), "dbgC")
            P.barrier()
            P.emit()
            return nc

        A.release(m_p1)
        P.barrier()
        junk = A.alloc([2048], BF16)
        bcs[BC_G2] = A.alloc([D], F32)
        bcs[BC_FG] = A.alloc([D], F32)
        P.dma("sp", lambda e: e.dma_start(out=bcs[BC_G2], in_=modd[0, 5 * D:6 * D].partition_broadcast(128)), "bc6", w=["bc6"])
        P.dma("act", lambda e: e.dma_start(out=bcs[BC_FG], in_=final_g[0, :].partition_broadcast(128)), "bc7", w=["bc7"])
        cb = A.alloc([S], I16)
        gath = A.alloc([4, 1056], BF16)
        tokT = [A.alloc([8, 512], BF16) for _ in range(2)]
        wgs = [A.alloc([8, 1024], BF16) for _ in range(2)]
        wus = [A.alloc([8, 1024], BF16) for _ in range(2)]
        wds = [A.alloc([8, 1024], BF16) for _ in range(2)]
        slu = [A.alloc([512], F32) for _ in range(2)]
        actT = A.alloc([8, 512], BF16)
        out1 = A.alloc([4, D], F32)
        outw = [A.alloc([D], F32) for _ in range(2)]
        otmp = A.alloc([512], F32)
        gath_f = gath.rearrange("p a b -> p (a b)").bitcast(F32).rearrange("p (a b) -> p a b", a=4)

        def load_weights(u):
            s_ = u % 2
            for nm, src, dst in (("wgs", ew_gate, wgs), ("wus", ew_up, wus), ("wds", ew_down, wds)):
                P.dma("pool", lambda e, src=src, dst=dst: e.dma_start(
                    out=dst[s_].rearrange("p a b -> p (a b)").rearrange("p (a b) -> p a b", b=2048),
                    in_=src[u].rearrange("p (a b) -> p a b", b=2048)), "ld_%s%d" % (nm, s_), w=["%s%d" % (nm, s_)])

        def route_idx(e_):
            P.dma("sp", lambda e: e.dma_start(out=cb, in_=cumd[e_].rearrange("s n -> (s n)").partition_broadcast(128)), "ld_cb",
                  r=["cumd"], w=["cb"])
            for cc in range(4):
                for hf in range(2):
                    P.op("dve", lambda e, cc=cc, hf=hf: e.tensor_scalar(
                        out=junk, in0=cb[:, hf * 2048:(hf + 1) * 2048], scalar1=cvals[:, cc:cc + 1], scalar2=0.0, op0=ALU.is_le, op1=ALU.add,
                        accum_out=idxp[:, e_ * 8 + cc * 2 + hf:e_ * 8 + cc * 2 + hf + 1]), r=["cb", "cvals"], w=["junk", "idxp%d" % e_])
            idxp_v = idxp[:, e_ * 8:(e_ + 1) * 8].rearrange("p (c h) -> p c h", h=2)
            P.op("dve", lambda e: e.tensor_tensor(out=idxf[:, e_ * 4:(e_ + 1) * 4], in0=idxp_v[:, :, 0], in1=idxp_v[:, :, 1], op=ALU.add),
                 r=["idxp%d" % e_], w=["idxf%d" % e_])
            P.op("dve", lambda e: e.tensor_copy(out=idxi[:, e_ * 4:(e_ + 1) * 4], in_=idxf[:, e_ * 4:(e_ + 1) * 4]), r=["idxf%d" % e_], w=["idxi%d" % e_])

        def gather_rows(e_):
            scr_keys = ["scr_%d" % t for t in range(NT)] + ["scra_%d" % q for q in range(4)]
            for cc in range(4):
                P.dma("pool", lambda e, cc=cc: e.indirect_dma_start(
                    out=gath[:, cc, :], out_offset=None, in_=scr[:, :],
                    in_offset=bass.IndirectOffsetOnAxis(ap=idxi[:, e_ * 4 + cc:e_ * 4 + cc + 1], axis=0)),
                    "ld_gath", r=["idxi%d" % e_] + scr_keys, w=["gath"])
            P.op("dve", lambda e: e.tensor_copy(out=wsel[:, e_ * 4:(e_ + 1) * 4], in_=gath_f[:, :, 512 + e_]), r=["gath"], w=["wsel%d" % e_])

        def transpose_tok(e_):
            tb = e_ % 2
            for cc in range(4):
                bank = 6 + (cc % 2)
                P.group("pe", [lambda e, k=k, cc=cc, bank=bank: e.transpose(out=PSB[bank][:, k * 128:(k + 1) * 128],
                                                                           in_=gath[:, cc, k * 128:(k + 1) * 128], identity=identb)
                               for k in range(8)], r=["gath", "identb"], w=[pk[bank]])
                if cc % 2 == 0:
                    P.op("act", lambda e, cc=cc, bank=bank: e.activation(out=tokT[tb][:, :, cc * 128:(cc + 1) * 128],
                                                                        in_=PSB[bank].rearrange("p (k t) -> p k t", k=8), func=AF.Copy),
                         r=[pk[bank]], w=["tokT%d" % tb])
                else:
                    P.op("dve", lambda e, cc=cc, bank=bank: e.tensor_copy(out=tokT[tb][:, :, cc * 128:(cc + 1) * 128],
                                                                         in_=PSB[bank].rearrange("p (k t) -> p k t", k=8)),
                         r=[pk[bank]], w=["tokT%d" % tb])

        hd_keys = ["hd_%d" % t for t in range(NT)]
        sc_keys = ["hd_sc0", "hd_sc1", "hd_sc2", "hd_sc3"]
        ocnt = [0]

        def unit(u):
            e_, fh = u // 2, u % 2
            s_ = u % 2
            tb = e_ % 2
            for fc in range(8):
                bg_, bu_ = 2 * (fc % 2), 2 * (fc % 2) + 1
                P.group("pe", [lambda e, k=k, fc=fc, bg_=bg_: e.matmul(PS[bg_], lhsT=wgs[s_][:, k, fc * 128:(fc + 1) * 128], rhs=tokT[tb][:, k, :],
                                                                      start=(k == 0), stop=(k == 7)) for k in range(8)],
                        r=["wgs%d" % s_, "tokT%d" % tb], w=[pk[bg_]])
                P.group("pe", [lambda e, k=k, fc=fc, bu_=bu_: e.matmul(PS[bu_], lhsT=wus[s_][:, k, fc * 128:(fc + 1) * 128], rhs=tokT[tb][:, k, :],
                                                                      start=(k == 0), stop=(k == 7)) for k in range(8)],
                        r=["wus%d" % s_, "tokT%d" % tb], w=[pk[bu_]])
                si = fc % 2
                P.op("act", lambda e, bg_=bg_, si=si: e.activation(out=slu[si], in_=PS[bg_], func=AF.Silu), r=[pk[bg_]], w=["slu%d" % si])
                P.op("dve", lambda e, fc=fc, bu_=bu_, si=si: e.tensor_tensor(out=actT[:, fc, :], in0=slu[si], in1=PS[bu_], op=ALU.mult),
                     r=["slu%d" % si, pk[bu_]], w=["actT"])
            for cc in range(4):
                if fh == 1:
                    ob = ocnt[0] % 2
                    ocnt[0] += 1
                for dh in range(2):
                    bo = 4 + dh
                    P.group("pe", [lambda e, fc=fc, cc=cc, dh=dh, bo=bo: e.matmul(PS[bo], lhsT=actT[:, fc, cc * 128:(cc + 1) * 128],
                                                                                 rhs=wds[s_][:, fc, dh * 512:(dh + 1) * 512],
                                                                                 start=(fc == 0), stop=(fc == 7)) for fc in range(8)],
                            r=["actT", "wds%d" % s_], w=[pk[bo]])
                    if fh == 0:
                        P.op("act", lambda e, cc=cc, dh=dh, bo=bo: e.activation(out=out1[:, cc, dh * 512:(dh + 1) * 512], in_=PS[bo], func=AF.Copy),
                             r=[pk[bo]], w=["out1"])
                    else:
                        P.op("dve", lambda e, cc=cc, dh=dh, bo=bo: e.tensor_tensor(out=otmp, in0=PS[bo], in1=out1[:, cc, dh * 512:(dh + 1) * 512], op=ALU.add),
                             r=[pk[bo], "out1"], w=["otmp"])
                        P.op("dve", lambda e, cc=cc, dh=dh, ob=ob: e.scalar_tensor_tensor(
                            out=outw[ob][:, dh * 512:(dh + 1) * 512], in0=otmp, scalar=wsel[:, e_ * 4 + cc:e_ * 4 + cc + 1],
                            in1=bcs[BC_G2][:, dh * 512:(dh + 1) * 512], op0=ALU.mult, op1=ALU.mult),
                            r=["otmp", "wsel%d" % e_, "bc6"], w=["outw%d" % ob])
                if fh == 1:
                    P.dma("pool", lambda e, cc=cc, ob=ob: e.indirect_dma_start(
                        out=hd[:, :], out_offset=bass.IndirectOffsetOnAxis(ap=idxi[:, e_ * 4 + cc:e_ * 4 + cc + 1], axis=0),
                        in_=outw[ob], in_offset=None, compute_op=ALU.add),
                        "sc_outw%d" % ob, r=["outw%d" % ob, "idxi%d" % e_] + hd_keys + (sc_keys if cc == 0 else []),
                        w=["hd_sc%d" % cc])

        P.op("dve", lambda e: e.memset(idxp, 0.0), w=["idxp%d" % i for i in range(E)])
        load_weights(0)
        load_weights(1)
        route_idx(0)
        gather_rows(0)
        transpose_tok(0)
        NEXP = int(os.environ.get('K_NEXP', E))
        for e_ in range(NEXP):
            if e_ + 1 < E:
                route_idx(e_ + 1)
            unit(2 * e_)
            if e_ + 1 < E:
                gather_rows(e_ + 1)
            if 2 * e_ + 2 < 2 * E:
                load_weights(2 * e_ + 2)
            if e_ + 1 < E:
                transpose_tok(e_ + 1)
            unit(2 * e_ + 1)
            if 2 * e_ + 3 < 2 * E:
                load_weights(2 * e_ + 3)

        fin = [A.alloc([D], F32) for _ in range(2)]
        fss = A.alloc([NT], F32)
        frs_ = A.alloc([NT], F32)
        last = []
        for T in range(NT):
            b = T % 2
            P.dma("sp", lambda e, T=T, b=b: e.dma_start(out=fin[b], in_=hd[T * 128:(T + 1) * 128, :]), "ld_fin%d" % b,
                  r=sc_keys + ["hd_%d" % T], w=["fin%d" % b])
            P.op("act", lambda e, T=T, b=b: e.activation(out=junk[:, 0:D], in_=fin[b], func=AF.Square, accum_out=fss[:, T:T + 1]),
                 r=["fin%d" % b], w=["junk", "fss%d" % T])
            P.op("dve", lambda e, T=T: e.tensor_scalar(out=fss[:, T:T + 1], in0=fss[:, T:T + 1], scalar1=1.0 / D, scalar2=1e-6,
                                                       op0=ALU.mult, op1=ALU.add), r=["fss%d" % T], w=["fss%d" % T])
            P.op("pool", lambda e, T=T: e.tensor_tensor(out=frs_[:, T:T + 1], in0=fss[:, T:T + 1], in1=nhalf[:, 0:1], op=ALU.pow),
                 r=["fss%d" % T, "nhalf"], w=["frs%d" % T])
            P.op("dve", lambda e, T=T, b=b: e.scalar_tensor_tensor(out=fin[b], in0=fin[b], scalar=frs_[:, T:T + 1], in1=bcs[BC_FG],
                                                                   op0=ALU.mult, op1=ALU.mult), r=["fin%d" % b, "frs%d" % T, "bc7"], w=["fin%d" % b])
            last.append(P.dma("act", lambda e, T=T, b=b: e.dma_start(out=out[T * 128:(T + 1) * 128, :], in_=fin[b]), "st_fin%d" % b,
                              r=["fin%d" % b], w=["out_%d" % T]))
        P.barrier()
        P.emit()
        print("arena high-water bytes", A.hw, "of", ARENA_BYTES)
    return nc


_CONST = {}


def _dft_layout(M):
    A_ = M.reshape(4, 8, 128, 8, 512)
    return np.ascontiguousarray(A_.transpose(3, 0, 2, 1, 4)).reshape(32, 128, 4096)


_WCACHE = {}


def _weight_layouts(inp):
    key = id(inp["expert_w_gate"])
    if _WCACHE.get("key") == key:
        return _WCACHE["val"]
    f = lambda a: np.asarray(a, dtype=np.float32)
    g = f(inp["expert_w_gate"][0]).reshape(E, 8, 128, 2, 1024).transpose(0, 3, 2, 1, 4)
    u = f(inp["expert_w_up"][0]).reshape(E, 8, 128, 2, 1024).transpose(0, 3, 2, 1, 4)
    d = f(inp["expert_w_down"][0]).reshape(E, 2, 8, 128, 1024).transpose(0, 1, 3, 2, 4)
    aw = f(inp["ada_w"][0]).reshape(8, 128, 12, 512).transpose(2, 1, 0, 3)
    val = dict(
        expert_w_gate_l=np.ascontiguousarray(g).reshape(E * 2, 128, 8192),
        expert_w_up_l=np.ascontiguousarray(u).reshape(E * 2, 128, 8192),
        expert_w_down_l=np.ascontiguousarray(d).reshape(E * 2, 128, 8192),
        ada_w_l=np.ascontiguousarray(aw).reshape(12, 128, 4096),
    )
    _WCACHE["key"] = key
    _WCACHE["val"] = val
    return val


def _constants():
    if _CONST:
        return _CONST
    bf = ml_dtypes.bfloat16
    d = np.arange(128)
    ang = 2.0 * np.pi * ((d[:, None] * d[None, :]) % 128) / 128.0
    cs128 = np.concatenate([np.cos(ang), np.sin(ang)], axis=1) / np.sqrt(128.0)
    s = np.arange(S, dtype=np.int64)
    m = (s[:, None] * s[None, :]) % S
    tab_c = np.cos(2.0 * np.pi * np.arange(S) / S) / 64.0
    tab_s = -np.sin(2.0 * np.pi * np.arange(S) / S) / 64.0
    q = np.arange(128)
    e_of = q % 16
    s_of = q // 16
    maskM = (e_of[:, None] == e_of[None, :]).astype(np.float32)
    maskM2 = ((e_of[:, None] == e_of[None, :]) & (s_of[:, None] < s_of[None, :])).astype(np.float32)
    cvals = (np.arange(4)[None, :] * 128 + np.arange(128)[:, None]).astype(np.float32)
    _CONST.update(
        cs128=cs128.astype(bf),
        dft_cos_l=_dft_layout(tab_c[m].astype(bf)),
        dft_sin_l=_dft_layout(tab_s[m].astype(bf)),
        maskM=maskM, maskM2=maskM2, cvals=cvals,
    )
    return _CONST


def make_in_map(b, inp):
    f = lambda a: np.ascontiguousarray(np.asarray(a, dtype=np.float32))
    C = _constants()
    WL = _weight_layouts(inp)
    smallrows = np.concatenate([
        f(inp["b_in"][0]).reshape(28, 128), f(inp["conv_dw_b"][0]).reshape(4, 128), f(inp["conv_ln_g"][0]).reshape(4, 128),
        f(inp["conv_ln_b"][0]).reshape(4, 128), f(inp["conv_b_out"][0]).reshape(8, 128), f(inp["fourier_b"][0]).reshape(8, 128)], axis=0)
    return {
        "x": f(inp["x"][b]),
        "c_col": np.ascontiguousarray(f(inp["c"][b]).reshape(8, 128).T),
        "ada_w_l": WL["ada_w_l"],
        "ada_b": f(inp["ada_b"][0]).reshape(1, -1),
        "norm1_g": f(inp["norm1_g"][0]).reshape(1, -1),
        "w_in": f(inp["w_in"][0]),
        "smallrows": np.ascontiguousarray(smallrows),
        "dw_rows": f(inp["conv_dw_w"][0]).reshape(124, 128),
        "conv_w_out": f(inp["conv_w_out"][0]),
        "fourier_w": f(inp["fourier_w"][0]),
        "w_out": f(inp["w_out"][0]),
        "b_out": f(inp["b_out"][0]).reshape(1, -1),
        "norm2_g": f(inp["norm2_g"][0]).reshape(1, -1),
        "router_w": f(inp["router_w"][0]),
        "expert_w_gate_l": WL["expert_w_gate_l"],
        "expert_w_up_l": WL["expert_w_up_l"],
        "expert_w_down_l": WL["expert_w_down_l"],
        "final_norm_g": f(inp["final_norm_g"]).reshape(1, -1),
        "cs128": C["cs128"], "dft_cos_l": C["dft_cos_l"], "dft_sin_l": C["dft_sin_l"],
        "maskM": C["maskM"], "maskM2": C["maskM2"], "cvals": C["cvals"],
    }


def kernel(**inputs):
    nc = build_program()
    in_maps = [make_in_map(b, inputs) for b in range(8)]
    res = run_bass_kernel_spmd(nc, in_maps, core_ids=list(range(8)))
    return np.stack([np.asarray(r["out"], dtype=np.float32) for r in res.results], axis=0)
```

```python
import os
from contextlib import ExitStack
import numpy as np
import ml_dtypes
import concourse.bass as bass
import concourse.mybir as mybir
from concourse.bass_utils import run_bass_kernel_spmd

F32 = mybir.dt.float32
BF16 = mybir.dt.bfloat16
I32 = mybir.dt.int32
I16 = mybir.dt.int16
AF = mybir.ActivationFunctionType
ALU = mybir.AluOpType

S = 4096
D = 1024
NT = 32
E = 16
CAP = 512
FF = 2048
KW = 31
ENGS = ("pe", "act", "dve", "pool", "sp")


class Prog:
    def __init__(self, nc, ctx):
        self.nc = nc
        self.ctx = ctx
        self.lists = {e: [] for e in ENGS}
        self.sem = {e: ctx.enter_context(nc.semaphore("prog_" + e)) for e in ENGS}
        self.cnt = {e: 0 for e in ENGS}
        self.waited = {}
        self.dma_sems = {}
        self.dma_cnt = {}
        self.dma_waited = {}
        self.last_w = {}
        self.readers = {}

    def _emit_waits(self, eng, deps):
        for d in deps:
            if d is None:
                continue
            if d[0] == "dma":
                _, name, val = d
                key = (eng, name)
                if self.dma_waited.get(key, 0) >= val:
                    continue
                self.dma_waited[key] = val
                sem = self.dma_sems[name]
                self.lists[eng].append(lambda e, sem=sem, val=val: e.wait_ge(sem, val))
            else:
                src, val = d
                key = (eng, src)
                if self.waited.get(key, 0) >= val:
                    continue
                self.waited[key] = val
                sem = self.sem[src]
                self.lists[eng].append(lambda e, sem=sem, val=val: e.wait_ge(sem, val))

    def _deps(self, r, w, extra):
        deps = list(extra)
        for k in r:
            if k in self.last_w:
                deps.append(self.last_w[k])
        for k in w:
            if k in self.last_w:
                deps.append(self.last_w[k])
            deps.extend(self.readers.get(k, []))
        return deps

    def _commit(self, tok, r, w):
        for k in r:
            self.readers.setdefault(k, []).append(tok)
        for k in w:
            self.last_w[k] = tok
            self.readers[k] = []

    def op(self, eng, fn, r=(), w=(), extra=()):
        self._emit_waits(eng, self._deps(r, w, extra))
        self.cnt[eng] += 1
        sem = self.sem[eng]
        self.lists[eng].append(lambda e, fn=fn, sem=sem: fn(e).then_inc(sem, 1))
        tok = (eng, self.cnt[eng])
        self._commit(tok, r, w)
        return tok

    def group(self, eng, fns, r=(), w=(), extra=()):
        self._emit_waits(eng, self._deps(r, w, extra))
        for fn in fns[:-1]:
            self.lists[eng].append(lambda e, fn=fn: fn(e))
        self.cnt[eng] += 1
        sem = self.sem[eng]
        fn = fns[-1]
        self.lists[eng].append(lambda e, fn=fn, sem=sem: fn(e).then_inc(sem, 1))
        tok = (eng, self.cnt[eng])
        self._commit(tok, r, w)
        return tok

    def dma(self, eng, fn, semname, r=(), w=(), extra=()):
        if semname not in self.dma_sems:
            self.dma_sems[semname] = self.ctx.enter_context(self.nc.semaphore("d_" + semname))
            self.dma_cnt[semname] = 0
        self._emit_waits(eng, self._deps(r, w, extra))
        self.dma_cnt[semname] += 16
        sem = self.dma_sems[semname]
        self.lists[eng].append(lambda e, fn=fn, sem=sem: fn(e).then_inc(sem, 16))
        tok = ("dma", semname, self.dma_cnt[semname])
        self._commit(tok, r, w)
        return tok

    def wait(self, eng, deps):
        self._emit_waits(eng, deps)

    def barrier(self):
        deps = [(src, self.cnt[src]) for src in ENGS if self.cnt[src] > 0]
        deps += [("dma", n, v) for n, v in self.dma_cnt.items() if v > 0]
        for eng in ENGS:
            self._emit_waits(eng, [d for d in deps if d[0] != eng])

    def emit(self):
        with self.nc.Block() as block:
            @block.tensor
            def _(e):
                for f in self.lists["pe"]:
                    f(e)

            @block.scalar
            def _(e):
                for f in self.lists["act"]:
                    f(e)

            @block.vector
            def _(e):
                for f in self.lists["dve"]:
                    f(e)

            @block.gpsimd
            def _(e):
                for f in self.lists["pool"]:
                    f(e)

            @block.sync
            def _(e):
                for f in self.lists["sp"]:
                    f(e)


class Arena:
    def __init__(self, big, total_bytes):
        self.big = big
        self.total = total_bytes
        self.off = 0

    def alloc(self, shape, dt):
        n = int(np.prod(shape))
        esz = 2 if dt in (BF16, I16) else 4
        nbytes = (n * esz + 63) // 64 * 64
        off = self.off
        self.off += nbytes
        self.hw = max(getattr(self, "hw", 0), self.off)
        assert self.off <= self.total, ("SBUF arena overflow", self.off, self.total)
        v = self.big[:, off // 4:(off + nbytes) // 4]
        if dt != F32:
            v = v.bitcast(dt)
        v = v[:, 0:n]
        if len(shape) == 2:
            v = v.rearrange("p (a b) -> p a b", a=shape[0])
        elif len(shape) == 3:
            v = v.rearrange("p (a b c) -> p a b c", a=shape[0], b=shape[1])
        return v

    def mark(self):
        return self.off

    def release(self, m):
        self.off = m


ARENA_BYTES = 200 * 1024


def build_program(stage=99):
    nc = bass.Bass("TRN2", target_bir_lowering=False)

    def din(name, shape, dt=F32):
        return nc.dram_tensor(name, list(shape), dt, kind="ExternalInput").ap()

    x = din("x", [S, D])
    c_col = din("c_col", [128, 8])
    ada_w = din("ada_w_l", [12, 128, 8 * 512])
    ada_b = din("ada_b", [1, 6 * D])
    norm1_g = din("norm1_g", [1, D])
    w_in = din("w_in", [D, 3584])
    smallrows = din("smallrows", [56, 128])
    dw_rows = din("dw_rows", [124, 128])
    conv_w_out = din("conv_w_out", [512, D])
    fourier_w = din("fourier_w", [512, D])
    w_out = din("w_out", [D, D])
    b_out = din("b_out", [1, D])
    norm2_g = din("norm2_g", [1, D])
    router_w = din("router_w", [D, E])
    ew_gate = din("expert_w_gate_l", [E * 2, 128, 8 * 1024])
    ew_up = din("expert_w_up_l", [E * 2, 128, 8 * 1024])
    ew_down = din("expert_w_down_l", [E * 2, 128, 8 * 1024])
    final_g = din("final_norm_g", [1, D])
    cs128_d = din("cs128", [128, 256], BF16)
    CSd = din("dft_cos_l", [32, 128, 4096], BF16)
    SSd = din("dft_sin_l", [32, 128, 4096], BF16)
    maskM_d = din("maskM", [128, 128])
    maskM2_d = din("maskM2", [128, 128])
    cvals_d = din("cvals", [128, 4])

    out = nc.dram_tensor("out", [S, D], F32, kind="ExternalOutput").ap()
    dbg = None
    if stage < 99:
        dbg = nc.dram_tensor("dbg", [128, 16384], F32, kind="ExternalOutput").ap()

    modd = nc.dram_tensor("modd", [1, 9 * D], F32, kind="Internal").ap()
    frd = nc.dram_tensor("frd", [512, S], BF16, kind="Internal").ap()
    scr = nc.dram_tensor("scr", [S, 1056], BF16, kind="Internal").ap()
    hd = nc.dram_tensor("hd", [S, D], F32, kind="Internal").ap()
    cumd = nc.dram_tensor("cumd", [E, 8, 512], I16, kind="Internal").ap()

    with ExitStack() as ctx:
        P = Prog(nc, ctx)
        big = ctx.enter_context(nc.sbuf_tensor("big", [128, ARENA_BYTES // 4], F32))
        A = Arena(big, ARENA_BYTES)
        psb = [ctx.enter_context(nc.psum_tensor("ps%d" % i, [128, 512], F32)) for i in range(8)]
        PS = [p[:, :] for p in psb]
        PSB = [p[:, :].bitcast(BF16) for p in psb]
        pk = ["ps%d" % i for i in range(8)]

        ident = A.alloc([128], F32)
        identb = A.alloc([128], BF16)
        smallcol = A.alloc([56], F32)
        dwcol = A.alloc([124], F32)
        rstd1 = A.alloc([NT], F32)
        ss1 = A.alloc([NT], F32)
        ss2 = A.alloc([NT], F32)
        rstd2 = A.alloc([NT], F32)
        logits = A.alloc([NT, E], F32)
        aff = A.alloc([NT, E], F32)
        maskM = A.alloc([128], F32)
        maskM2 = A.alloc([128], F32)
        cvals = A.alloc([4], F32)
        ones_ln = A.alloc([128], F32)
        nhalf = A.alloc([256], F32)
        eps6 = A.alloc([1], F32)
        eps5 = A.alloc([1], F32)
        cact = A.alloc([8], F32)
        rw = A.alloc([8, E], F32)
        idxf = A.alloc([E * 4], F32)
        idxp = A.alloc([E * 8], F32)
        idxi = A.alloc([E * 4], I32)
        wsel = A.alloc([E * 4], F32)
        cs128 = A.alloc([256], BF16)
        ones_lnb = A.alloc([128], BF16)
        BC_GS1, BC_SH1, BC_G1, BC_GB1, BC_GS2, BC_SH2, BC_G2, BC_FG = range(8)
        bcs = [None] * 8
        m_p1 = A.mark()
        for i in (BC_GS1, BC_SH1, BC_G1, BC_GB1, BC_GS2, BC_SH2):
            bcs[i] = A.alloc([D], F32)
        m_p2 = A.mark()

        P.op("pool", lambda e: e.memset(ident, 1.0), w=["ident"])
        P.op("pool", lambda e: e.affine_select(out=ident, in_=ident, pattern=[[-1, 128]], compare_op=ALU.is_equal,
                                               fill=0.0, base=0, channel_multiplier=1), w=["ident"])
        P.op("dve", lambda e: e.tensor_copy(out=identb, in_=ident), r=["ident"], w=["identb"])
        P.op("pool", lambda e: e.memset(ones_ln, 1.0 / 512.0), w=["ones_ln"])
        P.op("pool", lambda e: e.memset(ones_lnb, 1.0 / 512.0), w=["ones_lnb"])
        P.op("pool", lambda e: e.memset(nhalf, -0.5), w=["nhalf"])
        P.op("pool", lambda e: e.memset(eps6, 1e-6), w=["eps6"])
        P.op("pool", lambda e: e.memset(eps5, 1e-5), w=["eps5"])
        P.dma("sp", lambda e: e.dma_start(out=maskM, in_=maskM_d[:, :]), "c_maskM", w=["maskM"])
        P.dma("sp", lambda e: e.dma_start(out=maskM2, in_=maskM2_d[:, :]), "c_maskM2", w=["maskM2"])
        P.dma("sp", lambda e: e.dma_start(out=cvals, in_=cvals_d[:, :]), "c_cvals", w=["cvals"])
        P.dma("sp", lambda e: e.dma_start(out=cs128, in_=cs128_d[:, :]), "c_cs128", w=["cs128"])
        P.dma("sp", lambda e: e.dma_start(out=cact, in_=c_col[:, :]), "c_cact", w=["cact"])
        P.dma("sp", lambda e: e.dma_start(out=rw, in_=router_w.rearrange("(k p) n -> p k n", p=128)), "c_rw", w=["rw"])

        m0 = A.mark()
        modrow = A.alloc([9 * D], F32)
        adab = A.alloc([6 * D], F32)
        g1row = A.alloc([D], F32)
        g2row = A.alloc([D], F32)
        borow = A.alloc([D], F32)
        awb = [A.alloc([8, 512], F32) for _ in range(2)]
        rows_T = A.alloc([128], F32)
        P.dma("act", lambda e: e.dma_start(out=adab[0:1, :], in_=ada_b[:, :]), "c_adab", w=["adab"])
        P.dma("act", lambda e: e.dma_start(out=g1row[0:1, :], in_=norm1_g[:, :]), "c_g1", w=["g1row"])
        P.dma("act", lambda e: e.dma_start(out=g2row[0:1, :], in_=norm2_g[:, :]), "c_g2", w=["g2row"])
        P.dma("act", lambda e: e.dma_start(out=borow[0:1, :], in_=b_out[:, :]), "c_bo", w=["borow"])
        P.op("act", lambda e: e.activation(out=cact, in_=cact, func=AF.Silu), r=["cact"], w=["cact"])
        for pc in range(12):
            b = pc % 2
            P.dma("sp", lambda e, pc=pc, b=b: e.dma_start(out=awb[b].rearrange("p a b -> p (a b)"), in_=ada_w[pc]),
                  "awb%d" % b, w=["awb%d" % b])
            P.group("pe", [lambda e, k=k, b=b: e.matmul(PS[0][0:1, :], lhsT=cact[:, k:k + 1], rhs=awb[b][:, k, :],
                                                       start=(k == 0), stop=(k == 7)) for k in range(8)],
                    r=["cact", "awb%d" % b], w=[pk[0]])
            P.op("dve", lambda e, pc=pc: e.tensor_tensor(out=modrow[0:1, pc * 512:(pc + 1) * 512], in0=PS[0][0:1, :],
                                                        in1=adab[0:1, pc * 512:(pc + 1) * 512], op=ALU.add),
                 r=[pk[0], "adab"], w=["modrow"])
        P.op("dve", lambda e: e.scalar_tensor_tensor(out=modrow[0:1, 6 * D:7 * D], in0=modrow[0:1, D:2 * D], scalar=1.0,
                                                     in1=g1row[0:1, :], op0=ALU.add, op1=ALU.mult),
             r=["modrow", "g1row"], w=["modrow"])
        P.op("dve", lambda e: e.tensor_tensor(out=modrow[0:1, 7 * D:8 * D], in0=modrow[0:1, 2 * D:3 * D], in1=borow[0:1, :],
                                              op=ALU.mult), r=["modrow", "borow"], w=["modrow"])
        P.op("dve", lambda e: e.scalar_tensor_tensor(out=modrow[0:1, 8 * D:9 * D], in0=modrow[0:1, 4 * D:5 * D], scalar=1.0,
                                                     in1=g2row[0:1, :], op0=ALU.add, op1=ALU.mult),
             r=["modrow", "g2row"], w=["modrow"])
        P.dma("sp", lambda e: e.dma_start(out=modd[:, :], in_=modrow[0:1, :]), "st_modrow", r=["modrow"], w=["modd"])
        srcs = {BC_GS1: 6, BC_SH1: 0, BC_G1: 2, BC_GB1: 7, BC_GS2: 8, BC_SH2: 3}
        for i, off in srcs.items():
            P.dma("sp" if i % 2 == 0 else "act",
                  lambda e, i=i, off=off: e.dma_start(out=bcs[i], in_=modd[0, off * D:(off + 1) * D].partition_broadcast(128)),
                  "bc%d" % i, r=["modd"], w=["bc%d" % i])
        P.dma("sp", lambda e: e.dma_start(out=rows_T[0:56, :], in_=smallrows[:, :]), "c_rowsT", w=["rows_T"])
        P.group("pe", [lambda e: e.transpose(out=PS[1][:, 0:56], in_=rows_T[0:56, :], identity=ident[0:56, 0:56])],
                r=["rows_T", "ident"], w=[pk[1]])
        P.op("dve", lambda e: e.tensor_copy(out=smallcol, in_=PS[1][:, 0:56]), r=[pk[1]], w=["smallcol"])
        P.dma("sp", lambda e: e.dma_start(out=rows_T[0:124, :], in_=dw_rows[:, :]), "c_rowsT", w=["rows_T"])
        P.group("pe", [lambda e: e.transpose(out=PS[1][:, 0:124], in_=rows_T[0:124, :], identity=ident[0:124, 0:124])],
                r=["rows_T", "ident"], w=[pk[1]])
        P.op("dve", lambda e: e.tensor_copy(out=dwcol, in_=PS[1][:, 0:124]), r=[pk[1]], w=["dwcol"])
        SC_BIN, SC_DWB, SC_LNG, SC_LNB, SC_CBO, SC_FB = 0, 28, 32, 36, 40, 48

        if stage == 0:
            P.dma("sp", lambda e: e.dma_start(out=dbg[0:1, 0:9 * D], in_=modrow[0:1, :]), "dbg0", r=["modrow"])
            P.dma("sp", lambda e: e.dma_start(out=dbg[:, 9 * D:9 * D + 56], in_=smallcol), "dbg1", r=["smallcol"])
            P.dma("sp", lambda e: e.dma_start(out=dbg[:, 10 * D:10 * D + 124], in_=dwcol), "dbg2", r=["dwcol"])
            P.dma("sp", lambda e: e.dma_start(out=dbg[:, 11 * D:12 * D], in_=bcs[BC_GS1]), "dbg3", r=["bc0"])
            P.barrier()
            P.emit()
            return nc
        P.barrier()
        A.release(m0)

        def xhat_tile(T, xin, xin_k, tmp, xm, xhatT_dst, xhatT_k, psbank, first_pass, tmp_k="tmp", part=None, add_eng="pool"):
            if part == "B":
                P.group("pe", [lambda e, k=k: e.transpose(out=PSB[psbank][:, k * 128:(k + 1) * 128], in_=xm[:, k * 128:(k + 1) * 128],
                                                         identity=identb) for k in range(8)],
                        r=["xm", "identb"], w=[pk[psbank]])
                P.op("act", lambda e: e.activation(out=xhatT_dst, in_=PSB[psbank].rearrange("p (k t) -> p k t", k=8), func=AF.Copy),
                     r=[pk[psbank]], w=[xhatT_k])
                return
            P.dma("sp", lambda e: e.dma_start(out=xin, in_=x[T * 128:(T + 1) * 128, :]), "ld_" + xin_k, w=[xin_k])
            if first_pass:
                P.op("act", lambda e: e.activation(out=xm, in_=xin, func=AF.Square, accum_out=ss1[:, T:T + 1]),
                     r=[xin_k], w=["xm", "ss1_%d" % T])
                P.op("dve", lambda e: e.tensor_scalar(out=ss1[:, T:T + 1], in0=ss1[:, T:T + 1], scalar1=1.0 / D, scalar2=1e-6,
                                                      op0=ALU.mult, op1=ALU.add), r=["ss1_%d" % T], w=["ss1_%d" % T])
                P.op("pool", lambda e: e.tensor_tensor(out=rstd1[:, T:T + 1], in0=ss1[:, T:T + 1], in1=nhalf[:, 0:1], op=ALU.pow),
                     r=["ss1_%d" % T, "nhalf"], w=["rstd1_%d" % T])
            P.op("dve", lambda e: e.scalar_tensor_tensor(out=tmp, in0=xin, scalar=rstd1[:, T:T + 1], in1=bcs[BC_GS1],
                                                         op0=ALU.mult, op1=ALU.mult),
                 r=[xin_k, "rstd1_%d" % T, "bc0"], w=[tmp_k])
            P.op(add_eng, lambda e: e.tensor_tensor(out=xm, in0=tmp, in1=bcs[BC_SH1], op=ALU.add),
                 r=[tmp_k, "bc1"], w=["xm"])
            if part == "A":
                return
            P.group("pe", [lambda e, k=k: e.transpose(out=PSB[psbank][:, k * 128:(k + 1) * 128], in_=xm[:, k * 128:(k + 1) * 128],
                                                     identity=identb) for k in range(8)],
                    r=["xm", "identb"], w=[pk[psbank]])
            P.op("act", lambda e: e.activation(out=xhatT_dst, in_=PSB[psbank].rearrange("p (k t) -> p k t", k=8), func=AF.Copy),
                 r=[pk[psbank]], w=[xhatT_k])

        mA = A.mark()
        vT = A.alloc([4, S + 30], BF16)
        mV = A.mark()
        G = A.alloc([NT, 4, 256], BF16)
        mA2 = A.mark()
        wA = A.alloc([8, 1536], BF16)
        xin = [A.alloc([D], F32) for _ in range(2)]
        tmp = A.alloc([D], F32)
        xm = A.alloc([D], BF16)
        xhatT = [A.alloc([8, 512], BF16) for _ in range(2)]
        sg = [A.alloc([512], F32) for _ in range(2)]
        fT = [A.alloc([4, 512], BF16)] * 2

        P.op("pool", lambda e: e.memset(vT[:, :, 0:15], 0.0), w=["vT"])
        P.op("pool", lambda e: e.memset(vT[:, :, S + 15:S + 30], 0.0), w=["vT"])
        win_v = w_in.rearrange("(k p) n -> p k n", p=128)
        for k in range(8):
            P.dma("pool", lambda e, k=k: e.dma_start(out=wA[:, k, :], in_=win_v[:, k, 0:1536]), "ld_wA", w=["wA"])

        def xa_tile(sc_, tt_):
            T_ = sc_ * 4 + tt_
            xhat_tile(T_, xin[T_ % 2], "xin%d" % (T_ % 2), tmp, xm, xhatT[sc_ % 2][:, :, tt_ * 128:(tt_ + 1) * 128],
                      "xhatT%d" % (sc_ % 2), T_ % 2, True)

        for tt in range(4):
            xa_tile(0, tt)
        for sc in range(8):
            xb_ = sc % 2
            xk = "xhatT%d" % xb_
            for c in range(4):
                ba, bg = 2 + 2 * (c % 2), 3 + 2 * (c % 2)
                P.group("pe", [lambda e, k=k, c=c, ba=ba, xb_=xb_: e.matmul(PS[ba], lhsT=wA[:, k, c * 128:(c + 1) * 128], rhs=xhatT[xb_][:, k, :],
                                                                  start=(k == 0), stop=(k == 7)) for k in range(8)],
                        r=["wA", xk], w=[pk[ba]])
                P.group("pe", [lambda e, k=k, c=c, bg=bg, xb_=xb_: e.matmul(PS[bg], lhsT=wA[:, k, 512 + c * 128:512 + (c + 1) * 128], rhs=xhatT[xb_][:, k, :],
                                                                  start=(k == 0), stop=(k == 7)) for k in range(8)],
                        r=["wA", xk], w=[pk[bg]])
                sgi = c % 2
                P.op("act", lambda e, c=c, bg=bg, sgi=sgi: e.activation(out=sg[sgi], in_=PS[bg], func=AF.Sigmoid,
                                                                      bias=smallcol[:, SC_BIN + 4 + c:SC_BIN + 5 + c], scale=1.0),
                     r=[pk[bg], "smallcol"], w=["sg%d" % sgi])
                P.op("dve", lambda e, c=c, ba=ba, sgi=sgi, sc=sc: e.scalar_tensor_tensor(
                    out=vT[:, c, 15 + sc * 512:15 + (sc + 1) * 512], in0=PS[ba], scalar=smallcol[:, SC_BIN + c:SC_BIN + c + 1],
                    in1=sg[sgi], op0=ALU.add, op1=ALU.mult),
                    r=[pk[ba], "sg%d" % sgi, "smallcol"], w=["vT_%d" % sc])
                if sc + 1 < 8:
                    xa_tile(sc + 1, c)
            fb_ = sc % 2
            for g in range(4):
                bf = 2 + (g % 4)
                P.group("pe", [lambda e, k=k, g=g, bf=bf, xb_=xb_: e.matmul(PS[bf], lhsT=wA[:, k, 1024 + g * 128:1024 + (g + 1) * 128], rhs=xhatT[xb_][:, k, :],
                                                                  start=(k == 0), stop=(k == 7)) for k in range(8)],
                        r=["wA", xk], w=[pk[bf]])
                P.op("act", lambda e, g=g, bf=bf: e.activation(out=fT[fb_][:, g, :], in_=PS[bf], func=AF.Identity,
                                                              bias=smallcol[:, SC_BIN + 8 + g:SC_BIN + 9 + g], scale=1.0),
                     r=[pk[bf], "smallcol"], w=["fT"])
            for tt in range(4):
                T = sc * 4 + tt
                for hb in range(2):
                    bank = 6 + hb
                    P.group("pe", [lambda e, g=g, tt=tt, bank=bank: e.matmul(
                        PS[bank][:, (g % 2) * 256:(g % 2 + 1) * 256], lhsT=fT[fb_][:, g, tt * 128:(tt + 1) * 128], rhs=cs128,
                        start=True, stop=True) for g in (2 * hb, 2 * hb + 1)],
                        r=["fT", "cs128"], w=[pk[bank]])
                    eng = "dve" if hb == 0 else "act"
                    if eng == "dve":
                        P.op("dve", lambda e, T=T, hb=hb, bank=bank: e.tensor_copy(
                            out=G[:, T, 2 * hb:2 * hb + 2, :], in_=PS[bank].rearrange("p (a b) -> p a b", a=2)),
                            r=[pk[bank]], w=["G_%d" % T])
                    else:
                        P.op("act", lambda e, T=T, hb=hb, bank=bank: e.activation(
                            out=G[:, T, 2 * hb:2 * hb + 2, :], in_=PS[bank].rearrange("p (a b) -> p a b", a=2), func=AF.Copy),
                            r=[pk[bank]], w=["G_%d" % T])

        if stage == 1:
            P.barrier()
            dtmp = xin[0]
            n = [0]

            def dump(src, col0, ncols):
                n[0] += 1
                P.op("dve", lambda e: e.tensor_copy(out=dtmp[:, 0:ncols], in_=src), r=["xin0"], w=["xin0"])
                P.dma("sp", lambda e: e.dma_start(out=dbg[:, col0:col0 + ncols], in_=dtmp[:, 0:ncols]), "dbg%d" % n[0], r=["xin0"])
            for q in range(4):
                dump(vT[:, 1, 15 + q * 1024:15 + (q + 1) * 1024], q * 1024, 1024)
            dump(G[:, 5, :, :].rearrange("p a b -> p (a b)"), 4096, 1024)
            dump(rstd1, 5120, NT)
            P.barrier()
            P.emit()
            return nc

        A.release(mA2)
        P.barrier()
        csb = [A.alloc([8, 512], BF16) for _ in range(2)]
        ssb = [A.alloc([8, 512], BF16) for _ in range(2)]
        frs = [A.alloc([4, 512], BF16) for _ in range(2)]
        Gkeys = ["G_%d" % t for t in range(NT)]
        it = 0
        for kc in range(8):
            bset = 4 * (kc % 2)
            for tg in range(4):
                b = it % 2
                it += 1
                P.dma("sp", lambda e, b=b, tg=tg, kc=kc: e.dma_start(out=csb[b].rearrange("p a b -> p (a b)"), in_=CSd[kc * 4 + tg]),
                      "ld_csb%d" % b, w=["csb%d" % b])
                P.dma("act", lambda e, b=b, tg=tg, kc=kc: e.dma_start(out=ssb[b].rearrange("p a b -> p (a b)"), in_=SSd[kc * 4 + tg]),
                      "ld_ssb%d" % b, w=["ssb%d" % b])
                fns = []
                for t8 in range(8):
                    t = tg * 8 + t8
                    for g in range(4):
                        fns.append(lambda e, t=t, t8=t8, g=g, b=b, bset=bset: e.matmul(
                            PS[bset + g], lhsT=G[:, t, g, 0:128], rhs=csb[b][:, t8, :], start=(t == 0), stop=False))
                        fns.append(lambda e, t=t, t8=t8, g=g, b=b, bset=bset: e.matmul(
                            PS[bset + g], lhsT=G[:, t, g, 128:256], rhs=ssb[b][:, t8, :], start=False, stop=(t == NT - 1)))
                P.group("pe", fns, r=Gkeys + ["csb%d" % b, "ssb%d" % b], w=[pk[bset + g] for g in range(4)])
            fb_ = kc % 2
            for g in range(4):
                if g % 2 == 0:
                    P.op("dve", lambda e, g=g, bset=bset, fb_=fb_: e.tensor_copy(out=frs[fb_][:, g, :], in_=PS[bset + g]),
                         r=[pk[bset + g]], w=["frs%d" % fb_])
                else:
                    P.op("act", lambda e, g=g, bset=bset, fb_=fb_: e.activation(out=frs[fb_][:, g, :], in_=PS[bset + g], func=AF.Copy),
                         r=[pk[bset + g]], w=["frs%d" % fb_])
            P.dma("sp", lambda e, kc=kc, fb_=fb_: e.dma_start(
                out=frd.rearrange("(g j) n -> j g n", j=128)[:, :, kc * 512:(kc + 1) * 512], in_=frs[fb_]),
                "st_frs%d" % fb_, r=["frs%d" % fb_], w=["frd_%d" % kc])

        if stage == 2:
            P.barrier()
            ld = A.alloc([S], BF16)
            dtmp = A.alloc([D], F32)
            P.dma("sp", lambda e: e.dma_start(out=ld, in_=frd[128:256, :]), "dbgL", w=["ld"])
            for q in range(4):
                P.op("dve", lambda e, q=q: e.tensor_copy(out=dtmp, in_=ld[:, q * 1024:(q + 1) * 1024]), r=["ld", "dtmp"], w=["dtmp"])
                P.dma("sp", lambda e, q=q: e.dma_start(out=dbg[:, q * 1024:(q + 1) * 1024], in_=dtmp), "dbgA%d" % q, r=["dtmp"])
            P.barrier()
            P.emit()
            return nc

        A.release(mV)
        P.barrier()
        vkeys = ["vT_%d" % i for i in range(8)] + ["vT"]
        convT = A.alloc([4, S], BF16)
        conv_end = A.mark()
        diag = A.alloc([4, KW, 128], BF16)
        cbuf = [A.alloc([4, 512], F32) for _ in range(2)]
        sqb = A.alloc([4, 512], F32)
        mean_c = A.alloc([512], F32)
        var_c = A.alloc([512], F32)
        rstd_c = A.alloc([512], F32)
        ycen_c = [A.alloc([512], F32) for _ in range(2)]
        for c in range(4):
            for tap in range(KW):
                P.op("dve", lambda e, c=c, tap=tap: e.tensor_scalar(out=diag[:, c, tap, :], in0=identb, scalar1=dwcol[:, tap * 4 + c:tap * 4 + c + 1],
                                                                  scalar2=None, op0=ALU.mult), r=["identb", "dwcol"], w=["diag%d" % c])

        def ln_block(blk):
            cb_ = cbuf[blk % 2]
            ck = "cbuf%d" % (blk % 2)
            P.op("act", lambda e: e.activation(out=sqb, in_=cb_, func=AF.Square), r=[ck], w=["sqb"])
            P.group("pe", [lambda e, c=c: e.matmul(PS[0], lhsT=ones_ln, rhs=cb_[:, c, :], start=(c == 0), stop=(c == 3)) for c in range(4)],
                    r=[ck, "ones_ln"], w=[pk[0]])
            P.group("pe", [lambda e, c=c: e.matmul(PS[1], lhsT=ones_ln, rhs=sqb[:, c, :], start=(c == 0), stop=(c == 3)) for c in range(4)],
                    r=["sqb", "ones_ln"], w=[pk[1]])
            P.op("act", lambda e: e.activation(out=mean_c, in_=PS[0], func=AF.Copy), r=[pk[0]], w=["mean_c"])
            P.op("dve", lambda e: e.tensor_tensor(out=var_c, in0=mean_c, in1=mean_c, op=ALU.mult), r=["mean_c"], w=["var_c"])
            P.op("dve", lambda e: e.tensor_tensor(out=var_c, in0=PS[1], in1=var_c, op=ALU.subtract), r=[pk[1], "var_c"], w=["var_c"])
            P.op("act", lambda e: e.activation(out=rstd_c, in_=var_c, func=AF.Sqrt, bias=eps5[:, 0:1], scale=1.0), r=["var_c", "eps5"], w=["rstd_c"])
            P.op("dve", lambda e: e.reciprocal(out=rstd_c, in_=rstd_c), r=["rstd_c"], w=["rstd_c"])
            for c in range(4):
                y = ycen_c[c % 2]
                yk = "ycen_c%d" % (c % 2)
                P.op("dve", lambda e, c=c, y=y: e.tensor_tensor(out=y, in0=cb_[:, c, :], in1=mean_c, op=ALU.subtract), r=[ck, "mean_c"], w=[yk])
                P.op("dve", lambda e, y=y: e.tensor_tensor(out=y, in0=y, in1=rstd_c, op=ALU.mult), r=[yk, "rstd_c"], w=[yk])
                P.op("act", lambda e, c=c, y=y: e.activation(out=convT[:, c, blk * 512:(blk + 1) * 512], in_=y, func=AF.Silu,
                                                            bias=smallcol[:, SC_LNB + c:SC_LNB + c + 1], scale=smallcol[:, SC_LNG + c:SC_LNG + c + 1]),
                     r=[yk, "smallcol"], w=["convT"])

        ev = 0
        for blk in range(8):
            cb_ = cbuf[blk % 2]
            ck = "cbuf%d" % (blk % 2)
            for c in range(4):
                bank = 2 + ev % 6
                P.group("pe", [lambda e, c=c, blk=blk, tap=tap, bank=bank: e.matmul(
                    PS[bank], lhsT=diag[:, c, tap, :], rhs=vT[:, c, blk * 512 + tap:blk * 512 + tap + 512], start=(tap == 0), stop=(tap == KW - 1))
                    for tap in range(KW)], r=["diag%d" % c] + vkeys, w=[pk[bank]])
                if ev % 2 == 0:
                    P.op("act", lambda e, c=c, bank=bank, cb_=cb_: e.activation(out=cb_[:, c, :], in_=PS[bank], func=AF.Identity,
                                                                              bias=smallcol[:, SC_DWB + c:SC_DWB + c + 1], scale=1.0),
                         r=[pk[bank], "smallcol"], w=[ck])
                else:
                    P.op("dve", lambda e, c=c, bank=bank, cb_=cb_: e.tensor_scalar(out=cb_[:, c, :], in0=PS[bank],
                                                                                 scalar1=smallcol[:, SC_DWB + c:SC_DWB + c + 1], scalar2=None, op0=ALU.add),
                         r=[pk[bank], "smallcol"], w=[ck])
                ev += 1
                if c == 1 and blk > 0:
                    ln_block(blk - 1)
        ln_block(7)

        if stage == 25:
            P.barrier()
            A.release(mA2)
            dtmp = A.alloc([D], F32)
            for q in range(4):
                P.op("dve", lambda e, q=q: e.tensor_copy(out=dtmp, in_=convT[:, 1, q * 1024:(q + 1) * 1024]), r=["dtmp"], w=["dtmp"])
                P.dma("sp", lambda e, q=q: e.dma_start(out=dbg[:, q * 1024:(q + 1) * 1024], in_=dtmp), "dbgA%d" % q, r=["dtmp"])
            P.barrier()
            P.emit()
            return nc

        P.barrier()
        A.release(mA)
        cwo = A.alloc([4, D], BF16)
        fw = A.alloc([4, D], BF16)
        wo = A.alloc([8, D], BF16)
        assert A.mark() <= mV
        A.release(conv_end)
        wg = A.alloc([8, 2048], BF16)
        SCB = 256
        NSC = S // SCB
        TPS = SCB // 128
        xinB = [A.alloc([D], F32) for _ in range(2)]
        xmB = A.alloc([D], BF16)
        xhB = [A.alloc([8, SCB], BF16) for _ in range(2)]
        sg0 = [A.alloc([SCB], F32) for _ in range(2)]
        sg1 = [A.alloc([SCB], F32) for _ in range(2)]
        mm1 = [A.alloc([SCB], F32) for _ in range(2)]
        mm2 = [A.alloc([SCB], F32) for _ in range(2)]
        merged = [A.alloc([8, SCB], BF16) for _ in range(2)]
        hbuf = [A.alloc([D], F32) for _ in range(2)]
        htmp = [A.alloc([512], F32) for _ in range(2)]
        u2b = [A.alloc([D], BF16) for _ in range(2)]
        u2T = A.alloc([8, 128], F32)
        frc = [A.alloc([4, SCB], BF16) for _ in range(2)]

        for k in range(8):
            P.dma("pool", lambda e, k=k: e.dma_start(out=wg[:, k, :], in_=win_v[:, k, 1536:3584]), "ld_wg", w=["wg"])
            P.dma("pool", lambda e, k=k: e.dma_start(out=wo[:, k, :], in_=w_out.rearrange("(k p) n -> p k n", p=128)[:, k, :]), "ld_wo", w=["wo"])
        for c in range(4):
            P.dma("pool", lambda e, c=c: e.dma_start(out=cwo[:, c, :], in_=conv_w_out.rearrange("(k p) n -> p k n", p=128)[:, c, :]), "ld_cwo", w=["cwo"])
            P.dma("pool", lambda e, c=c: e.dma_start(out=fw[:, c, :], in_=fourier_w.rearrange("(k p) n -> p k n", p=128)[:, c, :]), "ld_fw", w=["fw"])
        frd_v = frd.rearrange("(g j) n -> j g n", j=128)

        def chunks_X(sc):
            p = sc % 2
            T0 = sc * SCB
            ch = []

            def c0():
                P.dma("act", lambda e: e.dma_start(out=frc[p], in_=frd_v[:, :, T0:T0 + SCB]), "ld_frc%d" % p, w=["frc%d" % p])
            ch.append(c0)
            for tt in range(TPS):
                T = sc * TPS + tt
                xk = "xinB%d" % (T % 2)
                for part in ("A", "B"):
                    ch.append(lambda T=T, xk=xk, tt=tt, part=part: xhat_tile(
                        T, xinB[T % 2], xk, xinB[T % 2], xmB, xhB[p][:, :, tt * 128:(tt + 1) * 128], "xhB%d" % p, 0, False,
                        tmp_k=xk, part=part, add_eng="dve"))
            return ch

        def stage_M(sc, inter=()):
            p = sc % 2
            T0 = sc * SCB
            inter = list(inter)
            per = (len(inter) + 7) // 8 if inter else 0
            for dc in range(8):
                for _ in range(per):
                    if inter:
                        inter.pop(0)()
                q = dc % 2
                b0 = 2 + 2 * q
                b1 = b0 + 1
                P.group("pe", [lambda e, c=c, dc=dc, b0=b0: e.matmul(PS[b0][:, 0:SCB], lhsT=cwo[:, c, dc * 128:(dc + 1) * 128], rhs=convT[:, c, T0:T0 + SCB],
                                                                    start=(c == 0), stop=(c == 3)) for c in range(4)]
                        + [lambda e, g=g, dc=dc, b0=b0: e.matmul(PS[b0][:, SCB:2 * SCB], lhsT=fw[:, g, dc * 128:(dc + 1) * 128], rhs=frc[p][:, g, :],
                                                                  start=(g == 0), stop=(g == 3)) for g in range(4)],
                        r=["cwo", "fw", "convT", "frc%d" % p], w=[pk[b0]])
                P.group("pe", [lambda e, k=k, dc=dc, b1=b1: e.matmul(PS[b1][:, 0:SCB], lhsT=wg[:, k, dc * 128:(dc + 1) * 128], rhs=xhB[p][:, k, :],
                                                                    start=(k == 0), stop=(k == 7)) for k in range(8)]
                        + [lambda e, k=k, dc=dc, b1=b1: e.matmul(PS[b1][:, SCB:2 * SCB], lhsT=wg[:, k, 1024 + dc * 128:1024 + (dc + 1) * 128], rhs=xhB[p][:, k, :],
                                                                  start=(k == 0), stop=(k == 7)) for k in range(8)],
                        r=["wg", "xhB%d" % p], w=[pk[b1]])
                P.op("act", lambda e, dc=dc, b1=b1, q=q: e.activation(out=sg0[q], in_=PS[b1][:, 0:SCB], func=AF.Sigmoid,
                                                                     bias=smallcol[:, SC_BIN + 12 + dc:SC_BIN + 13 + dc], scale=1.0),
                     r=[pk[b1], "smallcol"], w=["sg0B%d" % q])
                P.op("act", lambda e, dc=dc, b1=b1, q=q: e.activation(out=sg1[q], in_=PS[b1][:, SCB:2 * SCB], func=AF.Sigmoid,
                                                                     bias=smallcol[:, SC_BIN + 20 + dc:SC_BIN + 21 + dc], scale=1.0),
                     r=[pk[b1], "smallcol"], w=["sg1B%d" % q])
                P.op("dve", lambda e, dc=dc, b0=b0, q=q: e.scalar_tensor_tensor(out=mm1[q], in0=PS[b0][:, 0:SCB], scalar=smallcol[:, SC_CBO + dc:SC_CBO + dc + 1],
                                                                               in1=sg0[q], op0=ALU.add, op1=ALU.mult),
                     r=[pk[b0], "sg0B%d" % q, "smallcol"], w=["mm1%d" % q])
                P.op("dve", lambda e, dc=dc, b0=b0, q=q: e.scalar_tensor_tensor(out=mm2[q], in0=PS[b0][:, SCB:2 * SCB], scalar=smallcol[:, SC_FB + dc:SC_FB + dc + 1],
                                                                               in1=sg1[q], op0=ALU.add, op1=ALU.mult),
                     r=[pk[b0], "sg1B%d" % q, "smallcol"], w=["mm2%d" % q])
                P.op("pool", lambda e, dc=dc, q=q: e.tensor_tensor(out=merged[p][:, dc, :], in0=mm1[q], in1=mm2[q], op=ALU.add),
                     r=["mm1%d" % q, "mm2%d" % q], w=["merged%d" % p])

        def router_tr(T):
            hb = hbuf[T % 2]
            hk = "hbuf%d" % (T % 2)
            for hf in range(2):
                P.group("pe", [lambda e, k=k, hf=hf: e.transpose(out=PS[hf][:, (k % 4) * 128:(k % 4 + 1) * 128],
                                                                in_=hb[:, k * 128:(k + 1) * 128], identity=ident)
                               for k in range(4 * hf, 4 * hf + 4)], r=[hk, "ident"], w=[pk[hf]])
                if hf == 0:
                    P.op("dve", lambda e: e.tensor_copy(out=u2T[:, 0:4, :], in_=PS[0].rearrange("p (a b) -> p a b", a=4)), r=[pk[0]], w=["u2T"])
                else:
                    P.op("act", lambda e: e.activation(out=u2T[:, 4:8, :], in_=PS[1].rearrange("p (a b) -> p a b", a=4), func=AF.Copy), r=[pk[1]], w=["u2T"])

        def router_mm(T):
            P.group("pe", [lambda e, k=k: e.matmul(PS[1][:, 0:E], lhsT=u2T[:, k, :], rhs=rw[:, k, :], start=(k == 0), stop=(k == 7)) for k in range(8)],
                    r=["u2T", "rw"], w=[pk[1]])
            P.op("dve", lambda e: e.tensor_copy(out=logits[:, T, :], in_=PS[1][:, 0:E]), r=[pk[1]], w=["logits"])

        def stage_H(sc):
            for tt in range(TPS):
                T = sc * TPS + tt
                hb = hbuf[T % 2]
                hk = "hbuf%d" % (T % 2)
                ub = u2b[T % 2]
                uk = "u2b%d" % (T % 2)
                P.dma("sp", lambda e, hb=hb, T=T: e.dma_start(out=hb, in_=x[T * 128:(T + 1) * 128, :]), "ld_" + hk, w=[hk])
                P.op("pool", lambda e, hb=hb: e.tensor_tensor(out=hb, in0=hb, in1=bcs[BC_GB1], op=ALU.add), r=[hk, "bc3"], w=[hk])
                if T > 0:
                    router_tr(T - 1)
                toks = []
                for dh in range(2):
                    bh = 6 + dh
                    P.group("pe", [lambda e, k=k, tt=tt, dh=dh, bh=bh: e.matmul(PS[bh], lhsT=merged[sc % 2][:, k, tt * 128:(tt + 1) * 128],
                                                                               rhs=wo[:, k, dh * 512:(dh + 1) * 512], start=(k == 0), stop=(k == 7))
                                   for k in range(8)], r=["merged%d" % (sc % 2), "wo"], w=[pk[bh]])
                if T > 0:
                    router_mm(T - 1)
                for dh in range(2):
                    bh = 6 + dh
                    P.op("dve", lambda e, dh=dh, bh=bh: e.tensor_tensor(out=htmp[dh], in0=PS[bh], in1=bcs[BC_G1][:, dh * 512:(dh + 1) * 512], op=ALU.mult),
                         r=[pk[bh], "bc2"], w=["htmp%d" % dh])
                    P.op("dve", lambda e, dh=dh, hb=hb: e.tensor_tensor(out=hb[:, dh * 512:(dh + 1) * 512], in0=hb[:, dh * 512:(dh + 1) * 512],
                                                                       in1=htmp[dh], op=ALU.add), r=[hk, "htmp%d" % dh], w=[hk])
                P.dma("sp", lambda e, T=T, hb=hb: e.dma_start(out=hd[T * 128:(T + 1) * 128, :], in_=hb), "st_" + hk, r=[hk], w=["hd_%d" % T])
                P.op("act", lambda e, T=T, hb=hb, ub=ub: e.activation(out=ub, in_=hb, func=AF.Square, accum_out=ss2[:, T:T + 1]),
                     r=[hk], w=[uk, "ss2_%d" % T])
                P.op("dve", lambda e, T=T: e.tensor_scalar(out=ss2[:, T:T + 1], in0=ss2[:, T:T + 1], scalar1=1.0 / D, scalar2=1e-6,
                                                           op0=ALU.mult, op1=ALU.add), r=["ss2_%d" % T], w=["ss2_%d" % T])
                P.op("pool", lambda e, T=T: e.tensor_tensor(out=rstd2[:, T:T + 1], in0=ss2[:, T:T + 1], in1=nhalf[:, 0:1], op=ALU.pow),
                     r=["ss2_%d" % T, "nhalf"], w=["rstd2_%d" % T])
                P.op("dve", lambda e, T=T, hb=hb: e.scalar_tensor_tensor(out=hb, in0=hb, scalar=rstd2[:, T:T + 1], in1=bcs[BC_GS2],
                                                                        op0=ALU.mult, op1=ALU.mult), r=[hk, "rstd2_%d" % T, "bc4"], w=[hk])
                P.op("dve", lambda e, hb=hb: e.tensor_tensor(out=hb, in0=hb, in1=bcs[BC_SH2], op=ALU.add), r=[hk, "bc5"], w=[hk])
                P.op("act", lambda e, hb=hb, ub=ub: e.activation(out=ub, in_=hb, func=AF.Copy), r=[hk], w=[uk])
                P.dma("sp", lambda e, T=T, ub=ub: e.dma_start(out=scr[T * 128:(T + 1) * 128, 0:D], in_=ub), "st_" + uk, r=[uk], w=["scr_%d" % T])

        for f in chunks_X(0):
            f()
        for sc in range(NSC):
            inter = chunks_X(sc + 1) if sc + 1 < NSC else []
            stage_M(sc, inter)
            stage_H(sc)
        router_tr(NT - 1)
        router_mm(NT - 1)

        if stage == 3:
            P.barrier()
            P.dma("sp", lambda e: e.dma_start(out=dbg[:, 0:NT * E], in_=logits.rearrange("p a b -> p (a b)")), "dbgA")
            P.dma("sp", lambda e: e.dma_start(out=hbuf[0], in_=hd[3 * 128:4 * 128, :]), "dbgL", w=["hb0x"])
            P.dma("sp", lambda e: e.dma_start(out=dbg[:, 1024:2048], in_=hbuf[0]), "dbgC", r=["hb0x"])
            P.dma("sp", lambda e: e.dma_start(out=u2b[0], in_=scr[3 * 128:4 * 128, 0:D]), "dbgL2", w=["ub0x"])
            P.op("dve", lambda e: e.tensor_copy(out=hbuf[1], in_=u2b[0]), r=["ub0x"], w=["hb1x"])
            P.dma("sp", lambda e: e.dma_start(out=dbg[:, 2048:3072], in_=hbuf[1]), "dbgD", r=["hb1x"])
            P.barrier()
            P.emit()
            return nc

        A.release(m_p2)
        P.barrier()
        junk = A.alloc([2048], BF16)
        aff_es = A.alloc([512], F32)
        maskt = A.alloc([512], F32)
        onest = A.alloc([512], F32)
        cum = A.alloc([512], F32)
        affp = A.alloc([4, 128], F32)
        lo = A.alloc([1], F32)
        mid = A.alloc([1], F32)
        cntp = A.alloc([1], F32)
        pred = A.alloc([1], F32)
        offs = A.alloc([1], F32)
        mx = A.alloc([NT], F32)
        sm = A.alloc([NT], F32)
        cum16 = A.alloc([512], I16)
        P.op("dve", lambda e: e.tensor_reduce(out=mx, in_=logits, axis=mybir.AxisListType.X, op=ALU.max), r=["logits"], w=["mx"])
        P.op("dve", lambda e: e.tensor_scalar(out=mx, in0=mx, scalar1=-1.0, scalar2=None, op0=ALU.mult), r=["mx"], w=["mx"])
        for T in range(NT):
            P.op("act", lambda e, T=T: e.activation(out=aff[:, T, :], in_=logits[:, T, :], func=AF.Exp, bias=mx[:, T:T + 1], scale=1.0,
                                                   accum_out=sm[:, T:T + 1]), r=["logits", "mx"], w=["aff", "sm"])
        P.op("dve", lambda e: e.reciprocal(out=sm, in_=sm), r=["sm"], w=["sm"])
        for T in range(NT):
            P.op("dve", lambda e, T=T: e.tensor_scalar(out=aff[:, T, :], in0=aff[:, T, :], scalar1=sm[:, T:T + 1], scalar2=None, op0=ALU.mult),
                 r=["aff", "sm"], w=["aff"])
        scr_v = scr.rearrange("(t p) c -> p t c", p=128)
        aff_b = aff.rearrange("p a b -> p (a b)").bitcast(BF16).rearrange("p (a b) -> p a b", a=NT)
        for q in range(4):
            P.dma("sp", lambda e, q=q: e.dma_start(out=scr_v[:, q * 8:(q + 1) * 8, D:D + 32], in_=aff_b[:, q * 8:(q + 1) * 8, :]),
                  "st_aff", r=["aff"], w=["scra_%d" % q])
        aff4 = aff.rearrange("p (s c) e -> p s c e", c=4)
        for tc in range(4):
            P.op("dve", lambda e, tc=tc: e.tensor_copy(out=affp[:, tc, :].rearrange("p (s e) -> p s e", e=E), in_=aff4[:, :, tc, :]),
                 r=["aff"], w=["affp"])
        P.group("pe", [lambda e, tc=tc: e.transpose(out=PS[0][:, tc * 128:(tc + 1) * 128], in_=affp[:, tc, :], identity=ident) for tc in range(4)],
                r=["affp", "ident"], w=[pk[0]])
        P.op("dve", lambda e: e.tensor_copy(out=aff_es, in_=PS[0]), r=[pk[0]], w=["aff_es"])
        P.op("dve", lambda e: e.memset(lo, 0.0), w=["lo"])
        P.op("pool", lambda e: e.memset(onest, 1.0), w=["onest"])
        for i in range(26):
            step = 2.0 ** -(i + 1)
            P.op("dve", lambda e, step=step: e.tensor_scalar(out=mid, in0=lo, scalar1=step, scalar2=None, op0=ALU.add), r=["lo"], w=["mid"])
            P.op("dve", lambda e: e.tensor_scalar(out=junk[:, 0:512], in0=aff_es, scalar1=mid[:, 0:1], scalar2=0.0, op0=ALU.is_ge, op1=ALU.add,
                                                  accum_out=cntp), r=["aff_es", "mid"], w=["junk", "cntp"])
            P.group("pe", [lambda e: e.matmul(PS[1][:, 0:1], lhsT=maskM, rhs=cntp, start=True, stop=True)], r=["maskM", "cntp"], w=[pk[1]])
            P.op("dve", lambda e: e.tensor_scalar(out=pred, in0=PS[1][:, 0:1], scalar1=511.5, scalar2=None, op0=ALU.is_ge), r=[pk[1]], w=["pred"])
            P.op("dve", lambda e, step=step: e.scalar_tensor_tensor(out=lo, in0=pred, scalar=step, in1=lo, op0=ALU.mult, op1=ALU.add),
                 r=["pred", "lo"], w=["lo"])
        P.op("dve", lambda e: e.tensor_scalar(out=maskt, in0=aff_es, scalar1=lo[:, 0:1], scalar2=None, op0=ALU.is_ge), r=["aff_es", "lo"], w=["maskt"])
        P.op("dve", lambda e: e.tensor_tensor_scan(out=cum, data0=onest, data1=maskt, initial=0.0, op0=ALU.mult, op1=ALU.add),
             r=["onest", "maskt"], w=["cum"])
        P.group("pe", [lambda e: e.matmul(PS[1][:, 0:1], lhsT=maskM2, rhs=cum[:, 511:512], start=True, stop=True)], r=["maskM2", "cum"], w=[pk[1]])
        P.op("dve", lambda e: e.tensor_copy(out=offs, in_=PS[1][:, 0:1]), r=[pk[1]], w=["offs"])
        P.op("dve", lambda e: e.tensor_scalar(out=cum, in0=cum, scalar1=offs[:, 0:1], scalar2=None, op0=ALU.add), r=["cum", "offs"], w=["cum"])
        P.op("dve", lambda e: e.tensor_copy(out=cum16, in_=cum), r=["cum"], w=["cum16"])
        for s8 in range(8):
            P.dma("sp", lambda e, s8=s8: e.dma_start(out=cumd[:, s8, :], in_=cum16[s8 * 16:(s8 + 1) * 16, :]), "st_cum", r=["cum16"], w=["cumd"])

        if stage == 4:
            P.barrier()
            P.dma("sp", lambda e: e.dma_start(out=dbg[:, 0:512], in_=aff_es), "dbgA")
            P.dma("sp", lambda e: e.dma_start(out=dbg[:, 512:1024], in_=cum), "dbgB")
            P.dma("sp", lambda e: e.dma_start(out=dbg[:, 1024:1025], in_=lo, allow_slow_non_contiguous=True), "dbgC")
            P.barrier()
            P.emit()
            return nc

        A.release(m_p1)
        P.barrier()
        junk = A.alloc([2048], BF16)
        bcs[BC_G2] = A.alloc([D], F32)
        bcs[BC_FG] = A.alloc([D], F32)
        P.dma("sp", lambda e: e.dma_start(out=bcs[BC_G2], in_=modd[0, 5 * D:6 * D].partition_broadcast(128)), "bc6", w=["bc6"])
        P.dma("act", lambda e: e.dma_start(out=bcs[BC_FG], in_=final_g[0, :].partition_broadcast(128)), "bc7", w=["bc7"])
        cb = A.alloc([S], I16)
        gath = A.alloc([4, 1056], BF16)
        tokT = [A.alloc([8, 512], BF16) for _ in range(2)]
        wgs = [A.alloc([8, 1024], BF16) for _ in range(2)]
        wus = [A.alloc([8, 1024], BF16) for _ in range(2)]
        wds = [A.alloc([8, 1024], BF16) for _ in range(2)]
        slu = [A.alloc([512], F32) for _ in range(2)]
        actT = A.alloc([8, 512], BF16)
        out1 = A.alloc([4, D], F32)
        outw = [A.alloc([D], F32) for _ in range(2)]
        otmp = A.alloc([512], F32)
        gath_f = gath.rearrange("p a b -> p (a b)").bitcast(F32).rearrange("p (a b) -> p a b", a=4)

        def load_weights(u):
            s_ = u % 2
            for nm, src, dst in (("wgs", ew_gate, wgs), ("wus", ew_up, wus), ("wds", ew_down, wds)):
                P.dma("pool", lambda e, src=src, dst=dst: e.dma_start(
                    out=dst[s_].rearrange("p a b -> p (a b)").rearrange("p (a b) -> p a b", b=2048),
                    in_=src[u].rearrange("p (a b) -> p a b", b=2048)), "ld_%s%d" % (nm, s_), w=["%s%d" % (nm, s_)])

        def route_idx(e_):
            ch = []

            def r0():
                P.dma("sp", lambda e: e.dma_start(out=cb, in_=cumd[e_].rearrange("s n -> (s n)").partition_broadcast(128)), "ld_cb",
                      r=["cumd"], w=["cb"])
            ch.append(r0)
            for cc in range(4):
                for hf in range(2):
                    def rc(cc=cc, hf=hf):
                        P.op("dve", lambda e: e.tensor_scalar(
                            out=junk, in0=cb[:, hf * 2048:(hf + 1) * 2048], scalar1=cvals[:, cc:cc + 1], scalar2=0.0, op0=ALU.is_le, op1=ALU.add,
                            accum_out=idxp[:, e_ * 8 + cc * 2 + hf:e_ * 8 + cc * 2 + hf + 1]), r=["cb", "cvals"], w=["junk", "idxp%d" % e_])
                    ch.append(rc)

            def r9():
                idxp_v = idxp[:, e_ * 8:(e_ + 1) * 8].rearrange("p (c h) -> p c h", h=2)
                P.op("dve", lambda e: e.tensor_tensor(out=idxf[:, e_ * 4:(e_ + 1) * 4], in0=idxp_v[:, :, 0], in1=idxp_v[:, :, 1], op=ALU.add),
                     r=["idxp%d" % e_], w=["idxf%d" % e_])
                P.op("dve", lambda e: e.tensor_copy(out=idxi[:, e_ * 4:(e_ + 1) * 4], in_=idxf[:, e_ * 4:(e_ + 1) * 4]), r=["idxf%d" % e_], w=["idxi%d" % e_])
            ch.append(r9)
            return ch

        def gather_rows(e_):
            scr_keys = ["scr_%d" % t for t in range(NT)] + ["scra_%d" % q for q in range(4)]
            for cc in range(4):
                P.dma("pool", lambda e, cc=cc: e.indirect_dma_start(
                    out=gath[:, cc, :], out_offset=None, in_=scr[:, :],
                    in_offset=bass.IndirectOffsetOnAxis(ap=idxi[:, e_ * 4 + cc:e_ * 4 + cc + 1], axis=0)),
                    "ld_gath", r=["idxi%d" % e_] + scr_keys, w=["gath"])
            P.op("dve", lambda e: e.tensor_copy(out=wsel[:, e_ * 4:(e_ + 1) * 4], in_=gath_f[:, :, 512 + e_]), r=["gath"], w=["wsel%d" % e_])

        def transpose_tok(e_):
            tb = e_ % 2
            for cc in range(4):
                bank = 6 + (cc % 2)
                P.group("pe", [lambda e, k=k, cc=cc, bank=bank: e.transpose(out=PSB[bank][:, k * 128:(k + 1) * 128],
                                                                           in_=gath[:, cc, k * 128:(k + 1) * 128], identity=identb)
                               for k in range(8)], r=["gath", "identb"], w=[pk[bank]])
                if cc % 2 == 0:
                    P.op("act", lambda e, cc=cc, bank=bank: e.activation(out=tokT[tb][:, :, cc * 128:(cc + 1) * 128],
                                                                        in_=PSB[bank].rearrange("p (k t) -> p k t", k=8), func=AF.Copy),
                         r=[pk[bank]], w=["tokT%d" % tb])
                else:
                    P.op("dve", lambda e, cc=cc, bank=bank: e.tensor_copy(out=tokT[tb][:, :, cc * 128:(cc + 1) * 128],
                                                                         in_=PSB[bank].rearrange("p (k t) -> p k t", k=8)),
                         r=[pk[bank]], w=["tokT%d" % tb])

        hd_keys = ["hd_%d" % t for t in range(NT)]
        sc_keys = ["hd_sc0", "hd_sc1", "hd_sc2", "hd_sc3"]
        ocnt = [0]

        def unit(u, inter=()):
            inter = list(inter)
            e_, fh = u // 2, u % 2
            s_ = u % 2
            tb = e_ % 2
            for fc in range(8):
                bg_, bu_ = 2 * (fc % 2), 2 * (fc % 2) + 1
                P.group("pe", [lambda e, k=k, fc=fc, bg_=bg_: e.matmul(PS[bg_], lhsT=wgs[s_][:, k, fc * 128:(fc + 1) * 128], rhs=tokT[tb][:, k, :],
                                                                      start=(k == 0), stop=(k == 7)) for k in range(8)],
                        r=["wgs%d" % s_, "tokT%d" % tb], w=[pk[bg_]])
                P.group("pe", [lambda e, k=k, fc=fc, bu_=bu_: e.matmul(PS[bu_], lhsT=wus[s_][:, k, fc * 128:(fc + 1) * 128], rhs=tokT[tb][:, k, :],
                                                                      start=(k == 0), stop=(k == 7)) for k in range(8)],
                        r=["wus%d" % s_, "tokT%d" % tb], w=[pk[bu_]])
                si = fc % 2
                P.op("act", lambda e, bg_=bg_, si=si: e.activation(out=slu[si], in_=PS[bg_], func=AF.Silu), r=[pk[bg_]], w=["slu%d" % si])
                P.op("dve", lambda e, fc=fc, bu_=bu_, si=si: e.tensor_tensor(out=actT[:, fc, :], in0=slu[si], in1=PS[bu_], op=ALU.mult),
                     r=["slu%d" % si, pk[bu_]], w=["actT"])
                for _ in range(2):
                    if inter:
                        inter.pop(0)()
            while inter:
                inter.pop(0)()
            for cc in range(4):
                if fh == 1:
                    ob = ocnt[0] % 2
                    ocnt[0] += 1
                for dh in range(2):
                    bo = 4 + dh
                    P.group("pe", [lambda e, fc=fc, cc=cc, dh=dh, bo=bo: e.matmul(PS[bo], lhsT=actT[:, fc, cc * 128:(cc + 1) * 128],
                                                                                 rhs=wds[s_][:, fc, dh * 512:(dh + 1) * 512],
                                                                                 start=(fc == 0), stop=(fc == 7)) for fc in range(8)],
                            r=["actT", "wds%d" % s_], w=[pk[bo]])
                    if fh == 0:
                        P.op("act", lambda e, cc=cc, dh=dh, bo=bo: e.activation(out=out1[:, cc, dh * 512:(dh + 1) * 512], in_=PS[bo], func=AF.Copy),
                             r=[pk[bo]], w=["out1"])
                    else:
                        P.op("dve", lambda e, cc=cc, dh=dh, bo=bo: e.tensor_tensor(out=otmp, in0=PS[bo], in1=out1[:, cc, dh * 512:(dh + 1) * 512], op=ALU.add),
                             r=[pk[bo], "out1"], w=["otmp"])
                        P.op("dve", lambda e, cc=cc, dh=dh, ob=ob: e.scalar_tensor_tensor(
                            out=outw[ob][:, dh * 512:(dh + 1) * 512], in0=otmp, scalar=wsel[:, e_ * 4 + cc:e_ * 4 + cc + 1],
                            in1=bcs[BC_G2][:, dh * 512:(dh + 1) * 512], op0=ALU.mult, op1=ALU.mult),
                            r=["otmp", "wsel%d" % e_, "bc6"], w=["outw%d" % ob])
                if fh == 1:
                    P.dma("pool", lambda e, cc=cc, ob=ob: e.indirect_dma_start(
                        out=hd[:, :], out_offset=bass.IndirectOffsetOnAxis(ap=idxi[:, e_ * 4 + cc:e_ * 4 + cc + 1], axis=0),
                        in_=outw[ob], in_offset=None, compute_op=ALU.add),
                        "sc_outw%d" % ob, r=["outw%d" % ob, "idxi%d" % e_] + hd_keys + (sc_keys if cc == 0 else []),
                        w=["hd_sc%d" % cc])

        P.op("dve", lambda e: e.memset(idxp, 0.0), w=["idxp%d" % i for i in range(E)])
        load_weights(0)
        load_weights(1)
        for f in route_idx(0):
            f()
        gather_rows(0)
        transpose_tok(0)
        NEXP = int(os.environ.get('K_NEXP', E))
        for e_ in range(NEXP):
            unit(2 * e_, route_idx(e_ + 1) if e_ + 1 < E else ())
            if e_ + 1 < E:
                gather_rows(e_ + 1)
            if 2 * e_ + 2 < 2 * E:
                load_weights(2 * e_ + 2)
            if e_ + 1 < E:
                transpose_tok(e_ + 1)
            unit(2 * e_ + 1)
            if 2 * e_ + 3 < 2 * E:
                load_weights(2 * e_ + 3)

        fin = [A.alloc([D], F32) for _ in range(2)]
        fss = A.alloc([NT], F32)
        frs_ = A.alloc([NT], F32)
        last = []
        for T in range(NT):
            b = T % 2
            P.dma("sp", lambda e, T=T, b=b: e.dma_start(out=fin[b], in_=hd[T * 128:(T + 1) * 128, :]), "ld_fin%d" % b,
                  r=sc_keys + ["hd_%d" % T], w=["fin%d" % b])
            P.op("act", lambda e, T=T, b=b: e.activation(out=junk[:, 0:D], in_=fin[b], func=AF.Square, accum_out=fss[:, T:T + 1]),
                 r=["fin%d" % b], w=["junk", "fss%d" % T])
            P.op("dve", lambda e, T=T: e.tensor_scalar(out=fss[:, T:T + 1], in0=fss[:, T:T + 1], scalar1=1.0 / D, scalar2=1e-6,
                                                       op0=ALU.mult, op1=ALU.add), r=["fss%d" % T], w=["fss%d" % T])
            P.op("pool", lambda e, T=T: e.tensor_tensor(out=frs_[:, T:T + 1], in0=fss[:, T:T + 1], in1=nhalf[:, 0:1], op=ALU.pow),
                 r=["fss%d" % T, "nhalf"], w=["frs%d" % T])
            P.op("dve", lambda e, T=T, b=b: e.scalar_tensor_tensor(out=fin[b], in0=fin[b], scalar=frs_[:, T:T + 1], in1=bcs[BC_FG],
                                                                   op0=ALU.mult, op1=ALU.mult), r=["fin%d" % b, "frs%d" % T, "bc7"], w=["fin%d" % b])
            last.append(P.dma("act", lambda e, T=T, b=b: e.dma_start(out=out[T * 128:(T + 1) * 128, :], in_=fin[b]), "st_fin%d" % b,
                              r=["fin%d" % b], w=["out_%d" % T]))
        P.barrier()
        P.emit()
        print("arena high-water bytes", A.hw, "of", ARENA_BYTES)
    return nc


_CONST = {}


def _dft_layout(M):
    A_ = M.reshape(4, 8, 128, 8, 512)
    return np.ascontiguousarray(A_.transpose(3, 0, 2, 1, 4)).reshape(32, 128, 4096)


_WCACHE = {}


def _weight_layouts(inp):
    key = id(inp["expert_w_gate"])
    if _WCACHE.get("key") == key:
        return _WCACHE["val"]
    f = lambda a: np.asarray(a, dtype=np.float32)
    g = f(inp["expert_w_gate"][0]).reshape(E, 8, 128, 2, 1024).transpose(0, 3, 2, 1, 4)
    u = f(inp["expert_w_up"][0]).reshape(E, 8, 128, 2, 1024).transpose(0, 3, 2, 1, 4)
    d = f(inp["expert_w_down"][0]).reshape(E, 2, 8, 128, 1024).transpose(0, 1, 3, 2, 4)
    aw = f(inp["ada_w"][0]).reshape(8, 128, 12, 512).transpose(2, 1, 0, 3)
    val = dict(
        expert_w_gate_l=np.ascontiguousarray(g).reshape(E * 2, 128, 8192),
        expert_w_up_l=np.ascontiguousarray(u).reshape(E * 2, 128, 8192),
        expert_w_down_l=np.ascontiguousarray(d).reshape(E * 2, 128, 8192),
        ada_w_l=np.ascontiguousarray(aw).reshape(12, 128, 4096),
    )
    _WCACHE["key"] = key
    _WCACHE["val"] = val
    return val


def _constants():
    if _CONST:
        return _CONST
    bf = ml_dtypes.bfloat16
    d = np.arange(128)
    ang = 2.0 * np.pi * ((d[:, None] * d[None, :]) % 128) / 128.0
    cs128 = np.concatenate([np.cos(ang), np.sin(ang)], axis=1) / np.sqrt(128.0)
    s = np.arange(S, dtype=np.int64)
    m = (s[:, None] * s[None, :]) % S
    tab_c = np.cos(2.0 * np.pi * np.arange(S) / S) / 64.0
    tab_s = -np.sin(2.0 * np.pi * np.arange(S) / S) / 64.0
    q = np.arange(128)
    e_of = q % 16
    s_of = q // 16
    maskM = (e_of[:, None] == e_of[None, :]).astype(np.float32)
    maskM2 = ((e_of[:, None] == e_of[None, :]) & (s_of[:, None] < s_of[None, :])).astype(np.float32)
    cvals = (np.arange(4)[None, :] * 128 + np.arange(128)[:, None]).astype(np.float32)
    _CONST.update(
        cs128=cs128.astype(bf),
        dft_cos_l=_dft_layout(tab_c[m].astype(bf)),
        dft_sin_l=_dft_layout(tab_s[m].astype(bf)),
        maskM=maskM, maskM2=maskM2, cvals=cvals,
    )
    return _CONST


def make_in_map(b, inp):
    f = lambda a: np.ascontiguousarray(np.asarray(a, dtype=np.float32))
    C = _constants()
    WL = _weight_layouts(inp)
    smallrows = np.concatenate([
        f(inp["b_in"][0]).reshape(28, 128), f(inp["conv_dw_b"][0]).reshape(4, 128), f(inp["conv_ln_g"][0]).reshape(4, 128),
        f(inp["conv_ln_b"][0]).reshape(4, 128), f(inp["conv_b_out"][0]).reshape(8, 128), f(inp["fourier_b"][0]).reshape(8, 128)], axis=0)
    return {
        "x": f(inp["x"][b]),
        "c_col": np.ascontiguousarray(f(inp["c"][b]).reshape(8, 128).T),
        "ada_w_l": WL["ada_w_l"],
        "ada_b": f(inp["ada_b"][0]).reshape(1, -1),
        "norm1_g": f(inp["norm1_g"][0]).reshape(1, -1),
        "w_in": f(inp["w_in"][0]),
        "smallrows": np.ascontiguousarray(smallrows),
        "dw_rows": f(inp["conv_dw_w"][0]).reshape(124, 128),
        "conv_w_out": f(inp["conv_w_out"][0]),
        "fourier_w": f(inp["fourier_w"][0]),
        "w_out": f(inp["w_out"][0]),
        "b_out": f(inp["b_out"][0]).reshape(1, -1),
        "norm2_g": f(inp["norm2_g"][0]).reshape(1, -1),
        "router_w": f(inp["router_w"][0]),
        "expert_w_gate_l": WL["expert_w_gate_l"],
        "expert_w_up_l": WL["expert_w_up_l"],
        "expert_w_down_l": WL["expert_w_down_l"],
        "final_norm_g": f(inp["final_norm_g"]).reshape(1, -1),
        "cs128": C["cs128"], "dft_cos_l": C["dft_cos_l"], "dft_sin_l": C["dft_sin_l"],
        "maskM": C["maskM"], "maskM2": C["maskM2"], "cvals": C["cvals"],
    }


def kernel(**inputs):
    nc = build_program()
    in_maps = [make_in_map(b, inputs) for b in range(8)]
    res = run_bass_kernel_spmd(nc, in_maps, core_ids=list(range(8)))
    return np.stack([np.asarray(r["out"], dtype=np.float32) for r in res.results], axis=0)
```
